# Optimizing a Trainium2 kernel written in Bass

```python
import math
import jax, jax.numpy as jnp
from jax import lax
import numpy as np

D_MODEL = 1024
BATCH = 8
SEQ = 4096
DEPTH = 1

D_MIX = D_MODEL
D_HYENA = D_MIX // 2
D_RWKV = D_MIX - D_HYENA
N_DIRS = 2
HYENA_ORDER = 2
SHORT_CONV = 3
FILTER_EMB = 33
FILTER_BANDS = (FILTER_EMB - 1) // 2
FILTER_WIDTH = 64
FILTER_INNER = 2
DECAY_TARGET = 1e-2
FAST_DECAY_PCT = 0.3
SLOW_DECAY_PCT = 1.5
FILTER_NORM_EPS = 1e-6
RWKV_HEAD = 64
RWKV_HEADS = D_RWKV // RWKV_HEAD
DECAY_LORA = 32
ICLR_LORA = 32
GATE_LORA = 96
GN_EPS = 64e-5
HY_COLS = (HYENA_ORDER + 1) * D_HYENA
RW_COLS = 3 * D_RWKV + N_DIRS * (DECAY_LORA + ICLR_LORA) + GATE_LORA
N_IN = HY_COLS + RW_COLS
N_EXPERTS = 256
TOP_K = 8
N_GROUPS = 8
TOPK_GROUPS = 4
GROUP_SCORE_K = 2
D_EXPERT = 256
ROUTE_SCALE = 2.5
EXPERT_BLOCK = 128
NORM_EPS = 1e-6
N_MOD = 6

kernel_name = 'hybrid_hyena_rwkv7_moe_block'


def rmsnorm(x, g):
    xf = x.astype(jnp.float32)
    y = xf * lax.rsqrt(jnp.mean(xf * xf, axis=-1, keepdims=True) + NORM_EPS)
    return (y * g.astype(jnp.float32)).astype(x.dtype)


def centred_conv(u, w, b):
    half = SHORT_CONV // 2
    T = u.shape[1]
    up = jnp.pad(u, ((0, 0), (half, half), (0, 0)))
    return sum(up[:, j:j + T] * w[j] for j in range(SHORT_CONV)) + b


def centred_shift(u):
    T = u.shape[1]
    up = jnp.pad(u, ((0, 0), (1, 1), (0, 0)))
    return 0.5 * (up[:, :T] + up[:, 2:])


def hyena_filters(L, pw1, pb1, pw2, pb2, pw3, freq):
    f32 = jnp.float32
    pw1, pb1, pw2, pb2, pw3, freq = (t.astype(f32) for t in (pw1, pb1, pw2, pb2, pw3, freq))
    pos = jnp.arange(L, dtype=f32)
    t = pos / max(L - 1, 1)
    ang = (2.0 * math.pi / L) * pos[:, None] * jnp.linspace(1e-4, FILTER_BANDS - 1, FILTER_BANDS)[None]
    feats = jnp.concatenate([t[:, None], jnp.cos(ang), -jnp.sin(ang)], axis=-1)
    hdn = jnp.sin(freq * (feats @ pw1 + pb1))
    for i in range(FILTER_INNER):
        hdn = jnp.sin(freq * (hdn @ pw2[i] + pb2[i]))
    filt = (hdn @ pw3).reshape(L, HYENA_ORDER, N_DIRS, D_HYENA)
    deltas = jnp.abs(jnp.linspace(math.log(DECAY_TARGET) / SLOW_DECAY_PCT,
                                  math.log(DECAY_TARGET) / FAST_DECAY_PCT, D_HYENA))
    filt = filt * jnp.exp(-t[:, None] * deltas[None])[:, None, None, :]
    fwd, bwd = filt[:, :, 0], filt[:, :, 1]
    two_sided = jnp.concatenate([fwd, jnp.zeros_like(fwd[:1]), bwd[:0:-1]], axis=0)
    return two_sided * lax.rsqrt(jnp.sum(two_sided * two_sided, axis=0, keepdims=True) + FILTER_NORM_EPS)


def fft_long_conv(u, k, skip):
    L = u.shape[1]
    u_f = jnp.fft.rfft(u, n=2 * L, axis=1)
    k_f = jnp.fft.rfft(k, axis=0)
    y = jnp.fft.irfft(u_f * k_f[None], n=2 * L, axis=1)[:, :L]
    return y + u * skip


def hyena_mixer(p, conv_w, conv_b, pw1, pb1, pw2, pb2, pw3, freq, skip):
    f32 = jnp.float32
    u = centred_conv(p.astype(f32), conv_w.astype(f32), conv_b.astype(f32))
    streams = jnp.split(u, HYENA_ORDER + 1, axis=-1)
    k = hyena_filters(p.shape[1], pw1, pb1, pw2, pb2, pw3, freq)
    skip = skip.astype(f32)
    z = streams[0]
    for n in range(HYENA_ORDER):
        z = streams[n + 1] * fft_long_conv(z, k[:, n], skip[n])
    return z


def wkv_scan(r, w, k, v, kk, a, reverse):
    B, T, H, N = r.shape
    xs = tuple(jnp.moveaxis(t, 1, 0) for t in (r, w, k, v, -kk, kk * a))

    def step(S, inp):
        r_t, w_t, k_t, v_t, a_t, b_t = inp
        sa = jnp.einsum('bhvk,bhk->bhv', S, a_t)
        S = S * w_t[:, :, None, :] + sa[..., None] * b_t[:, :, None, :] + v_t[..., None] * k_t[:, :, None, :]
        return S, jnp.einsum('bhvk,bhk->bhv', S, r_t)

    S0 = jnp.zeros((B, H, N, N), jnp.float32)
    _, out = lax.scan(step, S0, xs, reverse=reverse)
    return jnp.moveaxis(out, 0, 1)


def rwkv_mixer(p, mu, w0, w_up, a0, a_up, g_up, k_k, k_a, r_k, ln_w, ln_b):
    f32 = jnp.float32
    B, T, _ = p.shape
    C, H, N = D_RWKV, RWKV_HEADS, RWKV_HEAD
    p = p.astype(f32)
    p = p + mu.astype(f32) * (centred_shift(p) - p)
    r, k, v = p[..., :C], p[..., C:2 * C], p[..., 2 * C:3 * C]
    col = 3 * C
    w_lo = p[..., col:col + N_DIRS * DECAY_LORA].reshape(B, T, N_DIRS, DECAY_LORA)
    col += N_DIRS * DECAY_LORA
    a_lo = p[..., col:col + N_DIRS * ICLR_LORA].reshape(B, T, N_DIRS, ICLR_LORA)
    col += N_DIRS * ICLR_LORA
    g_lo = p[..., col:col + GATE_LORA]
    w_log = -jax.nn.softplus(-(w0.astype(f32) + jnp.einsum('btdl,dlc->btdc', jnp.tanh(w_lo), w_up.astype(f32)))) - 0.5
    decay = jnp.exp(-jnp.exp(w_log))
    a = jax.nn.sigmoid(a0.astype(f32) + jnp.einsum('btdl,dlc->btdc', a_lo, a_up.astype(f32)))
    g = jax.nn.sigmoid(g_lo) @ g_up.astype(f32)
    heads = lambda t: t.reshape(B, T, H, N)
    kk = heads(k * k_k.astype(f32))
    kk = kk / jnp.maximum(jnp.sqrt(jnp.sum(kk * kk, axis=-1, keepdims=True)), 1e-12)
    k_dir = k[:, :, None, :] * (1.0 + (a - 1.0) * k_a.astype(f32))
    r_h, v_h = heads(r), heads(v)
    o_f = wkv_scan(r_h, heads(decay[:, :, 0]), heads(k_dir[:, :, 0]), v_h, kk, heads(a[:, :, 0]), False)
    o_b = wkv_scan(r_h, heads(decay[:, :, 1]), heads(k_dir[:, :, 1]), v_h, kk, heads(a[:, :, 1]), True)
    s = o_f + o_b
    mean = jnp.mean(s, axis=-1, keepdims=True)
    var = jnp.mean(jnp.square(s - mean), axis=-1, keepdims=True)
    s = ((s - mean) * lax.rsqrt(var + GN_EPS)).reshape(B, T, C) * ln_w.astype(f32) + ln_b.astype(f32)
    bonus = jnp.sum(heads(r * jnp.sum(k_dir, axis=2) * r_k.astype(f32)), axis=-1, keepdims=True) * v_h
    return (s + bonus.reshape(B, T, C)) * g


def swiglu(x, wg, wu, wd):
    return (jax.nn.silu(x @ wg) * (x @ wu)) @ wd


def route(h, router_w, router_bias):
    Ntok = h.shape[0]
    scores = jax.nn.sigmoid(h.astype(jnp.float32) @ router_w.astype(jnp.float32))
    biased = scores + router_bias.astype(jnp.float32)
    grp = biased.reshape(Ntok, N_GROUPS, N_EXPERTS // N_GROUPS)
    grp_score = jnp.sum(lax.top_k(grp, GROUP_SCORE_K)[0], axis=-1)
    _, gidx = lax.top_k(grp_score, TOPK_GROUPS)
    gmask = jnp.any(gidx[..., None] == jnp.arange(N_GROUPS), axis=1)
    emask = jnp.repeat(gmask, N_EXPERTS // N_GROUPS, axis=1)
    _, eidx = lax.top_k(jnp.where(emask, biased, -jnp.inf), TOP_K)
    wsel = jnp.take_along_axis(scores, eidx, axis=-1)
    wsel = wsel / jnp.sum(wsel, axis=-1, keepdims=True) * ROUTE_SCALE
    return eidx, wsel


def routed_experts(h, eidx, wsel, wg, wu, wd):
    Ntok, D = h.shape
    A = Ntok * TOP_K
    flat_e = eidx.reshape(-1).astype(jnp.int32)
    flat_w = wsel.reshape(-1).astype(h.dtype)
    order = jnp.argsort(flat_e)
    sorted_e = flat_e[order]
    counts = jnp.bincount(flat_e, length=N_EXPERTS).astype(jnp.int32)
    offsets = jnp.cumsum(counts) - counts
    padded = (counts + EXPERT_BLOCK - 1) // EXPERT_BLOCK * EXPERT_BLOCK
    pad_end = jnp.cumsum(padded)
    pad_off = pad_end - padded
    dest = pad_off[sorted_e] + jnp.arange(A, dtype=jnp.int32) - offsets[sorted_e]
    n_blocks = -(-(A + N_EXPERTS * (EXPERT_BLOCK - 1)) // EXPERT_BLOCK)
    P = n_blocks * EXPERT_BLOCK
    buf_tok = jnp.full((P,), Ntok, jnp.int32).at[dest].set((order // TOP_K).astype(jnp.int32))
    buf_w = jnp.zeros((P,), h.dtype).at[dest].set(flat_w[order])
    block_e = jnp.clip(jnp.searchsorted(pad_end, jnp.arange(n_blocks, dtype=jnp.int32) * EXPERT_BLOCK,
                                        side='right'), 0, N_EXPERTS - 1)
    h_pad = jnp.concatenate([h, jnp.zeros((1, D), h.dtype)], axis=0)

    def body(y, blk):
        tok, wt, e = blk
        out = swiglu(h_pad[tok], wg[e], wu[e], wd[e])
        return y.at[tok].add(out * wt[:, None]), None

    y0 = jnp.zeros((Ntok + 1, D), h.dtype)
    y, _ = lax.scan(body, y0, (buf_tok.reshape(n_blocks, EXPERT_BLOCK),
                               buf_w.reshape(n_blocks, EXPERT_BLOCK), block_e))
    return y[:Ntok]


def setup_inputs(seed: int = 0) -> dict:
    key = jax.random.key(seed)
    ks = iter(jax.random.split(key, 40))
    L = DEPTH

    def nrm(shape, scale):
        return scale * jax.random.normal(next(ks), shape, jnp.float32)

    def gain(shape):
        return 1.0 + 0.02 * jax.random.normal(next(ks), shape, jnp.float32)

    return {
        'x': nrm((BATCH, SEQ, D_MODEL), 1.0),
        'c': nrm((BATCH, D_MODEL), 1.0),
        'norm1_g': gain((L, D_MODEL)),
        'norm2_g': gain((L, D_MODEL)),
        'normf_g': gain((D_MODEL,)),
        'w_ada': nrm((L, D_MODEL, N_MOD * D_MODEL), 0.5 * D_MODEL ** -0.5),
        'b_ada': nrm((L, N_MOD * D_MODEL), 0.02),
        'w_in': nrm((L, D_MODEL, N_IN), D_MODEL ** -0.5),
        'w_out': nrm((L, D_MIX, D_MODEL), D_MIX ** -0.5),
        'hy_conv_w': nrm((L, SHORT_CONV, HY_COLS), SHORT_CONV ** -0.5),
        'hy_conv_b': nrm((L, HY_COLS), 0.02),
        'hy_pos_w1': nrm((L, FILTER_EMB, FILTER_WIDTH), FILTER_EMB ** -0.5),
        'hy_pos_b1': nrm((L, FILTER_WIDTH), 0.1),
        'hy_pos_w2': nrm((L, FILTER_INNER, FILTER_WIDTH, FILTER_WIDTH), FILTER_WIDTH ** -0.5),
        'hy_pos_b2': nrm((L, FILTER_INNER, FILTER_WIDTH), 0.1),
        'hy_pos_w3': nrm((L, FILTER_WIDTH, HYENA_ORDER * N_DIRS * D_HYENA), FILTER_WIDTH ** -0.5),
        'hy_sin_freq': gain((L, FILTER_WIDTH)),
        'hy_skip': nrm((L, HYENA_ORDER, D_HYENA), 1.0),
        'rw_mu': jax.random.uniform(next(ks), (L, RW_COLS), jnp.float32),
        'rw_w0': jax.random.uniform(next(ks), (L, N_DIRS, D_RWKV), jnp.float32, minval=-6.5, maxval=-1.5),
        'rw_w_up': nrm((L, N_DIRS, DECAY_LORA, D_RWKV), 0.1),
        'rw_a0': nrm((L, N_DIRS, D_RWKV), 0.1),
        'rw_a_up': nrm((L, N_DIRS, ICLR_LORA, D_RWKV), 0.1),
        'rw_g_up': nrm((L, GATE_LORA, D_RWKV), GATE_LORA ** -0.5),
        'rw_k_k': 0.85 + nrm((L, D_RWKV), 0.02),
        'rw_k_a': gain((L, D_RWKV)),
        'rw_r_k': nrm((L, D_RWKV), 0.1),
        'rw_ln_w': gain((L, D_RWKV)),
        'rw_ln_b': nrm((L, D_RWKV), 0.02),
        'router_w': nrm((L, D_MODEL, N_EXPERTS), D_MODEL ** -0.5),
        'router_bias': nrm((L, N_EXPERTS), 0.01),
        'exp_w_gate': nrm((L, N_EXPERTS, D_MODEL, D_EXPERT), D_MODEL ** -0.5),
        'exp_w_up': nrm((L, N_EXPERTS, D_MODEL, D_EXPERT), D_MODEL ** -0.5),
        'exp_w_down': nrm((L, N_EXPERTS, D_EXPERT, D_MODEL), D_EXPERT ** -0.5),
        'sh_w_gate': nrm((L, D_MODEL, D_EXPERT), D_MODEL ** -0.5),
        'sh_w_up': nrm((L, D_MODEL, D_EXPERT), D_MODEL ** -0.5),
        'sh_w_down': nrm((L, D_EXPERT, D_MODEL), D_EXPERT ** -0.5),
    }


def reference(x, c, norm1_g, norm2_g, normf_g, w_ada, b_ada, w_in, w_out,
              hy_conv_w, hy_conv_b, hy_pos_w1, hy_pos_b1, hy_pos_w2, hy_pos_b2, hy_pos_w3,
              hy_sin_freq, hy_skip, rw_mu, rw_w0, rw_w_up, rw_a0, rw_a_up, rw_g_up,
              rw_k_k, rw_k_a, rw_r_k, rw_ln_w, rw_ln_b, router_w, router_bias,
              exp_w_gate, exp_w_up, exp_w_down, sh_w_gate, sh_w_up, sh_w_down):
    B, T, D = x.shape
    for l in range(DEPTH):
        mod = jax.nn.silu(c) @ w_ada[l] + b_ada[l]
        sh1, sc1, g1, sh2, sc2, g2 = jnp.split(mod, N_MOD, axis=-1)
        h = rmsnorm(x, norm1_g[l]) * (1.0 + sc1[:, None, :]) + sh1[:, None, :]
        p = h @ w_in[l]
        y_hy = hyena_mixer(p[..., :HY_COLS], hy_conv_w[l], hy_conv_b[l], hy_pos_w1[l], hy_pos_b1[l],
                           hy_pos_w2[l], hy_pos_b2[l], hy_pos_w3[l], hy_sin_freq[l], hy_skip[l])
        y_rw = rwkv_mixer(p[..., HY_COLS:], rw_mu[l], rw_w0[l], rw_w_up[l], rw_a0[l], rw_a_up[l],
                          rw_g_up[l], rw_k_k[l], rw_k_a[l], rw_r_k[l], rw_ln_w[l], rw_ln_b[l])
        mix = jnp.concatenate([y_hy, y_rw], axis=-1).astype(x.dtype) @ w_out[l]
        x = x + g1[:, None, :] * mix
        h = rmsnorm(x, norm2_g[l]) * (1.0 + sc2[:, None, :]) + sh2[:, None, :]
        hf = h.reshape(B * T, D)
        eidx, wsel = route(hf, router_w[l], router_bias[l])
        ffn = swiglu(hf, sh_w_gate[l], sh_w_up[l], sh_w_down[l]) + \
            routed_experts(hf, eidx, wsel, exp_w_gate[l], exp_w_up[l], exp_w_down[l])
        x = x + g2[:, None, :] * ffn.reshape(B, T, D)
    return rmsnorm(x, normf_g)
```

```python
import numpy as np
from contextlib import ExitStack
import concourse.bass as bass
import concourse.mybir as mybir
from concourse.bass_utils import run_bass_kernel_spmd

F32 = mybir.dt.float32
BF16 = mybir.dt.bfloat16
I32 = mybir.dt.int32
ALU = mybir.AluOpType
AF = mybir.ActivationFunctionType
AX = mybir.AxisListType

NCORES = 8
T = 4096
D = 1024
NT = T // 128
NORM_EPS = 1e-6


class Sched:
    NDQ = 4

    def __init__(self, nc, es):
        self.nc = nc
        self.eng = {"pe": nc.tensor, "dve": nc.vector, "act": nc.scalar, "pool": nc.gpsimd}
        self.inc = {"pe": 1, "dve": 1, "act": 1, "pool": 1}
        self.stream = {"pe": "pe", "dve": "dve", "act": "act", "pool": "pool"}
        for base, eng, st in (("sp", nc.sync, "sp"), ("poolq", nc.gpsimd, "pool")):
            for i in range(self.NDQ):
                k = "%s%d" % (base, i)
                self.eng[k] = eng
                self.inc[k] = 16
                self.stream[k] = st
        self.sem = {k: es.enter_context(nc.semaphore("sem_" + k)) for k in self.eng}
        self.cnt = {k: 0 for k in self.eng}
        self.rr = {"sp": 0, "poolq": 0}
        self.last_w = {}
        self.readers = {}
        self.seen = {s: {} for s in ("pe", "dve", "act", "pool", "sp")}
        self.stream_eng = {"pe": nc.tensor, "dve": nc.vector, "act": nc.scalar, "pool": nc.gpsimd, "sp": nc.sync}

    def _wait(self, st, pq, seq):
        if self.seen[st].get(pq, 0) < seq:
            self.stream_eng[st].wait_ge(self.sem[pq], seq * self.inc[pq])
            self.seen[st][pq] = seq

    def op(self, q, fn, r=(), w=()):
        if q in self.rr:
            i = self.rr[q]
            self.rr[q] = (i + 1) % self.NDQ
            q = "%s%d" % (q, i)
        need = {}
        for b in r:
            for pq, seq in self.last_w.get(b, {}).items():
                need[pq] = max(need.get(pq, 0), seq)
        for b in w:
            for pq, seq in self.last_w.get(b, {}).items():
                need[pq] = max(need.get(pq, 0), seq)
            for pq, seq in self.readers.get(b, ()):
                need[pq] = max(need.get(pq, 0), seq)
        st = self.stream[q]
        if self.inc[q] == 16 and self.cnt[q] > 0:
            need[q] = max(need.get(q, 0), self.cnt[q])
        for pq, seq in need.items():
            self._wait(st, pq, seq)
        ins = fn()
        self.cnt[q] += 1
        seq = self.cnt[q]
        ins.then_inc(self.sem[q], self.inc[q])
        for b in w:
            self.last_w.setdefault(b, {})[q] = seq
            self.readers[b] = []
        for b in r:
            lst = self.readers.setdefault(b, [])
            lst.append((q, seq))
            if len(lst) > 16:
                best = {}
                for pq, s_ in lst:
                    best[pq] = max(best.get(pq, 0), s_)
                self.readers[b] = list(best.items())
        return ins

    def barrier(self):
        for st in ("pe", "dve", "act", "pool", "sp"):
            for k in self.eng:
                if self.cnt[k] > 0 and not (k == st and self.inc[k] == 1):
                    self._wait(st, k, self.cnt[k])

    def finish(self, q="pool"):
        for k in self.eng:
            if self.cnt[k] > 0:
                self.stream_eng[q].wait_ge(self.sem[k], self.cnt[k] * self.inc[k])


def bcast_rows(ap_row, parts=128):
    return ap_row.to_broadcast([parts, ap_row.shape[-1]])


HY_COLS = 1536
N_IN = 3296
DBG = False
SCAN_CHUNKS = 64
N_EXP_RUN = 256


def build_nc():
    nc = bass.Bass("TRN2", target_bir_lowering=False)
    dt_in = lambda name, shape, dt=F32: nc.dram_tensor(name, shape, dt, kind="ExternalInput").ap()
    x = dt_in("x", [T, D])
    c_in = dt_in("c", [1, D])
    normf_g = dt_in("normf_g", [1, D])
    norm1_g = dt_in("norm1_g", [1, D])
    w_ada = dt_in("w_ada", [D, 6 * D])
    b_ada = dt_in("b_ada", [1, 6 * D])
    w_in = dt_in("w_in", [D, N_IN])
    hy_conv_w = dt_in("hy_conv_w", [3, HY_COLS])
    hy_conv_b = dt_in("hy_conv_b", [1, HY_COLS])
    ident_in = dt_in("ident", [128, 128])
    featsT = dt_in("featsT", [33, T])
    env_in = dt_in("env", [T, 512])
    pw1 = dt_in("hy_pos_w1", [33, 64])
    pb1 = dt_in("hy_pos_b1", [64, 1])
    pw2 = dt_in("hy_pos_w2", [2, 64, 64])
    pb2 = dt_in("hy_pos_b2", [2, 64, 1])
    pw3 = dt_in("hy_pos_w3", [64, 2048])
    sfreq = dt_in("hy_sin_freq", [64, 1])
    hskip = dt_in("hy_skip", [1, 1024])
    dfw = dt_in("dfw", [64, 128, 32, 128], BF16)
    dinv = dt_in("dinv", [16, 128, 64, 256], BF16)
    KF = nc.dram_tensor("KF", [64, 128, 1024], F32, kind="Internal").ap()
    UD = nc.dram_tensor("UD", [12, 128, T], F32, kind="Internal").ap()
    ZD = nc.dram_tensor("ZD", [4, 128, T], F32, kind="Internal").ap()
    YD = nc.dram_tensor("YD", [4, 128, T], F32, kind="Internal").ap()
    MODD = nc.dram_tensor("MODD", [128, 6 * D], F32, kind="Internal").ap()
    rw_mu = dt_in("rw_mu", [1, 1760])
    rw_w0 = dt_in("rw_w0", [2, 512])
    rw_w_up = dt_in("rw_w_up", [2, 32, 512])
    rw_a0 = dt_in("rw_a0", [2, 512])
    rw_a_up = dt_in("rw_a_up", [2, 32, 512])
    rw_g_up = dt_in("rw_g_up", [96, 512])
    rw_k_k = dt_in("rw_k_k", [1, 512])
    rw_k_a = dt_in("rw_k_a", [1, 512])
    rw_r_k = dt_in("rw_r_k", [1, 512])
    rw_ln_w = dt_in("rw_ln_w", [1, 512])
    rw_ln_b = dt_in("rw_ln_b", [1, 512])
    blk_in = dt_in("blk", [128, 128])
    m48_in = dt_in("m48", [48, 512])
    m40_in = dt_in("m40", [40, 512])
    selF_in = dt_in("selF", [128, 128, 48], BF16)
    selB_in = dt_in("selB", [128, 128, 48], BF16)
    scr = lambda name, shape, dt=F32: nc.dram_tensor(name, shape, dt, kind="Internal").ap()
    KKN = scr("KKN", [512, T], BF16)
    RR = scr("RR", [512, T], BF16)
    DEC = scr("DEC", [2, 512, T])
    BT = scr("BT", [2, 2, T, 512], BF16)
    VT = scr("VT", [T, 512], BF16)
    BON = scr("BON", [512, T])
    GG = scr("GG", [512, T])
    OD = scr("OD", [2, T, 512])
    X1D = scr("X1D", [T, D])
    H2E = scr("H2E", [T + 1, D + 256])
    OUTACC = scr("OUTACC", [T + 1, D])
    norm2_g = dt_in("norm2_g", [1, D])
    router_w = dt_in("router_w", [D, 256])
    router_bias = dt_in("router_bias", [1, 256])
    sh_w_gate = dt_in("sh_w_gate", [D, 256])
    sh_w_up = dt_in("sh_w_up", [D, 256])
    sh_w_down = dt_in("sh_w_down", [256, D])
    exp_w_gate = dt_in("exp_w_gate", [256, D, 256])
    exp_w_up = dt_in("exp_w_up", [256, D, 256])
    exp_w_down = dt_in("exp_w_down", [256, 256, D])
    tokid_in = dt_in("tokid", [128, NT])
    w_out = dt_in("w_out", [D, D])
    out = nc.dram_tensor("out", [T, D], F32, kind="ExternalOutput").ap()
    if DBG:
        dbg_mod = nc.dram_tensor("dbg_mod", [128, 6 * D], F32, kind="ExternalOutput").ap()
        dbg_u = nc.dram_tensor("dbg_u", [12, 128, T], F32, kind="ExternalOutput").ap()
        dbg_kf = nc.dram_tensor("dbg_kf", [64, 128, 1024], F32, kind="ExternalOutput").ap()
        dbg_y = nc.dram_tensor("dbg_y", [4, 128, T], F32, kind="ExternalOutput").ap()
        dbg_od = nc.dram_tensor("dbg_od", [2, T, 512], F32, kind="ExternalOutput").ap()
        dbg_x1 = nc.dram_tensor("dbg_x1", [T, D], F32, kind="ExternalOutput").ap()
        dbg_idx = nc.dram_tensor("dbg_idx", [128, 8, 256], I32, kind="ExternalOutput").ap()
        dbg_rw = {
            "KKN": nc.dram_tensor("dbg_KKN", [512, T], BF16, kind="ExternalOutput").ap(),
            "RR": nc.dram_tensor("dbg_RR", [512, T], BF16, kind="ExternalOutput").ap(),
            "DEC": nc.dram_tensor("dbg_DEC", [2, 512, T], F32, kind="ExternalOutput").ap(),
            "BT": nc.dram_tensor("dbg_BT", [2, 2, T, 512], BF16, kind="ExternalOutput").ap(),
            "VT": nc.dram_tensor("dbg_VT", [T, 512], BF16, kind="ExternalOutput").ap(),
            "BON": nc.dram_tensor("dbg_BON", [512, T], F32, kind="ExternalOutput").ap(),
            "GG": nc.dram_tensor("dbg_GG", [512, T], F32, kind="ExternalOutput").ap(),
        }

    with ExitStack() as es:
        es.enter_context(nc.allow_low_precision("bf16 matmul operands, fp32 accumulation"))
        es.enter_context(nc.allow_non_contiguous_dma("small strided parameter loads"))
        S = Sched(nc, es)
        sb = lambda name, shape, dt=F32: es.enter_context(nc.sbuf_tensor(name, shape, dt))
        ps = [es.enter_context(nc.psum_tensor("ps%d" % i, [128, 512], F32)) for i in range(8)]
        PSN = ["ps%d" % i for i in range(8)]

        ident = sb("ident_sb", [128, 128])
        S.op("sp", lambda: nc.sync.dma_start(out=ident[:], in_=ident_in[:, :]), w=["ident"])


        TWO_PI = 6.283185307179586
        MAGIC = 12582912.0
        with ExitStack() as es1:
            sb1 = lambda name, shape, dt=F32: es1.enter_context(nc.sbuf_tensor(name, shape, dt))
            w1 = sb1("w1", [33, 64])
            w2 = sb1("w2", [64, 2, 64])
            w3 = sb1("w3", [64, 2048])
            fr = sb1("fr", [64, 1])
            fb = sb1("fb", [64, 3])
            ones = sb1("ones", [128, 128])
            skp = sb1("skp", [128, 1024])
            hd0 = sb1("hd0", [64, T])
            es1a = ExitStack()
            sb1a = lambda name, shape, dt=F32: es1a.enter_context(nc.sbuf_tensor(name, shape, dt))
            ft = sb1a("ft", [33, T])
            hd = [hd0, sb1a("hd1", [64, T])]
            rr = sb1a("rr", [64, 512])
            S.op("sp", lambda: nc.sync.dma_start(out=ft[:], in_=featsT[:, :]), w=["ft"])
            S.op("sp", lambda: nc.sync.dma_start(out=w1[:], in_=pw1[:, :]), w=["w1"])
            S.op("sp", lambda: nc.sync.dma_start(out=w2[:], in_=pw2.rearrange("i k m -> k i m")), w=["w2"])
            S.op("sp", lambda: nc.sync.dma_start(out=w3[:], in_=pw3[:, :]), w=["w3"])
            S.op("sp", lambda: nc.sync.dma_start(out=fr[:], in_=sfreq[:, :]), w=["fr"])
            S.op("sp", lambda: nc.sync.dma_start(out=fb[:, 0:1], in_=pb1[:, :]), w=["fb"])
            S.op("sp", lambda: nc.sync.dma_start(out=fb[:, 1:2], in_=pb2[0]), w=["fb"])
            S.op("sp", lambda: nc.sync.dma_start(out=fb[:, 2:3], in_=pb2[1]), w=["fb"])
            S.op("sp", lambda: nc.sync.dma_start(out=skp[:], in_=bcast_rows(hskip[0:1, :])), w=["skp"])
            S.op("dve", lambda: nc.vector.tensor_scalar(fb[:], fb[:], fr[:, 0:1], None, ALU.mult), r=["fb", "fr"], w=["fb"])
            S.op("pool", lambda: nc.gpsimd.memset(ones[:], 1.0), w=["ones"])
            for layer in range(3):
                src = ft if layer == 0 else hd[(layer - 1) % 2]
                srcn = "ft" if layer == 0 else "hd%d" % ((layer - 1) % 2)
                dst = hd[layer % 2]
                dstn = "hd%d" % (layer % 2)
                lw = w1[:, :] if layer == 0 else w2[:, layer - 1, :]
                lwn = "w1" if layer == 0 else "w2"
                for tcn in range(8):
                    pb = tcn % 2
                    sl = slice(tcn * 512, (tcn + 1) * 512)
                    S.op("pe", lambda: nc.tensor.matmul(ps[pb][0:64, :], lw, src[:, sl], start=True, stop=True),
                         r=[lwn, srcn], w=[PSN[pb]])
                    S.op("dve", lambda: nc.vector.tensor_scalar(dst[:, sl], ps[pb][0:64, :], fr[:, 0:1], fb[:, layer:layer + 1], ALU.mult, ALU.add),
                         r=[PSN[pb], "fr", "fb"], w=[dstn])
                    S.op("dve", lambda: nc.vector.tensor_scalar(rr[:], dst[:, sl], 1.0 / TWO_PI, MAGIC, ALU.mult, ALU.add), r=[dstn], w=["rr"])
                    S.op("dve", lambda: nc.vector.tensor_scalar(rr[:], rr[:], -MAGIC, -TWO_PI, ALU.add, ALU.mult), r=["rr"], w=["rr"])
                    S.op("dve", lambda: nc.vector.tensor_tensor(dst[:, sl], dst[:, sl], rr[:], ALU.add), r=[dstn, "rr"], w=[dstn])
                    S.op("dve", lambda: nc.vector.tensor_scalar(dst[:, sl], dst[:, sl], 3.14159, -3.14159, ALU.min, ALU.max), r=[dstn], w=[dstn])
                    S.op("act", lambda: nc.scalar.activation(out=dst[:, sl], in_=dst[:, sl], func=AF.Sin), r=[dstn], w=[dstn])
            S.barrier()
            es1a.close()
            envt = [sb1("envt%d" % i, [128, 512]) for i in range(2)]
            fw = [sb1("fw%d" % i, [128, 512]) for i in range(2)]
            bw = [sb1("bw%d" % i, [128, 512]) for i in range(2)]
            sqf = sb1("sqf", [128, 512])
            sqb = sb1("sqb", [128, 512])
            Eb = sb1("Eb", [128, NT, 1024], BF16)
            Ob = sb1("Ob", [128, NT, 1024], BF16)
            scl = sb1("scl", [128, 1024])
            dch = [sb1("dch%d" % i, [128, 32, 128], BF16) for i in range(2)]
            kst = [sb1("kst%d" % i, [128, 1024]) for i in range(2)]
            knq = sb1("knq", [128, 1024])
            hfin = hd[0]
            for i in range(NT):
                b = i % 2
                S.op("sp", lambda: nc.sync.dma_start(out=envt[b][:], in_=env_in[i * 128:(i + 1) * 128, :]), w=["envt%d" % b])
                for n in range(2):
                    for d in range(2):
                        pb = d
                        col = n * 1024 + d * 512
                        S.op("pe", lambda: nc.tensor.matmul(ps[pb][:], hfin[:, i * 128:(i + 1) * 128], w3[:, col:col + 512], start=True, stop=True),
                             r=["hd0", "w3"], w=[PSN[pb]])
                    S.op("dve", lambda: nc.vector.tensor_tensor(fw[n][:], ps[0][:], envt[b][:], ALU.mult), r=[PSN[0], "envt%d" % b], w=["fw%d" % n])
                    S.op("dve", lambda: nc.vector.tensor_tensor(bw[n][:], ps[1][:], envt[b][:], ALU.mult), r=[PSN[1], "envt%d" % b], w=["bw%d" % n])
                    if i == 0:
                        S.op("dve", lambda: nc.vector.memset(bw[n][0:1, :], 0.0), w=["bw%d" % n])
                    S.op("pool", lambda: nc.gpsimd.tensor_tensor(Eb[:, i, n * 512:(n + 1) * 512], fw[n][:], bw[n][:], ALU.add),
                         r=["fw%d" % n, "bw%d" % n], w=["Eb"])
                    S.op("pool", lambda: nc.gpsimd.tensor_tensor(Ob[:, i, n * 512:(n + 1) * 512], fw[n][:], bw[n][:], ALU.subtract),
                         r=["fw%d" % n, "bw%d" % n], w=["Ob"])
                    S.op("act", lambda: nc.scalar.activation(out=sqf[:], in_=fw[n][:], func=AF.Square), r=["fw%d" % n], w=["sqf"])
                    S.op("act", lambda: nc.scalar.activation(out=sqb[:], in_=bw[n][:], func=AF.Square), r=["bw%d" % n], w=["sqb"])
                    S.op("pe", lambda: nc.tensor.matmul(ps[2 + n][:], ones[:], sqf[:], start=(i == 0), stop=False), r=["ones", "sqf"], w=[PSN[2 + n]])
                    S.op("pe", lambda: nc.tensor.matmul(ps[2 + n][:], ones[:], sqb[:], start=False, stop=(i == NT - 1)), r=["ones", "sqb"], w=[PSN[2 + n]])
            for n in range(2):
                S.op("dve", lambda: nc.vector.tensor_scalar(scl[:, n * 512:(n + 1) * 512], ps[2 + n][:], 1e-6, None, ALU.add), r=[PSN[2 + n]], w=["scl"])
            S.op("act", lambda: nc.scalar.activation(out=scl[:], in_=scl[:], func=AF.Sqrt), r=["scl"], w=["scl"])
            S.op("dve", lambda: nc.vector.reciprocal(scl[:], scl[:]), r=["scl"], w=["scl"])
            for rc in range(64):
                b = rc % 2
                S.op("sp", lambda: nc.sync.dma_start(out=dch[b][:], in_=dfw[rc]), w=["dch%d" % b])
                src, srcn = (Eb, "Eb") if rc < 32 else (Ob, "Ob")
                for n in range(2):
                    for tcn in range(NT):
                        S.op("pe", lambda: nc.tensor.matmul(ps[4 + n][:], dch[b][:, tcn, :], src[:, tcn, n * 512:(n + 1) * 512],
                                                            start=(tcn == 0), stop=(tcn == NT - 1)),
                             r=["dch%d" % b, srcn], w=[PSN[4 + n]])
                    S.op("dve", lambda: nc.vector.tensor_tensor(kst[b][:, n * 512:(n + 1) * 512], ps[4 + n][:], scl[:, n * 512:(n + 1) * 512], ALU.mult),
                         r=[PSN[4 + n], "scl"], w=["kst%d" % b])
                if rc < 32:
                    S.op("pool", lambda: nc.gpsimd.tensor_tensor(kst[b][:], kst[b][:], skp[:], ALU.add), r=["kst%d" % b, "skp"], w=["kst%d" % b])
                if rc == 32:
                    for n in range(2):
                        for tcn in range(NT):
                            S.op("pe", lambda: nc.tensor.matmul(ps[6 + n][:], dch[b][:, tcn, :], Eb[:, tcn, n * 512:(n + 1) * 512],
                                                                start=(tcn == 0), stop=(tcn == NT - 1)),
                                 r=["dch%d" % b, "Eb"], w=[PSN[6 + n]])
                        S.op("dve", lambda: nc.vector.tensor_tensor(knq[:, n * 512:(n + 1) * 512], ps[6 + n][:], scl[:, n * 512:(n + 1) * 512], ALU.mult),
                             r=[PSN[6 + n], "scl"], w=["knq"])
                    S.op("dve", lambda: nc.vector.tensor_tensor(kst[b][0:1, :], knq[0:1, :], skp[0:1, :], ALU.add), r=["knq", "skp", "kst%d" % b], w=["kst%d" % b])
                S.op("poolq", lambda: nc.gpsimd.dma_start(out=KF[rc], in_=kst[b][:]), r=["kst%d" % b], w=["KF"])
            if DBG:
                S.op("poolq", lambda: nc.gpsimd.dma_start(out=dbg_kf[:, :, :], in_=KF[:, :, :]), r=["KF"])

        S.barrier()
        with ExitStack() as es1:
            sb1 = lambda name, shape, dt=F32: es1.enter_context(nc.sbuf_tensor(name, shape, dt))
            mod = sb1("mod", [128, 6 * D])
            cs = sb1("cs", [128, 8])
            crep = sb1("crep", [128, 8, 128])
            wa = [sb1("wa%d" % i, [128, 3 * D]) for i in range(2)]
            S.op("sp", lambda: nc.sync.dma_start(out=cs[:], in_=c_in.rearrange("o (kc p) -> p (o kc)", p=128)), w=["cs"])
            S.op("sp", lambda: nc.sync.dma_start(out=mod[:], in_=bcast_rows(b_ada[0:1, :])), w=["mod"])
            S.op("act", lambda: nc.scalar.activation(out=cs[:], in_=cs[:], func=AF.Silu), r=["cs"], w=["cs"])
            for kc in range(8):
                S.op("dve", lambda: nc.vector.tensor_copy(crep[:, kc, :], cs[:, kc:kc + 1].to_broadcast([128, 128])),
                     r=["cs"], w=["crep"])
            n = 0
            for half in range(2):
                for kc in range(8):
                    b = n % 2
                    n += 1
                    S.op("sp", lambda: nc.sync.dma_start(out=wa[b][:], in_=w_ada[kc * 128:(kc + 1) * 128, half * 3 * D:(half + 1) * 3 * D]),
                         w=["wa%d" % b])
                    for j in range(6):
                        S.op("pe", lambda: nc.tensor.matmul(ps[j][:], crep[:, kc, :], wa[b][:, j * 512:(j + 1) * 512],
                                                            start=(kc == 0), stop=(kc == 7)),
                             r=["crep", "wa%d" % b], w=[PSN[j]])
                for j in range(6):
                    col = half * 3 * D + j * 512
                    S.op("dve", lambda: nc.vector.tensor_tensor(mod[:, col:col + 512], mod[:, col:col + 512], ps[j][:], ALU.add),
                         r=[PSN[j], "mod"], w=["mod"])
            S.op("poolq", lambda: nc.gpsimd.dma_start(out=MODD[:, :], in_=mod[:]), r=["mod"], w=["MODD"])
            if DBG:
                S.op("poolq", lambda: nc.gpsimd.dma_start(out=dbg_mod[:, :], in_=mod[:]), r=["mod"])
        S.barrier()

        def rstd_of(sq, ss, rs, nm, src, srcname):
            S.op("act", lambda: nc.scalar.activation(out=sq[:], in_=src, func=AF.Square, accum_out=ss[:]),
                 r=[srcname], w=["sq" + nm, "ss" + nm])
            S.op("dve", lambda: nc.vector.tensor_scalar(rs[:], ss[:], 1.0 / D, NORM_EPS, ALU.mult, ALU.add),
                 r=["ss" + nm], w=["rs" + nm])
            S.op("act", lambda: nc.scalar.activation(out=rs[:], in_=rs[:], func=AF.Sqrt), r=["rs" + nm], w=["rs" + nm])
            S.op("dve", lambda: nc.vector.reciprocal(rs[:], rs[:]), r=["rs" + nm], w=["rs" + nm])

        es2 = ExitStack()
        sb2 = lambda name, shape, dt=F32: es2.enter_context(nc.sbuf_tensor(name, shape, dt))
        with es2:
            hT = sb2("hT", [128, 8, T + 2], BF16)
            S.op("pool", lambda: nc.gpsimd.memset(hT[:, :, 0:1], 0.0), w=["hT"])
            S.op("pool", lambda: nc.gpsimd.memset(hT[:, :, T + 1:T + 2], 0.0), w=["hT"])
            with ExitStack() as esN:
                sbN = lambda name, shape, dt=F32: esN.enter_context(nc.sbuf_tensor(name, shape, dt))
                gm1 = sbN("gm1", [128, D])
                sc1 = sbN("sc1", [128, D])
                sh1 = sbN("sh1", [128, D])
                S.op("sp", lambda: nc.sync.dma_start(out=gm1[:], in_=bcast_rows(norm1_g[0:1, :])), w=["gm1"])
                S.op("sp", lambda: nc.sync.dma_start(out=sh1[:], in_=MODD[:, 0:D]), r=["MODD"], w=["sh1"])
                S.op("sp", lambda: nc.sync.dma_start(out=sc1[:], in_=MODD[:, D:2 * D]), r=["MODD"], w=["sc1"])
                S.op("dve", lambda: nc.vector.scalar_tensor_tensor(gm1[:], sc1[:], 1.0, gm1[:], ALU.add, ALU.mult), r=["sc1", "gm1"], w=["gm1"])
                xt = [sbN("xt%d" % i, [128, D]) for i in range(2)]
                hh = [sbN("hh%d" % i, [128, D]) for i in range(2)]
                sq = sbN("sq", [128, D])
                ss = [sbN("ss%d" % i, [128, 1]) for i in range(2)]
                rs = [sbN("rs%d" % i, [128, 1]) for i in range(2)]
                for i in range(NT):
                    b = i % 2
                    S.op("sp", lambda: nc.sync.dma_start(out=xt[b][:], in_=x[i * 128:(i + 1) * 128, :]), w=["xt%d" % b])
                    rstd_of(sq, ss[b], rs[b], "n1_%d" % b, xt[b][:], "xt%d" % b)
                    S.op("dve", lambda: nc.vector.scalar_tensor_tensor(hh[b][:], xt[b][:], rs[b][:, 0:1], gm1[:], ALU.mult, ALU.mult),
                         r=["xt%d" % b, "rsn1_%d" % b, "gm1"], w=["hh%d" % b])
                    S.op("pool", lambda: nc.gpsimd.tensor_tensor(hh[b][:], hh[b][:], sh1[:], ALU.add), r=["hh%d" % b, "sh1"], w=["hh%d" % b])
                    for half in range(2):
                        pb = 6 + half
                        for q in range(4):
                            kc = half * 4 + q
                            S.op("pe", lambda: nc.tensor.transpose(ps[pb][:, q * 128:(q + 1) * 128], hh[b][:, kc * 128:(kc + 1) * 128], ident[:]),
                                 r=["hh%d" % b, "ident"], w=[PSN[pb]])
                        S.op("act", lambda: nc.scalar.copy(hT[:, half * 4:(half + 1) * 4, 1 + i * 128:1 + (i + 1) * 128],
                                                           ps[pb][:].rearrange("p (q t) -> p q t", q=4)),
                             r=[PSN[pb]], w=["hT"])
                S.barrier()

            wst = [sb2("wst%d" % i, [128, 8, 128]) for i in range(2)]
            wbf = [sb2("wbf%d" % i, [128, 8, 128], BF16) for i in range(2)]
            pfm = [sb2("pfm%d" % i, [128, T + 2]) for i in range(3)]
            for bb in range(3):
                S.op("pool", lambda: nc.gpsimd.memset(pfm[bb][:, 0:1], 0.0), w=["pfm%d" % bb])
                S.op("pool", lambda: nc.gpsimd.memset(pfm[bb][:, T + 1:T + 2], 0.0), w=["pfm%d" % bb])
            wcnt = [0]

            def inproj_group(cg, ncols, dst, dstname):
                b = wcnt[0] % 2
                wcnt[0] += 1
                S.op("sp", lambda: nc.sync.dma_start(out=wst[b][:, :, 0:ncols],
                                                     in_=w_in[:, cg * 128:cg * 128 + ncols].rearrange("(kc p) n -> p kc n", p=128)),
                     w=["wst%d" % b])
                S.op("pool", lambda: nc.gpsimd.tensor_copy(wbf[b][:, :, 0:ncols], wst[b][:, :, 0:ncols]), r=["wst%d" % b], w=["wbf%d" % b])
                for tcn in range(8):
                    pb = tcn % 2
                    for kc in range(8):
                        S.op("pe", lambda: nc.tensor.matmul(ps[pb][0:ncols, :], wbf[b][:, kc, 0:ncols], hT[:, kc, 1 + tcn * 512:1 + (tcn + 1) * 512],
                                                            start=(kc == 0), stop=(kc == 7)),
                             r=["wbf%d" % b, "hT"], w=[PSN[pb]])
                    S.op("act", lambda: nc.scalar.copy(dst[0:ncols, 1 + tcn * 512:1 + (tcn + 1) * 512], ps[pb][0:ncols, :]),
                         r=[PSN[pb]], w=[dstname])

            with ExitStack() as esH:
                sbH = lambda name, shape, dt=F32: esH.enter_context(nc.sbuf_tensor(name, shape, dt))
                ufm = [sbH("ufm%d" % i, [128, T]) for i in range(2)]
                cw = sbH("cw", [128, 3, 12])
                cb = sbH("cb", [128, 12])
                for j in range(3):
                    S.op("sp", lambda: nc.sync.dma_start(out=cw[:, j, :], in_=hy_conv_w[j:j + 1, :].rearrange("o (g p) -> p (o g)", p=128)), w=["cw"])
                S.op("sp", lambda: nc.sync.dma_start(out=cb[:], in_=hy_conv_b.rearrange("o (g p) -> p (o g)", p=128)), w=["cb"])
                for cg in range(12):
                    b = cg % 2
                    inproj_group(cg, 128, pfm[b], "pfm%d" % b)
                    S.op("dve", lambda: nc.vector.tensor_scalar(ufm[b][:], pfm[b][:, 0:T], cw[:, 0, cg:cg + 1], cb[:, cg:cg + 1], ALU.mult, ALU.add),
                         r=["pfm%d" % b, "cw", "cb"], w=["ufm%d" % b])
                    S.op("dve", lambda: nc.vector.scalar_tensor_tensor(ufm[b][:], pfm[b][:, 1:T + 1], cw[:, 1, cg:cg + 1], ufm[b][:], ALU.mult, ALU.add),
                         r=["pfm%d" % b, "cw", "ufm%d" % b], w=["ufm%d" % b])
                    S.op("dve", lambda: nc.vector.scalar_tensor_tensor(ufm[b][:], pfm[b][:, 2:T + 2], cw[:, 2, cg:cg + 1], ufm[b][:], ALU.mult, ALU.add),
                         r=["pfm%d" % b, "cw", "ufm%d" % b], w=["ufm%d" % b])
                    S.op("poolq", lambda: nc.gpsimd.dma_start(out=UD[cg, :, :], in_=ufm[b][:]), r=["ufm%d" % b], w=["UD"])
                    if DBG:
                        S.op("poolq", lambda: nc.gpsimd.dma_start(out=dbg_u[cg, :, :], in_=ufm[b][:]), r=["ufm%d" % b])
                S.barrier()

            with ExitStack() as esR:
                sbR = lambda name, shape, dt=F32: esR.enter_context(nc.sbuf_tensor(name, shape, dt))
                pvec = lambda ap_row, p=128: ap_row.rearrange("o (g p) -> p (o g)", p=p)
                mu_t = sbR("mu_t", [128, 14])
                w0_t = sbR("w0_t", [128, 8])
                a0_t = sbR("a0_t", [128, 8])
                kk_t = sbR("kk_t", [128, 4])
                ka_t = sbR("ka_t", [128, 4])
                oka_t = sbR("oka_t", [128, 4])
                rk_t = sbR("rk_t", [128, 4])
                S.op("sp", lambda: nc.sync.dma_start(out=mu_t[:, 0:13], in_=pvec(rw_mu[0:1, 0:1664])), w=["mu_t"])
                S.op("sp", lambda: nc.sync.dma_start(out=mu_t[0:96, 13:14], in_=pvec(rw_mu[0:1, 1664:1760], 96)), w=["mu_t"])
                for d in range(2):
                    S.op("sp", lambda: nc.sync.dma_start(out=w0_t[:, d * 4:(d + 1) * 4], in_=pvec(rw_w0[d:d + 1, :])), w=["w0_t"])
                    S.op("sp", lambda: nc.sync.dma_start(out=a0_t[:, d * 4:(d + 1) * 4], in_=pvec(rw_a0[d:d + 1, :])), w=["a0_t"])
                S.op("sp", lambda: nc.sync.dma_start(out=kk_t[:], in_=pvec(rw_k_k[0:1, :])), w=["kk_t"])
                S.op("sp", lambda: nc.sync.dma_start(out=ka_t[:], in_=pvec(rw_k_a[0:1, :])), w=["ka_t"])
                S.op("sp", lambda: nc.sync.dma_start(out=rk_t[:], in_=pvec(rw_r_k[0:1, :])), w=["rk_t"])
                S.op("dve", lambda: nc.vector.tensor_scalar(oka_t[:], ka_t[:], -1.0, 1.0, ALU.mult, ALU.add), r=["ka_t"], w=["oka_t"])
                Lw = sbR("Lw", [128, 4, 512], BF16)
                Gw = sbR("Gw", [96, 512], BF16)
                blk = sbR("blk_sb", [128, 128])
                Lbf = sbR("Lbf", [128, T], BF16)
                Glb = sbR("Glb", [96, T], BF16)
                esL = ExitStack()
                Lst = esL.enter_context(nc.sbuf_tensor("Lst", [128, 4, 512], F32))
                Gst = esL.enter_context(nc.sbuf_tensor("Gst", [96, 512], F32))
                S.op("pool", lambda: nc.gpsimd.memset(Lst[:], 0.0), w=["Lst"])
                for d in range(2):
                    S.op("sp", lambda: nc.sync.dma_start(out=Lst[d * 32:(d + 1) * 32, d, :], in_=rw_w_up[d]), w=["Lst"])
                    S.op("sp", lambda: nc.sync.dma_start(out=Lst[64 + d * 32:64 + (d + 1) * 32, 2 + d, :], in_=rw_a_up[d]), w=["Lst"])
                S.op("pool", lambda: nc.gpsimd.tensor_copy(Lw[:], Lst[:]), r=["Lst"], w=["Lw"])
                S.op("sp", lambda: nc.sync.dma_start(out=Gst[:], in_=rw_g_up[:, :]), w=["Gst"])
                S.op("pool", lambda: nc.gpsimd.tensor_copy(Gw[:], Gst[:]), r=["Gst"], w=["Gw"])
                S.op("sp", lambda: nc.sync.dma_start(out=blk[:], in_=blk_in[:, :]), w=["blk"])
                S.barrier()
                esL.close()

                singles = ("kkr", "sqk", "nrm", "sg", "tmpa", "bbf", "ad")

                def tmp2(name, shape=(128, 512), dt=F32):
                    if name in singles:
                        t_ = sbR(name + "0", list(shape), dt)
                        return [t_, t_]
                    return [sbR("%s%d" % (name, i), list(shape), dt) for i in range(2)]

                def shift(dst, dstn, src, srcn, mucol, c0, rows=128, n=512):
                    S.op("dve", lambda: nc.vector.tensor_tensor(dst[0:rows, :], src[0:rows, c0:c0 + n], src[0:rows, c0 + 2:c0 + 2 + n], ALU.add),
                         r=[srcn], w=[dstn])
                    S.op("dve", lambda: nc.vector.scalar_tensor_tensor(dst[0:rows, :], dst[0:rows, :], 0.5, src[0:rows, c0 + 1:c0 + 1 + n], ALU.mult, ALU.subtract),
                         r=[srcn, dstn], w=[dstn])
                    S.op("dve", lambda: nc.vector.scalar_tensor_tensor(dst[0:rows, :], dst[0:rows, :], mucol, src[0:rows, c0 + 1:c0 + 1 + n], ALU.mult, ALU.add),
                         r=[srcn, dstn, "mu_t"], w=[dstn])

                rf = tmp2("rf"); kf = tmp2("kf"); vf = tmp2("vf")
                kkr = tmp2("kkr"); sqk = tmp2("sqk"); nrm = tmp2("nrm"); kkf = tmp2("kkf")
                kkn = tmp2("kkn", dt=BF16); rbf = tmp2("rbf", dt=BF16)
                ad = tmp2("ad"); sg = tmp2("sg"); dec = tmp2("dec"); tmpa = tmp2("tmpa"); kd = tmp2("kd")
                ksum = tmp2("ksum"); bbf = tmp2("bbf"); bon = tmp2("bon"); gg = tmp2("gg")
                tmb = tmp2("tmb", (128, 4, 128), BF16); tmk = tmp2("tmk", (128, 4, 128), BF16); tmv = tmp2("tmv", (128, 4, 128), BF16)
                inproj_group(24, 128, pfm[0], "pfm0")
                inproj_group(25, 96, pfm[1], "pfm1")
                for ch in range(8):
                    c0 = ch * 512
                    p_ = ch % 2
                    shift(rf[p_], "rf%d" % p_, pfm[0], "pfm0", mu_t[:, 12:13], c0)
                    S.op("act", lambda: nc.scalar.activation(out=Lbf[0:64, c0:c0 + 512], in_=rf[p_][0:64, :], func=AF.Tanh), r=["rf%d" % p_], w=["Lbf"])
                    S.op("pool", lambda: nc.gpsimd.tensor_copy(Lbf[64:128, c0:c0 + 512], rf[p_][64:128, :]), r=["rf%d" % p_], w=["Lbf"])
                    shift(kf[p_], "kf%d" % p_, pfm[1], "pfm1", mu_t[0:96, 13:14], c0, rows=96)
                    S.op("act", lambda: nc.scalar.activation(out=Glb[0:96, c0:c0 + 512], in_=kf[p_][0:96, :], func=AF.Sigmoid), r=["kf%d" % p_], w=["Glb"])
                it = 0
                for g4 in range(4):
                    inproj_group(12 + g4, 128, pfm[0], "pfm0")
                    inproj_group(16 + g4, 128, pfm[1], "pfm1")
                    inproj_group(20 + g4, 128, pfm[2], "pfm2")
                    rows = slice(g4 * 128, (g4 + 1) * 128)
                    for ch in range(8):
                        c0 = ch * 512
                        cs_ = slice(c0, c0 + 512)
                        p_ = it % 2
                        it += 1
                        P = lambda nm: nm + ("0" if nm in singles else str(p_))
                        shift(rf[p_], P("rf"), pfm[0], "pfm0", mu_t[:, g4:g4 + 1], c0)
                        shift(kf[p_], P("kf"), pfm[1], "pfm1", mu_t[:, 4 + g4:5 + g4], c0)
                        shift(vf[p_], P("vf"), pfm[2], "pfm2", mu_t[:, 8 + g4:9 + g4], c0)
                        S.op("dve", lambda: nc.vector.tensor_scalar(kkr[p_][:], kf[p_][:], kk_t[:, g4:g4 + 1], None, ALU.mult), r=[P("kf"), "kk_t"], w=[P("kkr")])
                        S.op("act", lambda: nc.scalar.activation(out=sqk[p_][:], in_=kkr[p_][:], func=AF.Square), r=[P("kkr")], w=[P("sqk")])
                        S.op("pe", lambda: nc.tensor.matmul(ps[2][:], blk[:], sqk[p_][:], start=True, stop=True), r=["blk", P("sqk")], w=[PSN[2]])
                        S.op("act", lambda: nc.scalar.activation(out=nrm[p_][:], in_=ps[2][:], func=AF.Sqrt), r=[PSN[2]], w=[P("nrm")])
                        S.op("dve", lambda: nc.vector.tensor_scalar(nrm[p_][:], nrm[p_][:], 1e-12, None, ALU.max), r=[P("nrm")], w=[P("nrm")])
                        S.op("dve", lambda: nc.vector.reciprocal(nrm[p_][:], nrm[p_][:]), r=[P("nrm")], w=[P("nrm")])
                        S.op("dve", lambda: nc.vector.tensor_tensor(kkf[p_][:], kkr[p_][:], nrm[p_][:], ALU.mult), r=[P("kkr"), P("nrm")], w=[P("kkf")])
                        S.op("pool", lambda: nc.gpsimd.tensor_scalar(kkn[p_][:], kkf[p_][:], -1.0, None, ALU.mult), r=[P("kkf")], w=[P("kkn")])
                        S.op("poolq", lambda: nc.gpsimd.dma_start(out=KKN[rows, cs_], in_=kkn[p_][:]), r=[P("kkn")], w=["KKN"])
                        S.op("pool", lambda: nc.gpsimd.tensor_copy(rbf[p_][:], rf[p_][:]), r=[P("rf")], w=[P("rbf")])
                        S.op("poolq", lambda: nc.gpsimd.dma_start(out=RR[rows, cs_], in_=rbf[p_][:]), r=[P("rbf")], w=["RR"])
                        for d in range(2):
                            S.op("pe", lambda: nc.tensor.matmul(ps[3][:], Lw[:, 2 + d, rows], Lbf[:, cs_], start=True, stop=True), r=["Lw", "Lbf"], w=[PSN[3]])
                            S.op("act", lambda: nc.scalar.activation(out=ad[p_][:], in_=ps[3][:], func=AF.Sigmoid, bias=a0_t[:, d * 4 + g4:d * 4 + g4 + 1]),
                                 r=[PSN[3], "a0_t"], w=[P("ad")])
                            S.op("pe", lambda: nc.tensor.matmul(ps[4][:], Lw[:, d, rows], Lbf[:, cs_], start=True, stop=True), r=["Lw", "Lbf"], w=[PSN[4]])
                            S.op("act", lambda: nc.scalar.activation(out=sg[p_][:], in_=ps[4][:], func=AF.Sigmoid, bias=w0_t[:, d * 4 + g4:d * 4 + g4 + 1]),
                                 r=[PSN[4], "w0_t"], w=[P("sg")])
                            S.op("act", lambda: nc.scalar.activation(out=dec[p_][:], in_=sg[p_][:], func=AF.Exp, scale=-0.6065306597126334),
                                 r=[P("sg")], w=[P("dec")])
                            S.op("poolq", lambda: nc.gpsimd.dma_start(out=DEC[d, rows, cs_], in_=dec[p_][:]), r=[P("dec")], w=["DEC"])
                            S.op("dve", lambda: nc.vector.tensor_scalar(tmpa[p_][:], ad[p_][:], ka_t[:, g4:g4 + 1], oka_t[:, g4:g4 + 1], ALU.mult, ALU.add),
                                 r=[P("ad"), "ka_t", "oka_t"], w=[P("tmpa")])
                            S.op("pool", lambda: nc.gpsimd.tensor_tensor(kd[p_][:], tmpa[p_][:], kf[p_][:], ALU.mult), r=[P("tmpa"), P("kf")], w=[P("kd")])
                            if d == 0:
                                S.op("pool", lambda: nc.gpsimd.tensor_copy(ksum[p_][:], kd[p_][:]), r=[P("kd")], w=[P("ksum")])
                            else:
                                S.op("pool", lambda: nc.gpsimd.tensor_tensor(ksum[p_][:], ksum[p_][:], kd[p_][:], ALU.add), r=[P("kd"), P("ksum")], w=[P("ksum")])
                            S.op("dve", lambda: nc.vector.tensor_tensor(bbf[p_][:], kkf[p_][:], ad[p_][:], ALU.mult), r=[P("kkf"), P("ad")], w=[P("bbf")])
                            for q in range(4):
                                S.op("pe", lambda: nc.tensor.transpose(ps[5][:, q * 128:(q + 1) * 128], bbf[p_][:, q * 128:(q + 1) * 128], ident[:]),
                                     r=[P("bbf"), "ident"], w=[PSN[5]])
                            S.op("act", lambda: nc.scalar.copy(tmb[p_][:], ps[5][:].rearrange("p (q c) -> p q c", q=4)), r=[PSN[5]], w=[P("tmb")])
                            S.op("poolq", lambda: nc.gpsimd.dma_start(out=BT[0, d, cs_, rows].rearrange("(q p) c -> p q c", p=128), in_=tmb[p_][:]),
                                 r=[P("tmb")], w=["BT"])
                            for q in range(4):
                                S.op("pe", lambda: nc.tensor.transpose(ps[6][:, q * 128:(q + 1) * 128], kd[p_][:, q * 128:(q + 1) * 128], ident[:]),
                                     r=[P("kd"), "ident"], w=[PSN[6]])
                            S.op("act", lambda: nc.scalar.copy(tmk[p_][:], ps[6][:].rearrange("p (q c) -> p q c", q=4)), r=[PSN[6]], w=[P("tmk")])
                            S.op("poolq", lambda: nc.gpsimd.dma_start(out=BT[1, d, cs_, rows].rearrange("(q p) c -> p q c", p=128), in_=tmk[p_][:]),
                                 r=[P("tmk")], w=["BT"])
                        S.op("dve", lambda: nc.vector.scalar_tensor_tensor(bon[p_][:], rf[p_][:], rk_t[:, g4:g4 + 1], ksum[p_][:], ALU.mult, ALU.mult),
                             r=[P("rf"), "rk_t", P("ksum")], w=[P("bon")])
                        S.op("pe", lambda: nc.tensor.matmul(ps[7][:], blk[:], bon[p_][:], start=True, stop=True), r=["blk", P("bon")], w=[PSN[7]])
                        S.op("dve", lambda: nc.vector.tensor_tensor(bon[p_][:], ps[7][:], vf[p_][:], ALU.mult), r=[PSN[7], P("vf"), P("bon")], w=[P("bon")])
                        S.op("poolq", lambda: nc.gpsimd.dma_start(out=BON[rows, cs_], in_=bon[p_][:]), r=[P("bon")], w=["BON"])
                        for q in range(4):
                            S.op("pe", lambda: nc.tensor.transpose(ps[5][:, q * 128:(q + 1) * 128], vf[p_][:, q * 128:(q + 1) * 128], ident[:]),
                                 r=[P("vf"), "ident"], w=[PSN[5]])
                        S.op("act", lambda: nc.scalar.copy(tmv[p_][:], ps[5][:].rearrange("p (q c) -> p q c", q=4)), r=[PSN[5]], w=[P("tmv")])
                        S.op("poolq", lambda: nc.gpsimd.dma_start(out=VT[cs_, rows].rearrange("(q p) c -> p q c", p=128), in_=tmv[p_][:]),
                             r=[P("tmv")], w=["VT"])
                        S.op("pe", lambda: nc.tensor.matmul(ps[3][:], Gw[0:96, rows], Glb[0:96, cs_], start=True, stop=True), r=["Gw", "Glb"], w=[PSN[3]])
                        S.op("act", lambda: nc.scalar.copy(gg[p_][:], ps[3][:]), r=[PSN[3]], w=[P("gg")])
                        S.op("poolq", lambda: nc.gpsimd.dma_start(out=GG[rows, cs_], in_=gg[p_][:]), r=[P("gg")], w=["GG"])
                if DBG:
                    for nm_, src_ in (("KKN", KKN), ("RR", RR), ("DEC", DEC), ("BT", BT), ("VT", VT), ("BON", BON), ("GG", GG)):
                        S.op("poolq", lambda: nc.gpsimd.dma_start(out=dbg_rw[nm_], in_=src_), r=[nm_])
                S.barrier()

        S.barrier()
        with ExitStack() as es3:
            sb3 = lambda name, shape, dt=F32: es3.enter_context(nc.sbuf_tensor(name, shape, dt))
            utm = sb3("utm", [128, NT, 512], BF16)
            Yall = sb3("Yall", [128, 64, 512], BF16)
            for n in range(2):
                srcD = UD if n == 0 else ZD
                srcDn = "UD" if n == 0 else "ZD"
                esA = ExitStack()
                ufl = [esA.enter_context(nc.sbuf_tensor("ufl%d_%d" % (i, n), [128, T], F32)) for i in range(2)]
                for g in range(4):
                    fb_ = g % 2
                    S.op("sp", lambda: nc.sync.dma_start(out=ufl[fb_][:], in_=srcD[g, :, :]), r=[srcDn], w=["ufl%d" % fb_])
                    for i4 in range(NT // 4):
                        pb = i4 % 2
                        for q in range(4):
                            i = i4 * 4 + q
                            S.op("pe", lambda: nc.tensor.transpose(ps[pb][:, q * 128:(q + 1) * 128], ufl[fb_][:, i * 128:(i + 1) * 128], ident[:]),
                                 r=["ufl%d" % fb_, "ident"], w=[PSN[pb]])
                        S.op("act", lambda: nc.scalar.copy(utm[:, i4 * 4:(i4 + 1) * 4, g * 128:(g + 1) * 128],
                                                           ps[pb][:].rearrange("p (q c) -> p q c", q=4)),
                             r=[PSN[pb]], w=["utm"])
                S.barrier()
                esA.close()
                esB = ExitStack()
                sbB = lambda name, shape, dt=F32: esB.enter_context(nc.sbuf_tensor(name + "_%d" % n, shape, dt))
                dre = [sbB("dre%d" % i, [128, 32, 128], BF16) for i in range(2)]
                dim_ = [sbB("dim%d" % i, [128, 32, 128], BF16) for i in range(2)]
                kre = [sbB("kre%d" % i, [128, 512]) for i in range(2)]
                kim = [sbB("kim%d" % i, [128, 512]) for i in range(2)]
                ure = sbB("ure", [128, 512])
                uim = sbB("uim", [128, 512])
                t1 = sbB("t1", [128, 512])
                t2 = sbB("t2", [128, 512])
                t3 = sbB("t3", [128, 512])
                t4 = sbB("t4", [128, 512])
                for j in range(32):
                    b = j % 2
                    S.op("sp", lambda: nc.sync.dma_start(out=dre[b][:], in_=dfw[j]), w=["dre%d" % b])
                    S.op("sp", lambda: nc.sync.dma_start(out=dim_[b][:], in_=dfw[32 + j]), w=["dim%d" % b])
                    S.op("sp", lambda: nc.sync.dma_start(out=kre[b][:], in_=KF[j, :, n * 512:(n + 1) * 512]), r=["KF"], w=["kre%d" % b])
                    S.op("sp", lambda: nc.sync.dma_start(out=kim[b][:], in_=KF[32 + j, :, n * 512:(n + 1) * 512]), r=["KF"], w=["kim%d" % b])
                    pr, pi = 2 + 2 * b, 3 + 2 * b
                    for tcn in range(NT):
                        S.op("pe", lambda: nc.tensor.matmul(ps[pr][:], dre[b][:, tcn, :], utm[:, tcn, :], start=(tcn == 0), stop=(tcn == NT - 1)),
                             r=["dre%d" % b, "utm"], w=[PSN[pr]])
                    for tcn in range(NT):
                        S.op("pe", lambda: nc.tensor.matmul(ps[pi][:], dim_[b][:, tcn, :], utm[:, tcn, :], start=(tcn == 0), stop=(tcn == NT - 1)),
                             r=["dim%d" % b, "utm"], w=[PSN[pi]])
                    S.op("act", lambda: nc.scalar.copy(ure[:], ps[pr][:]), r=[PSN[pr]], w=["ure"])
                    S.op("act", lambda: nc.scalar.copy(uim[:], ps[pi][:]), r=[PSN[pi]], w=["uim"])
                    S.op("dve", lambda: nc.vector.tensor_tensor(t1[:], ure[:], kre[b][:], ALU.mult), r=["ure", "kre%d" % b], w=["t1"])
                    S.op("pool", lambda: nc.gpsimd.tensor_tensor(t2[:], uim[:], kim[b][:], ALU.mult), r=["uim", "kim%d" % b], w=["t2"])
                    S.op("dve", lambda: nc.vector.tensor_tensor(Yall[:, j, :], t1[:], t2[:], ALU.subtract), r=["t1", "t2"], w=["Yall"])
                    S.op("pool", lambda: nc.gpsimd.tensor_tensor(t3[:], ure[:], kim[b][:], ALU.mult), r=["ure", "kim%d" % b], w=["t3"])
                    S.op("dve", lambda: nc.vector.tensor_tensor(t4[:], uim[:], kre[b][:], ALU.mult), r=["uim", "kre%d" % b], w=["t4"])
                    S.op("pool", lambda: nc.gpsimd.tensor_tensor(Yall[:, 32 + j, :], t3[:], t4[:], ALU.add), r=["t3", "t4"], w=["Yall"])
                    if j == 0:
                        S.op("dve", lambda: nc.vector.tensor_tensor(Yall[0:1, 0, :], ure[0:1, :], kre[b][0:1, :], ALU.mult),
                             r=["ure", "kre%d" % b, "Yall"], w=["Yall"])
                        S.op("dve", lambda: nc.vector.tensor_tensor(Yall[0:1, 32, :], uim[0:1, :], kim[b][0:1, :], ALU.mult),
                             r=["uim", "kim%d" % b, "Yall"], w=["Yall"])
                S.barrier()
                esB.close()
                esC = ExitStack()
                sbC = lambda name, shape, dt=F32: esC.enter_context(nc.sbuf_tensor(name + "_%d" % n, shape, dt))
                dvt = sbC("dvt", [128, 64, 256], BF16)
                gate = [sbC("gate%d" % i, [128, 256]) for i in range(2)]
                zo = [sbC("zo%d" % i, [128, 256]) for i in range(2)]
                dstD = ZD if n == 0 else YD
                dstDn = "ZD" if n == 0 else "YD"
                for tq in range(16):
                    S.op("sp", lambda: nc.sync.dma_start(out=dvt[:], in_=dinv[tq]), w=["dvt"])
                    for g in range(4):
                        gb = g % 2
                        pb = 4 + g
                        gsrc = UD[4 + g] if n == 0 else UD[8 + g]
                        S.op("sp", lambda: nc.sync.dma_start(out=gate[gb][:], in_=gsrc[:, tq * 256:(tq + 1) * 256]), r=["UD"], w=["gate%d" % gb])
                        for rc in range(64):
                            S.op("pe", lambda: nc.tensor.matmul(ps[pb][:, 0:256], Yall[:, rc, g * 128:(g + 1) * 128], dvt[:, rc, :],
                                                                start=(rc == 0), stop=(rc == 63)),
                                 r=["Yall", "dvt"], w=[PSN[pb]])
                        S.op("dve", lambda: nc.vector.tensor_tensor(zo[gb][:], ps[pb][:, 0:256], gate[gb][:], ALU.mult),
                             r=[PSN[pb], "gate%d" % gb], w=["zo%d" % gb])
                        S.op("poolq", lambda: nc.gpsimd.dma_start(out=dstD[g, :, tq * 256:(tq + 1) * 256], in_=zo[gb][:]), r=["zo%d" % gb], w=[dstDn])
                S.barrier()
                esC.close()
            if DBG:
                S.op("poolq", lambda: nc.gpsimd.dma_start(out=dbg_y[:, :, :], in_=YD[:, :, :]), r=["YD"])

        S.barrier()
        with ExitStack() as esS:
            sbS = lambda name, shape, dt=F32: esS.enter_context(nc.sbuf_tensor(name, shape, dt))
            TC = 64
            St = sbS("St", [128, 512])
            Sbf = sbS("Sbf", [128, 512], BF16)
            rhs2 = sbS("rhs2", [48, 512], BF16)
            Vtm = sbS("Vtm", [128, NT, 512], BF16)
            m48 = sbS("m48_sb", [48, 512])
            m40 = sbS("m40_sb", [40, 1, 512])
            selF = sbS("selF_sb", [128, 128, 48], BF16)
            selB = sbS("selB_sb", [128, 128, 48], BF16)
            ARraw = sbS("ARraw", [128, 104, TC], BF16)
            AR2 = [sbS("AR2_%d" % i, [128, TC, 104], BF16) for i in range(2)]
            Wraw = sbS("Wraw", [128, 8, TC])
            W2 = [sbS("W2_%d" % i, [128, TC, 8, 1]) for i in range(2)]
            BKraw = sbS("BKraw", [48, TC, 128], BF16)
            BK2 = [sbS("BK2_%d" % i, [48, TC, 128], BF16) for i in range(2)]
            Oraw = [sbS("Oraw%d" % i, [40, 8, 512]) for i in range(2)]
            Ored = [sbS("Ored%d" % i, [40, 8, 64]) for i in range(2)]
            S.op("pool", lambda: nc.gpsimd.memset(St[:], 0.0), w=["St"])
            S.op("pool", lambda: nc.gpsimd.memset(Sbf[:], 0.0), w=["Sbf"])
            S.op("pool", lambda: nc.gpsimd.memset(rhs2[:], 0.0), w=["rhs2"])
            S.op("pool", lambda: nc.gpsimd.memset(ARraw[:], 0.0), w=["ARraw"])
            S.op("pool", lambda: nc.gpsimd.memset(BKraw[:], 0.0), w=["BKraw"])
            for i in range(2):
                S.op("pool", lambda: nc.gpsimd.memset(BK2[i][:], 0.0), w=["BK2_%d" % i])
            S.op("sp", lambda: nc.sync.dma_start(out=Vtm[:], in_=VT.rearrange("(i p) c -> p i c", p=128)), r=["VT"], w=["Vtm"])
            S.op("sp", lambda: nc.sync.dma_start(out=m48[:], in_=m48_in[:, :]), w=["m48"])
            S.op("sp", lambda: nc.sync.dma_start(out=m40[:, 0, :], in_=m40_in[:, :]), w=["m40"])
            S.op("sp", lambda: nc.sync.dma_start(out=selF[:], in_=selF_in[:, :, :]), w=["selF"])
            S.op("sp", lambda: nc.sync.dma_start(out=selB[:], in_=selB_in[:, :, :]), w=["selB"])
            KKN_v = KKN.rearrange("(j k) t -> k j t", k=64)
            RR_v = RR.rearrange("(j k) t -> k j t", k=64)
            for c in range(SCAN_CHUNKS):
                cb = c % 2
                s0 = c * TC
                fw_ = slice(s0, s0 + TC)
                bw_ = slice(T - s0 - TC, T - s0)
                A2n, W2n, B2n = "AR2_%d" % cb, "W2_%d" % cb, "BK2_%d" % cb
                S.op("sp", lambda: nc.sync.dma_start(out=ARraw[0:64, 0:8, :], in_=KKN_v[:, :, fw_]), r=["KKN"], w=["ARraw"])
                S.op("sp", lambda: nc.sync.dma_start(out=ARraw[64:128, 32:40, :], in_=KKN_v[:, :, bw_]), r=["KKN"], w=["ARraw"])
                S.op("sp", lambda: nc.sync.dma_start(out=ARraw[0:64, 64:72, :], in_=RR_v[:, :, fw_]), r=["RR"], w=["ARraw"])
                S.op("sp", lambda: nc.sync.dma_start(out=ARraw[64:128, 96:104, :], in_=RR_v[:, :, bw_]), r=["RR"], w=["ARraw"])
                S.op("pool", lambda: nc.gpsimd.tensor_copy(AR2[cb][0:64], ARraw[0:64].rearrange("p c t -> p t c")), r=["ARraw"], w=[A2n])
                S.op("pool", lambda: nc.gpsimd.tensor_copy(AR2[cb][64:128], ARraw[64:128, :, ::-1].rearrange("p c t -> p t c")), r=["ARraw"], w=[A2n])
                S.op("sp", lambda: nc.sync.dma_start(out=Wraw[0:64], in_=DEC[0].rearrange("(j k) t -> k j t", k=64)[:, :, fw_]), r=["DEC"], w=["Wraw"])
                S.op("sp", lambda: nc.sync.dma_start(out=Wraw[64:128], in_=DEC[1].rearrange("(j k) t -> k j t", k=64)[:, :, bw_]), r=["DEC"], w=["Wraw"])
                S.op("pool", lambda: nc.gpsimd.tensor_copy(W2[cb][0:64, :, :, 0], Wraw[0:64].rearrange("p j t -> p t j")), r=["Wraw"], w=[W2n])
                S.op("pool", lambda: nc.gpsimd.tensor_copy(W2[cb][64:128, :, :, 0], Wraw[64:128, :, ::-1].rearrange("p j t -> p t j")), r=["Wraw"], w=[W2n])
                for s_ in range(2):
                    S.op("sp", lambda: nc.sync.dma_start(out=BKraw[s_ * 8:(s_ + 1) * 8, :, 0:64],
                                                         in_=BT[s_, 0, fw_, :].rearrange("t (j k) -> j t k", k=64)), r=["BT"], w=["BKraw"])
                    S.op("sp", lambda: nc.sync.dma_start(out=BKraw[32 + s_ * 8:32 + (s_ + 1) * 8, :, 64:128],
                                                         in_=BT[s_, 1, bw_, :].rearrange("t (j k) -> j t k", k=64)), r=["BT"], w=["BKraw"])
                S.op("pool", lambda: nc.gpsimd.tensor_copy(BK2[cb][0:16], BKraw[0:16]), r=["BKraw"], w=[B2n])
                S.op("pool", lambda: nc.gpsimd.tensor_copy(BK2[cb][32:48], BKraw[32:48, ::-1, :]), r=["BKraw"], w=[B2n])
                for s in range(TC):
                    st = s0 + s
                    i_f, tl = st // 128, st % 128
                    i_b = NT - 1 - i_f
                    pA, pB, pO = st % 2, 2 + st % 2, 4 + st % 2
                    ob = (st // 8) % 2
                    S.op("pe", lambda: nc.tensor.matmul(ps[pA][0:48, :], selF[:, tl, :], Vtm[:, i_f, :], start=True, stop=False),
                         r=["selF", "Vtm"], w=[PSN[pA]])
                    S.op("pe", lambda: nc.tensor.matmul(ps[pA][0:48, :], selB[:, tl, :], Vtm[:, i_b, :], start=False, stop=False),
                         r=["selB", "Vtm"], w=[PSN[pA]])
                    S.op("pe", lambda: nc.tensor.matmul(ps[pA][0:48, :], AR2[cb][:, s, 0:48], Sbf[:], start=False, stop=True),
                         r=[A2n, "Sbf"], w=[PSN[pA]])
                    S.op("dve", lambda: nc.vector.tensor_tensor(rhs2[:], ps[pA][0:48, :], m48[:], ALU.mult), r=[PSN[pA], "m48"], w=["rhs2"])
                    S.op("dve", lambda: nc.vector.tensor_tensor(St[:].rearrange("p (j v) -> p j v", j=8), St[:].rearrange("p (j v) -> p j v", j=8),
                                                                W2[cb][:, s, :, :].to_broadcast([128, 8, 64]), ALU.mult),
                         r=["St", W2n], w=["St"])
                    S.op("pe", lambda: nc.tensor.matmul(ps[pB][:], BK2[cb][0:48, s, :], rhs2[:], start=True, stop=True), r=[B2n, "rhs2"], w=[PSN[pB]])
                    S.op("dve", lambda: nc.vector.tensor_tensor(St[:], St[:], ps[pB][:], ALU.add), r=["St", PSN[pB]], w=["St"])
                    S.op("act", lambda: nc.scalar.copy(Sbf[:], St[:]), r=["St"], w=["Sbf"])
                    S.op("pe", lambda: nc.tensor.matmul(ps[pO][0:40, :], AR2[cb][:, s, 64:104], Sbf[:], start=True, stop=True), r=[A2n, "Sbf"], w=[PSN[pO]])
                    S.op("act", lambda: nc.scalar.copy(Oraw[ob][:, st % 8, :], ps[pO][0:40, :]), r=[PSN[pO]], w=["Oraw%d" % ob])
                    if st % 8 == 7:
                        st0 = st - 7
                        S.op("pool", lambda: nc.gpsimd.tensor_tensor(Oraw[ob][:], Oraw[ob][:], m40[:].to_broadcast([40, 8, 512]), ALU.mult),
                             r=["Oraw%d" % ob, "m40"], w=["Oraw%d" % ob])
                        S.op("dve", lambda: nc.vector.tensor_reduce(out=Ored[ob][:], in_=Oraw[ob][:].rearrange("p s (j v) -> p s v j", j=8),
                                                                     axis=AX.X, op=ALU.add),
                             r=["Oraw%d" % ob], w=["Ored%d" % ob])
                        S.op("poolq", lambda: nc.gpsimd.dma_start(out=OD[0, st0:st0 + 8, :].rearrange("s (j v) -> j s v", v=64), in_=Ored[ob][0:8]),
                             r=["Ored%d" % ob], w=["OD"])
                        S.op("poolq", lambda: nc.gpsimd.dma_start(out=OD[1, st0:st0 + 8, :].rearrange("s (j v) -> j s v", v=64), in_=Ored[ob][32:40]),
                             r=["Ored%d" % ob], w=["OD"])
            if DBG:
                S.op("poolq", lambda: nc.gpsimd.dma_start(out=dbg_od, in_=OD), r=["OD"])
            S.barrier()

        with ExitStack() as esP:
            sbP = lambda name, shape, dt=F32: esP.enter_context(nc.sbuf_tensor(name, shape, dt))
            pvec = lambda ap_row, p=128: ap_row.rearrange("o (g p) -> p (o g)", p=p)
            yT = sbP("yT", [128, 8, T], BF16)
            ldb = [sbP("ldb%d" % i, [128, 1024]) for i in range(2)]
            n = 0
            for g in range(4):
                for c4 in range(4):
                    b = n % 2
                    n += 1
                    S.op("sp", lambda: nc.sync.dma_start(out=ldb[b][:], in_=YD[g, :, c4 * 1024:(c4 + 1) * 1024]), r=["YD"], w=["ldb%d" % b])
                    S.op("pool", lambda: nc.gpsimd.tensor_copy(yT[:, g, c4 * 1024:(c4 + 1) * 1024], ldb[b][:]), r=["ldb%d" % b], w=["yT"])
            blk2 = sbP("blk2", [128, 128])
            lnw_t = sbP("lnw_t", [128, 4])
            lnb_t = sbP("lnb_t", [128, 4])
            S.op("sp", lambda: nc.sync.dma_start(out=blk2[:], in_=blk_in[:, :]), w=["blk2"])
            S.op("sp", lambda: nc.sync.dma_start(out=lnw_t[:], in_=pvec(rw_ln_w[0:1, :])), w=["lnw_t"])
            S.op("sp", lambda: nc.sync.dma_start(out=lnb_t[:], in_=pvec(rw_ln_b[0:1, :])), w=["lnb_t"])
            oft = [sbP("oft%d" % i, [128, 4, 128]) for i in range(2)]
            obt = [sbP("obt%d" % i, [128, 4, 128]) for i in range(2)]
            bonc = [sbP("bonc%d" % i, [128, 512]) for i in range(2)]
            ggc = [sbP("ggc%d" % i, [128, 512]) for i in range(2)]
            obr = sbP("obr", [128, 4, 128])
            sfm = sbP("sfm", [128, 512])
            cen = sbP("cen", [128, 512])
            sqp = sbP("sqp", [128, 512])
            rstd = sbP("rstd", [128, 512])
            n = 0
            for g4 in range(4):
                rows = slice(g4 * 128, (g4 + 1) * 128)
                for i4 in range(8):
                    b = n % 2
                    n += 1
                    cs_ = slice(i4 * 512, (i4 + 1) * 512)
                    S.op("sp", lambda: nc.sync.dma_start(out=oft[b][:], in_=OD[0, cs_, rows].rearrange("(q p) c -> p q c", p=128)), r=["OD"], w=["oft%d" % b])
                    S.op("sp", lambda: nc.sync.dma_start(out=obt[b][:], in_=OD[1, T - (i4 + 1) * 512:T - i4 * 512, rows].rearrange("(m p) c -> p m c", p=128)),
                         r=["OD"], w=["obt%d" % b])
                    S.op("sp", lambda: nc.sync.dma_start(out=bonc[b][:], in_=BON[rows, cs_]), r=["BON"], w=["bonc%d" % b])
                    S.op("sp", lambda: nc.sync.dma_start(out=ggc[b][:], in_=GG[rows, cs_]), r=["GG"], w=["ggc%d" % b])
                    for q in range(4):
                        S.op("pe", lambda: nc.tensor.transpose(ps[0][:, q * 128:(q + 1) * 128], oft[b][:, q, :], ident[:]), r=["oft%d" % b, "ident"], w=[PSN[0]])
                    for q in range(4):
                        S.op("pe", lambda: nc.tensor.transpose(ps[1][:, q * 128:(q + 1) * 128], obt[b][:, 3 - q, :], ident[:]), r=["obt%d" % b, "ident"], w=[PSN[1]])
                    S.op("act", lambda: nc.scalar.copy(obr[:], ps[1][:].rearrange("p (q t) -> p q t", q=4)[:, :, ::-1]), r=[PSN[1]], w=["obr"])
                    S.op("dve", lambda: nc.vector.tensor_tensor(sfm[:], ps[0][:], obr[:].rearrange("p q t -> p (q t)"), ALU.add), r=[PSN[0], "obr"], w=["sfm"])
                    S.op("pe", lambda: nc.tensor.matmul(ps[2][:], blk2[:], sfm[:], start=True, stop=True), r=["blk2", "sfm"], w=[PSN[2]])
                    S.op("dve", lambda: nc.vector.scalar_tensor_tensor(cen[:], ps[2][:], -1.0 / 64, sfm[:], ALU.mult, ALU.add), r=[PSN[2], "sfm"], w=["cen"])
                    S.op("act", lambda: nc.scalar.activation(out=sqp[:], in_=cen[:], func=AF.Square), r=["cen"], w=["sqp"])
                    S.op("pe", lambda: nc.tensor.matmul(ps[3][:], blk2[:], sqp[:], start=True, stop=True), r=["blk2", "sqp"], w=[PSN[3]])
                    S.op("dve", lambda: nc.vector.tensor_scalar(rstd[:], ps[3][:], 1.0 / 64, 64e-5, ALU.mult, ALU.add), r=[PSN[3]], w=["rstd"])
                    S.op("act", lambda: nc.scalar.activation(out=rstd[:], in_=rstd[:], func=AF.Sqrt), r=["rstd"], w=["rstd"])
                    S.op("dve", lambda: nc.vector.reciprocal(rstd[:], rstd[:]), r=["rstd"], w=["rstd"])
                    S.op("dve", lambda: nc.vector.tensor_tensor(cen[:], cen[:], rstd[:], ALU.mult), r=["cen", "rstd"], w=["cen"])
                    S.op("dve", lambda: nc.vector.tensor_scalar(cen[:], cen[:], lnw_t[:, g4:g4 + 1], lnb_t[:, g4:g4 + 1], ALU.mult, ALU.add),
                         r=["cen", "lnw_t", "lnb_t"], w=["cen"])
                    S.op("pool", lambda: nc.gpsimd.tensor_tensor(cen[:], cen[:], bonc[b][:], ALU.add), r=["cen", "bonc%d" % b], w=["cen"])
                    S.op("pool", lambda: nc.gpsimd.tensor_tensor(yT[:, 4 + g4, cs_], cen[:], ggc[b][:], ALU.mult), r=["cen", "ggc%d" % b], w=["yT"])
            wo = sbP("wo", [128, 8, D], BF16)
            g1t = sbP("g1t", [128, D])
            xin = [sbP("xin%d" % i, [128, D]) for i in range(2)]
            x1t = [sbP("x1t%d" % i, [128, D]) for i in range(2)]
            for kc in range(8):
                b = kc % 2
                S.op("sp", lambda: nc.sync.dma_start(out=ldb[b][:], in_=w_out[kc * 128:(kc + 1) * 128, :]), w=["ldb%d" % b])
                S.op("pool", lambda: nc.gpsimd.tensor_copy(wo[:, kc, :], ldb[b][:]), r=["ldb%d" % b], w=["wo"])
            S.op("sp", lambda: nc.sync.dma_start(out=g1t[:], in_=MODD[:, 2 * D:3 * D]), r=["MODD"], w=["g1t"])
            for i in range(NT):
                b = i % 2
                S.op("sp", lambda: nc.sync.dma_start(out=xin[b][:], in_=x[i * 128:(i + 1) * 128, :]), w=["xin%d" % b])
                for half in range(2):
                    hs = slice(half * 512, (half + 1) * 512)
                    for kc in range(8):
                        S.op("pe", lambda: nc.tensor.matmul(ps[4 + half][:], yT[:, kc, i * 128:(i + 1) * 128], wo[:, kc, hs], start=(kc == 0), stop=(kc == 7)),
                             r=["yT", "wo"], w=[PSN[4 + half]])
                    S.op("dve", lambda: nc.vector.tensor_tensor(x1t[b][:, hs], ps[4 + half][:], g1t[:, hs], ALU.mult), r=[PSN[4 + half], "g1t"], w=["x1t%d" % b])
                    S.op("pool", lambda: nc.gpsimd.tensor_tensor(x1t[b][:, hs], x1t[b][:, hs], xin[b][:, hs], ALU.add), r=["x1t%d" % b, "xin%d" % b], w=["x1t%d" % b])
                S.op("poolq", lambda: nc.gpsimd.dma_start(out=X1D[i * 128:(i + 1) * 128, :], in_=x1t[b][:]), r=["x1t%d" % b], w=["X1D"])
            if DBG:
                S.op("poolq", lambda: nc.gpsimd.dma_start(out=dbg_x1, in_=X1D), r=["X1D"])
            S.barrier()

        NE = 256
        CAP = 1024
        NST = CAP // 128
        with ExitStack() as esF:
            sbF = lambda name, shape, dt=F32: esF.enter_context(nc.sbuf_tensor(name, shape, dt))
            g2t = sbF("g2t", [128, D])
            S.op("sp", lambda: nc.sync.dma_start(out=g2t[:], in_=MODD[:, 5 * D:6 * D]), r=["MODD"], w=["g2t"])
            keys = sbF("keys", [128, 2, CAP])
            idxI = sbF("idxI", [128, NST, NE], I32)
            with ExitStack() as esG:
                sbG = lambda name, shape, dt=F32: esG.enter_context(nc.sbuf_tensor(name, shape, dt))
                gm2 = sbG("gm2", [128, D]); sc2 = sbG("sc2", [128, D]); sh2 = sbG("sh2", [128, D])
                S.op("sp", lambda: nc.sync.dma_start(out=gm2[:], in_=bcast_rows(norm2_g[0:1, :])), w=["gm2"])
                S.op("sp", lambda: nc.sync.dma_start(out=sh2[:], in_=MODD[:, 3 * D:4 * D]), r=["MODD"], w=["sh2"])
                S.op("sp", lambda: nc.sync.dma_start(out=sc2[:], in_=MODD[:, 4 * D:5 * D]), r=["MODD"], w=["sc2"])
                S.op("dve", lambda: nc.vector.scalar_tensor_tensor(gm2[:], sc2[:], 1.0, gm2[:], ALU.add, ALU.mult), r=["sc2", "gm2"], w=["gm2"])
                rwt = sbG("rwt", [128, 8, NE])
                S.op("sp", lambda: nc.sync.dma_start(out=rwt[:], in_=router_w.rearrange("(kc p) n -> p kc n", p=128)), w=["rwt"])
                rbias = sbG("rbias", [128, NE])
                S.op("sp", lambda: nc.sync.dma_start(out=rbias[:], in_=bcast_rows(router_bias[0:1, :])), w=["rbias"])
                tokid = sbG("tokid_sb", [128, NT])
                S.op("sp", lambda: nc.sync.dma_start(out=tokid[:], in_=tokid_in[:, :]), w=["tokid"])
                zrow = sbG("zrow", [1, D + NE])
                S.op("pool", lambda: nc.gpsimd.memset(zrow[:], 0.0), w=["zrow"])
                S.op("poolq", lambda: nc.gpsimd.dma_start(out=H2E[T:T + 1, :], in_=zrow[:]), r=["zrow"], w=["H2E"])
                S.op("poolq", lambda: nc.gpsimd.dma_start(out=OUTACC[T:T + 1, :], in_=zrow[:, 0:D]), r=["zrow"], w=["OUTACC"])
                shgu = sbG("shgu", [128, 8, 512], BF16)
                shd = sbG("shd", [128, 2, D], BF16)
                stg = sbG("stg", [128, 8, 256])
                S.op("sp", lambda: nc.sync.dma_start(out=stg[:], in_=sh_w_gate.rearrange("(kc p) n -> p kc n", p=128)), w=["stg"])
                S.op("pool", lambda: nc.gpsimd.tensor_copy(shgu[:, :, 0:256], stg[:]), r=["stg"], w=["shgu"])
                S.op("sp", lambda: nc.sync.dma_start(out=stg[:], in_=sh_w_up.rearrange("(kc p) n -> p kc n", p=128)), w=["stg"])
                S.op("pool", lambda: nc.gpsimd.tensor_copy(shgu[:, :, 256:512], stg[:]), r=["stg"], w=["shgu"])
                S.op("sp", lambda: nc.sync.dma_start(out=stg[:].rearrange("p a b -> p (a b)").rearrange("p (j n) -> p j n", j=2),
                                                     in_=sh_w_down.rearrange("(jc p) n -> p jc n", p=128)), w=["stg"])
                S.op("pool", lambda: nc.gpsimd.tensor_copy(shd[:], stg[:].rearrange("p a b -> p (a b)").rearrange("p (j n) -> p j n", j=2)), r=["stg"], w=["shd"])
                KT = sbG("KT", [128, 2, T])
                xt2 = [sbG("xt2_%d" % i, [128, D]) for i in range(2)]
                h2 = [sbG("h2_%d" % i, [128, D]) for i in range(2)]
                sq2 = sbG("sq2", [128, D])
                ss2 = [sbG("ss2_%d" % i, [128, 1]) for i in range(2)]
                rs2 = [sbG("rs2_%d" % i, [128, 1]) for i in range(2)]
                h2T = sbG("h2T", [128, 8, 128])
                h2Tb = sbG("h2Tb", [128, 8, 128], BF16)
                sc = sbG("sc", [128, NE]); bia = sbG("bia", [128, NE]); msk = sbG("msk", [128, NE]); sel = sbG("sel", [128, NE])
                gat = [sbG("gat%d" % i, [128, NE]) for i in range(2)]
                key = sbG("key", [128, NE])
                m8 = sbG("m8", [128, 8, 8]); gs = sbG("gs", [128, 8]); gm8 = sbG("gm8", [128, 8]); gmk = sbG("gmk", [128, 8, 1]); pen = sbG("pen", [128, 8, 1])
                t8 = sbG("t8", [128, 8]); den = sbG("den", [128, 1])
                sgs = sbG("sgs", [128, 256]); acs = sbG("acs", [128, 256]); acT = sbG("acT", [128, 2, 128], BF16)
                acc = [sbG("acc%d" % i, [128, D]) for i in range(2)]
                for i in range(NT):
                    b = i % 2
                    tsl = slice(i * 128, (i + 1) * 128)
                    S.op("sp", lambda: nc.sync.dma_start(out=xt2[b][:], in_=X1D[tsl, :]), r=["X1D"], w=["xt2_%d" % b])
                    rstd_of(sq2, ss2[b], rs2[b], "n2_%d" % b, xt2[b][:], "xt2_%d" % b)
                    S.op("dve", lambda: nc.vector.scalar_tensor_tensor(h2[b][:], xt2[b][:], rs2[b][:, 0:1], gm2[:], ALU.mult, ALU.mult),
                         r=["xt2_%d" % b, "rsn2_%d" % b, "gm2"], w=["h2_%d" % b])
                    S.op("pool", lambda: nc.gpsimd.tensor_tensor(h2[b][:], h2[b][:], sh2[:], ALU.add), r=["h2_%d" % b, "sh2"], w=["h2_%d" % b])
                    S.op("poolq", lambda: nc.gpsimd.dma_start(out=H2E[tsl, 0:D], in_=h2[b][:]), r=["h2_%d" % b], w=["H2E"])
                    for half in range(2):
                        pb = half
                        for q in range(4):
                            kc = half * 4 + q
                            S.op("pe", lambda: nc.tensor.transpose(ps[pb][:, q * 128:(q + 1) * 128], h2[b][:, kc * 128:(kc + 1) * 128], ident[:]),
                                 r=["h2_%d" % b, "ident"], w=[PSN[pb]])
                        S.op("act", lambda: nc.scalar.copy(h2T[:, half * 4:(half + 1) * 4, :], ps[pb][:].rearrange("p (q t) -> p q t", q=4)), r=[PSN[pb]], w=["h2T"])
                    S.op("pool", lambda: nc.gpsimd.tensor_copy(h2Tb[:], h2T[:]), r=["h2T"], w=["h2Tb"])
                    for kc in range(8):
                        S.op("pe", lambda: nc.tensor.matmul(ps[2][:, 0:NE], h2T[:, kc, :], rwt[:, kc, :], start=(kc == 0), stop=(kc == 7)), r=["h2T", "rwt"], w=[PSN[2]])
                    S.op("act", lambda: nc.scalar.activation(out=sc[:], in_=ps[2][:, 0:NE], func=AF.Sigmoid), r=[PSN[2]], w=["sc"])
                    S.op("dve", lambda: nc.vector.tensor_tensor(bia[:], sc[:], rbias[:], ALU.add), r=["sc", "rbias"], w=["bia"])
                    for g in range(8):
                        S.op("dve", lambda: nc.vector.max(out=m8[:, g, :], in_=bia[:, g * 32:(g + 1) * 32]), r=["bia"], w=["m8"])
                    S.op("dve", lambda: nc.vector.tensor_tensor(gs[:], m8[:, :, 0], m8[:, :, 1], ALU.add), r=["m8"], w=["gs"])
                    S.op("dve", lambda: nc.vector.max(out=gm8[:], in_=gs[:]), r=["gs"], w=["gm8"])
                    S.op("dve", lambda: nc.vector.tensor_scalar(gmk[:, :, 0], gs[:], gm8[:, 3:4], None, ALU.is_ge), r=["gs", "gm8"], w=["gmk"])
                    S.op("dve", lambda: nc.vector.tensor_scalar(pen[:, :, 0], gmk[:, :, 0], 1e9, -1e9, ALU.mult, ALU.add), r=["gmk"], w=["pen"])
                    S.op("dve", lambda: nc.vector.tensor_tensor(msk[:].rearrange("p (g e) -> p g e", g=8), bia[:].rearrange("p (g e) -> p g e", g=8),
                                                                gmk[:].to_broadcast([128, 8, 32]), ALU.mult), r=["bia", "gmk"], w=["msk"])
                    S.op("dve", lambda: nc.vector.tensor_tensor(msk[:].rearrange("p (g e) -> p g e", g=8), msk[:].rearrange("p (g e) -> p g e", g=8),
                                                                pen[:].to_broadcast([128, 8, 32]), ALU.add), r=["msk", "pen"], w=["msk"])
                    S.op("dve", lambda: nc.vector.max(out=t8[:], in_=msk[:]), r=["msk"], w=["t8"])
                    S.op("dve", lambda: nc.vector.tensor_scalar(sel[:], msk[:], t8[:, 7:8], None, ALU.is_ge), r=["msk", "t8"], w=["sel"])
                    S.op("dve", lambda: nc.vector.tensor_tensor(gat[b][:], sc[:], sel[:], ALU.mult), r=["sc", "sel"], w=["gat%d" % b])
                    S.op("dve", lambda: nc.vector.tensor_reduce(out=den[:], in_=gat[b][:], axis=AX.X, op=ALU.add), r=["gat%d" % b], w=["den"])
                    S.op("dve", lambda: nc.vector.reciprocal(den[:], den[:]), r=["den"], w=["den"])
                    S.op("dve", lambda: nc.vector.tensor_scalar(gat[b][:], gat[b][:], den[:, 0:1], 2.5, ALU.mult, ALU.mult), r=["gat%d" % b, "den"], w=["gat%d" % b])
                    S.op("poolq", lambda: nc.gpsimd.dma_start(out=H2E[tsl, D:D + NE], in_=gat[b][:]), r=["gat%d" % b], w=["H2E"])
                    S.op("dve", lambda: nc.vector.tensor_scalar(key[:], sel[:], tokid[:, i:i + 1], None, ALU.mult), r=["sel", "tokid"], w=["key"])
                    for eh in range(2):
                        S.op("pe", lambda: nc.tensor.transpose(ps[3][:, eh * 128:(eh + 1) * 128], key[:, eh * 128:(eh + 1) * 128], ident[:]), r=["key", "ident"], w=[PSN[3]])
                    S.op("act", lambda: nc.scalar.copy(KT[:, :, tsl], ps[3][:, 0:256].rearrange("p (e t) -> p e t", e=2)), r=[PSN[3]], w=["KT"])
                    for kc in range(8):
                        S.op("pe", lambda: nc.tensor.matmul(ps[4][:], h2Tb[:, kc, :], shgu[:, kc, :], start=(kc == 0), stop=(kc == 7)), r=["h2Tb", "shgu"], w=[PSN[4]])
                    S.op("act", lambda: nc.scalar.activation(out=sgs[:], in_=ps[4][:, 0:256], func=AF.Silu), r=[PSN[4]], w=["sgs"])
                    S.op("dve", lambda: nc.vector.tensor_tensor(acs[:], sgs[:], ps[4][:, 256:512], ALU.mult), r=["sgs", PSN[4]], w=["acs"])
                    for jc in range(2):
                        S.op("pe", lambda: nc.tensor.transpose(ps[5][:, jc * 128:(jc + 1) * 128], acs[:, jc * 128:(jc + 1) * 128], ident[:]), r=["acs", "ident"], w=[PSN[5]])
                    S.op("act", lambda: nc.scalar.copy(acT[:], ps[5][:, 0:256].rearrange("p (j t) -> p j t", j=2)), r=[PSN[5]], w=["acT"])
                    for half in range(2):
                        hs = slice(half * 512, (half + 1) * 512)
                        for jc in range(2):
                            S.op("pe", lambda: nc.tensor.matmul(ps[6 + half][:], acT[:, jc, :], shd[:, jc, hs], start=(jc == 0), stop=(jc == 1)), r=["acT", "shd"], w=[PSN[6 + half]])
                        S.op("dve", lambda: nc.vector.tensor_tensor(acc[b][:, hs], ps[6 + half][:], g2t[:, hs], ALU.mult), r=[PSN[6 + half], "g2t"], w=["acc%d" % b])
                        S.op("pool", lambda: nc.gpsimd.tensor_tensor(acc[b][:, hs], acc[b][:, hs], xt2[b][:, hs], ALU.add), r=["acc%d" % b, "xt2_%d" % b], w=["acc%d" % b])
                    S.op("poolq", lambda: nc.gpsimd.dma_start(out=OUTACC[tsl, :], in_=acc[b][:]), r=["acc%d" % b], w=["OUTACC"])
                for eh in range(2):
                    for rnd in range(CAP // 8):
                        S.op("dve", lambda: nc.vector.max(out=keys[:, eh, rnd * 8:(rnd + 1) * 8], in_=KT[:, eh, :]), r=["KT"], w=["keys"])
                        S.op("dve", lambda: nc.vector.match_replace(out=KT[:, eh, :], in_to_replace=keys[:, eh, rnd * 8:(rnd + 1) * 8],
                                                                    in_values=KT[:, eh, :], imm_value=0.0), r=["KT", "keys"], w=["KT"])
                zk = sbG("zk", [128, 2, CAP])
                S.op("dve", lambda: nc.vector.tensor_scalar(zk[:], keys[:], 0.0, float(T + 1), ALU.is_equal, ALU.mult), r=["keys"], w=["zk"])
                S.op("dve", lambda: nc.vector.scalar_tensor_tensor(keys[:], keys[:], -1.0, zk[:], ALU.add, ALU.add), r=["keys", "zk"], w=["keys"])
                idxT = sbG("idxT", [128, NST, NE])
                for shf in range(NST):
                    for eh in range(2):
                        S.op("pe", lambda: nc.tensor.transpose(ps[0][:, eh * 128:(eh + 1) * 128], keys[:, eh, shf * 128:(shf + 1) * 128], ident[:]), r=["keys", "ident"], w=[PSN[0]])
                    S.op("act", lambda: nc.scalar.copy(idxT[:, shf, :], ps[0][:, 0:NE]), r=[PSN[0]], w=["idxT"])
                S.op("dve", lambda: nc.vector.tensor_copy(idxI[:], idxT[:]), r=["idxT"], w=["idxI"])
                if DBG:
                    S.op("poolq", lambda: nc.gpsimd.dma_start(out=dbg_idx, in_=idxI[:]), r=["idxI"])
                S.barrier()
            with ExitStack() as esE:
                sbE = lambda name, shape, dt=F32: esE.enter_context(nc.sbuf_tensor(name, shape, dt))
                wgs = [sbE("wgs%d" % i, [128, 8, 512]) for i in range(2)]
                wgb = [sbE("wgb%d" % i, [128, 8, 512], BF16) for i in range(2)]
                wds = [sbE("wds%d" % i, [128, 2, D]) for i in range(2)]
                wdb = [sbE("wdb%d" % i, [128, 2, D], BF16) for i in range(2)]
                Xg = [sbE("Xg%d" % i, [128, D + NE]) for i in range(2)]
                XgT = [sbE("XgT%d" % i, [128, 8, 128], BF16) for i in range(2)]
                sge = sbE("sge", [128, 256]); ace = sbE("ace", [128, 256]); aeT = sbE("aeT", [128, 2, 128], BF16)
                yo = [sbE("yo%d" % i, [128, D]) for i in range(2)]
                n = 0
                for e in range(N_EXP_RUN):
                    wb = e % 2
                    S.op("sp", lambda: nc.sync.dma_start(out=wgs[wb][:, :, 0:256], in_=exp_w_gate[e].rearrange("(kc p) n -> p kc n", p=128)), w=["wgs%d" % wb])
                    S.op("sp", lambda: nc.sync.dma_start(out=wgs[wb][:, :, 256:512], in_=exp_w_up[e].rearrange("(kc p) n -> p kc n", p=128)), w=["wgs%d" % wb])
                    S.op("sp", lambda: nc.sync.dma_start(out=wds[wb][:], in_=exp_w_down[e].rearrange("(jc p) n -> p jc n", p=128)), w=["wds%d" % wb])
                    S.op("pool", lambda: nc.gpsimd.tensor_copy(wgb[wb][:], wgs[wb][:]), r=["wgs%d" % wb], w=["wgb%d" % wb])
                    S.op("act", lambda: nc.scalar.copy(wdb[wb][:], wds[wb][:]), r=["wds%d" % wb], w=["wdb%d" % wb])
                    for shf in range(NST):
                        b = n % 2
                        n += 1
                        S.op("poolq", lambda: nc.gpsimd.indirect_dma_start(out=Xg[b][:, :], out_offset=None, in_=H2E[:, :],
                                                                           in_offset=bass.IndirectOffsetOnAxis(ap=idxI[:, shf, e:e + 1], axis=0)),
                             r=["H2E", "idxI"], w=["Xg%d" % b])
                        for half in range(2):
                            pb = half
                            for q in range(4):
                                kc = half * 4 + q
                                S.op("pe", lambda: nc.tensor.transpose(ps[pb][:, q * 128:(q + 1) * 128], Xg[b][:, kc * 128:(kc + 1) * 128], ident[:]),
                                     r=["Xg%d" % b, "ident"], w=[PSN[pb]])
                            S.op("act", lambda: nc.scalar.copy(XgT[b][:, half * 4:(half + 1) * 4, :], ps[pb][:].rearrange("p (q t) -> p q t", q=4)),
                                 r=[PSN[pb]], w=["XgT%d" % b])
                        for kc in range(8):
                            S.op("pe", lambda: nc.tensor.matmul(ps[2 + b][:], XgT[b][:, kc, :], wgb[wb][:, kc, :], start=(kc == 0), stop=(kc == 7)),
                                 r=["XgT%d" % b, "wgb%d" % wb], w=[PSN[2 + b]])
                        S.op("act", lambda: nc.scalar.activation(out=sge[:], in_=ps[2 + b][:, 0:256], func=AF.Silu), r=[PSN[2 + b]], w=["sge"])
                        S.op("dve", lambda: nc.vector.tensor_tensor(ace[:], sge[:], ps[2 + b][:, 256:512], ALU.mult), r=["sge", PSN[2 + b]], w=["ace"])
                        for jc in range(2):
                            S.op("pe", lambda: nc.tensor.transpose(ps[4][:, jc * 128:(jc + 1) * 128], ace[:, jc * 128:(jc + 1) * 128], ident[:]), r=["ace", "ident"], w=[PSN[4]])
                        S.op("act", lambda: nc.scalar.copy(aeT[:], ps[4][:, 0:256].rearrange("p (j t) -> p j t", j=2)), r=[PSN[4]], w=["aeT"])
                        for half in range(2):
                            hs = slice(half * 512, (half + 1) * 512)
                            for jc in range(2):
                                S.op("pe", lambda: nc.tensor.matmul(ps[6 + half][:], aeT[:, jc, :], wdb[wb][:, jc, hs], start=(jc == 0), stop=(jc == 1)),
                                     r=["aeT", "wdb%d" % wb], w=[PSN[6 + half]])
                            S.op("dve", lambda: nc.vector.scalar_tensor_tensor(yo[b][:, hs], ps[6 + half][:], Xg[b][:, D + e:D + e + 1], g2t[:, hs], ALU.mult, ALU.mult),
                                 r=[PSN[6 + half], "Xg%d" % b, "g2t"], w=["yo%d" % b])
                        S.op("poolq", lambda: nc.gpsimd.indirect_dma_start(out=OUTACC[:, :], out_offset=bass.IndirectOffsetOnAxis(ap=idxI[:, shf, e:e + 1], axis=0),
                                                                           in_=yo[b][:, :], in_offset=None, compute_op=ALU.add),
                             r=["yo%d" % b, "idxI"], w=["OUTACC"])
                S.barrier()

        S.barrier()
        gf = sb("gf", [128, D])
        ot = [sb("ot%d" % i, [128, D]) for i in range(2)]
        xt = [sb("xf%d" % i, [128, D]) for i in range(2)]
        sq = sb("sqf2", [128, D])
        ss = [sb("ssf%d" % i, [128, 1]) for i in range(2)]
        rs = [sb("rsf%d" % i, [128, 1]) for i in range(2)]

        def rstd_fin(b, src, srcname):
            S.op("act", lambda: nc.scalar.activation(out=sq[:], in_=src, func=AF.Square, accum_out=ss[b][:]),
                 r=[srcname], w=["sqf2", "ssf%d" % b])
            S.op("dve", lambda: nc.vector.tensor_scalar(rs[b][:], ss[b][:], 1.0 / D, NORM_EPS, ALU.mult, ALU.add),
                 r=["ssf%d" % b], w=["rsf%d" % b])
            S.op("act", lambda: nc.scalar.activation(out=rs[b][:], in_=rs[b][:], func=AF.Sqrt), r=["rsf%d" % b], w=["rsf%d" % b])
            S.op("dve", lambda: nc.vector.reciprocal(rs[b][:], rs[b][:]), r=["rsf%d" % b], w=["rsf%d" % b])

        S.op("sp", lambda: nc.sync.dma_start(out=gf[:], in_=bcast_rows(normf_g[0:1, :])), w=["gf"])
        for i in range(NT):
            b = i % 2
            S.op("sp", lambda: nc.sync.dma_start(out=xt[b][:], in_=OUTACC[i * 128:(i + 1) * 128, :]), r=["OUTACC"], w=["xf%d" % b])
            rstd_fin(b, xt[b][:], "xf%d" % b)
            S.op("dve", lambda: nc.vector.scalar_tensor_tensor(ot[b][:], xt[b][:], rs[b][:, 0:1], gf[:], ALU.mult, ALU.mult),
                 r=["xf%d" % b, "rsf%d" % b, "gf"], w=["ot%d" % b])
            S.op("poolq", lambda: nc.gpsimd.dma_start(out=out[i * 128:(i + 1) * 128, :], in_=ot[b][:]), r=["ot%d" % b])
        S.finish("pool")
    return nc


_NC_CACHE = {}


_CONST = {}


def _constants():
    if _CONST:
        return _CONST
    import ml_dtypes
    L = T
    N = 2 * T
    f32 = np.float32
    pos = np.arange(L, dtype=f32)
    t = pos / f32(L - 1)
    bands = np.linspace(1e-4, 15.0, 16, dtype=f32)
    ang = (f32(2.0 * np.pi / L) * pos[:, None]) * bands[None]
    feats = np.concatenate([t[:, None], np.cos(ang), -np.sin(ang)], axis=-1).astype(f32)
    deltas = np.abs(np.linspace(np.log(1e-2) / 1.5, np.log(1e-2) / 0.3, 512, dtype=f32))
    env = np.exp(-t[:, None] * deltas[None]).astype(f32)
    tt = np.arange(L, dtype=np.int64)
    ff = np.arange(L, dtype=np.int64)
    ph = (tt[:, None] * ff[None, :]) % N
    angm = ph.astype(np.float64) * (2.0 * np.pi / N)
    cosm = np.cos(angm)
    sinm = np.sin(angm)
    fw = np.empty((L, N), np.float32)
    fw[:, :L] = cosm
    fw[:, L:] = -sinm
    fw[:, L] = np.where(tt % 2 == 0, 1.0, -1.0)
    dfw = fw.reshape(NT, 128, 64, 128).transpose(2, 1, 0, 3)
    iv = np.empty((N, L), np.float32)
    iv[:L] = (2.0 / N) * cosm.T
    iv[0] = 1.0 / N
    iv[L:] = -(2.0 / N) * sinm.T
    iv[L] = np.where(tt % 2 == 0, 1.0, -1.0) / N
    dinv = iv.reshape(64, 128, 16, 256).transpose(2, 1, 0, 3)
    m48 = np.zeros((48, 512), np.float32)
    m40 = np.zeros((40, 512), np.float32)
    for base in (0, 8, 32, 40):
        for j in range(8):
            m48[base + j, j * 64:(j + 1) * 64] = 1.0
    for base in (0, 32):
        for j in range(8):
            m40[base + j, j * 64:(j + 1) * 64] = 1.0
    selF = np.zeros((128, 128, 48), np.float32)
    selB = np.zeros((128, 128, 48), np.float32)
    for tl in range(128):
        selF[tl, tl, 8:16] = 1.0
        selB[127 - tl, tl, 40:48] = 1.0
    _CONST.update({
        "m48": m48, "m40": m40,
        "selF": selF.astype(ml_dtypes.bfloat16), "selB": selB.astype(ml_dtypes.bfloat16),
        "featsT": np.ascontiguousarray(feats.T),
        "env": env,
        "dfw": np.ascontiguousarray(dfw).astype(ml_dtypes.bfloat16),
        "dinv": np.ascontiguousarray(dinv).astype(ml_dtypes.bfloat16),
    })
    return _CONST


def _f32(a):
    return np.ascontiguousarray(a, dtype=np.float32)


def make_in_maps(inputs, ncores=NCORES):
    x = _f32(inputs["x"])
    c = _f32(inputs["c"])
    shared = {
        "normf_g": _f32(inputs["normf_g"]).reshape(1, D),
        "norm1_g": _f32(inputs["norm1_g"]).reshape(1, D),
        "w_ada": _f32(inputs["w_ada"]).reshape(D, 6 * D),
        "b_ada": _f32(inputs["b_ada"]).reshape(1, 6 * D),
        "w_in": _f32(inputs["w_in"]).reshape(D, N_IN),
        "hy_conv_w": _f32(inputs["hy_conv_w"]).reshape(3, HY_COLS),
        "hy_conv_b": _f32(inputs["hy_conv_b"]).reshape(1, HY_COLS),
        "ident": np.eye(128, dtype=np.float32),
        "hy_pos_w1": _f32(inputs["hy_pos_w1"]).reshape(33, 64),
        "hy_pos_b1": _f32(inputs["hy_pos_b1"]).reshape(64, 1),
        "hy_pos_w2": _f32(inputs["hy_pos_w2"]).reshape(2, 64, 64),
        "hy_pos_b2": _f32(inputs["hy_pos_b2"]).reshape(2, 64, 1),
        "hy_pos_w3": _f32(inputs["hy_pos_w3"]).reshape(64, 2048),
        "hy_sin_freq": _f32(inputs["hy_sin_freq"]).reshape(64, 1),
        "hy_skip": _f32(inputs["hy_skip"]).reshape(1, 1024),
        "w_out": _f32(inputs["w_out"]).reshape(D, D),
        "norm2_g": _f32(inputs["norm2_g"]).reshape(1, D),
        "router_w": _f32(inputs["router_w"]).reshape(D, 256),
        "router_bias": _f32(inputs["router_bias"]).reshape(1, 256),
        "sh_w_gate": _f32(inputs["sh_w_gate"]).reshape(D, 256),
        "sh_w_up": _f32(inputs["sh_w_up"]).reshape(D, 256),
        "sh_w_down": _f32(inputs["sh_w_down"]).reshape(256, D),
        "exp_w_gate": _f32(inputs["exp_w_gate"]).reshape(256, D, 256),
        "exp_w_up": _f32(inputs["exp_w_up"]).reshape(256, D, 256),
        "exp_w_down": _f32(inputs["exp_w_down"]).reshape(256, 256, D),
        "tokid": (np.arange(T, dtype=np.float32).reshape(NT, 128).T + 1.0).copy(),
        "rw_mu": _f32(inputs["rw_mu"]).reshape(1, 1760),
        "rw_w0": _f32(inputs["rw_w0"]).reshape(2, 512),
        "rw_w_up": _f32(inputs["rw_w_up"]).reshape(2, 32, 512),
        "rw_a0": _f32(inputs["rw_a0"]).reshape(2, 512),
        "rw_a_up": _f32(inputs["rw_a_up"]).reshape(2, 32, 512),
        "rw_g_up": _f32(inputs["rw_g_up"]).reshape(96, 512),
        "rw_k_k": _f32(inputs["rw_k_k"]).reshape(1, 512),
        "rw_k_a": _f32(inputs["rw_k_a"]).reshape(1, 512),
        "rw_r_k": _f32(inputs["rw_r_k"]).reshape(1, 512),
        "rw_ln_w": _f32(inputs["rw_ln_w"]).reshape(1, 512),
        "rw_ln_b": _f32(inputs["rw_ln_b"]).reshape(1, 512),
        "blk": np.kron(np.eye(2, dtype=np.float32), np.ones((64, 64), np.float32)),
    }
    shared.update(_constants())
    in_maps = []
    for cidx in range(ncores):
        m = dict(shared)
        m["x"] = x[cidx]
        m["c"] = c[cidx:cidx + 1]
        in_maps.append(m)
    return in_maps


def kernel(**inputs):
    if "nc" not in _NC_CACHE:
        _NC_CACHE["nc"] = build_nc()
    nc = _NC_CACHE["nc"]
    in_maps = make_in_maps(inputs)
    res = run_bass_kernel_spmd(nc, in_maps, core_ids=list(range(NCORES)))
    return np.stack([res.results[c]["out"] for c in range(NCORES)], axis=0)
```

```python
import numpy as np
from contextlib import ExitStack
import concourse.bass as bass
import concourse.mybir as mybir
from concourse.bass_utils import run_bass_kernel_spmd

F32 = mybir.dt.float32
BF16 = mybir.dt.bfloat16
I32 = mybir.dt.int32
ALU = mybir.AluOpType
AF = mybir.ActivationFunctionType
AX = mybir.AxisListType

NCORES = 8
T = 4096
D = 1024
NT = T // 128
NORM_EPS = 1e-6


class Sched:
    NDQ = 4

    def __init__(self, nc, es):
        self.nc = nc
        self.eng = {"pe": nc.tensor, "dve": nc.vector, "act": nc.scalar, "pool": nc.gpsimd}
        self.inc = {"pe": 1, "dve": 1, "act": 1, "pool": 1}
        self.stream = {"pe": "pe", "dve": "dve", "act": "act", "pool": "pool"}
        for base, eng, st in (("sp", nc.sync, "sp"), ("poolq", nc.gpsimd, "pool")):
            for i in range(self.NDQ):
                k = "%s%d" % (base, i)
                self.eng[k] = eng
                self.inc[k] = 16
                self.stream[k] = st
        self.sem = {k: es.enter_context(nc.semaphore("sem_" + k)) for k in self.eng}
        self.cnt = {k: 0 for k in self.eng}
        self.rr = {"sp": 0, "poolq": 0}
        self.last_w = {}
        self.readers = {}
        self.seen = {s: {} for s in ("pe", "dve", "act", "pool", "sp")}
        self.stream_eng = {"pe": nc.tensor, "dve": nc.vector, "act": nc.scalar, "pool": nc.gpsimd, "sp": nc.sync}

    def _wait(self, st, pq, seq):
        if self.seen[st].get(pq, 0) < seq:
            self.stream_eng[st].wait_ge(self.sem[pq], seq * self.inc[pq])
            self.seen[st][pq] = seq

    def op(self, q, fn, r=(), w=()):
        if q in self.rr:
            i = self.rr[q]
            self.rr[q] = (i + 1) % self.NDQ
            q = "%s%d" % (q, i)
        need = {}
        for b in r:
            for pq, seq in self.last_w.get(b, {}).items():
                need[pq] = max(need.get(pq, 0), seq)
        for b in w:
            for pq, seq in self.last_w.get(b, {}).items():
                need[pq] = max(need.get(pq, 0), seq)
            for pq, seq in self.readers.get(b, ()):
                need[pq] = max(need.get(pq, 0), seq)
        st = self.stream[q]
        if self.inc[q] == 16 and self.cnt[q] > 0:
            need[q] = max(need.get(q, 0), self.cnt[q])
        for pq, seq in need.items():
            self._wait(st, pq, seq)
        ins = fn()
        self.cnt[q] += 1
        seq = self.cnt[q]
        ins.then_inc(self.sem[q], self.inc[q])
        for b in w:
            self.last_w.setdefault(b, {})[q] = seq
            self.readers[b] = []
        for b in r:
            lst = self.readers.setdefault(b, [])
            lst.append((q, seq))
            if len(lst) > 16:
                best = {}
                for pq, s_ in lst:
                    best[pq] = max(best.get(pq, 0), s_)
                self.readers[b] = list(best.items())
        return ins

    def barrier(self):
        for st in ("pe", "dve", "act", "pool", "sp"):
            for k in self.eng:
                if self.cnt[k] > 0 and not (k == st and self.inc[k] == 1):
                    self._wait(st, k, self.cnt[k])

    def finish(self, q="pool"):
        for k in self.eng:
            if self.cnt[k] > 0:
                self.stream_eng[q].wait_ge(self.sem[k], self.cnt[k] * self.inc[k])


def bcast_rows(ap_row, parts=128):
    return ap_row.to_broadcast([parts, ap_row.shape[-1]])


HY_COLS = 1536
N_IN = 3296
DBG = False
SCAN_CHUNKS = 64
N_EXP_RUN = 256


def build_nc():
    nc = bass.Bass("TRN2", target_bir_lowering=False)
    dt_in = lambda name, shape, dt=F32: nc.dram_tensor(name, shape, dt, kind="ExternalInput").ap()
    x = dt_in("x", [T, D])
    c_in = dt_in("c", [1, D])
    normf_g = dt_in("normf_g", [1, D])
    norm1_g = dt_in("norm1_g", [1, D])
    w_ada = dt_in("w_ada", [D, 6 * D])
    b_ada = dt_in("b_ada", [1, 6 * D])
    w_in = dt_in("w_in", [D, N_IN])
    hy_conv_w = dt_in("hy_conv_w", [3, HY_COLS])
    hy_conv_b = dt_in("hy_conv_b", [1, HY_COLS])
    ident_in = dt_in("ident", [128, 128])
    featsT = dt_in("featsT", [33, T])
    env_in = dt_in("env", [T, 512])
    pw1 = dt_in("hy_pos_w1", [33, 64])
    pb1 = dt_in("hy_pos_b1", [64, 1])
    pw2 = dt_in("hy_pos_w2", [2, 64, 64])
    pb2 = dt_in("hy_pos_b2", [2, 64, 1])
    pw3 = dt_in("hy_pos_w3", [64, 2048])
    sfreq = dt_in("hy_sin_freq", [64, 1])
    hskip = dt_in("hy_skip", [1, 1024])
    dfw = dt_in("dfw", [64, 128, 32, 128], BF16)
    dinv = dt_in("dinv", [16, 128, 64, 256], BF16)
    KF = nc.dram_tensor("KF", [64, 128, 1024], F32, kind="Internal").ap()
    UD = nc.dram_tensor("UD", [12, 128, T], F32, kind="Internal").ap()
    ZD = nc.dram_tensor("ZD", [4, 128, T], F32, kind="Internal").ap()
    YD = nc.dram_tensor("YD", [4, 128, T], F32, kind="Internal").ap()
    MODD = nc.dram_tensor("MODD", [128, 6 * D], F32, kind="Internal").ap()
    rw_mu = dt_in("rw_mu", [1, 1760])
    rw_w0 = dt_in("rw_w0", [2, 512])
    rw_w_up = dt_in("rw_w_up", [2, 32, 512])
    rw_a0 = dt_in("rw_a0", [2, 512])
    rw_a_up = dt_in("rw_a_up", [2, 32, 512])
    rw_g_up = dt_in("rw_g_up", [96, 512])
    rw_k_k = dt_in("rw_k_k", [1, 512])
    rw_k_a = dt_in("rw_k_a", [1, 512])
    rw_r_k = dt_in("rw_r_k", [1, 512])
    rw_ln_w = dt_in("rw_ln_w", [1, 512])
    rw_ln_b = dt_in("rw_ln_b", [1, 512])
    blk_in = dt_in("blk", [128, 128])
    m48_in = dt_in("m48", [48, 512])
    m40_in = dt_in("m40", [40, 512])
    selF_in = dt_in("selF", [128, 128, 48], BF16)
    selB_in = dt_in("selB", [128, 128, 48], BF16)
    scr = lambda name, shape, dt=F32: nc.dram_tensor(name, shape, dt, kind="Internal").ap()
    KKN = scr("KKN", [512, T], BF16)
    RR = scr("RR", [512, T], BF16)
    DEC = scr("DEC", [2, 512, T])
    BT = scr("BT", [2, 2, T, 512], BF16)
    VT = scr("VT", [T, 512], BF16)
    BON = scr("BON", [512, T])
    GG = scr("GG", [512, T])
    OD = scr("OD", [2, T, 512])
    X1D = scr("X1D", [T, D])
    H2E = scr("H2E", [T + 1, D + 256])
    OUTACC = scr("OUTACC", [T + 1, D])
    norm2_g = dt_in("norm2_g", [1, D])
    router_w = dt_in("router_w", [D, 256])
    router_bias = dt_in("router_bias", [1, 256])
    sh_w_gate = dt_in("sh_w_gate", [D, 256])
    sh_w_up = dt_in("sh_w_up", [D, 256])
    sh_w_down = dt_in("sh_w_down", [256, D])
    exp_w_gate = dt_in("exp_w_gate", [256, D, 256])
    exp_w_up = dt_in("exp_w_up", [256, D, 256])
    exp_w_down = dt_in("exp_w_down", [256, 256, D])
    tokid_in = dt_in("tokid", [128, NT])
    w_out = dt_in("w_out", [D, D])
    out = nc.dram_tensor("out", [T, D], F32, kind="ExternalOutput").ap()
    if DBG:
        dbg_mod = nc.dram_tensor("dbg_mod", [128, 6 * D], F32, kind="ExternalOutput").ap()
        dbg_u = nc.dram_tensor("dbg_u", [12, 128, T], F32, kind="ExternalOutput").ap()
        dbg_kf = nc.dram_tensor("dbg_kf", [64, 128, 1024], F32, kind="ExternalOutput").ap()
        dbg_y = nc.dram_tensor("dbg_y", [4, 128, T], F32, kind="ExternalOutput").ap()
        dbg_od = nc.dram_tensor("dbg_od", [2, T, 512], F32, kind="ExternalOutput").ap()
        dbg_x1 = nc.dram_tensor("dbg_x1", [T, D], F32, kind="ExternalOutput").ap()
        dbg_idx = nc.dram_tensor("dbg_idx", [128, 8, 256], I32, kind="ExternalOutput").ap()
        dbg_rw = {
            "KKN": nc.dram_tensor("dbg_KKN", [512, T], BF16, kind="ExternalOutput").ap(),
            "RR": nc.dram_tensor("dbg_RR", [512, T], BF16, kind="ExternalOutput").ap(),
            "DEC": nc.dram_tensor("dbg_DEC", [2, 512, T], F32, kind="ExternalOutput").ap(),
            "BT": nc.dram_tensor("dbg_BT", [2, 2, T, 512], BF16, kind="ExternalOutput").ap(),
            "VT": nc.dram_tensor("dbg_VT", [T, 512], BF16, kind="ExternalOutput").ap(),
            "BON": nc.dram_tensor("dbg_BON", [512, T], F32, kind="ExternalOutput").ap(),
            "GG": nc.dram_tensor("dbg_GG", [512, T], F32, kind="ExternalOutput").ap(),
        }

    with ExitStack() as es:
        es.enter_context(nc.allow_low_precision("bf16 matmul operands, fp32 accumulation"))
        es.enter_context(nc.allow_non_contiguous_dma("small strided parameter loads"))
        S = Sched(nc, es)
        sb = lambda name, shape, dt=F32: es.enter_context(nc.sbuf_tensor(name, shape, dt))
        ps = [es.enter_context(nc.psum_tensor("ps%d" % i, [128, 512], F32)) for i in range(8)]
        PSN = ["ps%d" % i for i in range(8)]

        ident = sb("ident_sb", [128, 128])
        S.op("sp", lambda: nc.sync.dma_start(out=ident[:], in_=ident_in[:, :]), w=["ident"])


        TWO_PI = 6.283185307179586
        MAGIC = 12582912.0
        with ExitStack() as es1:
            sb1 = lambda name, shape, dt=F32: es1.enter_context(nc.sbuf_tensor(name, shape, dt))
            w1 = sb1("w1", [33, 64])
            w2 = sb1("w2", [64, 2, 64])
            w3 = sb1("w3", [64, 2048])
            fr = sb1("fr", [64, 1])
            fb = sb1("fb", [64, 3])
            ones = sb1("ones", [128, 128])
            skp = sb1("skp", [128, 1024])
            hd0 = sb1("hd0", [64, T])
            es1a = ExitStack()
            sb1a = lambda name, shape, dt=F32: es1a.enter_context(nc.sbuf_tensor(name, shape, dt))
            ft = sb1a("ft", [33, T])
            hd = [hd0, sb1a("hd1", [64, T])]
            rr = sb1a("rr", [64, 512])
            S.op("sp", lambda: nc.sync.dma_start(out=ft[:], in_=featsT[:, :]), w=["ft"])
            S.op("sp", lambda: nc.sync.dma_start(out=w1[:], in_=pw1[:, :]), w=["w1"])
            S.op("sp", lambda: nc.sync.dma_start(out=w2[:], in_=pw2.rearrange("i k m -> k i m")), w=["w2"])
            S.op("sp", lambda: nc.sync.dma_start(out=w3[:], in_=pw3[:, :]), w=["w3"])
            S.op("sp", lambda: nc.sync.dma_start(out=fr[:], in_=sfreq[:, :]), w=["fr"])
            S.op("sp", lambda: nc.sync.dma_start(out=fb[:, 0:1], in_=pb1[:, :]), w=["fb"])
            S.op("sp", lambda: nc.sync.dma_start(out=fb[:, 1:2], in_=pb2[0]), w=["fb"])
            S.op("sp", lambda: nc.sync.dma_start(out=fb[:, 2:3], in_=pb2[1]), w=["fb"])
            S.op("sp", lambda: nc.sync.dma_start(out=skp[:], in_=bcast_rows(hskip[0:1, :])), w=["skp"])
            S.op("dve", lambda: nc.vector.tensor_scalar(fb[:], fb[:], fr[:, 0:1], None, ALU.mult), r=["fb", "fr"], w=["fb"])
            S.op("pool", lambda: nc.gpsimd.memset(ones[:], 1.0), w=["ones"])
            for layer in range(3):
                src = ft if layer == 0 else hd[(layer - 1) % 2]
                srcn = "ft" if layer == 0 else "hd%d" % ((layer - 1) % 2)
                dst = hd[layer % 2]
                dstn = "hd%d" % (layer % 2)
                lw = w1[:, :] if layer == 0 else w2[:, layer - 1, :]
                lwn = "w1" if layer == 0 else "w2"
                for tcn in range(8):
                    pb = tcn % 2
                    sl = slice(tcn * 512, (tcn + 1) * 512)
                    S.op("pe", lambda: nc.tensor.matmul(ps[pb][0:64, :], lw, src[:, sl], start=True, stop=True),
                         r=[lwn, srcn], w=[PSN[pb]])
                    S.op("dve", lambda: nc.vector.tensor_scalar(dst[:, sl], ps[pb][0:64, :], fr[:, 0:1], fb[:, layer:layer + 1], ALU.mult, ALU.add),
                         r=[PSN[pb], "fr", "fb"], w=[dstn])
                    S.op("dve", lambda: nc.vector.tensor_scalar(rr[:], dst[:, sl], 1.0 / TWO_PI, MAGIC, ALU.mult, ALU.add), r=[dstn], w=["rr"])
                    S.op("dve", lambda: nc.vector.tensor_scalar(rr[:], rr[:], -MAGIC, -TWO_PI, ALU.add, ALU.mult), r=["rr"], w=["rr"])
                    S.op("dve", lambda: nc.vector.tensor_tensor(dst[:, sl], dst[:, sl], rr[:], ALU.add), r=[dstn, "rr"], w=[dstn])
                    S.op("dve", lambda: nc.vector.tensor_scalar(dst[:, sl], dst[:, sl], 3.14159, -3.14159, ALU.min, ALU.max), r=[dstn], w=[dstn])
                    S.op("act", lambda: nc.scalar.activation(out=dst[:, sl], in_=dst[:, sl], func=AF.Sin), r=[dstn], w=[dstn])
            S.barrier()
            es1a.close()
            envt = [sb1("envt%d" % i, [128, 512]) for i in range(2)]
            fw = [sb1("fw%d" % i, [128, 512]) for i in range(2)]
            bw = [sb1("bw%d" % i, [128, 512]) for i in range(2)]
            sqf = sb1("sqf", [128, 512])
            sqb = sb1("sqb", [128, 512])
            Eb = sb1("Eb", [128, NT, 1024], BF16)
            Ob = sb1("Ob", [128, NT, 1024], BF16)
            scl = sb1("scl", [128, 1024])
            dch = [sb1("dch%d" % i, [128, 32, 128], BF16) for i in range(2)]
            kst = [sb1("kst%d" % i, [128, 1024]) for i in range(2)]
            knq = sb1("knq", [128, 1024])
            hfin = hd[0]
            for i in range(NT):
                b = i % 2
                S.op("sp", lambda: nc.sync.dma_start(out=envt[b][:], in_=env_in[i * 128:(i + 1) * 128, :]), w=["envt%d" % b])
                for n in range(2):
                    for d in range(2):
                        pb = d
                        col = n * 1024 + d * 512
                        S.op("pe", lambda: nc.tensor.matmul(ps[pb][:], hfin[:, i * 128:(i + 1) * 128], w3[:, col:col + 512], start=True, stop=True),
                             r=["hd0", "w3"], w=[PSN[pb]])
                    S.op("dve", lambda: nc.vector.tensor_tensor(fw[n][:], ps[0][:], envt[b][:], ALU.mult), r=[PSN[0], "envt%d" % b], w=["fw%d" % n])
                    S.op("dve", lambda: nc.vector.tensor_tensor(bw[n][:], ps[1][:], envt[b][:], ALU.mult), r=[PSN[1], "envt%d" % b], w=["bw%d" % n])
                    if i == 0:
                        S.op("dve", lambda: nc.vector.memset(bw[n][0:1, :], 0.0), w=["bw%d" % n])
                    S.op("pool", lambda: nc.gpsimd.tensor_tensor(Eb[:, i, n * 512:(n + 1) * 512], fw[n][:], bw[n][:], ALU.add),
                         r=["fw%d" % n, "bw%d" % n], w=["Eb"])
                    S.op("pool", lambda: nc.gpsimd.tensor_tensor(Ob[:, i, n * 512:(n + 1) * 512], fw[n][:], bw[n][:], ALU.subtract),
                         r=["fw%d" % n, "bw%d" % n], w=["Ob"])
                    S.op("act", lambda: nc.scalar.activation(out=sqf[:], in_=fw[n][:], func=AF.Square), r=["fw%d" % n], w=["sqf"])
                    S.op("act", lambda: nc.scalar.activation(out=sqb[:], in_=bw[n][:], func=AF.Square), r=["bw%d" % n], w=["sqb"])
                    S.op("pe", lambda: nc.tensor.matmul(ps[2 + n][:], ones[:], sqf[:], start=(i == 0), stop=False), r=["ones", "sqf"], w=[PSN[2 + n]])
                    S.op("pe", lambda: nc.tensor.matmul(ps[2 + n][:], ones[:], sqb[:], start=False, stop=(i == NT - 1)), r=["ones", "sqb"], w=[PSN[2 + n]])
            for n in range(2):
                S.op("dve", lambda: nc.vector.tensor_scalar(scl[:, n * 512:(n + 1) * 512], ps[2 + n][:], 1e-6, None, ALU.add), r=[PSN[2 + n]], w=["scl"])
            S.op("act", lambda: nc.scalar.activation(out=scl[:], in_=scl[:], func=AF.Sqrt), r=["scl"], w=["scl"])
            S.op("dve", lambda: nc.vector.reciprocal(scl[:], scl[:]), r=["scl"], w=["scl"])
            for rc in range(64):
                b = rc % 2
                S.op("sp", lambda: nc.sync.dma_start(out=dch[b][:], in_=dfw[rc]), w=["dch%d" % b])
                src, srcn = (Eb, "Eb") if rc < 32 else (Ob, "Ob")
                for n in range(2):
                    for tcn in range(NT):
                        S.op("pe", lambda: nc.tensor.matmul(ps[4 + n][:], dch[b][:, tcn, :], src[:, tcn, n * 512:(n + 1) * 512],
                                                            start=(tcn == 0), stop=(tcn == NT - 1)),
                             r=["dch%d" % b, srcn], w=[PSN[4 + n]])
                    S.op("dve", lambda: nc.vector.tensor_tensor(kst[b][:, n * 512:(n + 1) * 512], ps[4 + n][:], scl[:, n * 512:(n + 1) * 512], ALU.mult),
                         r=[PSN[4 + n], "scl"], w=["kst%d" % b])
                if rc < 32:
                    S.op("pool", lambda: nc.gpsimd.tensor_tensor(kst[b][:], kst[b][:], skp[:], ALU.add), r=["kst%d" % b, "skp"], w=["kst%d" % b])
                if rc == 32:
                    for n in range(2):
                        for tcn in range(NT):
                            S.op("pe", lambda: nc.tensor.matmul(ps[6 + n][:], dch[b][:, tcn, :], Eb[:, tcn, n * 512:(n + 1) * 512],
                                                                start=(tcn == 0), stop=(tcn == NT - 1)),
                                 r=["dch%d" % b, "Eb"], w=[PSN[6 + n]])
                        S.op("dve", lambda: nc.vector.tensor_tensor(knq[:, n * 512:(n + 1) * 512], ps[6 + n][:], scl[:, n * 512:(n + 1) * 512], ALU.mult),
                             r=[PSN[6 + n], "scl"], w=["knq"])
                    S.op("dve", lambda: nc.vector.tensor_tensor(kst[b][0:1, :], knq[0:1, :], skp[0:1, :], ALU.add), r=["knq", "skp", "kst%d" % b], w=["kst%d" % b])
                S.op("poolq", lambda: nc.gpsimd.dma_start(out=KF[rc], in_=kst[b][:]), r=["kst%d" % b], w=["KF"])
            if DBG:
                S.op("poolq", lambda: nc.gpsimd.dma_start(out=dbg_kf[:, :, :], in_=KF[:, :, :]), r=["KF"])

        S.barrier()
        with ExitStack() as es1:
            sb1 = lambda name, shape, dt=F32: es1.enter_context(nc.sbuf_tensor(name, shape, dt))
            mod = sb1("mod", [128, 6 * D])
            cs = sb1("cs", [128, 8])
            crep = sb1("crep", [128, 8, 128])
            wa = [sb1("wa%d" % i, [128, 3 * D]) for i in range(2)]
            S.op("sp", lambda: nc.sync.dma_start(out=cs[:], in_=c_in.rearrange("o (kc p) -> p (o kc)", p=128)), w=["cs"])
            S.op("sp", lambda: nc.sync.dma_start(out=mod[:], in_=bcast_rows(b_ada[0:1, :])), w=["mod"])
            S.op("act", lambda: nc.scalar.activation(out=cs[:], in_=cs[:], func=AF.Silu), r=["cs"], w=["cs"])
            for kc in range(8):
                S.op("dve", lambda: nc.vector.tensor_copy(crep[:, kc, :], cs[:, kc:kc + 1].to_broadcast([128, 128])),
                     r=["cs"], w=["crep"])
            n = 0
            for half in range(2):
                for kc in range(8):
                    b = n % 2
                    n += 1
                    S.op("sp", lambda: nc.sync.dma_start(out=wa[b][:], in_=w_ada[kc * 128:(kc + 1) * 128, half * 3 * D:(half + 1) * 3 * D]),
                         w=["wa%d" % b])
                    for j in range(6):
                        S.op("pe", lambda: nc.tensor.matmul(ps[j][:], crep[:, kc, :], wa[b][:, j * 512:(j + 1) * 512],
                                                            start=(kc == 0), stop=(kc == 7)),
                             r=["crep", "wa%d" % b], w=[PSN[j]])
                for j in range(6):
                    col = half * 3 * D + j * 512
                    S.op("dve", lambda: nc.vector.tensor_tensor(mod[:, col:col + 512], mod[:, col:col + 512], ps[j][:], ALU.add),
                         r=[PSN[j], "mod"], w=["mod"])
            S.op("poolq", lambda: nc.gpsimd.dma_start(out=MODD[:, :], in_=mod[:]), r=["mod"], w=["MODD"])
            if DBG:
                S.op("poolq", lambda: nc.gpsimd.dma_start(out=dbg_mod[:, :], in_=mod[:]), r=["mod"])
        S.barrier()

        def rstd_of(sq, ss, rs, nm, src, srcname):
            S.op("act", lambda: nc.scalar.activation(out=sq[:], in_=src, func=AF.Square, accum_out=ss[:]),
                 r=[srcname], w=["sq" + nm, "ss" + nm])
            S.op("dve", lambda: nc.vector.tensor_scalar(rs[:], ss[:], 1.0 / D, NORM_EPS, ALU.mult, ALU.add),
                 r=["ss" + nm], w=["rs" + nm])
            S.op("act", lambda: nc.scalar.activation(out=rs[:], in_=rs[:], func=AF.Sqrt), r=["rs" + nm], w=["rs" + nm])
            S.op("dve", lambda: nc.vector.reciprocal(rs[:], rs[:]), r=["rs" + nm], w=["rs" + nm])

        es2 = ExitStack()
        sb2 = lambda name, shape, dt=F32: es2.enter_context(nc.sbuf_tensor(name, shape, dt))
        with es2:
            hT = sb2("hT", [128, 8, T + 2], BF16)
            S.op("pool", lambda: nc.gpsimd.memset(hT[:, :, 0:1], 0.0), w=["hT"])
            S.op("pool", lambda: nc.gpsimd.memset(hT[:, :, T + 1:T + 2], 0.0), w=["hT"])
            with ExitStack() as esN:
                sbN = lambda name, shape, dt=F32: esN.enter_context(nc.sbuf_tensor(name, shape, dt))
                gm1 = sbN("gm1", [128, D])
                sc1 = sbN("sc1", [128, D])
                sh1 = sbN("sh1", [128, D])
                S.op("sp", lambda: nc.sync.dma_start(out=gm1[:], in_=bcast_rows(norm1_g[0:1, :])), w=["gm1"])
                S.op("sp", lambda: nc.sync.dma_start(out=sh1[:], in_=MODD[:, 0:D]), r=["MODD"], w=["sh1"])
                S.op("sp", lambda: nc.sync.dma_start(out=sc1[:], in_=MODD[:, D:2 * D]), r=["MODD"], w=["sc1"])
                S.op("dve", lambda: nc.vector.scalar_tensor_tensor(gm1[:], sc1[:], 1.0, gm1[:], ALU.add, ALU.mult), r=["sc1", "gm1"], w=["gm1"])
                xt = [sbN("xt%d" % i, [128, D]) for i in range(2)]
                hh = [sbN("hh%d" % i, [128, D]) for i in range(2)]
                sq = sbN("sq", [128, D])
                ss = [sbN("ss%d" % i, [128, 1]) for i in range(2)]
                rs = [sbN("rs%d" % i, [128, 1]) for i in range(2)]
                for i in range(NT):
                    b = i % 2
                    S.op("sp", lambda: nc.sync.dma_start(out=xt[b][:], in_=x[i * 128:(i + 1) * 128, :]), w=["xt%d" % b])
                    rstd_of(sq, ss[b], rs[b], "n1_%d" % b, xt[b][:], "xt%d" % b)
                    S.op("dve", lambda: nc.vector.scalar_tensor_tensor(hh[b][:], xt[b][:], rs[b][:, 0:1], gm1[:], ALU.mult, ALU.mult),
                         r=["xt%d" % b, "rsn1_%d" % b, "gm1"], w=["hh%d" % b])
                    S.op("pool", lambda: nc.gpsimd.tensor_tensor(hh[b][:], hh[b][:], sh1[:], ALU.add), r=["hh%d" % b, "sh1"], w=["hh%d" % b])
                    for half in range(2):
                        pb = 6 + half
                        for q in range(4):
                            kc = half * 4 + q
                            S.op("pe", lambda: nc.tensor.transpose(ps[pb][:, q * 128:(q + 1) * 128], hh[b][:, kc * 128:(kc + 1) * 128], ident[:]),
                                 r=["hh%d" % b, "ident"], w=[PSN[pb]])
                        S.op("act", lambda: nc.scalar.copy(hT[:, half * 4:(half + 1) * 4, 1 + i * 128:1 + (i + 1) * 128],
                                                           ps[pb][:].rearrange("p (q t) -> p q t", q=4)),
                             r=[PSN[pb]], w=["hT"])
                S.barrier()

            wst = [sb2("wst%d" % i, [128, 8, 128]) for i in range(2)]
            wbf = [sb2("wbf%d" % i, [128, 8, 128], BF16) for i in range(2)]
            pfm = [sb2("pfm%d" % i, [128, T + 2]) for i in range(3)]
            for bb in range(3):
                S.op("pool", lambda: nc.gpsimd.memset(pfm[bb][:, 0:1], 0.0), w=["pfm%d" % bb])
                S.op("pool", lambda: nc.gpsimd.memset(pfm[bb][:, T + 1:T + 2], 0.0), w=["pfm%d" % bb])
            wcnt = [0]

            def inproj_group(cg, ncols, dst, dstname):
                b = wcnt[0] % 2
                wcnt[0] += 1
                S.op("sp", lambda: nc.sync.dma_start(out=wst[b][:, :, 0:ncols],
                                                     in_=w_in[:, cg * 128:cg * 128 + ncols].rearrange("(kc p) n -> p kc n", p=128)),
                     w=["wst%d" % b])
                S.op("pool", lambda: nc.gpsimd.tensor_copy(wbf[b][:, :, 0:ncols], wst[b][:, :, 0:ncols]), r=["wst%d" % b], w=["wbf%d" % b])
                for tcn in range(8):
                    pb = tcn % 2
                    for kc in range(8):
                        S.op("pe", lambda: nc.tensor.matmul(ps[pb][0:ncols, :], wbf[b][:, kc, 0:ncols], hT[:, kc, 1 + tcn * 512:1 + (tcn + 1) * 512],
                                                            start=(kc == 0), stop=(kc == 7)),
                             r=["wbf%d" % b, "hT"], w=[PSN[pb]])
                    S.op("act", lambda: nc.scalar.copy(dst[0:ncols, 1 + tcn * 512:1 + (tcn + 1) * 512], ps[pb][0:ncols, :]),
                         r=[PSN[pb]], w=[dstname])

            with ExitStack() as esH:
                sbH = lambda name, shape, dt=F32: esH.enter_context(nc.sbuf_tensor(name, shape, dt))
                ufm = [sbH("ufm%d" % i, [128, T]) for i in range(2)]
                cw = sbH("cw", [128, 3, 12])
                cb = sbH("cb", [128, 12])
                for j in range(3):
                    S.op("sp", lambda: nc.sync.dma_start(out=cw[:, j, :], in_=hy_conv_w[j:j + 1, :].rearrange("o (g p) -> p (o g)", p=128)), w=["cw"])
                S.op("sp", lambda: nc.sync.dma_start(out=cb[:], in_=hy_conv_b.rearrange("o (g p) -> p (o g)", p=128)), w=["cb"])
                for cg in range(12):
                    b = cg % 2
                    inproj_group(cg, 128, pfm[b], "pfm%d" % b)
                    S.op("dve", lambda: nc.vector.tensor_scalar(ufm[b][:], pfm[b][:, 0:T], cw[:, 0, cg:cg + 1], cb[:, cg:cg + 1], ALU.mult, ALU.add),
                         r=["pfm%d" % b, "cw", "cb"], w=["ufm%d" % b])
                    S.op("dve", lambda: nc.vector.scalar_tensor_tensor(ufm[b][:], pfm[b][:, 1:T + 1], cw[:, 1, cg:cg + 1], ufm[b][:], ALU.mult, ALU.add),
                         r=["pfm%d" % b, "cw", "ufm%d" % b], w=["ufm%d" % b])
                    S.op("dve", lambda: nc.vector.scalar_tensor_tensor(ufm[b][:], pfm[b][:, 2:T + 2], cw[:, 2, cg:cg + 1], ufm[b][:], ALU.mult, ALU.add),
                         r=["pfm%d" % b, "cw", "ufm%d" % b], w=["ufm%d" % b])
                    S.op("poolq", lambda: nc.gpsimd.dma_start(out=UD[cg, :, :], in_=ufm[b][:]), r=["ufm%d" % b], w=["UD"])
                    if DBG:
                        S.op("poolq", lambda: nc.gpsimd.dma_start(out=dbg_u[cg, :, :], in_=ufm[b][:]), r=["ufm%d" % b])
                S.barrier()

            with ExitStack() as esR:
                sbR = lambda name, shape, dt=F32: esR.enter_context(nc.sbuf_tensor(name, shape, dt))
                pvec = lambda ap_row, p=128: ap_row.rearrange("o (g p) -> p (o g)", p=p)
                mu_t = sbR("mu_t", [128, 14])
                w0_t = sbR("w0_t", [128, 8])
                a0_t = sbR("a0_t", [128, 8])
                kk_t = sbR("kk_t", [128, 4])
                ka_t = sbR("ka_t", [128, 4])
                oka_t = sbR("oka_t", [128, 4])
                rk_t = sbR("rk_t", [128, 4])
                S.op("sp", lambda: nc.sync.dma_start(out=mu_t[:, 0:13], in_=pvec(rw_mu[0:1, 0:1664])), w=["mu_t"])
                S.op("sp", lambda: nc.sync.dma_start(out=mu_t[0:96, 13:14], in_=pvec(rw_mu[0:1, 1664:1760], 96)), w=["mu_t"])
                for d in range(2):
                    S.op("sp", lambda: nc.sync.dma_start(out=w0_t[:, d * 4:(d + 1) * 4], in_=pvec(rw_w0[d:d + 1, :])), w=["w0_t"])
                    S.op("sp", lambda: nc.sync.dma_start(out=a0_t[:, d * 4:(d + 1) * 4], in_=pvec(rw_a0[d:d + 1, :])), w=["a0_t"])
                S.op("sp", lambda: nc.sync.dma_start(out=kk_t[:], in_=pvec(rw_k_k[0:1, :])), w=["kk_t"])
                S.op("sp", lambda: nc.sync.dma_start(out=ka_t[:], in_=pvec(rw_k_a[0:1, :])), w=["ka_t"])
                S.op("sp", lambda: nc.sync.dma_start(out=rk_t[:], in_=pvec(rw_r_k[0:1, :])), w=["rk_t"])
                S.op("dve", lambda: nc.vector.tensor_scalar(oka_t[:], ka_t[:], -1.0, 1.0, ALU.mult, ALU.add), r=["ka_t"], w=["oka_t"])
                Lw = sbR("Lw", [128, 4, 512], BF16)
                Gw = sbR("Gw", [96, 512], BF16)
                blk = sbR("blk_sb", [128, 128])
                Lbf = sbR("Lbf", [128, T], BF16)
                Glb = sbR("Glb", [96, T], BF16)
                esL = ExitStack()
                Lst = esL.enter_context(nc.sbuf_tensor("Lst", [128, 4, 512], F32))
                Gst = esL.enter_context(nc.sbuf_tensor("Gst", [96, 512], F32))
                S.op("pool", lambda: nc.gpsimd.memset(Lst[:], 0.0), w=["Lst"])
                for d in range(2):
                    S.op("sp", lambda: nc.sync.dma_start(out=Lst[d * 32:(d + 1) * 32, d, :], in_=rw_w_up[d]), w=["Lst"])
                    S.op("sp", lambda: nc.sync.dma_start(out=Lst[64 + d * 32:64 + (d + 1) * 32, 2 + d, :], in_=rw_a_up[d]), w=["Lst"])
                S.op("pool", lambda: nc.gpsimd.tensor_copy(Lw[:], Lst[:]), r=["Lst"], w=["Lw"])
                S.op("sp", lambda: nc.sync.dma_start(out=Gst[:], in_=rw_g_up[:, :]), w=["Gst"])
                S.op("pool", lambda: nc.gpsimd.tensor_copy(Gw[:], Gst[:]), r=["Gst"], w=["Gw"])
                S.op("sp", lambda: nc.sync.dma_start(out=blk[:], in_=blk_in[:, :]), w=["blk"])
                S.barrier()
                esL.close()

                singles = ("kkr", "sqk", "nrm", "sg", "tmpa", "bbf", "ad")

                def tmp2(name, shape=(128, 512), dt=F32):
                    if name in singles:
                        t_ = sbR(name + "0", list(shape), dt)
                        return [t_, t_]
                    return [sbR("%s%d" % (name, i), list(shape), dt) for i in range(2)]

                def shift(dst, dstn, src, srcn, mucol, c0, rows=128, n=512):
                    S.op("dve", lambda: nc.vector.tensor_tensor(dst[0:rows, :], src[0:rows, c0:c0 + n], src[0:rows, c0 + 2:c0 + 2 + n], ALU.add),
                         r=[srcn], w=[dstn])
                    S.op("dve", lambda: nc.vector.scalar_tensor_tensor(dst[0:rows, :], dst[0:rows, :], 0.5, src[0:rows, c0 + 1:c0 + 1 + n], ALU.mult, ALU.subtract),
                         r=[srcn, dstn], w=[dstn])
                    S.op("dve", lambda: nc.vector.scalar_tensor_tensor(dst[0:rows, :], dst[0:rows, :], mucol, src[0:rows, c0 + 1:c0 + 1 + n], ALU.mult, ALU.add),
                         r=[srcn, dstn, "mu_t"], w=[dstn])

                rf = tmp2("rf"); kf = tmp2("kf"); vf = tmp2("vf")
                kkr = tmp2("kkr"); sqk = tmp2("sqk"); nrm = tmp2("nrm"); kkf = tmp2("kkf")
                kkn = tmp2("kkn", dt=BF16); rbf = tmp2("rbf", dt=BF16)
                ad = tmp2("ad"); sg = tmp2("sg"); dec = tmp2("dec"); tmpa = tmp2("tmpa"); kd = tmp2("kd")
                ksum = tmp2("ksum"); bbf = tmp2("bbf"); bon = tmp2("bon"); gg = tmp2("gg")
                tmb = tmp2("tmb", (128, 4, 128), BF16); tmk = tmp2("tmk", (128, 4, 128), BF16); tmv = tmp2("tmv", (128, 4, 128), BF16)
                inproj_group(24, 128, pfm[0], "pfm0")
                inproj_group(25, 96, pfm[1], "pfm1")
                for ch in range(8):
                    c0 = ch * 512
                    p_ = ch % 2
                    shift(rf[p_], "rf%d" % p_, pfm[0], "pfm0", mu_t[:, 12:13], c0)
                    S.op("act", lambda: nc.scalar.activation(out=Lbf[0:64, c0:c0 + 512], in_=rf[p_][0:64, :], func=AF.Tanh), r=["rf%d" % p_], w=["Lbf"])
                    S.op("pool", lambda: nc.gpsimd.tensor_copy(Lbf[64:128, c0:c0 + 512], rf[p_][64:128, :]), r=["rf%d" % p_], w=["Lbf"])
                    shift(kf[p_], "kf%d" % p_, pfm[1], "pfm1", mu_t[0:96, 13:14], c0, rows=96)
                    S.op("act", lambda: nc.scalar.activation(out=Glb[0:96, c0:c0 + 512], in_=kf[p_][0:96, :], func=AF.Sigmoid), r=["kf%d" % p_], w=["Glb"])
                it = 0
                for g4 in range(4):
                    inproj_group(12 + g4, 128, pfm[0], "pfm0")
                    inproj_group(16 + g4, 128, pfm[1], "pfm1")
                    inproj_group(20 + g4, 128, pfm[2], "pfm2")
                    rows = slice(g4 * 128, (g4 + 1) * 128)
                    for ch in range(8):
                        c0 = ch * 512
                        cs_ = slice(c0, c0 + 512)
                        p_ = it % 2
                        it += 1
                        P = lambda nm: nm + ("0" if nm in singles else str(p_))
                        shift(rf[p_], P("rf"), pfm[0], "pfm0", mu_t[:, g4:g4 + 1], c0)
                        shift(kf[p_], P("kf"), pfm[1], "pfm1", mu_t[:, 4 + g4:5 + g4], c0)
                        shift(vf[p_], P("vf"), pfm[2], "pfm2", mu_t[:, 8 + g4:9 + g4], c0)
                        S.op("dve", lambda: nc.vector.tensor_scalar(kkr[p_][:], kf[p_][:], kk_t[:, g4:g4 + 1], None, ALU.mult), r=[P("kf"), "kk_t"], w=[P("kkr")])
                        S.op("act", lambda: nc.scalar.activation(out=sqk[p_][:], in_=kkr[p_][:], func=AF.Square), r=[P("kkr")], w=[P("sqk")])
                        S.op("pe", lambda: nc.tensor.matmul(ps[2][:], blk[:], sqk[p_][:], start=True, stop=True), r=["blk", P("sqk")], w=[PSN[2]])
                        S.op("act", lambda: nc.scalar.activation(out=nrm[p_][:], in_=ps[2][:], func=AF.Sqrt), r=[PSN[2]], w=[P("nrm")])
                        S.op("dve", lambda: nc.vector.tensor_scalar(nrm[p_][:], nrm[p_][:], 1e-12, None, ALU.max), r=[P("nrm")], w=[P("nrm")])
                        S.op("dve", lambda: nc.vector.reciprocal(nrm[p_][:], nrm[p_][:]), r=[P("nrm")], w=[P("nrm")])
                        S.op("dve", lambda: nc.vector.tensor_tensor(kkf[p_][:], kkr[p_][:], nrm[p_][:], ALU.mult), r=[P("kkr"), P("nrm")], w=[P("kkf")])
                        S.op("pool", lambda: nc.gpsimd.tensor_scalar(kkn[p_][:], kkf[p_][:], -1.0, None, ALU.mult), r=[P("kkf")], w=[P("kkn")])
                        S.op("poolq", lambda: nc.gpsimd.dma_start(out=KKN[rows, cs_], in_=kkn[p_][:]), r=[P("kkn")], w=["KKN"])
                        S.op("pool", lambda: nc.gpsimd.tensor_copy(rbf[p_][:], rf[p_][:]), r=[P("rf")], w=[P("rbf")])
                        S.op("poolq", lambda: nc.gpsimd.dma_start(out=RR[rows, cs_], in_=rbf[p_][:]), r=[P("rbf")], w=["RR"])
                        for d in range(2):
                            S.op("pe", lambda: nc.tensor.matmul(ps[3][:], Lw[:, 2 + d, rows], Lbf[:, cs_], start=True, stop=True), r=["Lw", "Lbf"], w=[PSN[3]])
                            S.op("act", lambda: nc.scalar.activation(out=ad[p_][:], in_=ps[3][:], func=AF.Sigmoid, bias=a0_t[:, d * 4 + g4:d * 4 + g4 + 1]),
                                 r=[PSN[3], "a0_t"], w=[P("ad")])
                            S.op("pe", lambda: nc.tensor.matmul(ps[4][:], Lw[:, d, rows], Lbf[:, cs_], start=True, stop=True), r=["Lw", "Lbf"], w=[PSN[4]])
                            S.op("act", lambda: nc.scalar.activation(out=sg[p_][:], in_=ps[4][:], func=AF.Sigmoid, bias=w0_t[:, d * 4 + g4:d * 4 + g4 + 1]),
                                 r=[PSN[4], "w0_t"], w=[P("sg")])
                            S.op("act", lambda: nc.scalar.activation(out=dec[p_][:], in_=sg[p_][:], func=AF.Exp, scale=-0.6065306597126334),
                                 r=[P("sg")], w=[P("dec")])
                            S.op("poolq", lambda: nc.gpsimd.dma_start(out=DEC[d, rows, cs_], in_=dec[p_][:]), r=[P("dec")], w=["DEC"])
                            S.op("dve", lambda: nc.vector.tensor_scalar(tmpa[p_][:], ad[p_][:], ka_t[:, g4:g4 + 1], oka_t[:, g4:g4 + 1], ALU.mult, ALU.add),
                                 r=[P("ad"), "ka_t", "oka_t"], w=[P("tmpa")])
                            S.op("pool", lambda: nc.gpsimd.tensor_tensor(kd[p_][:], tmpa[p_][:], kf[p_][:], ALU.mult), r=[P("tmpa"), P("kf")], w=[P("kd")])
                            if d == 0:
                                S.op("pool", lambda: nc.gpsimd.tensor_copy(ksum[p_][:], kd[p_][:]), r=[P("kd")], w=[P("ksum")])
                            else:
                                S.op("pool", lambda: nc.gpsimd.tensor_tensor(ksum[p_][:], ksum[p_][:], kd[p_][:], ALU.add), r=[P("kd"), P("ksum")], w=[P("ksum")])
                            S.op("dve", lambda: nc.vector.tensor_tensor(bbf[p_][:], kkf[p_][:], ad[p_][:], ALU.mult), r=[P("kkf"), P("ad")], w=[P("bbf")])
                            for q in range(4):
                                S.op("pe", lambda: nc.tensor.transpose(ps[5][:, q * 128:(q + 1) * 128], bbf[p_][:, q * 128:(q + 1) * 128], ident[:]),
                                     r=[P("bbf"), "ident"], w=[PSN[5]])
                            S.op("act", lambda: nc.scalar.copy(tmb[p_][:], ps[5][:].rearrange("p (q c) -> p q c", q=4)), r=[PSN[5]], w=[P("tmb")])
                            S.op("poolq", lambda: nc.gpsimd.dma_start(out=BT[0, d, cs_, rows].rearrange("(q p) c -> p q c", p=128), in_=tmb[p_][:]),
                                 r=[P("tmb")], w=["BT"])
                            for q in range(4):
                                S.op("pe", lambda: nc.tensor.transpose(ps[6][:, q * 128:(q + 1) * 128], kd[p_][:, q * 128:(q + 1) * 128], ident[:]),
                                     r=[P("kd"), "ident"], w=[PSN[6]])
                            S.op("act", lambda: nc.scalar.copy(tmk[p_][:], ps[6][:].rearrange("p (q c) -> p q c", q=4)), r=[PSN[6]], w=[P("tmk")])
                            S.op("poolq", lambda: nc.gpsimd.dma_start(out=BT[1, d, cs_, rows].rearrange("(q p) c -> p q c", p=128), in_=tmk[p_][:]),
                                 r=[P("tmk")], w=["BT"])
                        S.op("dve", lambda: nc.vector.scalar_tensor_tensor(bon[p_][:], rf[p_][:], rk_t[:, g4:g4 + 1], ksum[p_][:], ALU.mult, ALU.mult),
                             r=[P("rf"), "rk_t", P("ksum")], w=[P("bon")])
                        S.op("pe", lambda: nc.tensor.matmul(ps[7][:], blk[:], bon[p_][:], start=True, stop=True), r=["blk", P("bon")], w=[PSN[7]])
                        S.op("dve", lambda: nc.vector.tensor_tensor(bon[p_][:], ps[7][:], vf[p_][:], ALU.mult), r=[PSN[7], P("vf"), P("bon")], w=[P("bon")])
                        S.op("poolq", lambda: nc.gpsimd.dma_start(out=BON[rows, cs_], in_=bon[p_][:]), r=[P("bon")], w=["BON"])
                        for q in range(4):
                            S.op("pe", lambda: nc.tensor.transpose(ps[5][:, q * 128:(q + 1) * 128], vf[p_][:, q * 128:(q + 1) * 128], ident[:]),
                                 r=[P("vf"), "ident"], w=[PSN[5]])
                        S.op("act", lambda: nc.scalar.copy(tmv[p_][:], ps[5][:].rearrange("p (q c) -> p q c", q=4)), r=[PSN[5]], w=[P("tmv")])
                        S.op("poolq", lambda: nc.gpsimd.dma_start(out=VT[cs_, rows].rearrange("(q p) c -> p q c", p=128), in_=tmv[p_][:]),
                             r=[P("tmv")], w=["VT"])
                        S.op("pe", lambda: nc.tensor.matmul(ps[3][:], Gw[0:96, rows], Glb[0:96, cs_], start=True, stop=True), r=["Gw", "Glb"], w=[PSN[3]])
                        S.op("act", lambda: nc.scalar.copy(gg[p_][:], ps[3][:]), r=[PSN[3]], w=[P("gg")])
                        S.op("poolq", lambda: nc.gpsimd.dma_start(out=GG[rows, cs_], in_=gg[p_][:]), r=[P("gg")], w=["GG"])
                if DBG:
                    for nm_, src_ in (("KKN", KKN), ("RR", RR), ("DEC", DEC), ("BT", BT), ("VT", VT), ("BON", BON), ("GG", GG)):
                        S.op("poolq", lambda: nc.gpsimd.dma_start(out=dbg_rw[nm_], in_=src_), r=[nm_])
                S.barrier()

        S.barrier()
        with ExitStack() as es3:
            sb3 = lambda name, shape, dt=F32: es3.enter_context(nc.sbuf_tensor(name, shape, dt))
            utm = sb3("utm", [128, NT, 512], BF16)
            Yall = sb3("Yall", [128, 64, 512], BF16)
            for n in range(2):
                srcD = UD if n == 0 else ZD
                srcDn = "UD" if n == 0 else "ZD"
                esA = ExitStack()
                ufl = [esA.enter_context(nc.sbuf_tensor("ufl%d_%d" % (i, n), [128, T], F32)) for i in range(2)]
                for g in range(4):
                    fb_ = g % 2
                    S.op("sp", lambda: nc.sync.dma_start(out=ufl[fb_][:], in_=srcD[g, :, :]), r=[srcDn], w=["ufl%d" % fb_])
                    for i4 in range(NT // 4):
                        pb = i4 % 2
                        for q in range(4):
                            i = i4 * 4 + q
                            S.op("pe", lambda: nc.tensor.transpose(ps[pb][:, q * 128:(q + 1) * 128], ufl[fb_][:, i * 128:(i + 1) * 128], ident[:]),
                                 r=["ufl%d" % fb_, "ident"], w=[PSN[pb]])
                        S.op("act", lambda: nc.scalar.copy(utm[:, i4 * 4:(i4 + 1) * 4, g * 128:(g + 1) * 128],
                                                           ps[pb][:].rearrange("p (q c) -> p q c", q=4)),
                             r=[PSN[pb]], w=["utm"])
                S.barrier()
                esA.close()
                esB = ExitStack()
                sbB = lambda name, shape, dt=F32: esB.enter_context(nc.sbuf_tensor(name + "_%d" % n, shape, dt))
                dre = [sbB("dre%d" % i, [128, 32, 128], BF16) for i in range(2)]
                dim_ = [sbB("dim%d" % i, [128, 32, 128], BF16) for i in range(2)]
                kre = [sbB("kre%d" % i, [128, 512]) for i in range(2)]
                kim = [sbB("kim%d" % i, [128, 512]) for i in range(2)]
                ure = sbB("ure", [128, 512])
                uim = sbB("uim", [128, 512])
                t1 = sbB("t1", [128, 512])
                t2 = sbB("t2", [128, 512])
                t3 = sbB("t3", [128, 512])
                t4 = sbB("t4", [128, 512])
                for j in range(32):
                    b = j % 2
                    S.op("sp", lambda: nc.sync.dma_start(out=dre[b][:], in_=dfw[j]), w=["dre%d" % b])
                    S.op("sp", lambda: nc.sync.dma_start(out=dim_[b][:], in_=dfw[32 + j]), w=["dim%d" % b])
                    S.op("sp", lambda: nc.sync.dma_start(out=kre[b][:], in_=KF[j, :, n * 512:(n + 1) * 512]), r=["KF"], w=["kre%d" % b])
                    S.op("sp", lambda: nc.sync.dma_start(out=kim[b][:], in_=KF[32 + j, :, n * 512:(n + 1) * 512]), r=["KF"], w=["kim%d" % b])
                    pr, pi = 2 + 2 * b, 3 + 2 * b
                    for tcn in range(NT):
                        S.op("pe", lambda: nc.tensor.matmul(ps[pr][:], dre[b][:, tcn, :], utm[:, tcn, :], start=(tcn == 0), stop=(tcn == NT - 1)),
                             r=["dre%d" % b, "utm"], w=[PSN[pr]])
                    for tcn in range(NT):
                        S.op("pe", lambda: nc.tensor.matmul(ps[pi][:], dim_[b][:, tcn, :], utm[:, tcn, :], start=(tcn == 0), stop=(tcn == NT - 1)),
                             r=["dim%d" % b, "utm"], w=[PSN[pi]])
                    S.op("act", lambda: nc.scalar.copy(ure[:], ps[pr][:]), r=[PSN[pr]], w=["ure"])
                    S.op("act", lambda: nc.scalar.copy(uim[:], ps[pi][:]), r=[PSN[pi]], w=["uim"])
                    S.op("dve", lambda: nc.vector.tensor_tensor(t1[:], ure[:], kre[b][:], ALU.mult), r=["ure", "kre%d" % b], w=["t1"])
                    S.op("pool", lambda: nc.gpsimd.tensor_tensor(t2[:], uim[:], kim[b][:], ALU.mult), r=["uim", "kim%d" % b], w=["t2"])
                    S.op("dve", lambda: nc.vector.tensor_tensor(Yall[:, j, :], t1[:], t2[:], ALU.subtract), r=["t1", "t2"], w=["Yall"])
                    S.op("pool", lambda: nc.gpsimd.tensor_tensor(t3[:], ure[:], kim[b][:], ALU.mult), r=["ure", "kim%d" % b], w=["t3"])
                    S.op("dve", lambda: nc.vector.tensor_tensor(t4[:], uim[:], kre[b][:], ALU.mult), r=["uim", "kre%d" % b], w=["t4"])
                    S.op("pool", lambda: nc.gpsimd.tensor_tensor(Yall[:, 32 + j, :], t3[:], t4[:], ALU.add), r=["t3", "t4"], w=["Yall"])
                    if j == 0:
                        S.op("dve", lambda: nc.vector.tensor_tensor(Yall[0:1, 0, :], ure[0:1, :], kre[b][0:1, :], ALU.mult),
                             r=["ure", "kre%d" % b, "Yall"], w=["Yall"])
                        S.op("dve", lambda: nc.vector.tensor_tensor(Yall[0:1, 32, :], uim[0:1, :], kim[b][0:1, :], ALU.mult),
                             r=["uim", "kim%d" % b, "Yall"], w=["Yall"])
                S.barrier()
                esB.close()
                esC = ExitStack()
                sbC = lambda name, shape, dt=F32: esC.enter_context(nc.sbuf_tensor(name + "_%d" % n, shape, dt))
                dvt = sbC("dvt", [128, 64, 256], BF16)
                gate = [sbC("gate%d" % i, [128, 256]) for i in range(2)]
                zo = [sbC("zo%d" % i, [128, 256]) for i in range(2)]
                dstD = ZD if n == 0 else YD
                dstDn = "ZD" if n == 0 else "YD"
                for tq in range(16):
                    S.op("sp", lambda: nc.sync.dma_start(out=dvt[:], in_=dinv[tq]), w=["dvt"])
                    for g in range(4):
                        gb = g % 2
                        pb = 4 + g
                        gsrc = UD[4 + g] if n == 0 else UD[8 + g]
                        S.op("sp", lambda: nc.sync.dma_start(out=gate[gb][:], in_=gsrc[:, tq * 256:(tq + 1) * 256]), r=["UD"], w=["gate%d" % gb])
                        for rc in range(64):
                            S.op("pe", lambda: nc.tensor.matmul(ps[pb][:, 0:256], Yall[:, rc, g * 128:(g + 1) * 128], dvt[:, rc, :],
                                                                start=(rc == 0), stop=(rc == 63)),
                                 r=["Yall", "dvt"], w=[PSN[pb]])
                        S.op("dve", lambda: nc.vector.tensor_tensor(zo[gb][:], ps[pb][:, 0:256], gate[gb][:], ALU.mult),
                             r=[PSN[pb], "gate%d" % gb], w=["zo%d" % gb])
                        S.op("poolq", lambda: nc.gpsimd.dma_start(out=dstD[g, :, tq * 256:(tq + 1) * 256], in_=zo[gb][:]), r=["zo%d" % gb], w=[dstDn])
                S.barrier()
                esC.close()
            if DBG:
                S.op("poolq", lambda: nc.gpsimd.dma_start(out=dbg_y[:, :, :], in_=YD[:, :, :]), r=["YD"])

        S.barrier()
        with ExitStack() as esS:
            sbS = lambda name, shape, dt=F32: esS.enter_context(nc.sbuf_tensor(name, shape, dt))
            TC = 64
            St = sbS("St", [128, 512])
            Sbf = sbS("Sbf", [128, 512], BF16)
            rhs2 = sbS("rhs2", [48, 512], BF16)
            Vtm = sbS("Vtm", [128, NT, 512], BF16)
            m48 = sbS("m48_sb", [48, 512])
            m40 = sbS("m40_sb", [40, 1, 512])
            selF = sbS("selF_sb", [128, 128, 48], BF16)
            selB = sbS("selB_sb", [128, 128, 48], BF16)
            ARraw = sbS("ARraw", [128, 104, TC], BF16)
            AR2 = [sbS("AR2_%d" % i, [128, TC, 104], BF16) for i in range(2)]
            Wraw = sbS("Wraw", [128, 8, TC])
            W2 = [sbS("W2_%d" % i, [128, TC, 8, 1]) for i in range(2)]
            BKraw = sbS("BKraw", [48, TC, 128], BF16)
            BK2 = [sbS("BK2_%d" % i, [48, TC, 128], BF16) for i in range(2)]
            Oraw = [sbS("Oraw%d" % i, [40, 8, 512]) for i in range(2)]
            Ored = [sbS("Ored%d" % i, [40, 8, 64]) for i in range(2)]
            S.op("pool", lambda: nc.gpsimd.memset(St[:], 0.0), w=["St"])
            S.op("pool", lambda: nc.gpsimd.memset(Sbf[:], 0.0), w=["Sbf"])
            S.op("pool", lambda: nc.gpsimd.memset(rhs2[:], 0.0), w=["rhs2"])
            S.op("pool", lambda: nc.gpsimd.memset(ARraw[:], 0.0), w=["ARraw"])
            S.op("pool", lambda: nc.gpsimd.memset(BKraw[:], 0.0), w=["BKraw"])
            for i in range(2):
                S.op("pool", lambda: nc.gpsimd.memset(BK2[i][:], 0.0), w=["BK2_%d" % i])
            S.op("sp", lambda: nc.sync.dma_start(out=Vtm[:], in_=VT.rearrange("(i p) c -> p i c", p=128)), r=["VT"], w=["Vtm"])
            S.op("sp", lambda: nc.sync.dma_start(out=m48[:], in_=m48_in[:, :]), w=["m48"])
            S.op("sp", lambda: nc.sync.dma_start(out=m40[:, 0, :], in_=m40_in[:, :]), w=["m40"])
            S.op("sp", lambda: nc.sync.dma_start(out=selF[:], in_=selF_in[:, :, :]), w=["selF"])
            S.op("sp", lambda: nc.sync.dma_start(out=selB[:], in_=selB_in[:, :, :]), w=["selB"])
            KKN_v = KKN.rearrange("(j k) t -> k j t", k=64)
            RR_v = RR.rearrange("(j k) t -> k j t", k=64)
            for c in range(SCAN_CHUNKS):
                cb = c % 2
                s0 = c * TC
                fw_ = slice(s0, s0 + TC)
                bw_ = slice(T - s0 - TC, T - s0)
                A2n, W2n, B2n = "AR2_%d" % cb, "W2_%d" % cb, "BK2_%d" % cb
                S.op("sp", lambda: nc.sync.dma_start(out=ARraw[0:64, 0:8, :], in_=KKN_v[:, :, fw_]), r=["KKN"], w=["ARraw"])
                S.op("sp", lambda: nc.sync.dma_start(out=ARraw[64:128, 32:40, :], in_=KKN_v[:, :, bw_]), r=["KKN"], w=["ARraw"])
                S.op("sp", lambda: nc.sync.dma_start(out=ARraw[0:64, 64:72, :], in_=RR_v[:, :, fw_]), r=["RR"], w=["ARraw"])
                S.op("sp", lambda: nc.sync.dma_start(out=ARraw[64:128, 96:104, :], in_=RR_v[:, :, bw_]), r=["RR"], w=["ARraw"])
                S.op("pool", lambda: nc.gpsimd.tensor_copy(AR2[cb][0:64], ARraw[0:64].rearrange("p c t -> p t c")), r=["ARraw"], w=[A2n])
                S.op("pool", lambda: nc.gpsimd.tensor_copy(AR2[cb][64:128], ARraw[64:128, :, ::-1].rearrange("p c t -> p t c")), r=["ARraw"], w=[A2n])
                S.op("sp", lambda: nc.sync.dma_start(out=Wraw[0:64], in_=DEC[0].rearrange("(j k) t -> k j t", k=64)[:, :, fw_]), r=["DEC"], w=["Wraw"])
                S.op("sp", lambda: nc.sync.dma_start(out=Wraw[64:128], in_=DEC[1].rearrange("(j k) t -> k j t", k=64)[:, :, bw_]), r=["DEC"], w=["Wraw"])
                S.op("pool", lambda: nc.gpsimd.tensor_copy(W2[cb][0:64, :, :, 0], Wraw[0:64].rearrange("p j t -> p t j")), r=["Wraw"], w=[W2n])
                S.op("pool", lambda: nc.gpsimd.tensor_copy(W2[cb][64:128, :, :, 0], Wraw[64:128, :, ::-1].rearrange("p j t -> p t j")), r=["Wraw"], w=[W2n])
                for s_ in range(2):
                    S.op("sp", lambda: nc.sync.dma_start(out=BKraw[s_ * 8:(s_ + 1) * 8, :, 0:64],
                                                         in_=BT[s_, 0, fw_, :].rearrange("t (j k) -> j t k", k=64)), r=["BT"], w=["BKraw"])
                    S.op("sp", lambda: nc.sync.dma_start(out=BKraw[32 + s_ * 8:32 + (s_ + 1) * 8, :, 64:128],
                                                         in_=BT[s_, 1, bw_, :].rearrange("t (j k) -> j t k", k=64)), r=["BT"], w=["BKraw"])
                S.op("pool", lambda: nc.gpsimd.tensor_copy(BK2[cb][0:16], BKraw[0:16]), r=["BKraw"], w=[B2n])
                S.op("pool", lambda: nc.gpsimd.tensor_copy(BK2[cb][32:48], BKraw[32:48, ::-1, :]), r=["BKraw"], w=[B2n])
                for s in range(TC):
                    st = s0 + s
                    i_f, tl = st // 128, st % 128
                    i_b = NT - 1 - i_f
                    pA, pB, pO = st % 2, 2 + st % 2, 4 + st % 2
                    ob = (st // 8) % 2
                    S.op("pe", lambda: nc.tensor.matmul(ps[pA][0:48, :], selF[:, tl, :], Vtm[:, i_f, :], start=True, stop=False),
                         r=["selF", "Vtm"], w=[PSN[pA]])
                    S.op("pe", lambda: nc.tensor.matmul(ps[pA][0:48, :], selB[:, tl, :], Vtm[:, i_b, :], start=False, stop=False),
                         r=["selB", "Vtm"], w=[PSN[pA]])
                    S.op("pe", lambda: nc.tensor.matmul(ps[pA][0:48, :], AR2[cb][:, s, 0:48], Sbf[:], start=False, stop=True),
                         r=[A2n, "Sbf"], w=[PSN[pA]])
                    S.op("dve", lambda: nc.vector.tensor_tensor(rhs2[:], ps[pA][0:48, :], m48[:], ALU.mult), r=[PSN[pA], "m48"], w=["rhs2"])
                    S.op("dve", lambda: nc.vector.tensor_tensor(St[:].rearrange("p (j v) -> p j v", j=8), St[:].rearrange("p (j v) -> p j v", j=8),
                                                                W2[cb][:, s, :, :].to_broadcast([128, 8, 64]), ALU.mult),
                         r=["St", W2n], w=["St"])
                    S.op("pe", lambda: nc.tensor.matmul(ps[pB][:], BK2[cb][0:48, s, :], rhs2[:], start=True, stop=True), r=[B2n, "rhs2"], w=[PSN[pB]])
                    S.op("dve", lambda: nc.vector.tensor_tensor(St[:], St[:], ps[pB][:], ALU.add), r=["St", PSN[pB]], w=["St"])
                    S.op("act", lambda: nc.scalar.copy(Sbf[:], St[:]), r=["St"], w=["Sbf"])
                    S.op("pe", lambda: nc.tensor.matmul(ps[pO][0:40, :], AR2[cb][:, s, 64:104], Sbf[:], start=True, stop=True), r=[A2n, "Sbf"], w=[PSN[pO]])
                    S.op("act", lambda: nc.scalar.copy(Oraw[ob][:, st % 8, :], ps[pO][0:40, :]), r=[PSN[pO]], w=["Oraw%d" % ob])
                    if st % 8 == 7:
                        st0 = st - 7
                        S.op("pool", lambda: nc.gpsimd.tensor_tensor(Oraw[ob][:], Oraw[ob][:], m40[:].to_broadcast([40, 8, 512]), ALU.mult),
                             r=["Oraw%d" % ob, "m40"], w=["Oraw%d" % ob])
                        S.op("dve", lambda: nc.vector.tensor_reduce(out=Ored[ob][:], in_=Oraw[ob][:].rearrange("p s (j v) -> p s v j", j=8),
                                                                     axis=AX.X, op=ALU.add),
                             r=["Oraw%d" % ob], w=["Ored%d" % ob])
                        S.op("poolq", lambda: nc.gpsimd.dma_start(out=OD[0, st0:st0 + 8, :].rearrange("s (j v) -> j s v", v=64), in_=Ored[ob][0:8]),
                             r=["Ored%d" % ob], w=["OD"])
                        S.op("poolq", lambda: nc.gpsimd.dma_start(out=OD[1, st0:st0 + 8, :].rearrange("s (j v) -> j s v", v=64), in_=Ored[ob][32:40]),
                             r=["Ored%d" % ob], w=["OD"])
            if DBG:
                S.op("poolq", lambda: nc.gpsimd.dma_start(out=dbg_od, in_=OD), r=["OD"])
            S.barrier()

        with ExitStack() as esP:
            sbP = lambda name, shape, dt=F32: esP.enter_context(nc.sbuf_tensor(name, shape, dt))
            pvec = lambda ap_row, p=128: ap_row.rearrange("o (g p) -> p (o g)", p=p)
            yT = sbP("yT", [128, 8, T], BF16)
            ldb = [sbP("ldb%d" % i, [128, 1024]) for i in range(2)]
            n = 0
            for g in range(4):
                for c4 in range(4):
                    b = n % 2
                    n += 1
                    S.op("sp", lambda: nc.sync.dma_start(out=ldb[b][:], in_=YD[g, :, c4 * 1024:(c4 + 1) * 1024]), r=["YD"], w=["ldb%d" % b])
                    S.op("pool", lambda: nc.gpsimd.tensor_copy(yT[:, g, c4 * 1024:(c4 + 1) * 1024], ldb[b][:]), r=["ldb%d" % b], w=["yT"])
            blk2 = sbP("blk2", [128, 128])
            lnw_t = sbP("lnw_t", [128, 4])
            lnb_t = sbP("lnb_t", [128, 4])
            S.op("sp", lambda: nc.sync.dma_start(out=blk2[:], in_=blk_in[:, :]), w=["blk2"])
            S.op("sp", lambda: nc.sync.dma_start(out=lnw_t[:], in_=pvec(rw_ln_w[0:1, :])), w=["lnw_t"])
            S.op("sp", lambda: nc.sync.dma_start(out=lnb_t[:], in_=pvec(rw_ln_b[0:1, :])), w=["lnb_t"])
            oft = [sbP("oft%d" % i, [128, 4, 128]) for i in range(2)]
            obt = [sbP("obt%d" % i, [128, 4, 128]) for i in range(2)]
            bonc = [sbP("bonc%d" % i, [128, 512]) for i in range(2)]
            ggc = [sbP("ggc%d" % i, [128, 512]) for i in range(2)]
            obr = sbP("obr", [128, 4, 128])
            sfm = sbP("sfm", [128, 512])
            cen = sbP("cen", [128, 512])
            sqp = sbP("sqp", [128, 512])
            rstd = sbP("rstd", [128, 512])
            n = 0
            for g4 in range(4):
                rows = slice(g4 * 128, (g4 + 1) * 128)
                for i4 in range(8):
                    b = n % 2
                    n += 1
                    cs_ = slice(i4 * 512, (i4 + 1) * 512)
                    S.op("sp", lambda: nc.sync.dma_start(out=oft[b][:], in_=OD[0, cs_, rows].rearrange("(q p) c -> p q c", p=128)), r=["OD"], w=["oft%d" % b])
                    S.op("sp", lambda: nc.sync.dma_start(out=obt[b][:], in_=OD[1, T - (i4 + 1) * 512:T - i4 * 512, rows].rearrange("(m p) c -> p m c", p=128)),
                         r=["OD"], w=["obt%d" % b])
                    S.op("sp", lambda: nc.sync.dma_start(out=bonc[b][:], in_=BON[rows, cs_]), r=["BON"], w=["bonc%d" % b])
                    S.op("sp", lambda: nc.sync.dma_start(out=ggc[b][:], in_=GG[rows, cs_]), r=["GG"], w=["ggc%d" % b])
                    for q in range(4):
                        S.op("pe", lambda: nc.tensor.transpose(ps[0][:, q * 128:(q + 1) * 128], oft[b][:, q, :], ident[:]), r=["oft%d" % b, "ident"], w=[PSN[0]])
                    for q in range(4):
                        S.op("pe", lambda: nc.tensor.transpose(ps[1][:, q * 128:(q + 1) * 128], obt[b][:, 3 - q, :], ident[:]), r=["obt%d" % b, "ident"], w=[PSN[1]])
                    S.op("act", lambda: nc.scalar.copy(obr[:], ps[1][:].rearrange("p (q t) -> p q t", q=4)[:, :, ::-1]), r=[PSN[1]], w=["obr"])
                    S.op("dve", lambda: nc.vector.tensor_tensor(sfm[:], ps[0][:], obr[:].rearrange("p q t -> p (q t)"), ALU.add), r=[PSN[0], "obr"], w=["sfm"])
                    S.op("pe", lambda: nc.tensor.matmul(ps[2][:], blk2[:], sfm[:], start=True, stop=True), r=["blk2", "sfm"], w=[PSN[2]])
                    S.op("dve", lambda: nc.vector.scalar_tensor_tensor(cen[:], ps[2][:], -1.0 / 64, sfm[:], ALU.mult, ALU.add), r=[PSN[2], "sfm"], w=["cen"])
                    S.op("act", lambda: nc.scalar.activation(out=sqp[:], in_=cen[:], func=AF.Square), r=["cen"], w=["sqp"])
                    S.op("pe", lambda: nc.tensor.matmul(ps[3][:], blk2[:], sqp[:], start=True, stop=True), r=["blk2", "sqp"], w=[PSN[3]])
                    S.op("dve", lambda: nc.vector.tensor_scalar(rstd[:], ps[3][:], 1.0 / 64, 64e-5, ALU.mult, ALU.add), r=[PSN[3]], w=["rstd"])
                    S.op("act", lambda: nc.scalar.activation(out=rstd[:], in_=rstd[:], func=AF.Sqrt), r=["rstd"], w=["rstd"])
                    S.op("dve", lambda: nc.vector.reciprocal(rstd[:], rstd[:]), r=["rstd"], w=["rstd"])
                    S.op("dve", lambda: nc.vector.tensor_tensor(cen[:], cen[:], rstd[:], ALU.mult), r=["cen", "rstd"], w=["cen"])
                    S.op("dve", lambda: nc.vector.tensor_scalar(cen[:], cen[:], lnw_t[:, g4:g4 + 1], lnb_t[:, g4:g4 + 1], ALU.mult, ALU.add),
                         r=["cen", "lnw_t", "lnb_t"], w=["cen"])
                    S.op("pool", lambda: nc.gpsimd.tensor_tensor(cen[:], cen[:], bonc[b][:], ALU.add), r=["cen", "bonc%d" % b], w=["cen"])
                    S.op("pool", lambda: nc.gpsimd.tensor_tensor(yT[:, 4 + g4, cs_], cen[:], ggc[b][:], ALU.mult), r=["cen", "ggc%d" % b], w=["yT"])
            wo = sbP("wo", [128, 8, D], BF16)
            g1t = sbP("g1t", [128, D])
            xin = [sbP("xin%d" % i, [128, D]) for i in range(2)]
            x1t = [sbP("x1t%d" % i, [128, D]) for i in range(2)]
            for kc in range(8):
                b = kc % 2
                S.op("sp", lambda: nc.sync.dma_start(out=ldb[b][:], in_=w_out[kc * 128:(kc + 1) * 128, :]), w=["ldb%d" % b])
                S.op("pool", lambda: nc.gpsimd.tensor_copy(wo[:, kc, :], ldb[b][:]), r=["ldb%d" % b], w=["wo"])
            S.op("sp", lambda: nc.sync.dma_start(out=g1t[:], in_=MODD[:, 2 * D:3 * D]), r=["MODD"], w=["g1t"])
            for i in range(NT):
                b = i % 2
                S.op("sp", lambda: nc.sync.dma_start(out=xin[b][:], in_=x[i * 128:(i + 1) * 128, :]), w=["xin%d" % b])
                for half in range(2):
                    hs = slice(half * 512, (half + 1) * 512)
                    for kc in range(8):
                        S.op("pe", lambda: nc.tensor.matmul(ps[4 + half][:], yT[:, kc, i * 128:(i + 1) * 128], wo[:, kc, hs], start=(kc == 0), stop=(kc == 7)),
                             r=["yT", "wo"], w=[PSN[4 + half]])
                    S.op("dve", lambda: nc.vector.tensor_tensor(x1t[b][:, hs], ps[4 + half][:], g1t[:, hs], ALU.mult), r=[PSN[4 + half], "g1t"], w=["x1t%d" % b])
                    S.op("pool", lambda: nc.gpsimd.tensor_tensor(x1t[b][:, hs], x1t[b][:, hs], xin[b][:, hs], ALU.add), r=["x1t%d" % b, "xin%d" % b], w=["x1t%d" % b])
                S.op("poolq", lambda: nc.gpsimd.dma_start(out=X1D[i * 128:(i + 1) * 128, :], in_=x1t[b][:]), r=["x1t%d" % b], w=["X1D"])
            if DBG:
                S.op("poolq", lambda: nc.gpsimd.dma_start(out=dbg_x1, in_=X1D), r=["X1D"])
            S.barrier()

        NE = 256
        CAP = 1024
        NST = CAP // 128
        with ExitStack() as esF:
            sbF = lambda name, shape, dt=F32: esF.enter_context(nc.sbuf_tensor(name, shape, dt))
            g2t = sbF("g2t", [128, D])
            S.op("sp", lambda: nc.sync.dma_start(out=g2t[:], in_=MODD[:, 5 * D:6 * D]), r=["MODD"], w=["g2t"])
            keys = sbF("keys", [128, 2, CAP])
            idxI = sbF("idxI", [128, NST, NE], I32)
            with ExitStack() as esG:
                sbG = lambda name, shape, dt=F32: esG.enter_context(nc.sbuf_tensor(name, shape, dt))
                gm2 = sbG("gm2", [128, D]); sc2 = sbG("sc2", [128, D]); sh2 = sbG("sh2", [128, D])
                S.op("sp", lambda: nc.sync.dma_start(out=gm2[:], in_=bcast_rows(norm2_g[0:1, :])), w=["gm2"])
                S.op("sp", lambda: nc.sync.dma_start(out=sh2[:], in_=MODD[:, 3 * D:4 * D]), r=["MODD"], w=["sh2"])
                S.op("sp", lambda: nc.sync.dma_start(out=sc2[:], in_=MODD[:, 4 * D:5 * D]), r=["MODD"], w=["sc2"])
                S.op("dve", lambda: nc.vector.scalar_tensor_tensor(gm2[:], sc2[:], 1.0, gm2[:], ALU.add, ALU.mult), r=["sc2", "gm2"], w=["gm2"])
                rwt = sbG("rwt", [128, 8, NE])
                S.op("sp", lambda: nc.sync.dma_start(out=rwt[:], in_=router_w.rearrange("(kc p) n -> p kc n", p=128)), w=["rwt"])
                rbias = sbG("rbias", [128, NE])
                S.op("sp", lambda: nc.sync.dma_start(out=rbias[:], in_=bcast_rows(router_bias[0:1, :])), w=["rbias"])
                tokid = sbG("tokid_sb", [128, NT])
                S.op("sp", lambda: nc.sync.dma_start(out=tokid[:], in_=tokid_in[:, :]), w=["tokid"])
                zrow = sbG("zrow", [1, D + NE])
                S.op("pool", lambda: nc.gpsimd.memset(zrow[:], 0.0), w=["zrow"])
                S.op("poolq", lambda: nc.gpsimd.dma_start(out=H2E[T:T + 1, :], in_=zrow[:]), r=["zrow"], w=["H2E"])
                S.op("poolq", lambda: nc.gpsimd.dma_start(out=OUTACC[T:T + 1, :], in_=zrow[:, 0:D]), r=["zrow"], w=["OUTACC"])
                shgu = sbG("shgu", [128, 8, 512], BF16)
                shd = sbG("shd", [128, 2, D], BF16)
                stg = sbG("stg", [128, 8, 256])
                S.op("sp", lambda: nc.sync.dma_start(out=stg[:], in_=sh_w_gate.rearrange("(kc p) n -> p kc n", p=128)), w=["stg"])
                S.op("pool", lambda: nc.gpsimd.tensor_copy(shgu[:, :, 0:256], stg[:]), r=["stg"], w=["shgu"])
                S.op("sp", lambda: nc.sync.dma_start(out=stg[:], in_=sh_w_up.rearrange("(kc p) n -> p kc n", p=128)), w=["stg"])
                S.op("pool", lambda: nc.gpsimd.tensor_copy(shgu[:, :, 256:512], stg[:]), r=["stg"], w=["shgu"])
                S.op("sp", lambda: nc.sync.dma_start(out=stg[:].rearrange("p a b -> p (a b)").rearrange("p (j n) -> p j n", j=2),
                                                     in_=sh_w_down.rearrange("(jc p) n -> p jc n", p=128)), w=["stg"])
                S.op("pool", lambda: nc.gpsimd.tensor_copy(shd[:], stg[:].rearrange("p a b -> p (a b)").rearrange("p (j n) -> p j n", j=2)), r=["stg"], w=["shd"])
                KT = sbG("KT", [128, 2, T])
                xt2 = [sbG("xt2_%d" % i, [128, D]) for i in range(2)]
                h2 = [sbG("h2_%d" % i, [128, D]) for i in range(2)]
                sq2 = sbG("sq2", [128, D])
                ss2 = [sbG("ss2_%d" % i, [128, 1]) for i in range(2)]
                rs2 = [sbG("rs2_%d" % i, [128, 1]) for i in range(2)]
                h2T = sbG("h2T", [128, 8, 128])
                h2Tb = sbG("h2Tb", [128, 8, 128], BF16)
                sc = sbG("sc", [128, NE]); bia = sbG("bia", [128, NE]); msk = sbG("msk", [128, NE]); sel = sbG("sel", [128, NE])
                gat = [sbG("gat%d" % i, [128, NE]) for i in range(2)]
                key = sbG("key", [128, NE])
                m8 = sbG("m8", [128, 8, 8]); gs = sbG("gs", [128, 8]); gm8 = sbG("gm8", [128, 8]); gmk = sbG("gmk", [128, 8, 1]); pen = sbG("pen", [128, 8, 1])
                t8 = sbG("t8", [128, 8]); den = sbG("den", [128, 1])
                sgs = sbG("sgs", [128, 256]); acs = sbG("acs", [128, 256]); acT = sbG("acT", [128, 2, 128], BF16)
                acc = [sbG("acc%d" % i, [128, D]) for i in range(2)]
                for i in range(NT):
                    b = i % 2
                    tsl = slice(i * 128, (i + 1) * 128)
                    S.op("sp", lambda: nc.sync.dma_start(out=xt2[b][:], in_=X1D[tsl, :]), r=["X1D"], w=["xt2_%d" % b])
                    rstd_of(sq2, ss2[b], rs2[b], "n2_%d" % b, xt2[b][:], "xt2_%d" % b)
                    S.op("dve", lambda: nc.vector.scalar_tensor_tensor(h2[b][:], xt2[b][:], rs2[b][:, 0:1], gm2[:], ALU.mult, ALU.mult),
                         r=["xt2_%d" % b, "rsn2_%d" % b, "gm2"], w=["h2_%d" % b])
                    S.op("pool", lambda: nc.gpsimd.tensor_tensor(h2[b][:], h2[b][:], sh2[:], ALU.add), r=["h2_%d" % b, "sh2"], w=["h2_%d" % b])
                    S.op("poolq", lambda: nc.gpsimd.dma_start(out=H2E[tsl, 0:D], in_=h2[b][:]), r=["h2_%d" % b], w=["H2E"])
                    for half in range(2):
                        pb = half
                        for q in range(4):
                            kc = half * 4 + q
                            S.op("pe", lambda: nc.tensor.transpose(ps[pb][:, q * 128:(q + 1) * 128], h2[b][:, kc * 128:(kc + 1) * 128], ident[:]),
                                 r=["h2_%d" % b, "ident"], w=[PSN[pb]])
                        S.op("act", lambda: nc.scalar.copy(h2T[:, half * 4:(half + 1) * 4, :], ps[pb][:].rearrange("p (q t) -> p q t", q=4)), r=[PSN[pb]], w=["h2T"])
                    S.op("pool", lambda: nc.gpsimd.tensor_copy(h2Tb[:], h2T[:]), r=["h2T"], w=["h2Tb"])
                    for kc in range(8):
                        S.op("pe", lambda: nc.tensor.matmul(ps[2][:, 0:NE], h2T[:, kc, :], rwt[:, kc, :], start=(kc == 0), stop=(kc == 7)), r=["h2T", "rwt"], w=[PSN[2]])
                    S.op("act", lambda: nc.scalar.activation(out=sc[:], in_=ps[2][:, 0:NE], func=AF.Sigmoid), r=[PSN[2]], w=["sc"])
                    S.op("dve", lambda: nc.vector.tensor_tensor(bia[:], sc[:], rbias[:], ALU.add), r=["sc", "rbias"], w=["bia"])
                    for g in range(8):
                        S.op("dve", lambda: nc.vector.max(out=m8[:, g, :], in_=bia[:, g * 32:(g + 1) * 32]), r=["bia"], w=["m8"])
                    S.op("dve", lambda: nc.vector.tensor_tensor(gs[:], m8[:, :, 0], m8[:, :, 1], ALU.add), r=["m8"], w=["gs"])
                    S.op("dve", lambda: nc.vector.max(out=gm8[:], in_=gs[:]), r=["gs"], w=["gm8"])
                    S.op("dve", lambda: nc.vector.tensor_scalar(gmk[:, :, 0], gs[:], gm8[:, 3:4], None, ALU.is_ge), r=["gs", "gm8"], w=["gmk"])
                    S.op("dve", lambda: nc.vector.tensor_scalar(pen[:, :, 0], gmk[:, :, 0], 1e9, -1e9, ALU.mult, ALU.add), r=["gmk"], w=["pen"])
                    S.op("dve", lambda: nc.vector.tensor_tensor(msk[:].rearrange("p (g e) -> p g e", g=8), bia[:].rearrange("p (g e) -> p g e", g=8),
                                                                gmk[:].to_broadcast([128, 8, 32]), ALU.mult), r=["bia", "gmk"], w=["msk"])
                    S.op("dve", lambda: nc.vector.tensor_tensor(msk[:].rearrange("p (g e) -> p g e", g=8), msk[:].rearrange("p (g e) -> p g e", g=8),
                                                                pen[:].to_broadcast([128, 8, 32]), ALU.add), r=["msk", "pen"], w=["msk"])
                    S.op("dve", lambda: nc.vector.max(out=t8[:], in_=msk[:]), r=["msk"], w=["t8"])
                    S.op("dve", lambda: nc.vector.tensor_scalar(sel[:], msk[:], t8[:, 7:8], None, ALU.is_ge), r=["msk", "t8"], w=["sel"])
                    S.op("dve", lambda: nc.vector.tensor_tensor(gat[b][:], sc[:], sel[:], ALU.mult), r=["sc", "sel"], w=["gat%d" % b])
                    S.op("dve", lambda: nc.vector.tensor_reduce(out=den[:], in_=gat[b][:], axis=AX.X, op=ALU.add), r=["gat%d" % b], w=["den"])
                    S.op("dve", lambda: nc.vector.reciprocal(den[:], den[:]), r=["den"], w=["den"])
                    S.op("dve", lambda: nc.vector.tensor_scalar(gat[b][:], gat[b][:], den[:, 0:1], 2.5, ALU.mult, ALU.mult), r=["gat%d" % b, "den"], w=["gat%d" % b])
                    S.op("poolq", lambda: nc.gpsimd.dma_start(out=H2E[tsl, D:D + NE], in_=gat[b][:]), r=["gat%d" % b], w=["H2E"])
                    S.op("dve", lambda: nc.vector.tensor_scalar(key[:], sel[:], tokid[:, i:i + 1], None, ALU.mult), r=["sel", "tokid"], w=["key"])
                    for eh in range(2):
                        S.op("pe", lambda: nc.tensor.transpose(ps[3][:, eh * 128:(eh + 1) * 128], key[:, eh * 128:(eh + 1) * 128], ident[:]), r=["key", "ident"], w=[PSN[3]])
                    S.op("act", lambda: nc.scalar.copy(KT[:, :, tsl], ps[3][:, 0:256].rearrange("p (e t) -> p e t", e=2)), r=[PSN[3]], w=["KT"])
                    for kc in range(8):
                        S.op("pe", lambda: nc.tensor.matmul(ps[4][:], h2Tb[:, kc, :], shgu[:, kc, :], start=(kc == 0), stop=(kc == 7)), r=["h2Tb", "shgu"], w=[PSN[4]])
                    S.op("act", lambda: nc.scalar.activation(out=sgs[:], in_=ps[4][:, 0:256], func=AF.Silu), r=[PSN[4]], w=["sgs"])
                    S.op("dve", lambda: nc.vector.tensor_tensor(acs[:], sgs[:], ps[4][:, 256:512], ALU.mult), r=["sgs", PSN[4]], w=["acs"])
                    for jc in range(2):
                        S.op("pe", lambda: nc.tensor.transpose(ps[5][:, jc * 128:(jc + 1) * 128], acs[:, jc * 128:(jc + 1) * 128], ident[:]), r=["acs", "ident"], w=[PSN[5]])
                    S.op("act", lambda: nc.scalar.copy(acT[:], ps[5][:, 0:256].rearrange("p (j t) -> p j t", j=2)), r=[PSN[5]], w=["acT"])
                    for half in range(2):
                        hs = slice(half * 512, (half + 1) * 512)
                        for jc in range(2):
                            S.op("pe", lambda: nc.tensor.matmul(ps[6 + half][:], acT[:, jc, :], shd[:, jc, hs], start=(jc == 0), stop=(jc == 1)), r=["acT", "shd"], w=[PSN[6 + half]])
                        S.op("dve", lambda: nc.vector.tensor_tensor(acc[b][:, hs], ps[6 + half][:], g2t[:, hs], ALU.mult), r=[PSN[6 + half], "g2t"], w=["acc%d" % b])
                        S.op("pool", lambda: nc.gpsimd.tensor_tensor(acc[b][:, hs], acc[b][:, hs], xt2[b][:, hs], ALU.add), r=["acc%d" % b, "xt2_%d" % b], w=["acc%d" % b])
                    S.op("poolq", lambda: nc.gpsimd.dma_start(out=OUTACC[tsl, :], in_=acc[b][:]), r=["acc%d" % b], w=["OUTACC"])
                for eh in range(2):
                    for rnd in range(CAP // 8):
                        S.op("dve", lambda: nc.vector.max(out=keys[:, eh, rnd * 8:(rnd + 1) * 8], in_=KT[:, eh, :]), r=["KT"], w=["keys"])
                        S.op("dve", lambda: nc.vector.match_replace(out=KT[:, eh, :], in_to_replace=keys[:, eh, rnd * 8:(rnd + 1) * 8],
                                                                    in_values=KT[:, eh, :], imm_value=0.0), r=["KT", "keys"], w=["KT"])
                zk = sbG("zk", [128, 2, CAP])
                S.op("dve", lambda: nc.vector.tensor_scalar(zk[:], keys[:], 0.0, float(T + 1), ALU.is_equal, ALU.mult), r=["keys"], w=["zk"])
                S.op("dve", lambda: nc.vector.scalar_tensor_tensor(keys[:], keys[:], -1.0, zk[:], ALU.add, ALU.add), r=["keys", "zk"], w=["keys"])
                idxT = sbG("idxT", [128, NST, NE])
                for shf in range(NST):
                    for eh in range(2):
                        S.op("pe", lambda: nc.tensor.transpose(ps[0][:, eh * 128:(eh + 1) * 128], keys[:, eh, shf * 128:(shf + 1) * 128], ident[:]), r=["keys", "ident"], w=[PSN[0]])
                    S.op("act", lambda: nc.scalar.copy(idxT[:, shf, :], ps[0][:, 0:NE]), r=[PSN[0]], w=["idxT"])
                S.op("dve", lambda: nc.vector.tensor_copy(idxI[:], idxT[:]), r=["idxT"], w=["idxI"])
                if DBG:
                    S.op("poolq", lambda: nc.gpsimd.dma_start(out=dbg_idx, in_=idxI[:]), r=["idxI"])
                S.barrier()
            with ExitStack() as esE:
                sbE = lambda name, shape, dt=F32: esE.enter_context(nc.sbuf_tensor(name, shape, dt))
                wgs = [sbE("wgs%d" % i, [128, 8, 512]) for i in range(2)]
                wgb = [sbE("wgb%d" % i, [128, 8, 512], BF16) for i in range(2)]
                wds = [sbE("wds%d" % i, [128, 2, D]) for i in range(2)]
                wdb = [sbE("wdb%d" % i, [128, 2, D], BF16) for i in range(2)]
                Xg = [sbE("Xg%d" % i, [128, D + NE]) for i in range(4)]
                XgT = [sbE("XgT%d" % i, [128, 8, 128], BF16) for i in range(2)]
                sge = [sbE("sge%d" % i, [128, 256]) for i in range(2)]
                ace = [sbE("ace%d" % i, [128, 256]) for i in range(2)]
                aeT = [sbE("aeT%d" % i, [128, 2, 128], BF16) for i in range(2)]
                yo = [sbE("yo%d" % i, [128, D]) for i in range(3)]
                tiles = [(e, shf) for e in range(N_EXP_RUN) for shf in range(NST)]
                NTL = len(tiles)

                def load_w(e):
                    wb = e % 2
                    S.op("sp", lambda: nc.sync.dma_start(out=wgs[wb][:, :, 0:256], in_=exp_w_gate[e].rearrange("(kc p) n -> p kc n", p=128)), w=["wgs%d" % wb])
                    S.op("sp", lambda: nc.sync.dma_start(out=wgs[wb][:, :, 256:512], in_=exp_w_up[e].rearrange("(kc p) n -> p kc n", p=128)), w=["wgs%d" % wb])
                    S.op("sp", lambda: nc.sync.dma_start(out=wds[wb][:], in_=exp_w_down[e].rearrange("(jc p) n -> p jc n", p=128)), w=["wds%d" % wb])

                def cast_w(e):
                    wb = e % 2
                    S.op("dve", lambda: nc.vector.tensor_copy(wgb[wb][:], wgs[wb][:]), r=["wgs%d" % wb], w=["wgb%d" % wb])
                    S.op("act", lambda: nc.scalar.copy(wdb[wb][:], wds[wb][:]), r=["wds%d" % wb], w=["wdb%d" % wb])

                def gather(k):
                    e, shf = tiles[k]
                    xb = k % 4
                    S.op("poolq", lambda: nc.gpsimd.indirect_dma_start(out=Xg[xb][:, :], out_offset=None, in_=H2E[:, :],
                                                                       in_offset=bass.IndirectOffsetOnAxis(ap=idxI[:, shf, e:e + 1], axis=0)),
                         r=["H2E", "idxI"], w=["Xg%d" % xb])

                def stageA(k):
                    xb, tb = k % 4, k % 2
                    for half in range(2):
                        for q in range(4):
                            kc = half * 4 + q
                            S.op("pe", lambda: nc.tensor.transpose(ps[half][:, q * 128:(q + 1) * 128], Xg[xb][:, kc * 128:(kc + 1) * 128], ident[:]),
                                 r=["Xg%d" % xb, "ident"], w=[PSN[half]])
                        S.op("act", lambda: nc.scalar.copy(XgT[tb][:, half * 4:(half + 1) * 4, :], ps[half][:].rearrange("p (q t) -> p q t", q=4)),
                             r=[PSN[half]], w=["XgT%d" % tb])

                def stageB(k):
                    e, shf = tiles[k]
                    wb, tb, gb = e % 2, k % 2, 2 + k % 2
                    for kc in range(8):
                        S.op("pe", lambda: nc.tensor.matmul(ps[gb][:], XgT[tb][:, kc, :], wgb[wb][:, kc, :], start=(kc == 0), stop=(kc == 7)),
                             r=["XgT%d" % tb, "wgb%d" % wb], w=[PSN[gb]])
                    S.op("act", lambda: nc.scalar.activation(out=sge[tb][:], in_=ps[gb][:, 0:256], func=AF.Silu), r=[PSN[gb]], w=["sge%d" % tb])
                    S.op("dve", lambda: nc.vector.tensor_tensor(ace[tb][:], sge[tb][:], ps[gb][:, 256:512], ALU.mult), r=["sge%d" % tb, PSN[gb]], w=["ace%d" % tb])

                def stageC(k):
                    tb = k % 2
                    for jc in range(2):
                        S.op("pe", lambda: nc.tensor.transpose(ps[4][:, jc * 128:(jc + 1) * 128], ace[tb][:, jc * 128:(jc + 1) * 128], ident[:]),
                             r=["ace%d" % tb, "ident"], w=[PSN[4]])
                    S.op("act", lambda: nc.scalar.copy(aeT[tb][:], ps[4][:, 0:256].rearrange("p (j t) -> p j t", j=2)), r=[PSN[4]], w=["aeT%d" % tb])

                def stageD(k):
                    e, shf = tiles[k]
                    wb, tb, xb, yb = e % 2, k % 2, k % 4, k % 3
                    for half in range(2):
                        hs = slice(half * 512, (half + 1) * 512)
                        for jc in range(2):
                            S.op("pe", lambda: nc.tensor.matmul(ps[6 + half][:], aeT[tb][:, jc, :], wdb[wb][:, jc, hs], start=(jc == 0), stop=(jc == 1)),
                                 r=["aeT%d" % tb, "wdb%d" % wb], w=[PSN[6 + half]])
                        S.op("dve", lambda: nc.vector.scalar_tensor_tensor(yo[yb][:, hs], ps[6 + half][:], Xg[xb][:, D + e:D + e + 1], g2t[:, hs], ALU.mult, ALU.mult),
                             r=[PSN[6 + half], "Xg%d" % xb, "g2t"], w=["yo%d" % yb])
                    S.op("poolq", lambda: nc.gpsimd.indirect_dma_start(out=OUTACC[:, :], out_offset=bass.IndirectOffsetOnAxis(ap=idxI[:, shf, e:e + 1], axis=0),
                                                                       in_=yo[yb][:, :], in_offset=None, compute_op=ALU.add),
                         r=["yo%d" % yb, "idxI"], w=["OUTACC"])

                if NTL > 0:
                    load_w(0)
                    cast_w(0)
                    if N_EXP_RUN > 1:
                        load_w(1)
                    gather(0)
                    if NTL > 1:
                        gather(1)
                    stageA(0)
                    for k in range(NTL):
                        e, shf = tiles[k]
                        if k + 2 < NTL:
                            gather(k + 2)
                        if k + 1 < NTL:
                            stageA(k + 1)
                        stageB(k)
                        if k >= 1:
                            stageD(k - 1)
                        stageC(k)
                        if shf == NST - 1 and e + 1 < N_EXP_RUN:
                            cast_w(e + 1)
                            if e + 2 < N_EXP_RUN:
                                load_w(e + 2)
                    stageD(NTL - 1)
                S.barrier()

        S.barrier()
        gf = sb("gf", [128, D])
        ot = [sb("ot%d" % i, [128, D]) for i in range(2)]
        xt = [sb("xf%d" % i, [128, D]) for i in range(2)]
        sq = sb("sqf2", [128, D])
        ss = [sb("ssf%d" % i, [128, 1]) for i in range(2)]
        rs = [sb("rsf%d" % i, [128, 1]) for i in range(2)]

        def rstd_fin(b, src, srcname):
            S.op("act", lambda: nc.scalar.activation(out=sq[:], in_=src, func=AF.Square, accum_out=ss[b][:]),
                 r=[srcname], w=["sqf2", "ssf%d" % b])
            S.op("dve", lambda: nc.vector.tensor_scalar(rs[b][:], ss[b][:], 1.0 / D, NORM_EPS, ALU.mult, ALU.add),
                 r=["ssf%d" % b], w=["rsf%d" % b])
            S.op("act", lambda: nc.scalar.activation(out=rs[b][:], in_=rs[b][:], func=AF.Sqrt), r=["rsf%d" % b], w=["rsf%d" % b])
            S.op("dve", lambda: nc.vector.reciprocal(rs[b][:], rs[b][:]), r=["rsf%d" % b], w=["rsf%d" % b])

        S.op("sp", lambda: nc.sync.dma_start(out=gf[:], in_=bcast_rows(normf_g[0:1, :])), w=["gf"])
        for i in range(NT):
            b = i % 2
            S.op("sp", lambda: nc.sync.dma_start(out=xt[b][:], in_=OUTACC[i * 128:(i + 1) * 128, :]), r=["OUTACC"], w=["xf%d" % b])
            rstd_fin(b, xt[b][:], "xf%d" % b)
            S.op("dve", lambda: nc.vector.scalar_tensor_tensor(ot[b][:], xt[b][:], rs[b][:, 0:1], gf[:], ALU.mult, ALU.mult),
                 r=["xf%d" % b, "rsf%d" % b, "gf"], w=["ot%d" % b])
            S.op("poolq", lambda: nc.gpsimd.dma_start(out=out[i * 128:(i + 1) * 128, :], in_=ot[b][:]), r=["ot%d" % b])
        S.finish("pool")
    return nc


_NC_CACHE = {}


_CONST = {}


def _constants():
    if _CONST:
        return _CONST
    import ml_dtypes
    L = T
    N = 2 * T
    f32 = np.float32
    pos = np.arange(L, dtype=f32)
    t = pos / f32(L - 1)
    bands = np.linspace(1e-4, 15.0, 16, dtype=f32)
    ang = (f32(2.0 * np.pi / L) * pos[:, None]) * bands[None]
    feats = np.concatenate([t[:, None], np.cos(ang), -np.sin(ang)], axis=-1).astype(f32)
    deltas = np.abs(np.linspace(np.log(1e-2) / 1.5, np.log(1e-2) / 0.3, 512, dtype=f32))
    env = np.exp(-t[:, None] * deltas[None]).astype(f32)
    tt = np.arange(L, dtype=np.int64)
    ff = np.arange(L, dtype=np.int64)
    ph = (tt[:, None] * ff[None, :]) % N
    angm = ph.astype(np.float64) * (2.0 * np.pi / N)
    cosm = np.cos(angm)
    sinm = np.sin(angm)
    fw = np.empty((L, N), np.float32)
    fw[:, :L] = cosm
    fw[:, L:] = -sinm
    fw[:, L] = np.where(tt % 2 == 0, 1.0, -1.0)
    dfw = fw.reshape(NT, 128, 64, 128).transpose(2, 1, 0, 3)
    iv = np.empty((N, L), np.float32)
    iv[:L] = (2.0 / N) * cosm.T
    iv[0] = 1.0 / N
    iv[L:] = -(2.0 / N) * sinm.T
    iv[L] = np.where(tt % 2 == 0, 1.0, -1.0) / N
    dinv = iv.reshape(64, 128, 16, 256).transpose(2, 1, 0, 3)
    m48 = np.zeros((48, 512), np.float32)
    m40 = np.zeros((40, 512), np.float32)
    for base in (0, 8, 32, 40):
        for j in range(8):
            m48[base + j, j * 64:(j + 1) * 64] = 1.0
    for base in (0, 32):
        for j in range(8):
            m40[base + j, j * 64:(j + 1) * 64] = 1.0
    selF = np.zeros((128, 128, 48), np.float32)
    selB = np.zeros((128, 128, 48), np.float32)
    for tl in range(128):
        selF[tl, tl, 8:16] = 1.0
        selB[127 - tl, tl, 40:48] = 1.0
    _CONST.update({
        "m48": m48, "m40": m40,
        "selF": selF.astype(ml_dtypes.bfloat16), "selB": selB.astype(ml_dtypes.bfloat16),
        "featsT": np.ascontiguousarray(feats.T),
        "env": env,
        "dfw": np.ascontiguousarray(dfw).astype(ml_dtypes.bfloat16),
        "dinv": np.ascontiguousarray(dinv).astype(ml_dtypes.bfloat16),
    })
    return _CONST


def _f32(a):
    return np.ascontiguousarray(a, dtype=np.float32)


def make_in_maps(inputs, ncores=NCORES):
    x = _f32(inputs["x"])
    c = _f32(inputs["c"])
    shared = {
        "normf_g": _f32(inputs["normf_g"]).reshape(1, D),
        "norm1_g": _f32(inputs["norm1_g"]).reshape(1, D),
        "w_ada": _f32(inputs["w_ada"]).reshape(D, 6 * D),
        "b_ada": _f32(inputs["b_ada"]).reshape(1, 6 * D),
        "w_in": _f32(inputs["w_in"]).reshape(D, N_IN),
        "hy_conv_w": _f32(inputs["hy_conv_w"]).reshape(3, HY_COLS),
        "hy_conv_b": _f32(inputs["hy_conv_b"]).reshape(1, HY_COLS),
        "ident": np.eye(128, dtype=np.float32),
        "hy_pos_w1": _f32(inputs["hy_pos_w1"]).reshape(33, 64),
        "hy_pos_b1": _f32(inputs["hy_pos_b1"]).reshape(64, 1),
        "hy_pos_w2": _f32(inputs["hy_pos_w2"]).reshape(2, 64, 64),
        "hy_pos_b2": _f32(inputs["hy_pos_b2"]).reshape(2, 64, 1),
        "hy_pos_w3": _f32(inputs["hy_pos_w3"]).reshape(64, 2048),
        "hy_sin_freq": _f32(inputs["hy_sin_freq"]).reshape(64, 1),
        "hy_skip": _f32(inputs["hy_skip"]).reshape(1, 1024),
        "w_out": _f32(inputs["w_out"]).reshape(D, D),
        "norm2_g": _f32(inputs["norm2_g"]).reshape(1, D),
        "router_w": _f32(inputs["router_w"]).reshape(D, 256),
        "router_bias": _f32(inputs["router_bias"]).reshape(1, 256),
        "sh_w_gate": _f32(inputs["sh_w_gate"]).reshape(D, 256),
        "sh_w_up": _f32(inputs["sh_w_up"]).reshape(D, 256),
        "sh_w_down": _f32(inputs["sh_w_down"]).reshape(256, D),
        "exp_w_gate": _f32(inputs["exp_w_gate"]).reshape(256, D, 256),
        "exp_w_up": _f32(inputs["exp_w_up"]).reshape(256, D, 256),
        "exp_w_down": _f32(inputs["exp_w_down"]).reshape(256, 256, D),
        "tokid": (np.arange(T, dtype=np.float32).reshape(NT, 128).T + 1.0).copy(),
        "rw_mu": _f32(inputs["rw_mu"]).reshape(1, 1760),
        "rw_w0": _f32(inputs["rw_w0"]).reshape(2, 512),
        "rw_w_up": _f32(inputs["rw_w_up"]).reshape(2, 32, 512),
        "rw_a0": _f32(inputs["rw_a0"]).reshape(2, 512),
        "rw_a_up": _f32(inputs["rw_a_up"]).reshape(2, 32, 512),
        "rw_g_up": _f32(inputs["rw_g_up"]).reshape(96, 512),
        "rw_k_k": _f32(inputs["rw_k_k"]).reshape(1, 512),
        "rw_k_a": _f32(inputs["rw_k_a"]).reshape(1, 512),
        "rw_r_k": _f32(inputs["rw_r_k"]).reshape(1, 512),
        "rw_ln_w": _f32(inputs["rw_ln_w"]).reshape(1, 512),
        "rw_ln_b": _f32(inputs["rw_ln_b"]).reshape(1, 512),
        "blk": np.kron(np.eye(2, dtype=np.float32), np.ones((64, 64), np.float32)),
    }
    shared.update(_constants())
    in_maps = []
    for cidx in range(ncores):
        m = dict(shared)
        m["x"] = x[cidx]
        m["c"] = c[cidx:cidx + 1]
        in_maps.append(m)
    return in_maps


def kernel(**inputs):
    if "nc" not in _NC_CACHE:
        _NC_CACHE["nc"] = build_nc()
    nc = _NC_CACHE["nc"]
    in_maps = make_in_maps(inputs)
    res = run_bass_kernel_spmd(nc, in_maps, core_ids=list(range(NCORES)))
    return np.stack([res.results[c]["out"] for c in range(NCORES)], axis=0)
```

```python
import numpy as np
from contextlib import ExitStack
import concourse.bass as bass
import concourse.mybir as mybir
from concourse.bass_utils import run_bass_kernel_spmd

F32 = mybir.dt.float32
BF16 = mybir.dt.bfloat16
I32 = mybir.dt.int32
ALU = mybir.AluOpType
AF = mybir.ActivationFunctionType
AX = mybir.AxisListType

NCORES = 8
T = 4096
D = 1024
NT = T // 128
NORM_EPS = 1e-6


class Sched:
    NDQ = 4

    def __init__(self, nc, es):
        self.nc = nc
        self.eng = {"pe": nc.tensor, "dve": nc.vector, "act": nc.scalar, "pool": nc.gpsimd}
        self.inc = {"pe": 1, "dve": 1, "act": 1, "pool": 1}
        self.stream = {"pe": "pe", "dve": "dve", "act": "act", "pool": "pool"}
        for base, eng, st in (("sp", nc.sync, "sp"), ("poolq", nc.gpsimd, "pool")):
            for i in range(self.NDQ):
                k = "%s%d" % (base, i)
                self.eng[k] = eng
                self.inc[k] = 16
                self.stream[k] = st
        self.sem = {k: es.enter_context(nc.semaphore("sem_" + k)) for k in self.eng}
        self.cnt = {k: 0 for k in self.eng}
        self.rr = {"sp": 0, "poolq": 0}
        self.last_w = {}
        self.readers = {}
        self.seen = {s: {} for s in ("pe", "dve", "act", "pool", "sp")}
        self.stream_eng = {"pe": nc.tensor, "dve": nc.vector, "act": nc.scalar, "pool": nc.gpsimd, "sp": nc.sync}

    def _wait(self, st, pq, seq):
        if self.seen[st].get(pq, 0) < seq:
            self.stream_eng[st].wait_ge(self.sem[pq], seq * self.inc[pq])
            self.seen[st][pq] = seq

    def op(self, q, fn, r=(), w=()):
        if q in self.rr:
            i = self.rr[q]
            self.rr[q] = (i + 1) % self.NDQ
            q = "%s%d" % (q, i)
        need = {}
        for b in r:
            for pq, seq in self.last_w.get(b, {}).items():
                need[pq] = max(need.get(pq, 0), seq)
        for b in w:
            for pq, seq in self.last_w.get(b, {}).items():
                need[pq] = max(need.get(pq, 0), seq)
            for pq, seq in self.readers.get(b, ()):
                need[pq] = max(need.get(pq, 0), seq)
        st = self.stream[q]
        if self.inc[q] == 16 and self.cnt[q] > 0:
            need[q] = max(need.get(q, 0), self.cnt[q])
        for pq, seq in need.items():
            self._wait(st, pq, seq)
        ins = fn()
        self.cnt[q] += 1
        seq = self.cnt[q]
        ins.then_inc(self.sem[q], self.inc[q])
        for b in w:
            self.last_w.setdefault(b, {})[q] = seq
            self.readers[b] = []
        for b in r:
            lst = self.readers.setdefault(b, [])
            lst.append((q, seq))
            if len(lst) > 16:
                best = {}
                for pq, s_ in lst:
                    best[pq] = max(best.get(pq, 0), s_)
                self.readers[b] = list(best.items())
        return ins

    def barrier(self):
        for st in ("pe", "dve", "act", "pool", "sp"):
            for k in self.eng:
                if self.cnt[k] > 0 and not (k == st and self.inc[k] == 1):
                    self._wait(st, k, self.cnt[k])

    def finish(self, q="pool"):
        for k in self.eng:
            if self.cnt[k] > 0:
                self.stream_eng[q].wait_ge(self.sem[k], self.cnt[k] * self.inc[k])


def bcast_rows(ap_row, parts=128):
    return ap_row.to_broadcast([parts, ap_row.shape[-1]])


HY_COLS = 1536
N_IN = 3296
DBG = False
SCAN_CHUNKS = 64
N_EXP_RUN = 256


def build_nc():
    nc = bass.Bass("TRN2", target_bir_lowering=False)
    dt_in = lambda name, shape, dt=F32: nc.dram_tensor(name, shape, dt, kind="ExternalInput").ap()
    x = dt_in("x", [T, D])
    c_in = dt_in("c", [1, D])
    normf_g = dt_in("normf_g", [1, D])
    norm1_g = dt_in("norm1_g", [1, D])
    w_ada = dt_in("w_ada", [D, 6 * D])
    b_ada = dt_in("b_ada", [1, 6 * D])
    w_in = dt_in("w_in", [D, N_IN])
    hy_conv_w = dt_in("hy_conv_w", [3, HY_COLS])
    hy_conv_b = dt_in("hy_conv_b", [1, HY_COLS])
    ident_in = dt_in("ident", [128, 128])
    featsT = dt_in("featsT", [33, T])
    env_in = dt_in("env", [T, 512])
    pw1 = dt_in("hy_pos_w1", [33, 64])
    pb1 = dt_in("hy_pos_b1", [64, 1])
    pw2 = dt_in("hy_pos_w2", [2, 64, 64])
    pb2 = dt_in("hy_pos_b2", [2, 64, 1])
    pw3 = dt_in("hy_pos_w3", [64, 2048])
    sfreq = dt_in("hy_sin_freq", [64, 1])
    hskip = dt_in("hy_skip", [1, 1024])
    dfw = dt_in("dfw", [64, 128, 32, 128], BF16)
    dinv = dt_in("dinv", [16, 128, 64, 256], BF16)
    KF = nc.dram_tensor("KF", [64, 128, 1024], F32, kind="Internal").ap()
    UD = nc.dram_tensor("UD", [12, 128, T], F32, kind="Internal").ap()
    ZD = nc.dram_tensor("ZD", [4, 128, T], F32, kind="Internal").ap()
    YD = nc.dram_tensor("YD", [4, 128, T], F32, kind="Internal").ap()
    MODD = nc.dram_tensor("MODD", [128, 6 * D], F32, kind="Internal").ap()
    rw_mu = dt_in("rw_mu", [1, 1760])
    rw_w0 = dt_in("rw_w0", [2, 512])
    rw_w_up = dt_in("rw_w_up", [2, 32, 512])
    rw_a0 = dt_in("rw_a0", [2, 512])
    rw_a_up = dt_in("rw_a_up", [2, 32, 512])
    rw_g_up = dt_in("rw_g_up", [96, 512])
    rw_k_k = dt_in("rw_k_k", [1, 512])
    rw_k_a = dt_in("rw_k_a", [1, 512])
    rw_r_k = dt_in("rw_r_k", [1, 512])
    rw_ln_w = dt_in("rw_ln_w", [1, 512])
    rw_ln_b = dt_in("rw_ln_b", [1, 512])
    blk_in = dt_in("blk", [128, 128])
    m48_in = dt_in("m48", [48, 512])
    m40_in = dt_in("m40", [40, 512])
    selF_in = dt_in("selF", [128, 128, 48], BF16)
    selB_in = dt_in("selB", [128, 128, 48], BF16)
    scr = lambda name, shape, dt=F32: nc.dram_tensor(name, shape, dt, kind="Internal").ap()
    KKN = scr("KKN", [512, T], BF16)
    RR = scr("RR", [512, T], BF16)
    DEC = scr("DEC", [2, 512, T])
    BT = scr("BT", [2, 2, T, 512], BF16)
    VT = scr("VT", [T, 512], BF16)
    BON = scr("BON", [512, T])
    GG = scr("GG", [512, T])
    OD = scr("OD", [2, T, 512])
    X1D = scr("X1D", [T, D])
    H2E = scr("H2E", [T + 1, D + 256])
    OUTACC = scr("OUTACC", [T + 1, D])
    norm2_g = dt_in("norm2_g", [1, D])
    router_w = dt_in("router_w", [D, 256])
    router_bias = dt_in("router_bias", [1, 256])
    sh_w_gate = dt_in("sh_w_gate", [D, 256])
    sh_w_up = dt_in("sh_w_up", [D, 256])
    sh_w_down = dt_in("sh_w_down", [256, D])
    exp_w_gate = dt_in("exp_w_gate", [256, D, 256])
    exp_w_up = dt_in("exp_w_up", [256, D, 256])
    exp_w_down = dt_in("exp_w_down", [256, 256, D])
    tokid_in = dt_in("tokid", [128, NT])
    w_out = dt_in("w_out", [D, D])
    out = nc.dram_tensor("out", [T, D], F32, kind="ExternalOutput").ap()
    if DBG:
        dbg_mod = nc.dram_tensor("dbg_mod", [128, 6 * D], F32, kind="ExternalOutput").ap()
        dbg_u = nc.dram_tensor("dbg_u", [12, 128, T], F32, kind="ExternalOutput").ap()
        dbg_kf = nc.dram_tensor("dbg_kf", [64, 128, 1024], F32, kind="ExternalOutput").ap()
        dbg_y = nc.dram_tensor("dbg_y", [4, 128, T], F32, kind="ExternalOutput").ap()
        dbg_od = nc.dram_tensor("dbg_od", [2, T, 512], F32, kind="ExternalOutput").ap()
        dbg_x1 = nc.dram_tensor("dbg_x1", [T, D], F32, kind="ExternalOutput").ap()
        dbg_idx = nc.dram_tensor("dbg_idx", [128, 8, 256], I32, kind="ExternalOutput").ap()
        dbg_rw = {
            "KKN": nc.dram_tensor("dbg_KKN", [512, T], BF16, kind="ExternalOutput").ap(),
            "RR": nc.dram_tensor("dbg_RR", [512, T], BF16, kind="ExternalOutput").ap(),
            "DEC": nc.dram_tensor("dbg_DEC", [2, 512, T], F32, kind="ExternalOutput").ap(),
            "BT": nc.dram_tensor("dbg_BT", [2, 2, T, 512], BF16, kind="ExternalOutput").ap(),
            "VT": nc.dram_tensor("dbg_VT", [T, 512], BF16, kind="ExternalOutput").ap(),
            "BON": nc.dram_tensor("dbg_BON", [512, T], F32, kind="ExternalOutput").ap(),
            "GG": nc.dram_tensor("dbg_GG", [512, T], F32, kind="ExternalOutput").ap(),
        }

    with ExitStack() as es:
        es.enter_context(nc.allow_low_precision("bf16 matmul operands, fp32 accumulation"))
        es.enter_context(nc.allow_non_contiguous_dma("small strided parameter loads"))
        S = Sched(nc, es)
        sb = lambda name, shape, dt=F32: es.enter_context(nc.sbuf_tensor(name, shape, dt))
        ps = [es.enter_context(nc.psum_tensor("ps%d" % i, [128, 512], F32)) for i in range(8)]
        PSN = ["ps%d" % i for i in range(8)]

        ident = sb("ident_sb", [128, 128])
        S.op("sp", lambda: nc.sync.dma_start(out=ident[:], in_=ident_in[:, :]), w=["ident"])


        TWO_PI = 6.283185307179586
        MAGIC = 12582912.0
        with ExitStack() as es1:
            sb1 = lambda name, shape, dt=F32: es1.enter_context(nc.sbuf_tensor(name, shape, dt))
            w1 = sb1("w1", [33, 64])
            w2 = sb1("w2", [64, 2, 64])
            w3 = sb1("w3", [64, 2048])
            fr = sb1("fr", [64, 1])
            fb = sb1("fb", [64, 3])
            ones = sb1("ones", [128, 128])
            skp = sb1("skp", [128, 1024])
            hd0 = sb1("hd0", [64, T])
            es1a = ExitStack()
            sb1a = lambda name, shape, dt=F32: es1a.enter_context(nc.sbuf_tensor(name, shape, dt))
            ft = sb1a("ft", [33, T])
            hd = [hd0, sb1a("hd1", [64, T])]
            rr = sb1a("rr", [64, 512])
            S.op("sp", lambda: nc.sync.dma_start(out=ft[:], in_=featsT[:, :]), w=["ft"])
            S.op("sp", lambda: nc.sync.dma_start(out=w1[:], in_=pw1[:, :]), w=["w1"])
            S.op("sp", lambda: nc.sync.dma_start(out=w2[:], in_=pw2.rearrange("i k m -> k i m")), w=["w2"])
            S.op("sp", lambda: nc.sync.dma_start(out=w3[:], in_=pw3[:, :]), w=["w3"])
            S.op("sp", lambda: nc.sync.dma_start(out=fr[:], in_=sfreq[:, :]), w=["fr"])
            S.op("sp", lambda: nc.sync.dma_start(out=fb[:, 0:1], in_=pb1[:, :]), w=["fb"])
            S.op("sp", lambda: nc.sync.dma_start(out=fb[:, 1:2], in_=pb2[0]), w=["fb"])
            S.op("sp", lambda: nc.sync.dma_start(out=fb[:, 2:3], in_=pb2[1]), w=["fb"])
            S.op("sp", lambda: nc.sync.dma_start(out=skp[:], in_=bcast_rows(hskip[0:1, :])), w=["skp"])
            S.op("dve", lambda: nc.vector.tensor_scalar(fb[:], fb[:], fr[:, 0:1], None, ALU.mult), r=["fb", "fr"], w=["fb"])
            S.op("pool", lambda: nc.gpsimd.memset(ones[:], 1.0), w=["ones"])
            for layer in range(3):
                src = ft if layer == 0 else hd[(layer - 1) % 2]
                srcn = "ft" if layer == 0 else "hd%d" % ((layer - 1) % 2)
                dst = hd[layer % 2]
                dstn = "hd%d" % (layer % 2)
                lw = w1[:, :] if layer == 0 else w2[:, layer - 1, :]
                lwn = "w1" if layer == 0 else "w2"
                for tcn in range(8):
                    pb = tcn % 2
                    sl = slice(tcn * 512, (tcn + 1) * 512)
                    S.op("pe", lambda: nc.tensor.matmul(ps[pb][0:64, :], lw, src[:, sl], start=True, stop=True),
                         r=[lwn, srcn], w=[PSN[pb]])
                    S.op("dve", lambda: nc.vector.tensor_scalar(dst[:, sl], ps[pb][0:64, :], fr[:, 0:1], fb[:, layer:layer + 1], ALU.mult, ALU.add),
                         r=[PSN[pb], "fr", "fb"], w=[dstn])
                    S.op("dve", lambda: nc.vector.tensor_scalar(rr[:], dst[:, sl], 1.0 / TWO_PI, MAGIC, ALU.mult, ALU.add), r=[dstn], w=["rr"])
                    S.op("dve", lambda: nc.vector.tensor_scalar(rr[:], rr[:], -MAGIC, -TWO_PI, ALU.add, ALU.mult), r=["rr"], w=["rr"])
                    S.op("dve", lambda: nc.vector.tensor_tensor(dst[:, sl], dst[:, sl], rr[:], ALU.add), r=[dstn, "rr"], w=[dstn])
                    S.op("dve", lambda: nc.vector.tensor_scalar(dst[:, sl], dst[:, sl], 3.14159, -3.14159, ALU.min, ALU.max), r=[dstn], w=[dstn])
                    S.op("act", lambda: nc.scalar.activation(out=dst[:, sl], in_=dst[:, sl], func=AF.Sin), r=[dstn], w=[dstn])
            S.barrier()
            es1a.close()
            envt = [sb1("envt%d" % i, [128, 512]) for i in range(2)]
            fw = [sb1("fw%d" % i, [128, 512]) for i in range(2)]
            bw = [sb1("bw%d" % i, [128, 512]) for i in range(2)]
            sqf = sb1("sqf", [128, 512])
            sqb = sb1("sqb", [128, 512])
            Eb = sb1("Eb", [128, NT, 1024], BF16)
            Ob = sb1("Ob", [128, NT, 1024], BF16)
            scl = sb1("scl", [128, 1024])
            dch = [sb1("dch%d" % i, [128, 32, 128], BF16) for i in range(2)]
            kst = [sb1("kst%d" % i, [128, 1024]) for i in range(2)]
            knq = sb1("knq", [128, 1024])
            hfin = hd[0]
            for i in range(NT):
                b = i % 2
                S.op("sp", lambda: nc.sync.dma_start(out=envt[b][:], in_=env_in[i * 128:(i + 1) * 128, :]), w=["envt%d" % b])
                for n in range(2):
                    for d in range(2):
                        pb = d
                        col = n * 1024 + d * 512
                        S.op("pe", lambda: nc.tensor.matmul(ps[pb][:], hfin[:, i * 128:(i + 1) * 128], w3[:, col:col + 512], start=True, stop=True),
                             r=["hd0", "w3"], w=[PSN[pb]])
                    S.op("dve", lambda: nc.vector.tensor_tensor(fw[n][:], ps[0][:], envt[b][:], ALU.mult), r=[PSN[0], "envt%d" % b], w=["fw%d" % n])
                    S.op("dve", lambda: nc.vector.tensor_tensor(bw[n][:], ps[1][:], envt[b][:], ALU.mult), r=[PSN[1], "envt%d" % b], w=["bw%d" % n])
                    if i == 0:
                        S.op("dve", lambda: nc.vector.memset(bw[n][0:1, :], 0.0), w=["bw%d" % n])
                    S.op("pool", lambda: nc.gpsimd.tensor_tensor(Eb[:, i, n * 512:(n + 1) * 512], fw[n][:], bw[n][:], ALU.add),
                         r=["fw%d" % n, "bw%d" % n], w=["Eb"])
                    S.op("pool", lambda: nc.gpsimd.tensor_tensor(Ob[:, i, n * 512:(n + 1) * 512], fw[n][:], bw[n][:], ALU.subtract),
                         r=["fw%d" % n, "bw%d" % n], w=["Ob"])
                    S.op("act", lambda: nc.scalar.activation(out=sqf[:], in_=fw[n][:], func=AF.Square), r=["fw%d" % n], w=["sqf"])
                    S.op("act", lambda: nc.scalar.activation(out=sqb[:], in_=bw[n][:], func=AF.Square), r=["bw%d" % n], w=["sqb"])
                    S.op("pe", lambda: nc.tensor.matmul(ps[2 + n][:], ones[:], sqf[:], start=(i == 0), stop=False), r=["ones", "sqf"], w=[PSN[2 + n]])
                    S.op("pe", lambda: nc.tensor.matmul(ps[2 + n][:], ones[:], sqb[:], start=False, stop=(i == NT - 1)), r=["ones", "sqb"], w=[PSN[2 + n]])
            for n in range(2):
                S.op("dve", lambda: nc.vector.tensor_scalar(scl[:, n * 512:(n + 1) * 512], ps[2 + n][:], 1e-6, None, ALU.add), r=[PSN[2 + n]], w=["scl"])
            S.op("act", lambda: nc.scalar.activation(out=scl[:], in_=scl[:], func=AF.Sqrt), r=["scl"], w=["scl"])
            S.op("dve", lambda: nc.vector.reciprocal(scl[:], scl[:]), r=["scl"], w=["scl"])
            for rc in range(64):
                b = rc % 2
                S.op("sp", lambda: nc.sync.dma_start(out=dch[b][:], in_=dfw[rc]), w=["dch%d" % b])
                src, srcn = (Eb, "Eb") if rc < 32 else (Ob, "Ob")
                for n in range(2):
                    for tcn in range(NT):
                        S.op("pe", lambda: nc.tensor.matmul(ps[4 + n][:], dch[b][:, tcn, :], src[:, tcn, n * 512:(n + 1) * 512],
                                                            start=(tcn == 0), stop=(tcn == NT - 1)),
                             r=["dch%d" % b, srcn], w=[PSN[4 + n]])
                    S.op("dve", lambda: nc.vector.tensor_tensor(kst[b][:, n * 512:(n + 1) * 512], ps[4 + n][:], scl[:, n * 512:(n + 1) * 512], ALU.mult),
                         r=[PSN[4 + n], "scl"], w=["kst%d" % b])
                if rc < 32:
                    S.op("pool", lambda: nc.gpsimd.tensor_tensor(kst[b][:], kst[b][:], skp[:], ALU.add), r=["kst%d" % b, "skp"], w=["kst%d" % b])
                if rc == 32:
                    for n in range(2):
                        for tcn in range(NT):
                            S.op("pe", lambda: nc.tensor.matmul(ps[6 + n][:], dch[b][:, tcn, :], Eb[:, tcn, n * 512:(n + 1) * 512],
                                                                start=(tcn == 0), stop=(tcn == NT - 1)),
                                 r=["dch%d" % b, "Eb"], w=[PSN[6 + n]])
                        S.op("dve", lambda: nc.vector.tensor_tensor(knq[:, n * 512:(n + 1) * 512], ps[6 + n][:], scl[:, n * 512:(n + 1) * 512], ALU.mult),
                             r=[PSN[6 + n], "scl"], w=["knq"])
                    S.op("dve", lambda: nc.vector.tensor_tensor(kst[b][0:1, :], knq[0:1, :], skp[0:1, :], ALU.add), r=["knq", "skp", "kst%d" % b], w=["kst%d" % b])
                S.op("poolq", lambda: nc.gpsimd.dma_start(out=KF[rc], in_=kst[b][:]), r=["kst%d" % b], w=["KF"])
            if DBG:
                S.op("poolq", lambda: nc.gpsimd.dma_start(out=dbg_kf[:, :, :], in_=KF[:, :, :]), r=["KF"])

        S.barrier()
        with ExitStack() as es1:
            sb1 = lambda name, shape, dt=F32: es1.enter_context(nc.sbuf_tensor(name, shape, dt))
            mod = sb1("mod", [128, 6 * D])
            cs = sb1("cs", [128, 8])
            crep = sb1("crep", [128, 8, 128])
            wa = [sb1("wa%d" % i, [128, 3 * D]) for i in range(2)]
            S.op("sp", lambda: nc.sync.dma_start(out=cs[:], in_=c_in.rearrange("o (kc p) -> p (o kc)", p=128)), w=["cs"])
            S.op("sp", lambda: nc.sync.dma_start(out=mod[:], in_=bcast_rows(b_ada[0:1, :])), w=["mod"])
            S.op("act", lambda: nc.scalar.activation(out=cs[:], in_=cs[:], func=AF.Silu), r=["cs"], w=["cs"])
            for kc in range(8):
                S.op("dve", lambda: nc.vector.tensor_copy(crep[:, kc, :], cs[:, kc:kc + 1].to_broadcast([128, 128])),
                     r=["cs"], w=["crep"])
            n = 0
            for half in range(2):
                for kc in range(8):
                    b = n % 2
                    n += 1
                    S.op("sp", lambda: nc.sync.dma_start(out=wa[b][:], in_=w_ada[kc * 128:(kc + 1) * 128, half * 3 * D:(half + 1) * 3 * D]),
                         w=["wa%d" % b])
                    for j in range(6):
                        S.op("pe", lambda: nc.tensor.matmul(ps[j][:], crep[:, kc, :], wa[b][:, j * 512:(j + 1) * 512],
                                                            start=(kc == 0), stop=(kc == 7)),
                             r=["crep", "wa%d" % b], w=[PSN[j]])
                for j in range(6):
                    col = half * 3 * D + j * 512
                    S.op("dve", lambda: nc.vector.tensor_tensor(mod[:, col:col + 512], mod[:, col:col + 512], ps[j][:], ALU.add),
                         r=[PSN[j], "mod"], w=["mod"])
            S.op("poolq", lambda: nc.gpsimd.dma_start(out=MODD[:, :], in_=mod[:]), r=["mod"], w=["MODD"])
            if DBG:
                S.op("poolq", lambda: nc.gpsimd.dma_start(out=dbg_mod[:, :], in_=mod[:]), r=["mod"])
        S.barrier()

        def rstd_of(sq, ss, rs, nm, src, srcname):
            S.op("act", lambda: nc.scalar.activation(out=sq[:], in_=src, func=AF.Square, accum_out=ss[:]),
                 r=[srcname], w=["sq" + nm, "ss" + nm])
            S.op("dve", lambda: nc.vector.tensor_scalar(rs[:], ss[:], 1.0 / D, NORM_EPS, ALU.mult, ALU.add),
                 r=["ss" + nm], w=["rs" + nm])
            S.op("act", lambda: nc.scalar.activation(out=rs[:], in_=rs[:], func=AF.Sqrt), r=["rs" + nm], w=["rs" + nm])
            S.op("dve", lambda: nc.vector.reciprocal(rs[:], rs[:]), r=["rs" + nm], w=["rs" + nm])

        es2 = ExitStack()
        sb2 = lambda name, shape, dt=F32: es2.enter_context(nc.sbuf_tensor(name, shape, dt))
        with es2:
            hT = sb2("hT", [128, 8, T + 2], BF16)
            S.op("pool", lambda: nc.gpsimd.memset(hT[:, :, 0:1], 0.0), w=["hT"])
            S.op("pool", lambda: nc.gpsimd.memset(hT[:, :, T + 1:T + 2], 0.0), w=["hT"])
            with ExitStack() as esN:
                sbN = lambda name, shape, dt=F32: esN.enter_context(nc.sbuf_tensor(name, shape, dt))
                gm1 = sbN("gm1", [128, D])
                sc1 = sbN("sc1", [128, D])
                sh1 = sbN("sh1", [128, D])
                S.op("sp", lambda: nc.sync.dma_start(out=gm1[:], in_=bcast_rows(norm1_g[0:1, :])), w=["gm1"])
                S.op("sp", lambda: nc.sync.dma_start(out=sh1[:], in_=MODD[:, 0:D]), r=["MODD"], w=["sh1"])
                S.op("sp", lambda: nc.sync.dma_start(out=sc1[:], in_=MODD[:, D:2 * D]), r=["MODD"], w=["sc1"])
                S.op("dve", lambda: nc.vector.scalar_tensor_tensor(gm1[:], sc1[:], 1.0, gm1[:], ALU.add, ALU.mult), r=["sc1", "gm1"], w=["gm1"])
                xt = [sbN("xt%d" % i, [128, D]) for i in range(2)]
                hh = [sbN("hh%d" % i, [128, D]) for i in range(2)]
                sq = sbN("sq", [128, D])
                ss = [sbN("ss%d" % i, [128, 1]) for i in range(2)]
                rs = [sbN("rs%d" % i, [128, 1]) for i in range(2)]
                for i in range(NT):
                    b = i % 2
                    S.op("sp", lambda: nc.sync.dma_start(out=xt[b][:], in_=x[i * 128:(i + 1) * 128, :]), w=["xt%d" % b])
                    rstd_of(sq, ss[b], rs[b], "n1_%d" % b, xt[b][:], "xt%d" % b)
                    S.op("dve", lambda: nc.vector.scalar_tensor_tensor(hh[b][:], xt[b][:], rs[b][:, 0:1], gm1[:], ALU.mult, ALU.mult),
                         r=["xt%d" % b, "rsn1_%d" % b, "gm1"], w=["hh%d" % b])
                    S.op("pool", lambda: nc.gpsimd.tensor_tensor(hh[b][:], hh[b][:], sh1[:], ALU.add), r=["hh%d" % b, "sh1"], w=["hh%d" % b])
                    for half in range(2):
                        pb = 6 + half
                        for q in range(4):
                            kc = half * 4 + q
                            S.op("pe", lambda: nc.tensor.transpose(ps[pb][:, q * 128:(q + 1) * 128], hh[b][:, kc * 128:(kc + 1) * 128], ident[:]),
                                 r=["hh%d" % b, "ident"], w=[PSN[pb]])
                        S.op("act", lambda: nc.scalar.copy(hT[:, half * 4:(half + 1) * 4, 1 + i * 128:1 + (i + 1) * 128],
                                                           ps[pb][:].rearrange("p (q t) -> p q t", q=4)),
                             r=[PSN[pb]], w=["hT"])
                S.barrier()

            wst = [sb2("wst%d" % i, [128, 8, 128]) for i in range(2)]
            wbf = [sb2("wbf%d" % i, [128, 8, 128], BF16) for i in range(2)]
            pfm = [sb2("pfm%d" % i, [128, T + 2]) for i in range(3)]
            for bb in range(3):
                S.op("pool", lambda: nc.gpsimd.memset(pfm[bb][:, 0:1], 0.0), w=["pfm%d" % bb])
                S.op("pool", lambda: nc.gpsimd.memset(pfm[bb][:, T + 1:T + 2], 0.0), w=["pfm%d" % bb])
            wcnt = [0]

            def inproj_group(cg, ncols, dst, dstname):
                b = wcnt[0] % 2
                wcnt[0] += 1
                S.op("sp", lambda: nc.sync.dma_start(out=wst[b][:, :, 0:ncols],
                                                     in_=w_in[:, cg * 128:cg * 128 + ncols].rearrange("(kc p) n -> p kc n", p=128)),
                     w=["wst%d" % b])
                S.op("pool", lambda: nc.gpsimd.tensor_copy(wbf[b][:, :, 0:ncols], wst[b][:, :, 0:ncols]), r=["wst%d" % b], w=["wbf%d" % b])
                for tcn in range(8):
                    pb = tcn % 2
                    for kc in range(8):
                        S.op("pe", lambda: nc.tensor.matmul(ps[pb][0:ncols, :], wbf[b][:, kc, 0:ncols], hT[:, kc, 1 + tcn * 512:1 + (tcn + 1) * 512],
                                                            start=(kc == 0), stop=(kc == 7)),
                             r=["wbf%d" % b, "hT"], w=[PSN[pb]])
                    S.op("act", lambda: nc.scalar.copy(dst[0:ncols, 1 + tcn * 512:1 + (tcn + 1) * 512], ps[pb][0:ncols, :]),
                         r=[PSN[pb]], w=[dstname])

            with ExitStack() as esH:
                sbH = lambda name, shape, dt=F32: esH.enter_context(nc.sbuf_tensor(name, shape, dt))
                ufm = [sbH("ufm%d" % i, [128, T]) for i in range(2)]
                cw = sbH("cw", [128, 3, 12])
                cb = sbH("cb", [128, 12])
                for j in range(3):
                    S.op("sp", lambda: nc.sync.dma_start(out=cw[:, j, :], in_=hy_conv_w[j:j + 1, :].rearrange("o (g p) -> p (o g)", p=128)), w=["cw"])
                S.op("sp", lambda: nc.sync.dma_start(out=cb[:], in_=hy_conv_b.rearrange("o (g p) -> p (o g)", p=128)), w=["cb"])
                for cg in range(12):
                    b = cg % 2
                    inproj_group(cg, 128, pfm[b], "pfm%d" % b)
                    S.op("dve", lambda: nc.vector.tensor_scalar(ufm[b][:], pfm[b][:, 0:T], cw[:, 0, cg:cg + 1], cb[:, cg:cg + 1], ALU.mult, ALU.add),
                         r=["pfm%d" % b, "cw", "cb"], w=["ufm%d" % b])
                    S.op("dve", lambda: nc.vector.scalar_tensor_tensor(ufm[b][:], pfm[b][:, 1:T + 1], cw[:, 1, cg:cg + 1], ufm[b][:], ALU.mult, ALU.add),
                         r=["pfm%d" % b, "cw", "ufm%d" % b], w=["ufm%d" % b])
                    S.op("dve", lambda: nc.vector.scalar_tensor_tensor(ufm[b][:], pfm[b][:, 2:T + 2], cw[:, 2, cg:cg + 1], ufm[b][:], ALU.mult, ALU.add),
                         r=["pfm%d" % b, "cw", "ufm%d" % b], w=["ufm%d" % b])
                    S.op("poolq", lambda: nc.gpsimd.dma_start(out=UD[cg, :, :], in_=ufm[b][:]), r=["ufm%d" % b], w=["UD"])
                    if DBG:
                        S.op("poolq", lambda: nc.gpsimd.dma_start(out=dbg_u[cg, :, :], in_=ufm[b][:]), r=["ufm%d" % b])
                S.barrier()

            with ExitStack() as esR:
                sbR = lambda name, shape, dt=F32: esR.enter_context(nc.sbuf_tensor(name, shape, dt))
                pvec = lambda ap_row, p=128: ap_row.rearrange("o (g p) -> p (o g)", p=p)
                mu_t = sbR("mu_t", [128, 14])
                w0_t = sbR("w0_t", [128, 8])
                a0_t = sbR("a0_t", [128, 8])
                kk_t = sbR("kk_t", [128, 4])
                ka_t = sbR("ka_t", [128, 4])
                oka_t = sbR("oka_t", [128, 4])
                rk_t = sbR("rk_t", [128, 4])
                S.op("sp", lambda: nc.sync.dma_start(out=mu_t[:, 0:13], in_=pvec(rw_mu[0:1, 0:1664])), w=["mu_t"])
                S.op("sp", lambda: nc.sync.dma_start(out=mu_t[0:96, 13:14], in_=pvec(rw_mu[0:1, 1664:1760], 96)), w=["mu_t"])
                for d in range(2):
                    S.op("sp", lambda: nc.sync.dma_start(out=w0_t[:, d * 4:(d + 1) * 4], in_=pvec(rw_w0[d:d + 1, :])), w=["w0_t"])
                    S.op("sp", lambda: nc.sync.dma_start(out=a0_t[:, d * 4:(d + 1) * 4], in_=pvec(rw_a0[d:d + 1, :])), w=["a0_t"])
                S.op("sp", lambda: nc.sync.dma_start(out=kk_t[:], in_=pvec(rw_k_k[0:1, :])), w=["kk_t"])
                S.op("sp", lambda: nc.sync.dma_start(out=ka_t[:], in_=pvec(rw_k_a[0:1, :])), w=["ka_t"])
                S.op("sp", lambda: nc.sync.dma_start(out=rk_t[:], in_=pvec(rw_r_k[0:1, :])), w=["rk_t"])
                S.op("dve", lambda: nc.vector.tensor_scalar(oka_t[:], ka_t[:], -1.0, 1.0, ALU.mult, ALU.add), r=["ka_t"], w=["oka_t"])
                Lw = sbR("Lw", [128, 4, 512], BF16)
                Gw = sbR("Gw", [96, 512], BF16)
                blk = sbR("blk_sb", [128, 128])
                Lbf = sbR("Lbf", [128, T], BF16)
                Glb = sbR("Glb", [96, T], BF16)
                esL = ExitStack()
                Lst = esL.enter_context(nc.sbuf_tensor("Lst", [128, 4, 512], F32))
                Gst = esL.enter_context(nc.sbuf_tensor("Gst", [96, 512], F32))
                S.op("pool", lambda: nc.gpsimd.memset(Lst[:], 0.0), w=["Lst"])
                for d in range(2):
                    S.op("sp", lambda: nc.sync.dma_start(out=Lst[d * 32:(d + 1) * 32, d, :], in_=rw_w_up[d]), w=["Lst"])
                    S.op("sp", lambda: nc.sync.dma_start(out=Lst[64 + d * 32:64 + (d + 1) * 32, 2 + d, :], in_=rw_a_up[d]), w=["Lst"])
                S.op("pool", lambda: nc.gpsimd.tensor_copy(Lw[:], Lst[:]), r=["Lst"], w=["Lw"])
                S.op("sp", lambda: nc.sync.dma_start(out=Gst[:], in_=rw_g_up[:, :]), w=["Gst"])
                S.op("pool", lambda: nc.gpsimd.tensor_copy(Gw[:], Gst[:]), r=["Gst"], w=["Gw"])
                S.op("sp", lambda: nc.sync.dma_start(out=blk[:], in_=blk_in[:, :]), w=["blk"])
                S.barrier()
                esL.close()

                singles = ("kkr", "sqk", "nrm", "sg", "tmpa", "bbf", "ad")

                def tmp2(name, shape=(128, 512), dt=F32):
                    if name in singles:
                        t_ = sbR(name + "0", list(shape), dt)
                        return [t_, t_]
                    return [sbR("%s%d" % (name, i), list(shape), dt) for i in range(2)]

                def shift(dst, dstn, src, srcn, mucol, c0, rows=128, n=512):
                    S.op("dve", lambda: nc.vector.tensor_tensor(dst[0:rows, :], src[0:rows, c0:c0 + n], src[0:rows, c0 + 2:c0 + 2 + n], ALU.add),
                         r=[srcn], w=[dstn])
                    S.op("dve", lambda: nc.vector.scalar_tensor_tensor(dst[0:rows, :], dst[0:rows, :], 0.5, src[0:rows, c0 + 1:c0 + 1 + n], ALU.mult, ALU.subtract),
                         r=[srcn, dstn], w=[dstn])
                    S.op("dve", lambda: nc.vector.scalar_tensor_tensor(dst[0:rows, :], dst[0:rows, :], mucol, src[0:rows, c0 + 1:c0 + 1 + n], ALU.mult, ALU.add),
                         r=[srcn, dstn, "mu_t"], w=[dstn])

                rf = tmp2("rf"); kf = tmp2("kf"); vf = tmp2("vf")
                kkr = tmp2("kkr"); sqk = tmp2("sqk"); nrm = tmp2("nrm"); kkf = tmp2("kkf")
                kkn = tmp2("kkn", dt=BF16); rbf = tmp2("rbf", dt=BF16)
                ad = tmp2("ad"); sg = tmp2("sg"); dec = tmp2("dec"); tmpa = tmp2("tmpa"); kd = tmp2("kd")
                ksum = tmp2("ksum"); bbf = tmp2("bbf"); bon = tmp2("bon"); gg = tmp2("gg")
                tmb = tmp2("tmb", (128, 4, 128), BF16); tmk = tmp2("tmk", (128, 4, 128), BF16); tmv = tmp2("tmv", (128, 4, 128), BF16)
                inproj_group(24, 128, pfm[0], "pfm0")
                inproj_group(25, 96, pfm[1], "pfm1")
                for ch in range(8):
                    c0 = ch * 512
                    p_ = ch % 2
                    shift(rf[p_], "rf%d" % p_, pfm[0], "pfm0", mu_t[:, 12:13], c0)
                    S.op("act", lambda: nc.scalar.activation(out=Lbf[0:64, c0:c0 + 512], in_=rf[p_][0:64, :], func=AF.Tanh), r=["rf%d" % p_], w=["Lbf"])
                    S.op("pool", lambda: nc.gpsimd.tensor_copy(Lbf[64:128, c0:c0 + 512], rf[p_][64:128, :]), r=["rf%d" % p_], w=["Lbf"])
                    shift(kf[p_], "kf%d" % p_, pfm[1], "pfm1", mu_t[0:96, 13:14], c0, rows=96)
                    S.op("act", lambda: nc.scalar.activation(out=Glb[0:96, c0:c0 + 512], in_=kf[p_][0:96, :], func=AF.Sigmoid), r=["kf%d" % p_], w=["Glb"])
                it = 0
                for g4 in range(4):
                    inproj_group(12 + g4, 128, pfm[0], "pfm0")
                    inproj_group(16 + g4, 128, pfm[1], "pfm1")
                    inproj_group(20 + g4, 128, pfm[2], "pfm2")
                    rows = slice(g4 * 128, (g4 + 1) * 128)
                    for ch in range(8):
                        c0 = ch * 512
                        cs_ = slice(c0, c0 + 512)
                        p_ = it % 2
                        it += 1
                        P = lambda nm: nm + ("0" if nm in singles else str(p_))
                        shift(rf[p_], P("rf"), pfm[0], "pfm0", mu_t[:, g4:g4 + 1], c0)
                        shift(kf[p_], P("kf"), pfm[1], "pfm1", mu_t[:, 4 + g4:5 + g4], c0)
                        shift(vf[p_], P("vf"), pfm[2], "pfm2", mu_t[:, 8 + g4:9 + g4], c0)
                        S.op("dve", lambda: nc.vector.tensor_scalar(kkr[p_][:], kf[p_][:], kk_t[:, g4:g4 + 1], None, ALU.mult), r=[P("kf"), "kk_t"], w=[P("kkr")])
                        S.op("act", lambda: nc.scalar.activation(out=sqk[p_][:], in_=kkr[p_][:], func=AF.Square), r=[P("kkr")], w=[P("sqk")])
                        S.op("pe", lambda: nc.tensor.matmul(ps[2][:], blk[:], sqk[p_][:], start=True, stop=True), r=["blk", P("sqk")], w=[PSN[2]])
                        S.op("act", lambda: nc.scalar.activation(out=nrm[p_][:], in_=ps[2][:], func=AF.Sqrt), r=[PSN[2]], w=[P("nrm")])
                        S.op("dve", lambda: nc.vector.tensor_scalar(nrm[p_][:], nrm[p_][:], 1e-12, None, ALU.max), r=[P("nrm")], w=[P("nrm")])
                        S.op("dve", lambda: nc.vector.reciprocal(nrm[p_][:], nrm[p_][:]), r=[P("nrm")], w=[P("nrm")])
                        S.op("dve", lambda: nc.vector.tensor_tensor(kkf[p_][:], kkr[p_][:], nrm[p_][:], ALU.mult), r=[P("kkr"), P("nrm")], w=[P("kkf")])
                        S.op("pool", lambda: nc.gpsimd.tensor_scalar(kkn[p_][:], kkf[p_][:], -1.0, None, ALU.mult), r=[P("kkf")], w=[P("kkn")])
                        S.op("poolq", lambda: nc.gpsimd.dma_start(out=KKN[rows, cs_], in_=kkn[p_][:]), r=[P("kkn")], w=["KKN"])
                        S.op("pool", lambda: nc.gpsimd.tensor_copy(rbf[p_][:], rf[p_][:]), r=[P("rf")], w=[P("rbf")])
                        S.op("poolq", lambda: nc.gpsimd.dma_start(out=RR[rows, cs_], in_=rbf[p_][:]), r=[P("rbf")], w=["RR"])
                        for d in range(2):
                            S.op("pe", lambda: nc.tensor.matmul(ps[3][:], Lw[:, 2 + d, rows], Lbf[:, cs_], start=True, stop=True), r=["Lw", "Lbf"], w=[PSN[3]])
                            S.op("act", lambda: nc.scalar.activation(out=ad[p_][:], in_=ps[3][:], func=AF.Sigmoid, bias=a0_t[:, d * 4 + g4:d * 4 + g4 + 1]),
                                 r=[PSN[3], "a0_t"], w=[P("ad")])
                            S.op("pe", lambda: nc.tensor.matmul(ps[4][:], Lw[:, d, rows], Lbf[:, cs_], start=True, stop=True), r=["Lw", "Lbf"], w=[PSN[4]])
                            S.op("act", lambda: nc.scalar.activation(out=sg[p_][:], in_=ps[4][:], func=AF.Sigmoid, bias=w0_t[:, d * 4 + g4:d * 4 + g4 + 1]),
                                 r=[PSN[4], "w0_t"], w=[P("sg")])
                            S.op("act", lambda: nc.scalar.activation(out=dec[p_][:], in_=sg[p_][:], func=AF.Exp, scale=-0.6065306597126334),
                                 r=[P("sg")], w=[P("dec")])
                            S.op("poolq", lambda: nc.gpsimd.dma_start(out=DEC[d, rows, cs_], in_=dec[p_][:]), r=[P("dec")], w=["DEC"])
                            S.op("dve", lambda: nc.vector.tensor_scalar(tmpa[p_][:], ad[p_][:], ka_t[:, g4:g4 + 1], oka_t[:, g4:g4 + 1], ALU.mult, ALU.add),
                                 r=[P("ad"), "ka_t", "oka_t"], w=[P("tmpa")])
                            S.op("pool", lambda: nc.gpsimd.tensor_tensor(kd[p_][:], tmpa[p_][:], kf[p_][:], ALU.mult), r=[P("tmpa"), P("kf")], w=[P("kd")])
                            if d == 0:
                                S.op("pool", lambda: nc.gpsimd.tensor_copy(ksum[p_][:], kd[p_][:]), r=[P("kd")], w=[P("ksum")])
                            else:
                                S.op("pool", lambda: nc.gpsimd.tensor_tensor(ksum[p_][:], ksum[p_][:], kd[p_][:], ALU.add), r=[P("kd"), P("ksum")], w=[P("ksum")])
                            S.op("dve", lambda: nc.vector.tensor_tensor(bbf[p_][:], kkf[p_][:], ad[p_][:], ALU.mult), r=[P("kkf"), P("ad")], w=[P("bbf")])
                            for q in range(4):
                                S.op("pe", lambda: nc.tensor.transpose(ps[5][:, q * 128:(q + 1) * 128], bbf[p_][:, q * 128:(q + 1) * 128], ident[:]),
                                     r=[P("bbf"), "ident"], w=[PSN[5]])
                            S.op("act", lambda: nc.scalar.copy(tmb[p_][:], ps[5][:].rearrange("p (q c) -> p q c", q=4)), r=[PSN[5]], w=[P("tmb")])
                            S.op("poolq", lambda: nc.gpsimd.dma_start(out=BT[0, d, cs_, rows].rearrange("(q p) c -> p q c", p=128), in_=tmb[p_][:]),
                                 r=[P("tmb")], w=["BT"])
                            for q in range(4):
                                S.op("pe", lambda: nc.tensor.transpose(ps[6][:, q * 128:(q + 1) * 128], kd[p_][:, q * 128:(q + 1) * 128], ident[:]),
                                     r=[P("kd"), "ident"], w=[PSN[6]])
                            S.op("act", lambda: nc.scalar.copy(tmk[p_][:], ps[6][:].rearrange("p (q c) -> p q c", q=4)), r=[PSN[6]], w=[P("tmk")])
                            S.op("poolq", lambda: nc.gpsimd.dma_start(out=BT[1, d, cs_, rows].rearrange("(q p) c -> p q c", p=128), in_=tmk[p_][:]),
                                 r=[P("tmk")], w=["BT"])
                        S.op("dve", lambda: nc.vector.scalar_tensor_tensor(bon[p_][:], rf[p_][:], rk_t[:, g4:g4 + 1], ksum[p_][:], ALU.mult, ALU.mult),
                             r=[P("rf"), "rk_t", P("ksum")], w=[P("bon")])
                        S.op("pe", lambda: nc.tensor.matmul(ps[7][:], blk[:], bon[p_][:], start=True, stop=True), r=["blk", P("bon")], w=[PSN[7]])
                        S.op("dve", lambda: nc.vector.tensor_tensor(bon[p_][:], ps[7][:], vf[p_][:], ALU.mult), r=[PSN[7], P("vf"), P("bon")], w=[P("bon")])
                        S.op("poolq", lambda: nc.gpsimd.dma_start(out=BON[rows, cs_], in_=bon[p_][:]), r=[P("bon")], w=["BON"])
                        for q in range(4):
                            S.op("pe", lambda: nc.tensor.transpose(ps[5][:, q * 128:(q + 1) * 128], vf[p_][:, q * 128:(q + 1) * 128], ident[:]),
                                 r=[P("vf"), "ident"], w=[PSN[5]])
                        S.op("act", lambda: nc.scalar.copy(tmv[p_][:], ps[5][:].rearrange("p (q c) -> p q c", q=4)), r=[PSN[5]], w=[P("tmv")])
                        S.op("poolq", lambda: nc.gpsimd.dma_start(out=VT[cs_, rows].rearrange("(q p) c -> p q c", p=128), in_=tmv[p_][:]),
                             r=[P("tmv")], w=["VT"])
                        S.op("pe", lambda: nc.tensor.matmul(ps[3][:], Gw[0:96, rows], Glb[0:96, cs_], start=True, stop=True), r=["Gw", "Glb"], w=[PSN[3]])
                        S.op("act", lambda: nc.scalar.copy(gg[p_][:], ps[3][:]), r=[PSN[3]], w=[P("gg")])
                        S.op("poolq", lambda: nc.gpsimd.dma_start(out=GG[rows, cs_], in_=gg[p_][:]), r=[P("gg")], w=["GG"])
                if DBG:
                    for nm_, src_ in (("KKN", KKN), ("RR", RR), ("DEC", DEC), ("BT", BT), ("VT", VT), ("BON", BON), ("GG", GG)):
                        S.op("poolq", lambda: nc.gpsimd.dma_start(out=dbg_rw[nm_], in_=src_), r=[nm_])
                S.barrier()

        S.barrier()
        with ExitStack() as es3:
            sb3 = lambda name, shape, dt=F32: es3.enter_context(nc.sbuf_tensor(name, shape, dt))
            utm = sb3("utm", [128, NT, 512], BF16)
            Yall = sb3("Yall", [128, 64, 512], BF16)
            for n in range(2):
                srcD = UD if n == 0 else ZD
                srcDn = "UD" if n == 0 else "ZD"
                esA = ExitStack()
                ufl = [esA.enter_context(nc.sbuf_tensor("ufl%d_%d" % (i, n), [128, T], F32)) for i in range(2)]
                for g in range(4):
                    fb_ = g % 2
                    S.op("sp", lambda: nc.sync.dma_start(out=ufl[fb_][:], in_=srcD[g, :, :]), r=[srcDn], w=["ufl%d" % fb_])
                    for i4 in range(NT // 4):
                        pb = i4 % 2
                        for q in range(4):
                            i = i4 * 4 + q
                            S.op("pe", lambda: nc.tensor.transpose(ps[pb][:, q * 128:(q + 1) * 128], ufl[fb_][:, i * 128:(i + 1) * 128], ident[:]),
                                 r=["ufl%d" % fb_, "ident"], w=[PSN[pb]])
                        S.op("act", lambda: nc.scalar.copy(utm[:, i4 * 4:(i4 + 1) * 4, g * 128:(g + 1) * 128],
                                                           ps[pb][:].rearrange("p (q c) -> p q c", q=4)),
                             r=[PSN[pb]], w=["utm"])
                S.barrier()
                esA.close()
                esB = ExitStack()
                sbB = lambda name, shape, dt=F32: esB.enter_context(nc.sbuf_tensor(name + "_%d" % n, shape, dt))
                dre = [sbB("dre%d" % i, [128, 32, 128], BF16) for i in range(2)]
                dim_ = [sbB("dim%d" % i, [128, 32, 128], BF16) for i in range(2)]
                kre = [sbB("kre%d" % i, [128, 512]) for i in range(2)]
                kim = [sbB("kim%d" % i, [128, 512]) for i in range(2)]
                ure = sbB("ure", [128, 512])
                uim = sbB("uim", [128, 512])
                t1 = sbB("t1", [128, 512])
                t2 = sbB("t2", [128, 512])
                t3 = sbB("t3", [128, 512])
                t4 = sbB("t4", [128, 512])
                for j in range(32):
                    b = j % 2
                    S.op("sp", lambda: nc.sync.dma_start(out=dre[b][:], in_=dfw[j]), w=["dre%d" % b])
                    S.op("sp", lambda: nc.sync.dma_start(out=dim_[b][:], in_=dfw[32 + j]), w=["dim%d" % b])
                    S.op("sp", lambda: nc.sync.dma_start(out=kre[b][:], in_=KF[j, :, n * 512:(n + 1) * 512]), r=["KF"], w=["kre%d" % b])
                    S.op("sp", lambda: nc.sync.dma_start(out=kim[b][:], in_=KF[32 + j, :, n * 512:(n + 1) * 512]), r=["KF"], w=["kim%d" % b])
                    pr, pi = 2 + 2 * b, 3 + 2 * b
                    for tcn in range(NT):
                        S.op("pe", lambda: nc.tensor.matmul(ps[pr][:], dre[b][:, tcn, :], utm[:, tcn, :], start=(tcn == 0), stop=(tcn == NT - 1)),
                             r=["dre%d" % b, "utm"], w=[PSN[pr]])
                    for tcn in range(NT):
                        S.op("pe", lambda: nc.tensor.matmul(ps[pi][:], dim_[b][:, tcn, :], utm[:, tcn, :], start=(tcn == 0), stop=(tcn == NT - 1)),
                             r=["dim%d" % b, "utm"], w=[PSN[pi]])
                    S.op("act", lambda: nc.scalar.copy(ure[:], ps[pr][:]), r=[PSN[pr]], w=["ure"])
                    S.op("act", lambda: nc.scalar.copy(uim[:], ps[pi][:]), r=[PSN[pi]], w=["uim"])
                    S.op("dve", lambda: nc.vector.tensor_tensor(t1[:], ure[:], kre[b][:], ALU.mult), r=["ure", "kre%d" % b], w=["t1"])
                    S.op("pool", lambda: nc.gpsimd.tensor_tensor(t2[:], uim[:], kim[b][:], ALU.mult), r=["uim", "kim%d" % b], w=["t2"])
                    S.op("dve", lambda: nc.vector.tensor_tensor(Yall[:, j, :], t1[:], t2[:], ALU.subtract), r=["t1", "t2"], w=["Yall"])
                    S.op("pool", lambda: nc.gpsimd.tensor_tensor(t3[:], ure[:], kim[b][:], ALU.mult), r=["ure", "kim%d" % b], w=["t3"])
                    S.op("dve", lambda: nc.vector.tensor_tensor(t4[:], uim[:], kre[b][:], ALU.mult), r=["uim", "kre%d" % b], w=["t4"])
                    S.op("pool", lambda: nc.gpsimd.tensor_tensor(Yall[:, 32 + j, :], t3[:], t4[:], ALU.add), r=["t3", "t4"], w=["Yall"])
                    if j == 0:
                        S.op("dve", lambda: nc.vector.tensor_tensor(Yall[0:1, 0, :], ure[0:1, :], kre[b][0:1, :], ALU.mult),
                             r=["ure", "kre%d" % b, "Yall"], w=["Yall"])
                        S.op("dve", lambda: nc.vector.tensor_tensor(Yall[0:1, 32, :], uim[0:1, :], kim[b][0:1, :], ALU.mult),
                             r=["uim", "kim%d" % b, "Yall"], w=["Yall"])
                S.barrier()
                esB.close()
                esC = ExitStack()
                sbC = lambda name, shape, dt=F32: esC.enter_context(nc.sbuf_tensor(name + "_%d" % n, shape, dt))
                dvt = sbC("dvt", [128, 64, 256], BF16)
                gate = [sbC("gate%d" % i, [128, 256]) for i in range(2)]
                zo = [sbC("zo%d" % i, [128, 256]) for i in range(2)]
                dstD = ZD if n == 0 else YD
                dstDn = "ZD" if n == 0 else "YD"
                for tq in range(16):
                    S.op("sp", lambda: nc.sync.dma_start(out=dvt[:], in_=dinv[tq]), w=["dvt"])
                    for g in range(4):
                        gb = g % 2
                        pb = 4 + g
                        gsrc = UD[4 + g] if n == 0 else UD[8 + g]
                        S.op("sp", lambda: nc.sync.dma_start(out=gate[gb][:], in_=gsrc[:, tq * 256:(tq + 1) * 256]), r=["UD"], w=["gate%d" % gb])
                        for rc in range(64):
                            S.op("pe", lambda: nc.tensor.matmul(ps[pb][:, 0:256], Yall[:, rc, g * 128:(g + 1) * 128], dvt[:, rc, :],
                                                                start=(rc == 0), stop=(rc == 63)),
                                 r=["Yall", "dvt"], w=[PSN[pb]])
                        S.op("dve", lambda: nc.vector.tensor_tensor(zo[gb][:], ps[pb][:, 0:256], gate[gb][:], ALU.mult),
                             r=[PSN[pb], "gate%d" % gb], w=["zo%d" % gb])
                        S.op("poolq", lambda: nc.gpsimd.dma_start(out=dstD[g, :, tq * 256:(tq + 1) * 256], in_=zo[gb][:]), r=["zo%d" % gb], w=[dstDn])
                S.barrier()
                esC.close()
            if DBG:
                S.op("poolq", lambda: nc.gpsimd.dma_start(out=dbg_y[:, :, :], in_=YD[:, :, :]), r=["YD"])

        S.barrier()
        with ExitStack() as esS:
            sbS = lambda name, shape, dt=F32: esS.enter_context(nc.sbuf_tensor(name, shape, dt))
            TC = 64
            St = sbS("St", [128, 512])
            Sbf = sbS("Sbf", [128, 512], BF16)
            rhs2 = sbS("rhs2", [48, 512], BF16)
            Vtm = sbS("Vtm", [128, NT, 512], BF16)
            m48 = sbS("m48_sb", [48, 512])
            m40 = sbS("m40_sb", [40, 1, 512])
            selF = sbS("selF_sb", [128, 128, 48], BF16)
            selB = sbS("selB_sb", [128, 128, 48], BF16)
            ARraw = sbS("ARraw", [128, 104, TC], BF16)
            AR2 = [sbS("AR2_%d" % i, [128, TC, 104], BF16) for i in range(2)]
            Wraw = sbS("Wraw", [128, 8, TC])
            W2 = [sbS("W2_%d" % i, [128, TC, 8, 1]) for i in range(2)]
            BKraw = sbS("BKraw", [48, TC, 128], BF16)
            BK2 = [sbS("BK2_%d" % i, [48, TC, 128], BF16) for i in range(2)]
            Oraw = [sbS("Oraw%d" % i, [40, 8, 512]) for i in range(2)]
            Ored = [sbS("Ored%d" % i, [40, 8, 64]) for i in range(2)]
            S.op("pool", lambda: nc.gpsimd.memset(St[:], 0.0), w=["St0", "St1"])
            S.op("pool", lambda: nc.gpsimd.memset(Sbf[:], 0.0), w=["Sbf0", "Sbf1"])
            S.op("pool", lambda: nc.gpsimd.memset(rhs2[:], 0.0), w=["rhs2_0", "rhs2_1"])
            S.op("pool", lambda: nc.gpsimd.memset(ARraw[:], 0.0), w=["ARraw"])
            S.op("pool", lambda: nc.gpsimd.memset(BKraw[:], 0.0), w=["BKraw"])
            for i in range(2):
                S.op("pool", lambda: nc.gpsimd.memset(BK2[i][:], 0.0), w=["BK2_%d" % i])
            S.op("sp", lambda: nc.sync.dma_start(out=Vtm[:], in_=VT.rearrange("(i p) c -> p i c", p=128)), r=["VT"], w=["Vtm"])
            S.op("sp", lambda: nc.sync.dma_start(out=m48[:], in_=m48_in[:, :]), w=["m48"])
            S.op("sp", lambda: nc.sync.dma_start(out=m40[:, 0, :], in_=m40_in[:, :]), w=["m40"])
            S.op("sp", lambda: nc.sync.dma_start(out=selF[:], in_=selF_in[:, :, :]), w=["selF"])
            S.op("sp", lambda: nc.sync.dma_start(out=selB[:], in_=selB_in[:, :, :]), w=["selB"])
            KKN_v = KKN.rearrange("(j k) t -> k j t", k=64)
            RR_v = RR.rearrange("(j k) t -> k j t", k=64)
            for c in range(SCAN_CHUNKS):
                cb = c % 2
                s0 = c * TC
                fw_ = slice(s0, s0 + TC)
                bw_ = slice(T - s0 - TC, T - s0)
                A2n, W2n, B2n = "AR2_%d" % cb, "W2_%d" % cb, "BK2_%d" % cb
                S.op("sp", lambda: nc.sync.dma_start(out=ARraw[0:64, 0:8, :], in_=KKN_v[:, :, fw_]), r=["KKN"], w=["ARraw"])
                S.op("sp", lambda: nc.sync.dma_start(out=ARraw[64:128, 32:40, :], in_=KKN_v[:, :, bw_]), r=["KKN"], w=["ARraw"])
                S.op("sp", lambda: nc.sync.dma_start(out=ARraw[0:64, 64:72, :], in_=RR_v[:, :, fw_]), r=["RR"], w=["ARraw"])
                S.op("sp", lambda: nc.sync.dma_start(out=ARraw[64:128, 96:104, :], in_=RR_v[:, :, bw_]), r=["RR"], w=["ARraw"])
                S.op("pool", lambda: nc.gpsimd.tensor_copy(AR2[cb][0:64], ARraw[0:64].rearrange("p c t -> p t c")), r=["ARraw"], w=[A2n])
                S.op("pool", lambda: nc.gpsimd.tensor_copy(AR2[cb][64:128], ARraw[64:128, :, ::-1].rearrange("p c t -> p t c")), r=["ARraw"], w=[A2n])
                S.op("sp", lambda: nc.sync.dma_start(out=Wraw[0:64], in_=DEC[0].rearrange("(j k) t -> k j t", k=64)[:, :, fw_]), r=["DEC"], w=["Wraw"])
                S.op("sp", lambda: nc.sync.dma_start(out=Wraw[64:128], in_=DEC[1].rearrange("(j k) t -> k j t", k=64)[:, :, bw_]), r=["DEC"], w=["Wraw"])
                S.op("pool", lambda: nc.gpsimd.tensor_copy(W2[cb][0:64, :, :, 0], Wraw[0:64].rearrange("p j t -> p t j")), r=["Wraw"], w=[W2n])
                S.op("pool", lambda: nc.gpsimd.tensor_copy(W2[cb][64:128, :, :, 0], Wraw[64:128, :, ::-1].rearrange("p j t -> p t j")), r=["Wraw"], w=[W2n])
                for s_ in range(2):
                    S.op("sp", lambda: nc.sync.dma_start(out=BKraw[s_ * 8:(s_ + 1) * 8, :, 0:64],
                                                         in_=BT[s_, 0, fw_, :].rearrange("t (j k) -> j t k", k=64)), r=["BT"], w=["BKraw"])
                    S.op("sp", lambda: nc.sync.dma_start(out=BKraw[32 + s_ * 8:32 + (s_ + 1) * 8, :, 64:128],
                                                         in_=BT[s_, 1, bw_, :].rearrange("t (j k) -> j t k", k=64)), r=["BT"], w=["BKraw"])
                S.op("pool", lambda: nc.gpsimd.tensor_copy(BK2[cb][0:16], BKraw[0:16]), r=["BKraw"], w=[B2n])
                S.op("pool", lambda: nc.gpsimd.tensor_copy(BK2[cb][32:48], BKraw[32:48, ::-1, :]), r=["BKraw"], w=[B2n])
                for s in range(TC):
                    st = s0 + s
                    i_f, tl = st // 128, st % 128
                    i_b = NT - 1 - i_f
                    ob = (st // 8) % 2
                    HH = (0, 1)
                    csl = [slice(h_ * 256, (h_ + 1) * 256) for h_ in HH]
                    for h_ in HH:
                        S.op("pe", lambda: nc.tensor.matmul(ps[h_][0:48, 0:256], selF[:, tl, :], Vtm[:, i_f, csl[h_]], start=True, stop=False),
                             r=["selF", "Vtm"], w=[PSN[h_]])
                        S.op("pe", lambda: nc.tensor.matmul(ps[h_][0:48, 0:256], selB[:, tl, :], Vtm[:, i_b, csl[h_]], start=False, stop=False),
                             r=["selB", "Vtm"], w=[PSN[h_]])
                        S.op("pe", lambda: nc.tensor.matmul(ps[h_][0:48, 0:256], AR2[cb][:, s, 0:48], Sbf[:, csl[h_]], start=False, stop=True),
                             r=[A2n, "Sbf%d" % h_], w=[PSN[h_]])
                    for h_ in HH:
                        S.op("dve", lambda: nc.vector.tensor_tensor(rhs2[:, csl[h_]], ps[h_][0:48, 0:256], m48[:, csl[h_]], ALU.mult),
                             r=[PSN[h_], "m48"], w=["rhs2_%d" % h_])
                    for h_ in HH:
                        S.op("dve", lambda: nc.vector.tensor_tensor(St[:, csl[h_]].rearrange("p (j v) -> p j v", j=4), St[:, csl[h_]].rearrange("p (j v) -> p j v", j=4),
                                                                    W2[cb][:, s, h_ * 4:(h_ + 1) * 4, :].to_broadcast([128, 4, 64]), ALU.mult),
                             r=["St%d" % h_, W2n], w=["St%d" % h_])
                    for h_ in HH:
                        S.op("pe", lambda: nc.tensor.matmul(ps[2 + h_][:, 0:256], BK2[cb][0:48, s, :], rhs2[:, csl[h_]], start=True, stop=True),
                             r=[B2n, "rhs2_%d" % h_], w=[PSN[2 + h_]])
                    for h_ in HH:
                        S.op("dve", lambda: nc.vector.tensor_tensor(St[:, csl[h_]], St[:, csl[h_]], ps[2 + h_][:, 0:256], ALU.add),
                             r=["St%d" % h_, PSN[2 + h_]], w=["St%d" % h_])
                    for h_ in HH:
                        S.op("act", lambda: nc.scalar.copy(Sbf[:, csl[h_]], St[:, csl[h_]]), r=["St%d" % h_], w=["Sbf%d" % h_])
                    for h_ in HH:
                        S.op("pe", lambda: nc.tensor.matmul(ps[4 + h_][0:40, 0:256], AR2[cb][:, s, 64:104], Sbf[:, csl[h_]], start=True, stop=True),
                             r=[A2n, "Sbf%d" % h_], w=[PSN[4 + h_]])
                    for h_ in HH:
                        S.op("act", lambda: nc.scalar.copy(Oraw[ob][:, st % 8, csl[h_]], ps[4 + h_][0:40, 0:256]), r=[PSN[4 + h_]], w=["Oraw%d" % ob])
                    if st % 8 == 7:
                        st0 = st - 7
                        S.op("pool", lambda: nc.gpsimd.tensor_tensor(Oraw[ob][:], Oraw[ob][:], m40[:].to_broadcast([40, 8, 512]), ALU.mult),
                             r=["Oraw%d" % ob, "m40"], w=["Oraw%d" % ob])
                        S.op("dve", lambda: nc.vector.tensor_reduce(out=Ored[ob][:], in_=Oraw[ob][:].rearrange("p s (j v) -> p s v j", j=8),
                                                                     axis=AX.X, op=ALU.add),
                             r=["Oraw%d" % ob], w=["Ored%d" % ob])
                        S.op("poolq", lambda: nc.gpsimd.dma_start(out=OD[0, st0:st0 + 8, :].rearrange("s (j v) -> j s v", v=64), in_=Ored[ob][0:8]),
                             r=["Ored%d" % ob], w=["OD"])
                        S.op("poolq", lambda: nc.gpsimd.dma_start(out=OD[1, st0:st0 + 8, :].rearrange("s (j v) -> j s v", v=64), in_=Ored[ob][32:40]),
                             r=["Ored%d" % ob], w=["OD"])
            if DBG:
                S.op("poolq", lambda: nc.gpsimd.dma_start(out=dbg_od, in_=OD), r=["OD"])
            S.barrier()

        with ExitStack() as esP:
            sbP = lambda name, shape, dt=F32: esP.enter_context(nc.sbuf_tensor(name, shape, dt))
            pvec = lambda ap_row, p=128: ap_row.rearrange("o (g p) -> p (o g)", p=p)
            yT = sbP("yT", [128, 8, T], BF16)
            ldb = [sbP("ldb%d" % i, [128, 1024]) for i in range(2)]
            n = 0
            for g in range(4):
                for c4 in range(4):
                    b = n % 2
                    n += 1
                    S.op("sp", lambda: nc.sync.dma_start(out=ldb[b][:], in_=YD[g, :, c4 * 1024:(c4 + 1) * 1024]), r=["YD"], w=["ldb%d" % b])
                    S.op("pool", lambda: nc.gpsimd.tensor_copy(yT[:, g, c4 * 1024:(c4 + 1) * 1024], ldb[b][:]), r=["ldb%d" % b], w=["yT"])
            blk2 = sbP("blk2", [128, 128])
            lnw_t = sbP("lnw_t", [128, 4])
            lnb_t = sbP("lnb_t", [128, 4])
            S.op("sp", lambda: nc.sync.dma_start(out=blk2[:], in_=blk_in[:, :]), w=["blk2"])
            S.op("sp", lambda: nc.sync.dma_start(out=lnw_t[:], in_=pvec(rw_ln_w[0:1, :])), w=["lnw_t"])
            S.op("sp", lambda: nc.sync.dma_start(out=lnb_t[:], in_=pvec(rw_ln_b[0:1, :])), w=["lnb_t"])
            oft = [sbP("oft%d" % i, [128, 4, 128]) for i in range(2)]
            obt = [sbP("obt%d" % i, [128, 4, 128]) for i in range(2)]
            bonc = [sbP("bonc%d" % i, [128, 512]) for i in range(2)]
            ggc = [sbP("ggc%d" % i, [128, 512]) for i in range(2)]
            obr = sbP("obr", [128, 4, 128])
            sfm = sbP("sfm", [128, 512])
            cen = sbP("cen", [128, 512])
            sqp = sbP("sqp", [128, 512])
            rstd = sbP("rstd", [128, 512])
            n = 0
            for g4 in range(4):
                rows = slice(g4 * 128, (g4 + 1) * 128)
                for i4 in range(8):
                    b = n % 2
                    n += 1
                    cs_ = slice(i4 * 512, (i4 + 1) * 512)
                    S.op("sp", lambda: nc.sync.dma_start(out=oft[b][:], in_=OD[0, cs_, rows].rearrange("(q p) c -> p q c", p=128)), r=["OD"], w=["oft%d" % b])
                    S.op("sp", lambda: nc.sync.dma_start(out=obt[b][:], in_=OD[1, T - (i4 + 1) * 512:T - i4 * 512, rows].rearrange("(m p) c -> p m c", p=128)),
                         r=["OD"], w=["obt%d" % b])
                    S.op("sp", lambda: nc.sync.dma_start(out=bonc[b][:], in_=BON[rows, cs_]), r=["BON"], w=["bonc%d" % b])
                    S.op("sp", lambda: nc.sync.dma_start(out=ggc[b][:], in_=GG[rows, cs_]), r=["GG"], w=["ggc%d" % b])
                    for q in range(4):
                        S.op("pe", lambda: nc.tensor.transpose(ps[0][:, q * 128:(q + 1) * 128], oft[b][:, q, :], ident[:]), r=["oft%d" % b, "ident"], w=[PSN[0]])
                    for q in range(4):
                        S.op("pe", lambda: nc.tensor.transpose(ps[1][:, q * 128:(q + 1) * 128], obt[b][:, 3 - q, :], ident[:]), r=["obt%d" % b, "ident"], w=[PSN[1]])
                    S.op("act", lambda: nc.scalar.copy(obr[:], ps[1][:].rearrange("p (q t) -> p q t", q=4)[:, :, ::-1]), r=[PSN[1]], w=["obr"])
                    S.op("dve", lambda: nc.vector.tensor_tensor(sfm[:], ps[0][:], obr[:].rearrange("p q t -> p (q t)"), ALU.add), r=[PSN[0], "obr"], w=["sfm"])
                    S.op("pe", lambda: nc.tensor.matmul(ps[2][:], blk2[:], sfm[:], start=True, stop=True), r=["blk2", "sfm"], w=[PSN[2]])
                    S.op("dve", lambda: nc.vector.scalar_tensor_tensor(cen[:], ps[2][:], -1.0 / 64, sfm[:], ALU.mult, ALU.add), r=[PSN[2], "sfm"], w=["cen"])
                    S.op("act", lambda: nc.scalar.activation(out=sqp[:], in_=cen[:], func=AF.Square), r=["cen"], w=["sqp"])
                    S.op("pe", lambda: nc.tensor.matmul(ps[3][:], blk2[:], sqp[:], start=True, stop=True), r=["blk2", "sqp"], w=[PSN[3]])
                    S.op("dve", lambda: nc.vector.tensor_scalar(rstd[:], ps[3][:], 1.0 / 64, 64e-5, ALU.mult, ALU.add), r=[PSN[3]], w=["rstd"])
                    S.op("act", lambda: nc.scalar.activation(out=rstd[:], in_=rstd[:], func=AF.Sqrt), r=["rstd"], w=["rstd"])
                    S.op("dve", lambda: nc.vector.reciprocal(rstd[:], rstd[:]), r=["rstd"], w=["rstd"])
                    S.op("dve", lambda: nc.vector.tensor_tensor(cen[:], cen[:], rstd[:], ALU.mult), r=["cen", "rstd"], w=["cen"])
                    S.op("dve", lambda: nc.vector.tensor_scalar(cen[:], cen[:], lnw_t[:, g4:g4 + 1], lnb_t[:, g4:g4 + 1], ALU.mult, ALU.add),
                         r=["cen", "lnw_t", "lnb_t"], w=["cen"])
                    S.op("pool", lambda: nc.gpsimd.tensor_tensor(cen[:], cen[:], bonc[b][:], ALU.add), r=["cen", "bonc%d" % b], w=["cen"])
                    S.op("pool", lambda: nc.gpsimd.tensor_tensor(yT[:, 4 + g4, cs_], cen[:], ggc[b][:], ALU.mult), r=["cen", "ggc%d" % b], w=["yT"])
            wo = sbP("wo", [128, 8, D], BF16)
            g1t = sbP("g1t", [128, D])
            xin = [sbP("xin%d" % i, [128, D]) for i in range(2)]
            x1t = [sbP("x1t%d" % i, [128, D]) for i in range(2)]
            for kc in range(8):
                b = kc % 2
                S.op("sp", lambda: nc.sync.dma_start(out=ldb[b][:], in_=w_out[kc * 128:(kc + 1) * 128, :]), w=["ldb%d" % b])
                S.op("pool", lambda: nc.gpsimd.tensor_copy(wo[:, kc, :], ldb[b][:]), r=["ldb%d" % b], w=["wo"])
            S.op("sp", lambda: nc.sync.dma_start(out=g1t[:], in_=MODD[:, 2 * D:3 * D]), r=["MODD"], w=["g1t"])
            for i in range(NT):
                b = i % 2
                S.op("sp", lambda: nc.sync.dma_start(out=xin[b][:], in_=x[i * 128:(i + 1) * 128, :]), w=["xin%d" % b])
                for half in range(2):
                    hs = slice(half * 512, (half + 1) * 512)
                    for kc in range(8):
                        S.op("pe", lambda: nc.tensor.matmul(ps[4 + half][:], yT[:, kc, i * 128:(i + 1) * 128], wo[:, kc, hs], start=(kc == 0), stop=(kc == 7)),
                             r=["yT", "wo"], w=[PSN[4 + half]])
                    S.op("dve", lambda: nc.vector.tensor_tensor(x1t[b][:, hs], ps[4 + half][:], g1t[:, hs], ALU.mult), r=[PSN[4 + half], "g1t"], w=["x1t%d" % b])
                    S.op("pool", lambda: nc.gpsimd.tensor_tensor(x1t[b][:, hs], x1t[b][:, hs], xin[b][:, hs], ALU.add), r=["x1t%d" % b, "xin%d" % b], w=["x1t%d" % b])
                S.op("poolq", lambda: nc.gpsimd.dma_start(out=X1D[i * 128:(i + 1) * 128, :], in_=x1t[b][:]), r=["x1t%d" % b], w=["X1D"])
            if DBG:
                S.op("poolq", lambda: nc.gpsimd.dma_start(out=dbg_x1, in_=X1D), r=["X1D"])
            S.barrier()

        NE = 256
        CAP = 1024
        NST = CAP // 128
        with ExitStack() as esF:
            sbF = lambda name, shape, dt=F32: esF.enter_context(nc.sbuf_tensor(name, shape, dt))
            g2t = sbF("g2t", [128, D])
            S.op("sp", lambda: nc.sync.dma_start(out=g2t[:], in_=MODD[:, 5 * D:6 * D]), r=["MODD"], w=["g2t"])
            keys = sbF("keys", [128, 2, CAP])
            idxI = sbF("idxI", [128, NST, NE], I32)
            with ExitStack() as esG:
                sbG = lambda name, shape, dt=F32: esG.enter_context(nc.sbuf_tensor(name, shape, dt))
                gm2 = sbG("gm2", [128, D]); sc2 = sbG("sc2", [128, D]); sh2 = sbG("sh2", [128, D])
                S.op("sp", lambda: nc.sync.dma_start(out=gm2[:], in_=bcast_rows(norm2_g[0:1, :])), w=["gm2"])
                S.op("sp", lambda: nc.sync.dma_start(out=sh2[:], in_=MODD[:, 3 * D:4 * D]), r=["MODD"], w=["sh2"])
                S.op("sp", lambda: nc.sync.dma_start(out=sc2[:], in_=MODD[:, 4 * D:5 * D]), r=["MODD"], w=["sc2"])
                S.op("dve", lambda: nc.vector.scalar_tensor_tensor(gm2[:], sc2[:], 1.0, gm2[:], ALU.add, ALU.mult), r=["sc2", "gm2"], w=["gm2"])
                rwt = sbG("rwt", [128, 8, NE])
                S.op("sp", lambda: nc.sync.dma_start(out=rwt[:], in_=router_w.rearrange("(kc p) n -> p kc n", p=128)), w=["rwt"])
                rbias = sbG("rbias", [128, NE])
                S.op("sp", lambda: nc.sync.dma_start(out=rbias[:], in_=bcast_rows(router_bias[0:1, :])), w=["rbias"])
                tokid = sbG("tokid_sb", [128, NT])
                S.op("sp", lambda: nc.sync.dma_start(out=tokid[:], in_=tokid_in[:, :]), w=["tokid"])
                zrow = sbG("zrow", [1, D + NE])
                S.op("pool", lambda: nc.gpsimd.memset(zrow[:], 0.0), w=["zrow"])
                S.op("poolq", lambda: nc.gpsimd.dma_start(out=H2E[T:T + 1, :], in_=zrow[:]), r=["zrow"], w=["H2E"])
                S.op("poolq", lambda: nc.gpsimd.dma_start(out=OUTACC[T:T + 1, :], in_=zrow[:, 0:D]), r=["zrow"], w=["OUTACC"])
                shgu = sbG("shgu", [128, 8, 512], BF16)
                shd = sbG("shd", [128, 2, D], BF16)
                stg = sbG("stg", [128, 8, 256])
                S.op("sp", lambda: nc.sync.dma_start(out=stg[:], in_=sh_w_gate.rearrange("(kc p) n -> p kc n", p=128)), w=["stg"])
                S.op("pool", lambda: nc.gpsimd.tensor_copy(shgu[:, :, 0:256], stg[:]), r=["stg"], w=["shgu"])
                S.op("sp", lambda: nc.sync.dma_start(out=stg[:], in_=sh_w_up.rearrange("(kc p) n -> p kc n", p=128)), w=["stg"])
                S.op("pool", lambda: nc.gpsimd.tensor_copy(shgu[:, :, 256:512], stg[:]), r=["stg"], w=["shgu"])
                S.op("sp", lambda: nc.sync.dma_start(out=stg[:].rearrange("p a b -> p (a b)").rearrange("p (j n) -> p j n", j=2),
                                                     in_=sh_w_down.rearrange("(jc p) n -> p jc n", p=128)), w=["stg"])
                S.op("pool", lambda: nc.gpsimd.tensor_copy(shd[:], stg[:].rearrange("p a b -> p (a b)").rearrange("p (j n) -> p j n", j=2)), r=["stg"], w=["shd"])
                KT = sbG("KT", [128, 2, T])
                xt2 = [sbG("xt2_%d" % i, [128, D]) for i in range(2)]
                h2 = [sbG("h2_%d" % i, [128, D]) for i in range(2)]
                sq2 = sbG("sq2", [128, D])
                ss2 = [sbG("ss2_%d" % i, [128, 1]) for i in range(2)]
                rs2 = [sbG("rs2_%d" % i, [128, 1]) for i in range(2)]
                h2T = sbG("h2T", [128, 8, 128])
                h2Tb = sbG("h2Tb", [128, 8, 128], BF16)
                sc = sbG("sc", [128, NE]); bia = sbG("bia", [128, NE]); msk = sbG("msk", [128, NE]); sel = sbG("sel", [128, NE])
                gat = [sbG("gat%d" % i, [128, NE]) for i in range(2)]
                key = sbG("key", [128, NE])
                m8 = sbG("m8", [128, 8, 8]); gs = sbG("gs", [128, 8]); gm8 = sbG("gm8", [128, 8]); gmk = sbG("gmk", [128, 8, 1]); pen = sbG("pen", [128, 8, 1])
                t8 = sbG("t8", [128, 8]); den = sbG("den", [128, 1])
                sgs = sbG("sgs", [128, 256]); acs = sbG("acs", [128, 256]); acT = sbG("acT", [128, 2, 128], BF16)
                acc = [sbG("acc%d" % i, [128, D]) for i in range(2)]
                for i in range(NT):
                    b = i % 2
                    tsl = slice(i * 128, (i + 1) * 128)
                    S.op("sp", lambda: nc.sync.dma_start(out=xt2[b][:], in_=X1D[tsl, :]), r=["X1D"], w=["xt2_%d" % b])
                    rstd_of(sq2, ss2[b], rs2[b], "n2_%d" % b, xt2[b][:], "xt2_%d" % b)
                    S.op("dve", lambda: nc.vector.scalar_tensor_tensor(h2[b][:], xt2[b][:], rs2[b][:, 0:1], gm2[:], ALU.mult, ALU.mult),
                         r=["xt2_%d" % b, "rsn2_%d" % b, "gm2"], w=["h2_%d" % b])
                    S.op("pool", lambda: nc.gpsimd.tensor_tensor(h2[b][:], h2[b][:], sh2[:], ALU.add), r=["h2_%d" % b, "sh2"], w=["h2_%d" % b])
                    S.op("poolq", lambda: nc.gpsimd.dma_start(out=H2E[tsl, 0:D], in_=h2[b][:]), r=["h2_%d" % b], w=["H2E"])
                    for half in range(2):
                        pb = half
                        for q in range(4):
                            kc = half * 4 + q
                            S.op("pe", lambda: nc.tensor.transpose(ps[pb][:, q * 128:(q + 1) * 128], h2[b][:, kc * 128:(kc + 1) * 128], ident[:]),
                                 r=["h2_%d" % b, "ident"], w=[PSN[pb]])
                        S.op("act", lambda: nc.scalar.copy(h2T[:, half * 4:(half + 1) * 4, :], ps[pb][:].rearrange("p (q t) -> p q t", q=4)), r=[PSN[pb]], w=["h2T"])
                    S.op("pool", lambda: nc.gpsimd.tensor_copy(h2Tb[:], h2T[:]), r=["h2T"], w=["h2Tb"])
                    for kc in range(8):
                        S.op("pe", lambda: nc.tensor.matmul(ps[2][:, 0:NE], h2T[:, kc, :], rwt[:, kc, :], start=(kc == 0), stop=(kc == 7)), r=["h2T", "rwt"], w=[PSN[2]])
                    S.op("act", lambda: nc.scalar.activation(out=sc[:], in_=ps[2][:, 0:NE], func=AF.Sigmoid), r=[PSN[2]], w=["sc"])
                    S.op("dve", lambda: nc.vector.tensor_tensor(bia[:], sc[:], rbias[:], ALU.add), r=["sc", "rbias"], w=["bia"])
                    for g in range(8):
                        S.op("dve", lambda: nc.vector.max(out=m8[:, g, :], in_=bia[:, g * 32:(g + 1) * 32]), r=["bia"], w=["m8"])
                    S.op("dve", lambda: nc.vector.tensor_tensor(gs[:], m8[:, :, 0], m8[:, :, 1], ALU.add), r=["m8"], w=["gs"])
                    S.op("dve", lambda: nc.vector.max(out=gm8[:], in_=gs[:]), r=["gs"], w=["gm8"])
                    S.op("dve", lambda: nc.vector.tensor_scalar(gmk[:, :, 0], gs[:], gm8[:, 3:4], None, ALU.is_ge), r=["gs", "gm8"], w=["gmk"])
                    S.op("dve", lambda: nc.vector.tensor_scalar(pen[:, :, 0], gmk[:, :, 0], 1e9, -1e9, ALU.mult, ALU.add), r=["gmk"], w=["pen"])
                    S.op("dve", lambda: nc.vector.tensor_tensor(msk[:].rearrange("p (g e) -> p g e", g=8), bia[:].rearrange("p (g e) -> p g e", g=8),
                                                                gmk[:].to_broadcast([128, 8, 32]), ALU.mult), r=["bia", "gmk"], w=["msk"])
                    S.op("dve", lambda: nc.vector.tensor_tensor(msk[:].rearrange("p (g e) -> p g e", g=8), msk[:].rearrange("p (g e) -> p g e", g=8),
                                                                pen[:].to_broadcast([128, 8, 32]), ALU.add), r=["msk", "pen"], w=["msk"])
                    S.op("dve", lambda: nc.vector.max(out=t8[:], in_=msk[:]), r=["msk"], w=["t8"])
                    S.op("dve", lambda: nc.vector.tensor_scalar(sel[:], msk[:], t8[:, 7:8], None, ALU.is_ge), r=["msk", "t8"], w=["sel"])
                    S.op("dve", lambda: nc.vector.tensor_tensor(gat[b][:], sc[:], sel[:], ALU.mult), r=["sc", "sel"], w=["gat%d" % b])
                    S.op("dve", lambda: nc.vector.tensor_reduce(out=den[:], in_=gat[b][:], axis=AX.X, op=ALU.add), r=["gat%d" % b], w=["den"])
                    S.op("dve", lambda: nc.vector.reciprocal(den[:], den[:]), r=["den"], w=["den"])
                    S.op("dve", lambda: nc.vector.tensor_scalar(gat[b][:], gat[b][:], den[:, 0:1], 2.5, ALU.mult, ALU.mult), r=["gat%d" % b, "den"], w=["gat%d" % b])
                    S.op("poolq", lambda: nc.gpsimd.dma_start(out=H2E[tsl, D:D + NE], in_=gat[b][:]), r=["gat%d" % b], w=["H2E"])
                    S.op("dve", lambda: nc.vector.tensor_scalar(key[:], sel[:], tokid[:, i:i + 1], None, ALU.mult), r=["sel", "tokid"], w=["key"])
                    for eh in range(2):
                        S.op("pe", lambda: nc.tensor.transpose(ps[3][:, eh * 128:(eh + 1) * 128], key[:, eh * 128:(eh + 1) * 128], ident[:]), r=["key", "ident"], w=[PSN[3]])
                    S.op("act", lambda: nc.scalar.copy(KT[:, :, tsl], ps[3][:, 0:256].rearrange("p (e t) -> p e t", e=2)), r=[PSN[3]], w=["KT"])
                    for kc in range(8):
                        S.op("pe", lambda: nc.tensor.matmul(ps[4][:], h2Tb[:, kc, :], shgu[:, kc, :], start=(kc == 0), stop=(kc == 7)), r=["h2Tb", "shgu"], w=[PSN[4]])
                    S.op("act", lambda: nc.scalar.activation(out=sgs[:], in_=ps[4][:, 0:256], func=AF.Silu), r=[PSN[4]], w=["sgs"])
                    S.op("dve", lambda: nc.vector.tensor_tensor(acs[:], sgs[:], ps[4][:, 256:512], ALU.mult), r=["sgs", PSN[4]], w=["acs"])
                    for jc in range(2):
                        S.op("pe", lambda: nc.tensor.transpose(ps[5][:, jc * 128:(jc + 1) * 128], acs[:, jc * 128:(jc + 1) * 128], ident[:]), r=["acs", "ident"], w=[PSN[5]])
                    S.op("act", lambda: nc.scalar.copy(acT[:], ps[5][:, 0:256].rearrange("p (j t) -> p j t", j=2)), r=[PSN[5]], w=["acT"])
                    for half in range(2):
                        hs = slice(half * 512, (half + 1) * 512)
                        for jc in range(2):
                            S.op("pe", lambda: nc.tensor.matmul(ps[6 + half][:], acT[:, jc, :], shd[:, jc, hs], start=(jc == 0), stop=(jc == 1)), r=["acT", "shd"], w=[PSN[6 + half]])
                        S.op("dve", lambda: nc.vector.tensor_tensor(acc[b][:, hs], ps[6 + half][:], g2t[:, hs], ALU.mult), r=[PSN[6 + half], "g2t"], w=["acc%d" % b])
                        S.op("pool", lambda: nc.gpsimd.tensor_tensor(acc[b][:, hs], acc[b][:, hs], xt2[b][:, hs], ALU.add), r=["acc%d" % b, "xt2_%d" % b], w=["acc%d" % b])
                    S.op("poolq", lambda: nc.gpsimd.dma_start(out=OUTACC[tsl, :], in_=acc[b][:]), r=["acc%d" % b], w=["OUTACC"])
                for eh in range(2):
                    for rnd in range(CAP // 8):
                        S.op("dve", lambda: nc.vector.max(out=keys[:, eh, rnd * 8:(rnd + 1) * 8], in_=KT[:, eh, :]), r=["KT"], w=["keys"])
                        S.op("dve", lambda: nc.vector.match_replace(out=KT[:, eh, :], in_to_replace=keys[:, eh, rnd * 8:(rnd + 1) * 8],
                                                                    in_values=KT[:, eh, :], imm_value=0.0), r=["KT", "keys"], w=["KT"])
                zk = sbG("zk", [128, 2, CAP])
                S.op("dve", lambda: nc.vector.tensor_scalar(zk[:], keys[:], 0.0, float(T + 1), ALU.is_equal, ALU.mult), r=["keys"], w=["zk"])
                S.op("dve", lambda: nc.vector.scalar_tensor_tensor(keys[:], keys[:], -1.0, zk[:], ALU.add, ALU.add), r=["keys", "zk"], w=["keys"])
                idxT = sbG("idxT", [128, NST, NE])
                for shf in range(NST):
                    for eh in range(2):
                        S.op("pe", lambda: nc.tensor.transpose(ps[0][:, eh * 128:(eh + 1) * 128], keys[:, eh, shf * 128:(shf + 1) * 128], ident[:]), r=["keys", "ident"], w=[PSN[0]])
                    S.op("act", lambda: nc.scalar.copy(idxT[:, shf, :], ps[0][:, 0:NE]), r=[PSN[0]], w=["idxT"])
                S.op("dve", lambda: nc.vector.tensor_copy(idxI[:], idxT[:]), r=["idxT"], w=["idxI"])
                if DBG:
                    S.op("poolq", lambda: nc.gpsimd.dma_start(out=dbg_idx, in_=idxI[:]), r=["idxI"])
                S.barrier()
            with ExitStack() as esE:
                sbE = lambda name, shape, dt=F32: esE.enter_context(nc.sbuf_tensor(name, shape, dt))
                wgs = [sbE("wgs%d" % i, [128, 8, 512]) for i in range(2)]
                wgb = [sbE("wgb%d" % i, [128, 8, 512], BF16) for i in range(2)]
                wds = [sbE("wds%d" % i, [128, 2, D]) for i in range(2)]
                wdb = [sbE("wdb%d" % i, [128, 2, D], BF16) for i in range(2)]
                Xg = [sbE("Xg%d" % i, [128, D + NE]) for i in range(4)]
                XgT = [sbE("XgT%d" % i, [128, 8, 128], BF16) for i in range(2)]
                sge = [sbE("sge%d" % i, [128, 256]) for i in range(2)]
                ace = [sbE("ace%d" % i, [128, 256]) for i in range(2)]
                aeT = [sbE("aeT%d" % i, [128, 2, 128], BF16) for i in range(2)]
                yo = [sbE("yo%d" % i, [128, D]) for i in range(3)]
                tiles = [(e, shf) for e in range(N_EXP_RUN) for shf in range(NST)]
                NTL = len(tiles)

                def load_w(e):
                    wb = e % 2
                    S.op("sp", lambda: nc.sync.dma_start(out=wgs[wb][:, :, 0:256], in_=exp_w_gate[e].rearrange("(kc p) n -> p kc n", p=128)), w=["wgs%d" % wb])
                    S.op("sp", lambda: nc.sync.dma_start(out=wgs[wb][:, :, 256:512], in_=exp_w_up[e].rearrange("(kc p) n -> p kc n", p=128)), w=["wgs%d" % wb])
                    S.op("sp", lambda: nc.sync.dma_start(out=wds[wb][:], in_=exp_w_down[e].rearrange("(jc p) n -> p jc n", p=128)), w=["wds%d" % wb])

                def cast_w(e):
                    wb = e % 2
                    S.op("dve", lambda: nc.vector.tensor_copy(wgb[wb][:], wgs[wb][:]), r=["wgs%d" % wb], w=["wgb%d" % wb])
                    S.op("act", lambda: nc.scalar.copy(wdb[wb][:], wds[wb][:]), r=["wds%d" % wb], w=["wdb%d" % wb])

                def gather(k):
                    e, shf = tiles[k]
                    xb = k % 4
                    S.op("poolq", lambda: nc.gpsimd.indirect_dma_start(out=Xg[xb][:, :], out_offset=None, in_=H2E[:, :],
                                                                       in_offset=bass.IndirectOffsetOnAxis(ap=idxI[:, shf, e:e + 1], axis=0)),
                         r=["H2E", "idxI"], w=["Xg%d" % xb])

                def stageA(k):
                    xb, tb = k % 4, k % 2
                    for half in range(2):
                        for q in range(4):
                            kc = half * 4 + q
                            S.op("pe", lambda: nc.tensor.transpose(ps[half][:, q * 128:(q + 1) * 128], Xg[xb][:, kc * 128:(kc + 1) * 128], ident[:]),
                                 r=["Xg%d" % xb, "ident"], w=[PSN[half]])
                        S.op("act", lambda: nc.scalar.copy(XgT[tb][:, half * 4:(half + 1) * 4, :], ps[half][:].rearrange("p (q t) -> p q t", q=4)),
                             r=[PSN[half]], w=["XgT%d" % tb])

                def stageB(k):
                    e, shf = tiles[k]
                    wb, tb, gb = e % 2, k % 2, 2 + k % 2
                    for kc in range(8):
                        S.op("pe", lambda: nc.tensor.matmul(ps[gb][:], XgT[tb][:, kc, :], wgb[wb][:, kc, :], start=(kc == 0), stop=(kc == 7)),
                             r=["XgT%d" % tb, "wgb%d" % wb], w=[PSN[gb]])
                    S.op("act", lambda: nc.scalar.activation(out=sge[tb][:], in_=ps[gb][:, 0:256], func=AF.Silu), r=[PSN[gb]], w=["sge%d" % tb])
                    S.op("dve", lambda: nc.vector.tensor_tensor(ace[tb][:], sge[tb][:], ps[gb][:, 256:512], ALU.mult), r=["sge%d" % tb, PSN[gb]], w=["ace%d" % tb])

                def stageC(k):
                    tb = k % 2
                    for jc in range(2):
                        S.op("pe", lambda: nc.tensor.transpose(ps[4][:, jc * 128:(jc + 1) * 128], ace[tb][:, jc * 128:(jc + 1) * 128], ident[:]),
                             r=["ace%d" % tb, "ident"], w=[PSN[4]])
                    S.op("act", lambda: nc.scalar.copy(aeT[tb][:], ps[4][:, 0:256].rearrange("p (j t) -> p j t", j=2)), r=[PSN[4]], w=["aeT%d" % tb])

                def stageD(k):
                    e, shf = tiles[k]
                    wb, tb, xb, yb = e % 2, k % 2, k % 4, k % 3
                    for half in range(2):
                        hs = slice(half * 512, (half + 1) * 512)
                        for jc in range(2):
                            S.op("pe", lambda: nc.tensor.matmul(ps[6 + half][:], aeT[tb][:, jc, :], wdb[wb][:, jc, hs], start=(jc == 0), stop=(jc == 1)),
                                 r=["aeT%d" % tb, "wdb%d" % wb], w=[PSN[6 + half]])
                        S.op("dve", lambda: nc.vector.scalar_tensor_tensor(yo[yb][:, hs], ps[6 + half][:], Xg[xb][:, D + e:D + e + 1], g2t[:, hs], ALU.mult, ALU.mult),
                             r=[PSN[6 + half], "Xg%d" % xb, "g2t"], w=["yo%d" % yb])
                    S.op("poolq", lambda: nc.gpsimd.indirect_dma_start(out=OUTACC[:, :], out_offset=bass.IndirectOffsetOnAxis(ap=idxI[:, shf, e:e + 1], axis=0),
                                                                       in_=yo[yb][:, :], in_offset=None, compute_op=ALU.add),
                         r=["yo%d" % yb, "idxI"], w=["OUTACC"])

                if NTL > 0:
                    load_w(0)
                    cast_w(0)
                    if N_EXP_RUN > 1:
                        load_w(1)
                    gather(0)
                    if NTL > 1:
                        gather(1)
                    stageA(0)
                    for k in range(NTL):
                        e, shf = tiles[k]
                        if k + 2 < NTL:
                            gather(k + 2)
                        if k + 1 < NTL:
                            stageA(k + 1)
                        stageB(k)
                        if k >= 1:
                            stageD(k - 1)
                        stageC(k)
                        if shf == NST - 1 and e + 1 < N_EXP_RUN:
                            cast_w(e + 1)
                            if e + 2 < N_EXP_RUN:
                                load_w(e + 2)
                    stageD(NTL - 1)
                S.barrier()

        S.barrier()
        gf = sb("gf", [128, D])
        ot = [sb("ot%d" % i, [128, D]) for i in range(2)]
        xt = [sb("xf%d" % i, [128, D]) for i in range(2)]
        sq = sb("sqf2", [128, D])
        ss = [sb("ssf%d" % i, [128, 1]) for i in range(2)]
        rs = [sb("rsf%d" % i, [128, 1]) for i in range(2)]

        def rstd_fin(b, src, srcname):
            S.op("act", lambda: nc.scalar.activation(out=sq[:], in_=src, func=AF.Square, accum_out=ss[b][:]),
                 r=[srcname], w=["sqf2", "ssf%d" % b])
            S.op("dve", lambda: nc.vector.tensor_scalar(rs[b][:], ss[b][:], 1.0 / D, NORM_EPS, ALU.mult, ALU.add),
                 r=["ssf%d" % b], w=["rsf%d" % b])
            S.op("act", lambda: nc.scalar.activation(out=rs[b][:], in_=rs[b][:], func=AF.Sqrt), r=["rsf%d" % b], w=["rsf%d" % b])
            S.op("dve", lambda: nc.vector.reciprocal(rs[b][:], rs[b][:]), r=["rsf%d" % b], w=["rsf%d" % b])

        S.op("sp", lambda: nc.sync.dma_start(out=gf[:], in_=bcast_rows(normf_g[0:1, :])), w=["gf"])
        for i in range(NT):
            b = i % 2
            S.op("sp", lambda: nc.sync.dma_start(out=xt[b][:], in_=OUTACC[i * 128:(i + 1) * 128, :]), r=["OUTACC"], w=["xf%d" % b])
            rstd_fin(b, xt[b][:], "xf%d" % b)
            S.op("dve", lambda: nc.vector.scalar_tensor_tensor(ot[b][:], xt[b][:], rs[b][:, 0:1], gf[:], ALU.mult, ALU.mult),
                 r=["xf%d" % b, "rsf%d" % b, "gf"], w=["ot%d" % b])
            S.op("poolq", lambda: nc.gpsimd.dma_start(out=out[i * 128:(i + 1) * 128, :], in_=ot[b][:]), r=["ot%d" % b])
        S.finish("pool")
    return nc


_NC_CACHE = {}


_CONST = {}


def _constants():
    if _CONST:
        return _CONST
    import ml_dtypes
    L = T
    N = 2 * T
    f32 = np.float32
    pos = np.arange(L, dtype=f32)
    t = pos / f32(L - 1)
    bands = np.linspace(1e-4, 15.0, 16, dtype=f32)
    ang = (f32(2.0 * np.pi / L) * pos[:, None]) * bands[None]
    feats = np.concatenate([t[:, None], np.cos(ang), -np.sin(ang)], axis=-1).astype(f32)
    deltas = np.abs(np.linspace(np.log(1e-2) / 1.5, np.log(1e-2) / 0.3, 512, dtype=f32))
    env = np.exp(-t[:, None] * deltas[None]).astype(f32)
    tt = np.arange(L, dtype=np.int64)
    ff = np.arange(L, dtype=np.int64)
    ph = (tt[:, None] * ff[None, :]) % N
    angm = ph.astype(np.float64) * (2.0 * np.pi / N)
    cosm = np.cos(angm)
    sinm = np.sin(angm)
    fw = np.empty((L, N), np.float32)
    fw[:, :L] = cosm
    fw[:, L:] = -sinm
    fw[:, L] = np.where(tt % 2 == 0, 1.0, -1.0)
    dfw = fw.reshape(NT, 128, 64, 128).transpose(2, 1, 0, 3)
    iv = np.empty((N, L), np.float32)
    iv[:L] = (2.0 / N) * cosm.T
    iv[0] = 1.0 / N
    iv[L:] = -(2.0 / N) * sinm.T
    iv[L] = np.where(tt % 2 == 0, 1.0, -1.0) / N
    dinv = iv.reshape(64, 128, 16, 256).transpose(2, 1, 0, 3)
    m48 = np.zeros((48, 512), np.float32)
    m40 = np.zeros((40, 512), np.float32)
    for base in (0, 8, 32, 40):
        for j in range(8):
            m48[base + j, j * 64:(j + 1) * 64] = 1.0
    for base in (0, 32):
        for j in range(8):
            m40[base + j, j * 64:(j + 1) * 64] = 1.0
    selF = np.zeros((128, 128, 48), np.float32)
    selB = np.zeros((128, 128, 48), np.float32)
    for tl in range(128):
        selF[tl, tl, 8:16] = 1.0
        selB[127 - tl, tl, 40:48] = 1.0
    _CONST.update({
        "m48": m48, "m40": m40,
        "selF": selF.astype(ml_dtypes.bfloat16), "selB": selB.astype(ml_dtypes.bfloat16),
        "featsT": np.ascontiguousarray(feats.T),
        "env": env,
        "dfw": np.ascontiguousarray(dfw).astype(ml_dtypes.bfloat16),
        "dinv": np.ascontiguousarray(dinv).astype(ml_dtypes.bfloat16),
    })
    return _CONST


def _f32(a):
    return np.ascontiguousarray(a, dtype=np.float32)


def make_in_maps(inputs, ncores=NCORES):
    x = _f32(inputs["x"])
    c = _f32(inputs["c"])
    shared = {
        "normf_g": _f32(inputs["normf_g"]).reshape(1, D),
        "norm1_g": _f32(inputs["norm1_g"]).reshape(1, D),
        "w_ada": _f32(inputs["w_ada"]).reshape(D, 6 * D),
        "b_ada": _f32(inputs["b_ada"]).reshape(1, 6 * D),
        "w_in": _f32(inputs["w_in"]).reshape(D, N_IN),
        "hy_conv_w": _f32(inputs["hy_conv_w"]).reshape(3, HY_COLS),
        "hy_conv_b": _f32(inputs["hy_conv_b"]).reshape(1, HY_COLS),
        "ident": np.eye(128, dtype=np.float32),
        "hy_pos_w1": _f32(inputs["hy_pos_w1"]).reshape(33, 64),
        "hy_pos_b1": _f32(inputs["hy_pos_b1"]).reshape(64, 1),
        "hy_pos_w2": _f32(inputs["hy_pos_w2"]).reshape(2, 64, 64),
        "hy_pos_b2": _f32(inputs["hy_pos_b2"]).reshape(2, 64, 1),
        "hy_pos_w3": _f32(inputs["hy_pos_w3"]).reshape(64, 2048),
        "hy_sin_freq": _f32(inputs["hy_sin_freq"]).reshape(64, 1),
        "hy_skip": _f32(inputs["hy_skip"]).reshape(1, 1024),
        "w_out": _f32(inputs["w_out"]).reshape(D, D),
        "norm2_g": _f32(inputs["norm2_g"]).reshape(1, D),
        "router_w": _f32(inputs["router_w"]).reshape(D, 256),
        "router_bias": _f32(inputs["router_bias"]).reshape(1, 256),
        "sh_w_gate": _f32(inputs["sh_w_gate"]).reshape(D, 256),
        "sh_w_up": _f32(inputs["sh_w_up"]).reshape(D, 256),
        "sh_w_down": _f32(inputs["sh_w_down"]).reshape(256, D),
        "exp_w_gate": _f32(inputs["exp_w_gate"]).reshape(256, D, 256),
        "exp_w_up": _f32(inputs["exp_w_up"]).reshape(256, D, 256),
        "exp_w_down": _f32(inputs["exp_w_down"]).reshape(256, 256, D),
        "tokid": (np.arange(T, dtype=np.float32).reshape(NT, 128).T + 1.0).copy(),
        "rw_mu": _f32(inputs["rw_mu"]).reshape(1, 1760),
        "rw_w0": _f32(inputs["rw_w0"]).reshape(2, 512),
        "rw_w_up": _f32(inputs["rw_w_up"]).reshape(2, 32, 512),
        "rw_a0": _f32(inputs["rw_a0"]).reshape(2, 512),
        "rw_a_up": _f32(inputs["rw_a_up"]).reshape(2, 32, 512),
        "rw_g_up": _f32(inputs["rw_g_up"]).reshape(96, 512),
        "rw_k_k": _f32(inputs["rw_k_k"]).reshape(1, 512),
        "rw_k_a": _f32(inputs["rw_k_a"]).reshape(1, 512),
        "rw_r_k": _f32(inputs["rw_r_k"]).reshape(1, 512),
        "rw_ln_w": _f32(inputs["rw_ln_w"]).reshape(1, 512),
        "rw_ln_b": _f32(inputs["rw_ln_b"]).reshape(1, 512),
        "blk": np.kron(np.eye(2, dtype=np.float32), np.ones((64, 64), np.float32)),
    }
    shared.update(_constants())
    in_maps = []
    for cidx in range(ncores):
        m = dict(shared)
        m["x"] = x[cidx]
        m["c"] = c[cidx:cidx + 1]
        in_maps.append(m)
    return in_maps


def kernel(**inputs):
    if "nc" not in _NC_CACHE:
        _NC_CACHE["nc"] = build_nc()
    nc = _NC_CACHE["nc"]
    in_maps = make_in_maps(inputs)
    res = run_bass_kernel_spmd(nc, in_maps, core_ids=list(range(NCORES)))
    return np.stack([res.results[c]["out"] for c in range(NCORES)], axis=0)
```

```python
import numpy as np
from contextlib import ExitStack
import concourse.bass as bass
import concourse.mybir as mybir
from concourse.bass_utils import run_bass_kernel_spmd

F32 = mybir.dt.float32
BF16 = mybir.dt.bfloat16
I32 = mybir.dt.int32
ALU = mybir.AluOpType
AF = mybir.ActivationFunctionType
AX = mybir.AxisListType

NCORES = 8
T = 4096
D = 1024
NT = T // 128
NORM_EPS = 1e-6


class Sched:
    NDQ = 4

    def __init__(self, nc, es):
        self.nc = nc
        self.eng = {"pe": nc.tensor, "dve": nc.vector, "act": nc.scalar, "pool": nc.gpsimd}
        self.inc = {"pe": 1, "dve": 1, "act": 1, "pool": 1}
        self.stream = {"pe": "pe", "dve": "dve", "act": "act", "pool": "pool"}
        for base, eng, st in (("sp", nc.sync, "sp"), ("poolq", nc.gpsimd, "pool")):
            for i in range(self.NDQ):
                k = "%s%d" % (base, i)
                self.eng[k] = eng
                self.inc[k] = 16
                self.stream[k] = st
        self.sem = {k: es.enter_context(nc.semaphore("sem_" + k)) for k in self.eng}
        self.cnt = {k: 0 for k in self.eng}
        self.rr = {"sp": 0, "poolq": 0}
        self.last_w = {}
        self.readers = {}
        self.seen = {s: {} for s in ("pe", "dve", "act", "pool", "sp")}
        self.stream_eng = {"pe": nc.tensor, "dve": nc.vector, "act": nc.scalar, "pool": nc.gpsimd, "sp": nc.sync}

    def _wait(self, st, pq, seq):
        if self.seen[st].get(pq, 0) < seq:
            self.stream_eng[st].wait_ge(self.sem[pq], seq * self.inc[pq])
            self.seen[st][pq] = seq

    def op(self, q, fn, r=(), w=()):
        if q in self.rr:
            i = self.rr[q]
            self.rr[q] = (i + 1) % self.NDQ
            q = "%s%d" % (q, i)
        need = {}
        for b in r:
            for pq, seq in self.last_w.get(b, {}).items():
                need[pq] = max(need.get(pq, 0), seq)
        for b in w:
            for pq, seq in self.last_w.get(b, {}).items():
                need[pq] = max(need.get(pq, 0), seq)
            for pq, seq in self.readers.get(b, ()):
                need[pq] = max(need.get(pq, 0), seq)
        st = self.stream[q]
        if self.inc[q] == 16 and self.cnt[q] > 0:
            need[q] = max(need.get(q, 0), self.cnt[q])
        pending = [(pq, seq) for pq, seq in need.items() if self.seen[st].get(pq, 0) < seq]
        embed = None
        if pending and self.inc[q] == 1:
            embed = pending.pop()
        for pq, seq in pending:
            self._wait(st, pq, seq)
        ins = fn()
        if embed is not None:
            ins._wait_ge(self.sem[embed[0]], embed[1] * self.inc[embed[0]])
            self.seen[st][embed[0]] = embed[1]
        self.cnt[q] += 1
        seq = self.cnt[q]
        ins.then_inc(self.sem[q], self.inc[q])
        for b in w:
            self.last_w.setdefault(b, {})[q] = seq
            self.readers[b] = []
        for b in r:
            lst = self.readers.setdefault(b, [])
            lst.append((q, seq))
            if len(lst) > 16:
                best = {}
                for pq, s_ in lst:
                    best[pq] = max(best.get(pq, 0), s_)
                self.readers[b] = list(best.items())
        return ins

    def barrier(self):
        for st in ("pe", "dve", "act", "pool", "sp"):
            for k in self.eng:
                if self.cnt[k] > 0 and not (k == st and self.inc[k] == 1):
                    self._wait(st, k, self.cnt[k])

    def finish(self, q="pool"):
        for k in self.eng:
            if self.cnt[k] > 0:
                self.stream_eng[q].wait_ge(self.sem[k], self.cnt[k] * self.inc[k])


def bcast_rows(ap_row, parts=128):
    return ap_row.to_broadcast([parts, ap_row.shape[-1]])


HY_COLS = 1536
N_IN = 3296
DBG = False
SCAN_CHUNKS = 64
N_EXP_RUN = 256


def build_nc():
    nc = bass.Bass("TRN2", target_bir_lowering=False)
    dt_in = lambda name, shape, dt=F32: nc.dram_tensor(name, shape, dt, kind="ExternalInput").ap()
    x = dt_in("x", [T, D])
    c_in = dt_in("c", [1, D])
    normf_g = dt_in("normf_g", [1, D])
    norm1_g = dt_in("norm1_g", [1, D])
    w_ada = dt_in("w_ada", [D, 6 * D])
    b_ada = dt_in("b_ada", [1, 6 * D])
    w_in = dt_in("w_in", [D, N_IN])
    hy_conv_w = dt_in("hy_conv_w", [3, HY_COLS])
    hy_conv_b = dt_in("hy_conv_b", [1, HY_COLS])
    ident_in = dt_in("ident", [128, 128])
    featsT = dt_in("featsT", [33, T])
    env_in = dt_in("env", [T, 512])
    pw1 = dt_in("hy_pos_w1", [33, 64])
    pb1 = dt_in("hy_pos_b1", [64, 1])
    pw2 = dt_in("hy_pos_w2", [2, 64, 64])
    pb2 = dt_in("hy_pos_b2", [2, 64, 1])
    pw3 = dt_in("hy_pos_w3", [64, 2048])
    sfreq = dt_in("hy_sin_freq", [64, 1])
    hskip = dt_in("hy_skip", [1, 1024])
    dfw = dt_in("dfw", [64, 128, 32, 128], BF16)
    dinv = dt_in("dinv", [16, 128, 64, 256], BF16)
    KF = nc.dram_tensor("KF", [64, 128, 1024], F32, kind="Internal").ap()
    UD = nc.dram_tensor("UD", [12, 128, T], F32, kind="Internal").ap()
    ZD = nc.dram_tensor("ZD", [4, 128, T], F32, kind="Internal").ap()
    YD = nc.dram_tensor("YD", [4, 128, T], F32, kind="Internal").ap()
    MODD = nc.dram_tensor("MODD", [128, 6 * D], F32, kind="Internal").ap()
    rw_mu = dt_in("rw_mu", [1, 1760])
    rw_w0 = dt_in("rw_w0", [2, 512])
    rw_w_up = dt_in("rw_w_up", [2, 32, 512])
    rw_a0 = dt_in("rw_a0", [2, 512])
    rw_a_up = dt_in("rw_a_up", [2, 32, 512])
    rw_g_up = dt_in("rw_g_up", [96, 512])
    rw_k_k = dt_in("rw_k_k", [1, 512])
    rw_k_a = dt_in("rw_k_a", [1, 512])
    rw_r_k = dt_in("rw_r_k", [1, 512])
    rw_ln_w = dt_in("rw_ln_w", [1, 512])
    rw_ln_b = dt_in("rw_ln_b", [1, 512])
    blk_in = dt_in("blk", [128, 128])
    m48_in = dt_in("m48", [48, 512])
    m40_in = dt_in("m40", [40, 512])
    selF_in = dt_in("selF", [128, 128, 48], BF16)
    selB_in = dt_in("selB", [128, 128, 48], BF16)
    scr = lambda name, shape, dt=F32: nc.dram_tensor(name, shape, dt, kind="Internal").ap()
    KKN = scr("KKN", [512, T], BF16)
    RR = scr("RR", [512, T], BF16)
    DEC = scr("DEC", [2, 512, T])
    BT = scr("BT", [2, 2, T, 512], BF16)
    VT = scr("VT", [T, 512], BF16)
    BON = scr("BON", [512, T])
    GG = scr("GG", [512, T])
    OD = scr("OD", [2, T, 512])
    X1D = scr("X1D", [T, D])
    H2E = scr("H2E", [T + 1, D + 256])
    OUTACC = scr("OUTACC", [T + 1, D])
    norm2_g = dt_in("norm2_g", [1, D])
    router_w = dt_in("router_w", [D, 256])
    router_bias = dt_in("router_bias", [1, 256])
    sh_w_gate = dt_in("sh_w_gate", [D, 256])
    sh_w_up = dt_in("sh_w_up", [D, 256])
    sh_w_down = dt_in("sh_w_down", [256, D])
    exp_w_gate = dt_in("exp_w_gate", [256, D, 256])
    exp_w_up = dt_in("exp_w_up", [256, D, 256])
    exp_w_down = dt_in("exp_w_down", [256, 256, D])
    tokid_in = dt_in("tokid", [128, NT])
    w_out = dt_in("w_out", [D, D])
    out = nc.dram_tensor("out", [T, D], F32, kind="ExternalOutput").ap()
    if DBG:
        dbg_mod = nc.dram_tensor("dbg_mod", [128, 6 * D], F32, kind="ExternalOutput").ap()
        dbg_u = nc.dram_tensor("dbg_u", [12, 128, T], F32, kind="ExternalOutput").ap()
        dbg_kf = nc.dram_tensor("dbg_kf", [64, 128, 1024], F32, kind="ExternalOutput").ap()
        dbg_y = nc.dram_tensor("dbg_y", [4, 128, T], F32, kind="ExternalOutput").ap()
        dbg_od = nc.dram_tensor("dbg_od", [2, T, 512], F32, kind="ExternalOutput").ap()
        dbg_x1 = nc.dram_tensor("dbg_x1", [T, D], F32, kind="ExternalOutput").ap()
        dbg_idx = nc.dram_tensor("dbg_idx", [128, 8, 256], I32, kind="ExternalOutput").ap()
        dbg_rw = {
            "KKN": nc.dram_tensor("dbg_KKN", [512, T], BF16, kind="ExternalOutput").ap(),
            "RR": nc.dram_tensor("dbg_RR", [512, T], BF16, kind="ExternalOutput").ap(),
            "DEC": nc.dram_tensor("dbg_DEC", [2, 512, T], F32, kind="ExternalOutput").ap(),
            "BT": nc.dram_tensor("dbg_BT", [2, 2, T, 512], BF16, kind="ExternalOutput").ap(),
            "VT": nc.dram_tensor("dbg_VT", [T, 512], BF16, kind="ExternalOutput").ap(),
            "BON": nc.dram_tensor("dbg_BON", [512, T], F32, kind="ExternalOutput").ap(),
            "GG": nc.dram_tensor("dbg_GG", [512, T], F32, kind="ExternalOutput").ap(),
        }

    with ExitStack() as es:
        es.enter_context(nc.allow_low_precision("bf16 matmul operands, fp32 accumulation"))
        es.enter_context(nc.allow_non_contiguous_dma("small strided parameter loads"))
        S = Sched(nc, es)
        sb = lambda name, shape, dt=F32: es.enter_context(nc.sbuf_tensor(name, shape, dt))
        ps = [es.enter_context(nc.psum_tensor("ps%d" % i, [128, 512], F32)) for i in range(8)]
        PSN = ["ps%d" % i for i in range(8)]

        ident = sb("ident_sb", [128, 128])
        S.op("sp", lambda: nc.sync.dma_start(out=ident[:], in_=ident_in[:, :]), w=["ident"])


        TWO_PI = 6.283185307179586
        MAGIC = 12582912.0
        with ExitStack() as es1:
            sb1 = lambda name, shape, dt=F32: es1.enter_context(nc.sbuf_tensor(name, shape, dt))
            w1 = sb1("w1", [33, 64])
            w2 = sb1("w2", [64, 2, 64])
            w3 = sb1("w3", [64, 2048])
            fr = sb1("fr", [64, 1])
            fb = sb1("fb", [64, 3])
            ones = sb1("ones", [128, 128])
            skp = sb1("skp", [128, 1024])
            hd0 = sb1("hd0", [64, T])
            es1a = ExitStack()
            sb1a = lambda name, shape, dt=F32: es1a.enter_context(nc.sbuf_tensor(name, shape, dt))
            ft = sb1a("ft", [33, T])
            hd = [hd0, sb1a("hd1", [64, T])]
            rr = sb1a("rr", [64, 512])
            S.op("sp", lambda: nc.sync.dma_start(out=ft[:], in_=featsT[:, :]), w=["ft"])
            S.op("sp", lambda: nc.sync.dma_start(out=w1[:], in_=pw1[:, :]), w=["w1"])
            S.op("sp", lambda: nc.sync.dma_start(out=w2[:], in_=pw2.rearrange("i k m -> k i m")), w=["w2"])
            S.op("sp", lambda: nc.sync.dma_start(out=w3[:], in_=pw3[:, :]), w=["w3"])
            S.op("sp", lambda: nc.sync.dma_start(out=fr[:], in_=sfreq[:, :]), w=["fr"])
            S.op("sp", lambda: nc.sync.dma_start(out=fb[:, 0:1], in_=pb1[:, :]), w=["fb"])
            S.op("sp", lambda: nc.sync.dma_start(out=fb[:, 1:2], in_=pb2[0]), w=["fb"])
            S.op("sp", lambda: nc.sync.dma_start(out=fb[:, 2:3], in_=pb2[1]), w=["fb"])
            S.op("sp", lambda: nc.sync.dma_start(out=skp[:], in_=bcast_rows(hskip[0:1, :])), w=["skp"])
            S.op("dve", lambda: nc.vector.tensor_scalar(fb[:], fb[:], fr[:, 0:1], None, ALU.mult), r=["fb", "fr"], w=["fb"])
            S.op("pool", lambda: nc.gpsimd.memset(ones[:], 1.0), w=["ones"])
            for layer in range(3):
                src = ft if layer == 0 else hd[(layer - 1) % 2]
                srcn = "ft" if layer == 0 else "hd%d" % ((layer - 1) % 2)
                dst = hd[layer % 2]
                dstn = "hd%d" % (layer % 2)
                lw = w1[:, :] if layer == 0 else w2[:, layer - 1, :]
                lwn = "w1" if layer == 0 else "w2"
                for tcn in range(8):
                    pb = tcn % 2
                    sl = slice(tcn * 512, (tcn + 1) * 512)
                    S.op("pe", lambda: nc.tensor.matmul(ps[pb][0:64, :], lw, src[:, sl], start=True, stop=True),
                         r=[lwn, srcn], w=[PSN[pb]])
                    S.op("dve", lambda: nc.vector.tensor_scalar(dst[:, sl], ps[pb][0:64, :], fr[:, 0:1], fb[:, layer:layer + 1], ALU.mult, ALU.add),
                         r=[PSN[pb], "fr", "fb"], w=[dstn])
                    S.op("dve", lambda: nc.vector.tensor_scalar(rr[:], dst[:, sl], 1.0 / TWO_PI, MAGIC, ALU.mult, ALU.add), r=[dstn], w=["rr"])
                    S.op("dve", lambda: nc.vector.tensor_scalar(rr[:], rr[:], -MAGIC, -TWO_PI, ALU.add, ALU.mult), r=["rr"], w=["rr"])
                    S.op("dve", lambda: nc.vector.tensor_tensor(dst[:, sl], dst[:, sl], rr[:], ALU.add), r=[dstn, "rr"], w=[dstn])
                    S.op("dve", lambda: nc.vector.tensor_scalar(dst[:, sl], dst[:, sl], 3.14159, -3.14159, ALU.min, ALU.max), r=[dstn], w=[dstn])
                    S.op("act", lambda: nc.scalar.activation(out=dst[:, sl], in_=dst[:, sl], func=AF.Sin), r=[dstn], w=[dstn])
            S.barrier()
            es1a.close()
            envt = [sb1("envt%d" % i, [128, 512]) for i in range(2)]
            fw = [sb1("fw%d" % i, [128, 512]) for i in range(2)]
            bw = [sb1("bw%d" % i, [128, 512]) for i in range(2)]
            sqf = sb1("sqf", [128, 512])
            sqb = sb1("sqb", [128, 512])
            Eb = sb1("Eb", [128, NT, 1024], BF16)
            Ob = sb1("Ob", [128, NT, 1024], BF16)
            scl = sb1("scl", [128, 1024])
            dch = [sb1("dch%d" % i, [128, 32, 128], BF16) for i in range(2)]
            kst = [sb1("kst%d" % i, [128, 1024]) for i in range(2)]
            knq = sb1("knq", [128, 1024])
            hfin = hd[0]
            for i in range(NT):
                b = i % 2
                S.op("sp", lambda: nc.sync.dma_start(out=envt[b][:], in_=env_in[i * 128:(i + 1) * 128, :]), w=["envt%d" % b])
                for n in range(2):
                    for d in range(2):
                        pb = d
                        col = n * 1024 + d * 512
                        S.op("pe", lambda: nc.tensor.matmul(ps[pb][:], hfin[:, i * 128:(i + 1) * 128], w3[:, col:col + 512], start=True, stop=True),
                             r=["hd0", "w3"], w=[PSN[pb]])
                    S.op("dve", lambda: nc.vector.tensor_tensor(fw[n][:], ps[0][:], envt[b][:], ALU.mult), r=[PSN[0], "envt%d" % b], w=["fw%d" % n])
                    S.op("dve", lambda: nc.vector.tensor_tensor(bw[n][:], ps[1][:], envt[b][:], ALU.mult), r=[PSN[1], "envt%d" % b], w=["bw%d" % n])
                    if i == 0:
                        S.op("dve", lambda: nc.vector.memset(bw[n][0:1, :], 0.0), w=["bw%d" % n])
                    S.op("pool", lambda: nc.gpsimd.tensor_tensor(Eb[:, i, n * 512:(n + 1) * 512], fw[n][:], bw[n][:], ALU.add),
                         r=["fw%d" % n, "bw%d" % n], w=["Eb"])
                    S.op("pool", lambda: nc.gpsimd.tensor_tensor(Ob[:, i, n * 512:(n + 1) * 512], fw[n][:], bw[n][:], ALU.subtract),
                         r=["fw%d" % n, "bw%d" % n], w=["Ob"])
                    S.op("act", lambda: nc.scalar.activation(out=sqf[:], in_=fw[n][:], func=AF.Square), r=["fw%d" % n], w=["sqf"])
                    S.op("act", lambda: nc.scalar.activation(out=sqb[:], in_=bw[n][:], func=AF.Square), r=["bw%d" % n], w=["sqb"])
                    S.op("pe", lambda: nc.tensor.matmul(ps[2 + n][:], ones[:], sqf[:], start=(i == 0), stop=False), r=["ones", "sqf"], w=[PSN[2 + n]])
                    S.op("pe", lambda: nc.tensor.matmul(ps[2 + n][:], ones[:], sqb[:], start=False, stop=(i == NT - 1)), r=["ones", "sqb"], w=[PSN[2 + n]])
            for n in range(2):
                S.op("dve", lambda: nc.vector.tensor_scalar(scl[:, n * 512:(n + 1) * 512], ps[2 + n][:], 1e-6, None, ALU.add), r=[PSN[2 + n]], w=["scl"])
            S.op("act", lambda: nc.scalar.activation(out=scl[:], in_=scl[:], func=AF.Sqrt), r=["scl"], w=["scl"])
            S.op("dve", lambda: nc.vector.reciprocal(scl[:], scl[:]), r=["scl"], w=["scl"])
            for rc in range(64):
                b = rc % 2
                S.op("sp", lambda: nc.sync.dma_start(out=dch[b][:], in_=dfw[rc]), w=["dch%d" % b])
                src, srcn = (Eb, "Eb") if rc < 32 else (Ob, "Ob")
                for n in range(2):
                    for tcn in range(NT):
                        S.op("pe", lambda: nc.tensor.matmul(ps[4 + n][:], dch[b][:, tcn, :], src[:, tcn, n * 512:(n + 1) * 512],
                                                            start=(tcn == 0), stop=(tcn == NT - 1)),
                             r=["dch%d" % b, srcn], w=[PSN[4 + n]])
                    S.op("dve", lambda: nc.vector.tensor_tensor(kst[b][:, n * 512:(n + 1) * 512], ps[4 + n][:], scl[:, n * 512:(n + 1) * 512], ALU.mult),
                         r=[PSN[4 + n], "scl"], w=["kst%d" % b])
                if rc < 32:
                    S.op("pool", lambda: nc.gpsimd.tensor_tensor(kst[b][:], kst[b][:], skp[:], ALU.add), r=["kst%d" % b, "skp"], w=["kst%d" % b])
                if rc == 32:
                    for n in range(2):
                        for tcn in range(NT):
                            S.op("pe", lambda: nc.tensor.matmul(ps[6 + n][:], dch[b][:, tcn, :], Eb[:, tcn, n * 512:(n + 1) * 512],
                                                                start=(tcn == 0), stop=(tcn == NT - 1)),
                                 r=["dch%d" % b, "Eb"], w=[PSN[6 + n]])
                        S.op("dve", lambda: nc.vector.tensor_tensor(knq[:, n * 512:(n + 1) * 512], ps[6 + n][:], scl[:, n * 512:(n + 1) * 512], ALU.mult),
                             r=[PSN[6 + n], "scl"], w=["knq"])
                    S.op("dve", lambda: nc.vector.tensor_tensor(kst[b][0:1, :], knq[0:1, :], skp[0:1, :], ALU.add), r=["knq", "skp", "kst%d" % b], w=["kst%d" % b])
                S.op("poolq", lambda: nc.gpsimd.dma_start(out=KF[rc], in_=kst[b][:]), r=["kst%d" % b], w=["KF"])
            if DBG:
                S.op("poolq", lambda: nc.gpsimd.dma_start(out=dbg_kf[:, :, :], in_=KF[:, :, :]), r=["KF"])

        S.barrier()
        with ExitStack() as es1:
            sb1 = lambda name, shape, dt=F32: es1.enter_context(nc.sbuf_tensor(name, shape, dt))
            mod = sb1("mod", [128, 6 * D])
            cs = sb1("cs", [128, 8])
            crep = sb1("crep", [128, 8, 128])
            wa = [sb1("wa%d" % i, [128, 3 * D]) for i in range(2)]
            S.op("sp", lambda: nc.sync.dma_start(out=cs[:], in_=c_in.rearrange("o (kc p) -> p (o kc)", p=128)), w=["cs"])
            S.op("sp", lambda: nc.sync.dma_start(out=mod[:], in_=bcast_rows(b_ada[0:1, :])), w=["mod"])
            S.op("act", lambda: nc.scalar.activation(out=cs[:], in_=cs[:], func=AF.Silu), r=["cs"], w=["cs"])
            for kc in range(8):
                S.op("dve", lambda: nc.vector.tensor_copy(crep[:, kc, :], cs[:, kc:kc + 1].to_broadcast([128, 128])),
                     r=["cs"], w=["crep"])
            n = 0
            for half in range(2):
                for kc in range(8):
                    b = n % 2
                    n += 1
                    S.op("sp", lambda: nc.sync.dma_start(out=wa[b][:], in_=w_ada[kc * 128:(kc + 1) * 128, half * 3 * D:(half + 1) * 3 * D]),
                         w=["wa%d" % b])
                    for j in range(6):
                        S.op("pe", lambda: nc.tensor.matmul(ps[j][:], crep[:, kc, :], wa[b][:, j * 512:(j + 1) * 512],
                                                            start=(kc == 0), stop=(kc == 7)),
                             r=["crep", "wa%d" % b], w=[PSN[j]])
                for j in range(6):
                    col = half * 3 * D + j * 512
                    S.op("dve", lambda: nc.vector.tensor_tensor(mod[:, col:col + 512], mod[:, col:col + 512], ps[j][:], ALU.add),
                         r=[PSN[j], "mod"], w=["mod"])
            S.op("poolq", lambda: nc.gpsimd.dma_start(out=MODD[:, :], in_=mod[:]), r=["mod"], w=["MODD"])
            if DBG:
                S.op("poolq", lambda: nc.gpsimd.dma_start(out=dbg_mod[:, :], in_=mod[:]), r=["mod"])
        S.barrier()

        def rstd_of(sq, ss, rs, nm, src, srcname):
            S.op("act", lambda: nc.scalar.activation(out=sq[:], in_=src, func=AF.Square, accum_out=ss[:]),
                 r=[srcname], w=["sq" + nm, "ss" + nm])
            S.op("dve", lambda: nc.vector.tensor_scalar(rs[:], ss[:], 1.0 / D, NORM_EPS, ALU.mult, ALU.add),
                 r=["ss" + nm], w=["rs" + nm])
            S.op("act", lambda: nc.scalar.activation(out=rs[:], in_=rs[:], func=AF.Sqrt), r=["rs" + nm], w=["rs" + nm])
            S.op("dve", lambda: nc.vector.reciprocal(rs[:], rs[:]), r=["rs" + nm], w=["rs" + nm])

        es2 = ExitStack()
        sb2 = lambda name, shape, dt=F32: es2.enter_context(nc.sbuf_tensor(name, shape, dt))
        with es2:
            hT = sb2("hT", [128, 8, T + 2], BF16)
            S.op("pool", lambda: nc.gpsimd.memset(hT[:, :, 0:1], 0.0), w=["hT"])
            S.op("pool", lambda: nc.gpsimd.memset(hT[:, :, T + 1:T + 2], 0.0), w=["hT"])
            with ExitStack() as esN:
                sbN = lambda name, shape, dt=F32: esN.enter_context(nc.sbuf_tensor(name, shape, dt))
                gm1 = sbN("gm1", [128, D])
                sc1 = sbN("sc1", [128, D])
                sh1 = sbN("sh1", [128, D])
                S.op("sp", lambda: nc.sync.dma_start(out=gm1[:], in_=bcast_rows(norm1_g[0:1, :])), w=["gm1"])
                S.op("sp", lambda: nc.sync.dma_start(out=sh1[:], in_=MODD[:, 0:D]), r=["MODD"], w=["sh1"])
                S.op("sp", lambda: nc.sync.dma_start(out=sc1[:], in_=MODD[:, D:2 * D]), r=["MODD"], w=["sc1"])
                S.op("dve", lambda: nc.vector.scalar_tensor_tensor(gm1[:], sc1[:], 1.0, gm1[:], ALU.add, ALU.mult), r=["sc1", "gm1"], w=["gm1"])
                xt = [sbN("xt%d" % i, [128, D]) for i in range(2)]
                hh = [sbN("hh%d" % i, [128, D]) for i in range(2)]
                sq = sbN("sq", [128, D])
                ss = [sbN("ss%d" % i, [128, 1]) for i in range(2)]
                rs = [sbN("rs%d" % i, [128, 1]) for i in range(2)]
                for i in range(NT):
                    b = i % 2
                    S.op("sp", lambda: nc.sync.dma_start(out=xt[b][:], in_=x[i * 128:(i + 1) * 128, :]), w=["xt%d" % b])
                    rstd_of(sq, ss[b], rs[b], "n1_%d" % b, xt[b][:], "xt%d" % b)
                    S.op("dve", lambda: nc.vector.scalar_tensor_tensor(hh[b][:], xt[b][:], rs[b][:, 0:1], gm1[:], ALU.mult, ALU.mult),
                         r=["xt%d" % b, "rsn1_%d" % b, "gm1"], w=["hh%d" % b])
                    S.op("pool", lambda: nc.gpsimd.tensor_tensor(hh[b][:], hh[b][:], sh1[:], ALU.add), r=["hh%d" % b, "sh1"], w=["hh%d" % b])
                    for half in range(2):
                        pb = 6 + half
                        for q in range(4):
                            kc = half * 4 + q
                            S.op("pe", lambda: nc.tensor.transpose(ps[pb][:, q * 128:(q + 1) * 128], hh[b][:, kc * 128:(kc + 1) * 128], ident[:]),
                                 r=["hh%d" % b, "ident"], w=[PSN[pb]])
                        S.op("act", lambda: nc.scalar.copy(hT[:, half * 4:(half + 1) * 4, 1 + i * 128:1 + (i + 1) * 128],
                                                           ps[pb][:].rearrange("p (q t) -> p q t", q=4)),
                             r=[PSN[pb]], w=["hT"])
                S.barrier()

            wst = [sb2("wst%d" % i, [128, 8, 128]) for i in range(2)]
            wbf = [sb2("wbf%d" % i, [128, 8, 128], BF16) for i in range(2)]
            pfm = [sb2("pfm%d" % i, [128, T + 2]) for i in range(3)]
            for bb in range(3):
                S.op("pool", lambda: nc.gpsimd.memset(pfm[bb][:, 0:1], 0.0), w=["pfm%d" % bb])
                S.op("pool", lambda: nc.gpsimd.memset(pfm[bb][:, T + 1:T + 2], 0.0), w=["pfm%d" % bb])
            wcnt = [0]

            def inproj_group(cg, ncols, dst, dstname):
                b = wcnt[0] % 2
                wcnt[0] += 1
                S.op("sp", lambda: nc.sync.dma_start(out=wst[b][:, :, 0:ncols],
                                                     in_=w_in[:, cg * 128:cg * 128 + ncols].rearrange("(kc p) n -> p kc n", p=128)),
                     w=["wst%d" % b])
                S.op("pool", lambda: nc.gpsimd.tensor_copy(wbf[b][:, :, 0:ncols], wst[b][:, :, 0:ncols]), r=["wst%d" % b], w=["wbf%d" % b])
                for tcn in range(8):
                    pb = tcn % 2
                    for kc in range(8):
                        S.op("pe", lambda: nc.tensor.matmul(ps[pb][0:ncols, :], wbf[b][:, kc, 0:ncols], hT[:, kc, 1 + tcn * 512:1 + (tcn + 1) * 512],
                                                            start=(kc == 0), stop=(kc == 7)),
                             r=["wbf%d" % b, "hT"], w=[PSN[pb]])
                    S.op("act", lambda: nc.scalar.copy(dst[0:ncols, 1 + tcn * 512:1 + (tcn + 1) * 512], ps[pb][0:ncols, :]),
                         r=[PSN[pb]], w=[dstname])

            with ExitStack() as esH:
                sbH = lambda name, shape, dt=F32: esH.enter_context(nc.sbuf_tensor(name, shape, dt))
                ufm = [sbH("ufm%d" % i, [128, T]) for i in range(2)]
                cw = sbH("cw", [128, 3, 12])
                cb = sbH("cb", [128, 12])
                for j in range(3):
                    S.op("sp", lambda: nc.sync.dma_start(out=cw[:, j, :], in_=hy_conv_w[j:j + 1, :].rearrange("o (g p) -> p (o g)", p=128)), w=["cw"])
                S.op("sp", lambda: nc.sync.dma_start(out=cb[:], in_=hy_conv_b.rearrange("o (g p) -> p (o g)", p=128)), w=["cb"])
                for cg in range(12):
                    b = cg % 2
                    inproj_group(cg, 128, pfm[b], "pfm%d" % b)
                    S.op("dve", lambda: nc.vector.tensor_scalar(ufm[b][:], pfm[b][:, 0:T], cw[:, 0, cg:cg + 1], cb[:, cg:cg + 1], ALU.mult, ALU.add),
                         r=["pfm%d" % b, "cw", "cb"], w=["ufm%d" % b])
                    S.op("dve", lambda: nc.vector.scalar_tensor_tensor(ufm[b][:], pfm[b][:, 1:T + 1], cw[:, 1, cg:cg + 1], ufm[b][:], ALU.mult, ALU.add),
                         r=["pfm%d" % b, "cw", "ufm%d" % b], w=["ufm%d" % b])
                    S.op("dve", lambda: nc.vector.scalar_tensor_tensor(ufm[b][:], pfm[b][:, 2:T + 2], cw[:, 2, cg:cg + 1], ufm[b][:], ALU.mult, ALU.add),
                         r=["pfm%d" % b, "cw", "ufm%d" % b], w=["ufm%d" % b])
                    S.op("poolq", lambda: nc.gpsimd.dma_start(out=UD[cg, :, :], in_=ufm[b][:]), r=["ufm%d" % b], w=["UD"])
                    if DBG:
                        S.op("poolq", lambda: nc.gpsimd.dma_start(out=dbg_u[cg, :, :], in_=ufm[b][:]), r=["ufm%d" % b])
                S.barrier()

            with ExitStack() as esR:
                sbR = lambda name, shape, dt=F32: esR.enter_context(nc.sbuf_tensor(name, shape, dt))
                pvec = lambda ap_row, p=128: ap_row.rearrange("o (g p) -> p (o g)", p=p)
                mu_t = sbR("mu_t", [128, 14])
                w0_t = sbR("w0_t", [128, 8])
                a0_t = sbR("a0_t", [128, 8])
                kk_t = sbR("kk_t", [128, 4])
                ka_t = sbR("ka_t", [128, 4])
                oka_t = sbR("oka_t", [128, 4])
                rk_t = sbR("rk_t", [128, 4])
                S.op("sp", lambda: nc.sync.dma_start(out=mu_t[:, 0:13], in_=pvec(rw_mu[0:1, 0:1664])), w=["mu_t"])
                S.op("sp", lambda: nc.sync.dma_start(out=mu_t[0:96, 13:14], in_=pvec(rw_mu[0:1, 1664:1760], 96)), w=["mu_t"])
                for d in range(2):
                    S.op("sp", lambda: nc.sync.dma_start(out=w0_t[:, d * 4:(d + 1) * 4], in_=pvec(rw_w0[d:d + 1, :])), w=["w0_t"])
                    S.op("sp", lambda: nc.sync.dma_start(out=a0_t[:, d * 4:(d + 1) * 4], in_=pvec(rw_a0[d:d + 1, :])), w=["a0_t"])
                S.op("sp", lambda: nc.sync.dma_start(out=kk_t[:], in_=pvec(rw_k_k[0:1, :])), w=["kk_t"])
                S.op("sp", lambda: nc.sync.dma_start(out=ka_t[:], in_=pvec(rw_k_a[0:1, :])), w=["ka_t"])
                S.op("sp", lambda: nc.sync.dma_start(out=rk_t[:], in_=pvec(rw_r_k[0:1, :])), w=["rk_t"])
                S.op("dve", lambda: nc.vector.tensor_scalar(oka_t[:], ka_t[:], -1.0, 1.0, ALU.mult, ALU.add), r=["ka_t"], w=["oka_t"])
                Lw = sbR("Lw", [128, 4, 512], BF16)
                Gw = sbR("Gw", [96, 512], BF16)
                blk = sbR("blk_sb", [128, 128])
                Lbf = sbR("Lbf", [128, T], BF16)
                Glb = sbR("Glb", [96, T], BF16)
                esL = ExitStack()
                Lst = esL.enter_context(nc.sbuf_tensor("Lst", [128, 4, 512], F32))
                Gst = esL.enter_context(nc.sbuf_tensor("Gst", [96, 512], F32))
                S.op("pool", lambda: nc.gpsimd.memset(Lst[:], 0.0), w=["Lst"])
                for d in range(2):
                    S.op("sp", lambda: nc.sync.dma_start(out=Lst[d * 32:(d + 1) * 32, d, :], in_=rw_w_up[d]), w=["Lst"])
                    S.op("sp", lambda: nc.sync.dma_start(out=Lst[64 + d * 32:64 + (d + 1) * 32, 2 + d, :], in_=rw_a_up[d]), w=["Lst"])
                S.op("pool", lambda: nc.gpsimd.tensor_copy(Lw[:], Lst[:]), r=["Lst"], w=["Lw"])
                S.op("sp", lambda: nc.sync.dma_start(out=Gst[:], in_=rw_g_up[:, :]), w=["Gst"])
                S.op("pool", lambda: nc.gpsimd.tensor_copy(Gw[:], Gst[:]), r=["Gst"], w=["Gw"])
                S.op("sp", lambda: nc.sync.dma_start(out=blk[:], in_=blk_in[:, :]), w=["blk"])
                S.barrier()
                esL.close()

                singles = ("kkr", "sqk", "nrm", "sg", "tmpa", "bbf", "ad")

                def tmp2(name, shape=(128, 512), dt=F32):
                    if name in singles:
                        t_ = sbR(name + "0", list(shape), dt)
                        return [t_, t_]
                    return [sbR("%s%d" % (name, i), list(shape), dt) for i in range(2)]

                def shift(dst, dstn, src, srcn, mucol, c0, rows=128, n=512):
                    S.op("dve", lambda: nc.vector.tensor_tensor(dst[0:rows, :], src[0:rows, c0:c0 + n], src[0:rows, c0 + 2:c0 + 2 + n], ALU.add),
                         r=[srcn], w=[dstn])
                    S.op("dve", lambda: nc.vector.scalar_tensor_tensor(dst[0:rows, :], dst[0:rows, :], 0.5, src[0:rows, c0 + 1:c0 + 1 + n], ALU.mult, ALU.subtract),
                         r=[srcn, dstn], w=[dstn])
                    S.op("dve", lambda: nc.vector.scalar_tensor_tensor(dst[0:rows, :], dst[0:rows, :], mucol, src[0:rows, c0 + 1:c0 + 1 + n], ALU.mult, ALU.add),
                         r=[srcn, dstn, "mu_t"], w=[dstn])

                rf = tmp2("rf"); kf = tmp2("kf"); vf = tmp2("vf")
                kkr = tmp2("kkr"); sqk = tmp2("sqk"); nrm = tmp2("nrm"); kkf = tmp2("kkf")
                kkn = tmp2("kkn", dt=BF16); rbf = tmp2("rbf", dt=BF16)
                ad = tmp2("ad"); sg = tmp2("sg"); dec = tmp2("dec"); tmpa = tmp2("tmpa"); kd = tmp2("kd")
                ksum = tmp2("ksum"); bbf = tmp2("bbf"); bon = tmp2("bon"); gg = tmp2("gg")
                tmb = tmp2("tmb", (128, 4, 128), BF16); tmk = tmp2("tmk", (128, 4, 128), BF16); tmv = tmp2("tmv", (128, 4, 128), BF16)
                inproj_group(24, 128, pfm[0], "pfm0")
                inproj_group(25, 96, pfm[1], "pfm1")
                for ch in range(8):
                    c0 = ch * 512
                    p_ = ch % 2
                    shift(rf[p_], "rf%d" % p_, pfm[0], "pfm0", mu_t[:, 12:13], c0)
                    S.op("act", lambda: nc.scalar.activation(out=Lbf[0:64, c0:c0 + 512], in_=rf[p_][0:64, :], func=AF.Tanh), r=["rf%d" % p_], w=["Lbf"])
                    S.op("pool", lambda: nc.gpsimd.tensor_copy(Lbf[64:128, c0:c0 + 512], rf[p_][64:128, :]), r=["rf%d" % p_], w=["Lbf"])
                    shift(kf[p_], "kf%d" % p_, pfm[1], "pfm1", mu_t[0:96, 13:14], c0, rows=96)
                    S.op("act", lambda: nc.scalar.activation(out=Glb[0:96, c0:c0 + 512], in_=kf[p_][0:96, :], func=AF.Sigmoid), r=["kf%d" % p_], w=["Glb"])
                it = 0
                for g4 in range(4):
                    inproj_group(12 + g4, 128, pfm[0], "pfm0")
                    inproj_group(16 + g4, 128, pfm[1], "pfm1")
                    inproj_group(20 + g4, 128, pfm[2], "pfm2")
                    rows = slice(g4 * 128, (g4 + 1) * 128)
                    for ch in range(8):
                        c0 = ch * 512
                        cs_ = slice(c0, c0 + 512)
                        p_ = it % 2
                        it += 1
                        P = lambda nm: nm + ("0" if nm in singles else str(p_))
                        shift(rf[p_], P("rf"), pfm[0], "pfm0", mu_t[:, g4:g4 + 1], c0)
                        shift(kf[p_], P("kf"), pfm[1], "pfm1", mu_t[:, 4 + g4:5 + g4], c0)
                        shift(vf[p_], P("vf"), pfm[2], "pfm2", mu_t[:, 8 + g4:9 + g4], c0)
                        S.op("dve", lambda: nc.vector.tensor_scalar(kkr[p_][:], kf[p_][:], kk_t[:, g4:g4 + 1], None, ALU.mult), r=[P("kf"), "kk_t"], w=[P("kkr")])
                        S.op("act", lambda: nc.scalar.activation(out=sqk[p_][:], in_=kkr[p_][:], func=AF.Square), r=[P("kkr")], w=[P("sqk")])
                        S.op("pe", lambda: nc.tensor.matmul(ps[2][:], blk[:], sqk[p_][:], start=True, stop=True), r=["blk", P("sqk")], w=[PSN[2]])
                        S.op("act", lambda: nc.scalar.activation(out=nrm[p_][:], in_=ps[2][:], func=AF.Sqrt), r=[PSN[2]], w=[P("nrm")])
                        S.op("dve", lambda: nc.vector.tensor_scalar(nrm[p_][:], nrm[p_][:], 1e-12, None, ALU.max), r=[P("nrm")], w=[P("nrm")])
                        S.op("dve", lambda: nc.vector.reciprocal(nrm[p_][:], nrm[p_][:]), r=[P("nrm")], w=[P("nrm")])
                        S.op("dve", lambda: nc.vector.tensor_tensor(kkf[p_][:], kkr[p_][:], nrm[p_][:], ALU.mult), r=[P("kkr"), P("nrm")], w=[P("kkf")])
                        S.op("pool", lambda: nc.gpsimd.tensor_scalar(kkn[p_][:], kkf[p_][:], -1.0, None, ALU.mult), r=[P("kkf")], w=[P("kkn")])
                        S.op("poolq", lambda: nc.gpsimd.dma_start(out=KKN[rows, cs_], in_=kkn[p_][:]), r=[P("kkn")], w=["KKN"])
                        S.op("pool", lambda: nc.gpsimd.tensor_copy(rbf[p_][:], rf[p_][:]), r=[P("rf")], w=[P("rbf")])
                        S.op("poolq", lambda: nc.gpsimd.dma_start(out=RR[rows, cs_], in_=rbf[p_][:]), r=[P("rbf")], w=["RR"])
                        for d in range(2):
                            S.op("pe", lambda: nc.tensor.matmul(ps[3][:], Lw[:, 2 + d, rows], Lbf[:, cs_], start=True, stop=True), r=["Lw", "Lbf"], w=[PSN[3]])
                            S.op("act", lambda: nc.scalar.activation(out=ad[p_][:], in_=ps[3][:], func=AF.Sigmoid, bias=a0_t[:, d * 4 + g4:d * 4 + g4 + 1]),
                                 r=[PSN[3], "a0_t"], w=[P("ad")])
                            S.op("pe", lambda: nc.tensor.matmul(ps[4][:], Lw[:, d, rows], Lbf[:, cs_], start=True, stop=True), r=["Lw", "Lbf"], w=[PSN[4]])
                            S.op("act", lambda: nc.scalar.activation(out=sg[p_][:], in_=ps[4][:], func=AF.Sigmoid, bias=w0_t[:, d * 4 + g4:d * 4 + g4 + 1]),
                                 r=[PSN[4], "w0_t"], w=[P("sg")])
                            S.op("act", lambda: nc.scalar.activation(out=dec[p_][:], in_=sg[p_][:], func=AF.Exp, scale=-0.6065306597126334),
                                 r=[P("sg")], w=[P("dec")])
                            S.op("poolq", lambda: nc.gpsimd.dma_start(out=DEC[d, rows, cs_], in_=dec[p_][:]), r=[P("dec")], w=["DEC"])
                            S.op("dve", lambda: nc.vector.tensor_scalar(tmpa[p_][:], ad[p_][:], ka_t[:, g4:g4 + 1], oka_t[:, g4:g4 + 1], ALU.mult, ALU.add),
                                 r=[P("ad"), "ka_t", "oka_t"], w=[P("tmpa")])
                            S.op("pool", lambda: nc.gpsimd.tensor_tensor(kd[p_][:], tmpa[p_][:], kf[p_][:], ALU.mult), r=[P("tmpa"), P("kf")], w=[P("kd")])
                            if d == 0:
                                S.op("pool", lambda: nc.gpsimd.tensor_copy(ksum[p_][:], kd[p_][:]), r=[P("kd")], w=[P("ksum")])
                            else:
                                S.op("pool", lambda: nc.gpsimd.tensor_tensor(ksum[p_][:], ksum[p_][:], kd[p_][:], ALU.add), r=[P("kd"), P("ksum")], w=[P("ksum")])
                            S.op("dve", lambda: nc.vector.tensor_tensor(bbf[p_][:], kkf[p_][:], ad[p_][:], ALU.mult), r=[P("kkf"), P("ad")], w=[P("bbf")])
                            for q in range(4):
                                S.op("pe", lambda: nc.tensor.transpose(ps[5][:, q * 128:(q + 1) * 128], bbf[p_][:, q * 128:(q + 1) * 128], ident[:]),
                                     r=[P("bbf"), "ident"], w=[PSN[5]])
                            S.op("act", lambda: nc.scalar.copy(tmb[p_][:], ps[5][:].rearrange("p (q c) -> p q c", q=4)), r=[PSN[5]], w=[P("tmb")])
                            S.op("poolq", lambda: nc.gpsimd.dma_start(out=BT[0, d, cs_, rows].rearrange("(q p) c -> p q c", p=128), in_=tmb[p_][:]),
                                 r=[P("tmb")], w=["BT"])
                            for q in range(4):
                                S.op("pe", lambda: nc.tensor.transpose(ps[6][:, q * 128:(q + 1) * 128], kd[p_][:, q * 128:(q + 1) * 128], ident[:]),
                                     r=[P("kd"), "ident"], w=[PSN[6]])
                            S.op("act", lambda: nc.scalar.copy(tmk[p_][:], ps[6][:].rearrange("p (q c) -> p q c", q=4)), r=[PSN[6]], w=[P("tmk")])
                            S.op("poolq", lambda: nc.gpsimd.dma_start(out=BT[1, d, cs_, rows].rearrange("(q p) c -> p q c", p=128), in_=tmk[p_][:]),
                                 r=[P("tmk")], w=["BT"])
                        S.op("dve", lambda: nc.vector.scalar_tensor_tensor(bon[p_][:], rf[p_][:], rk_t[:, g4:g4 + 1], ksum[p_][:], ALU.mult, ALU.mult),
                             r=[P("rf"), "rk_t", P("ksum")], w=[P("bon")])
                        S.op("pe", lambda: nc.tensor.matmul(ps[7][:], blk[:], bon[p_][:], start=True, stop=True), r=["blk", P("bon")], w=[PSN[7]])
                        S.op("dve", lambda: nc.vector.tensor_tensor(bon[p_][:], ps[7][:], vf[p_][:], ALU.mult), r=[PSN[7], P("vf"), P("bon")], w=[P("bon")])
                        S.op("poolq", lambda: nc.gpsimd.dma_start(out=BON[rows, cs_], in_=bon[p_][:]), r=[P("bon")], w=["BON"])
                        for q in range(4):
                            S.op("pe", lambda: nc.tensor.transpose(ps[5][:, q * 128:(q + 1) * 128], vf[p_][:, q * 128:(q + 1) * 128], ident[:]),
                                 r=[P("vf"), "ident"], w=[PSN[5]])
                        S.op("act", lambda: nc.scalar.copy(tmv[p_][:], ps[5][:].rearrange("p (q c) -> p q c", q=4)), r=[PSN[5]], w=[P("tmv")])
                        S.op("poolq", lambda: nc.gpsimd.dma_start(out=VT[cs_, rows].rearrange("(q p) c -> p q c", p=128), in_=tmv[p_][:]),
                             r=[P("tmv")], w=["VT"])
                        S.op("pe", lambda: nc.tensor.matmul(ps[3][:], Gw[0:96, rows], Glb[0:96, cs_], start=True, stop=True), r=["Gw", "Glb"], w=[PSN[3]])
                        S.op("act", lambda: nc.scalar.copy(gg[p_][:], ps[3][:]), r=[PSN[3]], w=[P("gg")])
                        S.op("poolq", lambda: nc.gpsimd.dma_start(out=GG[rows, cs_], in_=gg[p_][:]), r=[P("gg")], w=["GG"])
                if DBG:
                    for nm_, src_ in (("KKN", KKN), ("RR", RR), ("DEC", DEC), ("BT", BT), ("VT", VT), ("BON", BON), ("GG", GG)):
                        S.op("poolq", lambda: nc.gpsimd.dma_start(out=dbg_rw[nm_], in_=src_), r=[nm_])
                S.barrier()

        S.barrier()
        with ExitStack() as es3:
            sb3 = lambda name, shape, dt=F32: es3.enter_context(nc.sbuf_tensor(name, shape, dt))
            utm = sb3("utm", [128, NT, 512], BF16)
            Yall = sb3("Yall", [128, 64, 512], BF16)
            for n in range(2):
                srcD = UD if n == 0 else ZD
                srcDn = "UD" if n == 0 else "ZD"
                esA = ExitStack()
                ufl = [esA.enter_context(nc.sbuf_tensor("ufl%d_%d" % (i, n), [128, T], F32)) for i in range(2)]
                for g in range(4):
                    fb_ = g % 2
                    S.op("sp", lambda: nc.sync.dma_start(out=ufl[fb_][:], in_=srcD[g, :, :]), r=[srcDn], w=["ufl%d" % fb_])
                    for i4 in range(NT // 4):
                        pb = i4 % 2
                        for q in range(4):
                            i = i4 * 4 + q
                            S.op("pe", lambda: nc.tensor.transpose(ps[pb][:, q * 128:(q + 1) * 128], ufl[fb_][:, i * 128:(i + 1) * 128], ident[:]),
                                 r=["ufl%d" % fb_, "ident"], w=[PSN[pb]])
                        S.op("act", lambda: nc.scalar.copy(utm[:, i4 * 4:(i4 + 1) * 4, g * 128:(g + 1) * 128],
                                                           ps[pb][:].rearrange("p (q c) -> p q c", q=4)),
                             r=[PSN[pb]], w=["utm"])
                S.barrier()
                esA.close()
                esB = ExitStack()
                sbB = lambda name, shape, dt=F32: esB.enter_context(nc.sbuf_tensor(name + "_%d" % n, shape, dt))
                dre = [sbB("dre%d" % i, [128, 32, 128], BF16) for i in range(2)]
                dim_ = [sbB("dim%d" % i, [128, 32, 128], BF16) for i in range(2)]
                kre = [sbB("kre%d" % i, [128, 512]) for i in range(2)]
                kim = [sbB("kim%d" % i, [128, 512]) for i in range(2)]
                ure = sbB("ure", [128, 512])
                uim = sbB("uim", [128, 512])
                t1 = sbB("t1", [128, 512])
                t2 = sbB("t2", [128, 512])
                t3 = sbB("t3", [128, 512])
                t4 = sbB("t4", [128, 512])
                for j in range(32):
                    b = j % 2
                    S.op("sp", lambda: nc.sync.dma_start(out=dre[b][:], in_=dfw[j]), w=["dre%d" % b])
                    S.op("sp", lambda: nc.sync.dma_start(out=dim_[b][:], in_=dfw[32 + j]), w=["dim%d" % b])
                    S.op("sp", lambda: nc.sync.dma_start(out=kre[b][:], in_=KF[j, :, n * 512:(n + 1) * 512]), r=["KF"], w=["kre%d" % b])
                    S.op("sp", lambda: nc.sync.dma_start(out=kim[b][:], in_=KF[32 + j, :, n * 512:(n + 1) * 512]), r=["KF"], w=["kim%d" % b])
                    pr, pi = 2 + 2 * b, 3 + 2 * b
                    for tcn in range(NT):
                        S.op("pe", lambda: nc.tensor.matmul(ps[pr][:], dre[b][:, tcn, :], utm[:, tcn, :], start=(tcn == 0), stop=(tcn == NT - 1)),
                             r=["dre%d" % b, "utm"], w=[PSN[pr]])
                    for tcn in range(NT):
                        S.op("pe", lambda: nc.tensor.matmul(ps[pi][:], dim_[b][:, tcn, :], utm[:, tcn, :], start=(tcn == 0), stop=(tcn == NT - 1)),
                             r=["dim%d" % b, "utm"], w=[PSN[pi]])
                    S.op("act", lambda: nc.scalar.copy(ure[:], ps[pr][:]), r=[PSN[pr]], w=["ure"])
                    S.op("act", lambda: nc.scalar.copy(uim[:], ps[pi][:]), r=[PSN[pi]], w=["uim"])
                    S.op("dve", lambda: nc.vector.tensor_tensor(t1[:], ure[:], kre[b][:], ALU.mult), r=["ure", "kre%d" % b], w=["t1"])
                    S.op("pool", lambda: nc.gpsimd.tensor_tensor(t2[:], uim[:], kim[b][:], ALU.mult), r=["uim", "kim%d" % b], w=["t2"])
                    S.op("dve", lambda: nc.vector.tensor_tensor(Yall[:, j, :], t1[:], t2[:], ALU.subtract), r=["t1", "t2"], w=["Yall"])
                    S.op("pool", lambda: nc.gpsimd.tensor_tensor(t3[:], ure[:], kim[b][:], ALU.mult), r=["ure", "kim%d" % b], w=["t3"])
                    S.op("dve", lambda: nc.vector.tensor_tensor(t4[:], uim[:], kre[b][:], ALU.mult), r=["uim", "kre%d" % b], w=["t4"])
                    S.op("pool", lambda: nc.gpsimd.tensor_tensor(Yall[:, 32 + j, :], t3[:], t4[:], ALU.add), r=["t3", "t4"], w=["Yall"])
                    if j == 0:
                        S.op("dve", lambda: nc.vector.tensor_tensor(Yall[0:1, 0, :], ure[0:1, :], kre[b][0:1, :], ALU.mult),
                             r=["ure", "kre%d" % b, "Yall"], w=["Yall"])
                        S.op("dve", lambda: nc.vector.tensor_tensor(Yall[0:1, 32, :], uim[0:1, :], kim[b][0:1, :], ALU.mult),
                             r=["uim", "kim%d" % b, "Yall"], w=["Yall"])
                S.barrier()
                esB.close()
                esC = ExitStack()
                sbC = lambda name, shape, dt=F32: esC.enter_context(nc.sbuf_tensor(name + "_%d" % n, shape, dt))
                dvt = sbC("dvt", [128, 64, 256], BF16)
                gate = [sbC("gate%d" % i, [128, 256]) for i in range(2)]
                zo = [sbC("zo%d" % i, [128, 256]) for i in range(2)]
                dstD = ZD if n == 0 else YD
                dstDn = "ZD" if n == 0 else "YD"
                for tq in range(16):
                    S.op("sp", lambda: nc.sync.dma_start(out=dvt[:], in_=dinv[tq]), w=["dvt"])
                    for g in range(4):
                        gb = g % 2
                        pb = 4 + g
                        gsrc = UD[4 + g] if n == 0 else UD[8 + g]
                        S.op("sp", lambda: nc.sync.dma_start(out=gate[gb][:], in_=gsrc[:, tq * 256:(tq + 1) * 256]), r=["UD"], w=["gate%d" % gb])
                        for rc in range(64):
                            S.op("pe", lambda: nc.tensor.matmul(ps[pb][:, 0:256], Yall[:, rc, g * 128:(g + 1) * 128], dvt[:, rc, :],
                                                                start=(rc == 0), stop=(rc == 63)),
                                 r=["Yall", "dvt"], w=[PSN[pb]])
                        S.op("dve", lambda: nc.vector.tensor_tensor(zo[gb][:], ps[pb][:, 0:256], gate[gb][:], ALU.mult),
                             r=[PSN[pb], "gate%d" % gb], w=["zo%d" % gb])
                        S.op("poolq", lambda: nc.gpsimd.dma_start(out=dstD[g, :, tq * 256:(tq + 1) * 256], in_=zo[gb][:]), r=["zo%d" % gb], w=[dstDn])
                S.barrier()
                esC.close()
            if DBG:
                S.op("poolq", lambda: nc.gpsimd.dma_start(out=dbg_y[:, :, :], in_=YD[:, :, :]), r=["YD"])

        S.barrier()
        with ExitStack() as esS:
            sbS = lambda name, shape, dt=F32: esS.enter_context(nc.sbuf_tensor(name, shape, dt))
            TC = 64
            St = sbS("St", [128, 512])
            Sbf = sbS("Sbf", [128, 512], BF16)
            rhs2 = sbS("rhs2", [48, 512], BF16)
            Vtm = sbS("Vtm", [128, NT, 512], BF16)
            m48 = sbS("m48_sb", [48, 512])
            m40 = sbS("m40_sb", [40, 1, 512])
            selF = sbS("selF_sb", [128, 128, 48], BF16)
            selB = sbS("selB_sb", [128, 128, 48], BF16)
            ARraw = sbS("ARraw", [128, 104, TC], BF16)
            AR2 = [sbS("AR2_%d" % i, [128, TC, 104], BF16) for i in range(2)]
            Wraw = sbS("Wraw", [128, 8, TC])
            W2 = [sbS("W2_%d" % i, [128, TC, 8, 1]) for i in range(2)]
            BKraw = sbS("BKraw", [48, TC, 128], BF16)
            BK2 = [sbS("BK2_%d" % i, [48, TC, 128], BF16) for i in range(2)]
            Oraw = [sbS("Oraw%d" % i, [40, 8, 512]) for i in range(2)]
            Ored = [sbS("Ored%d" % i, [40, 8, 64]) for i in range(2)]
            S.op("pool", lambda: nc.gpsimd.memset(St[:], 0.0), w=["St0", "St1"])
            S.op("pool", lambda: nc.gpsimd.memset(Sbf[:], 0.0), w=["Sbf0", "Sbf1"])
            S.op("pool", lambda: nc.gpsimd.memset(rhs2[:], 0.0), w=["rhs2_0", "rhs2_1"])
            S.op("pool", lambda: nc.gpsimd.memset(ARraw[:], 0.0), w=["ARraw"])
            S.op("pool", lambda: nc.gpsimd.memset(BKraw[:], 0.0), w=["BKraw"])
            for i in range(2):
                S.op("pool", lambda: nc.gpsimd.memset(BK2[i][:], 0.0), w=["BK2_%d" % i])
            S.op("sp", lambda: nc.sync.dma_start(out=Vtm[:], in_=VT.rearrange("(i p) c -> p i c", p=128)), r=["VT"], w=["Vtm"])
            S.op("sp", lambda: nc.sync.dma_start(out=m48[:], in_=m48_in[:, :]), w=["m48"])
            S.op("sp", lambda: nc.sync.dma_start(out=m40[:, 0, :], in_=m40_in[:, :]), w=["m40"])
            S.op("sp", lambda: nc.sync.dma_start(out=selF[:], in_=selF_in[:, :, :]), w=["selF"])
            S.op("sp", lambda: nc.sync.dma_start(out=selB[:], in_=selB_in[:, :, :]), w=["selB"])
            KKN_v = KKN.rearrange("(j k) t -> k j t", k=64)
            RR_v = RR.rearrange("(j k) t -> k j t", k=64)
            for c in range(SCAN_CHUNKS):
                cb = c % 2
                s0 = c * TC
                fw_ = slice(s0, s0 + TC)
                bw_ = slice(T - s0 - TC, T - s0)
                A2n, W2n, B2n = "AR2_%d" % cb, "W2_%d" % cb, "BK2_%d" % cb
                S.op("sp", lambda: nc.sync.dma_start(out=ARraw[0:64, 0:8, :], in_=KKN_v[:, :, fw_]), r=["KKN"], w=["ARraw"])
                S.op("sp", lambda: nc.sync.dma_start(out=ARraw[64:128, 32:40, :], in_=KKN_v[:, :, bw_]), r=["KKN"], w=["ARraw"])
                S.op("sp", lambda: nc.sync.dma_start(out=ARraw[0:64, 64:72, :], in_=RR_v[:, :, fw_]), r=["RR"], w=["ARraw"])
                S.op("sp", lambda: nc.sync.dma_start(out=ARraw[64:128, 96:104, :], in_=RR_v[:, :, bw_]), r=["RR"], w=["ARraw"])
                S.op("pool", lambda: nc.gpsimd.tensor_copy(AR2[cb][0:64], ARraw[0:64].rearrange("p c t -> p t c")), r=["ARraw"], w=[A2n])
                S.op("pool", lambda: nc.gpsimd.tensor_copy(AR2[cb][64:128], ARraw[64:128, :, ::-1].rearrange("p c t -> p t c")), r=["ARraw"], w=[A2n])
                S.op("sp", lambda: nc.sync.dma_start(out=Wraw[0:64], in_=DEC[0].rearrange("(j k) t -> k j t", k=64)[:, :, fw_]), r=["DEC"], w=["Wraw"])
                S.op("sp", lambda: nc.sync.dma_start(out=Wraw[64:128], in_=DEC[1].rearrange("(j k) t -> k j t", k=64)[:, :, bw_]), r=["DEC"], w=["Wraw"])
                S.op("pool", lambda: nc.gpsimd.tensor_copy(W2[cb][0:64, :, :, 0], Wraw[0:64].rearrange("p j t -> p t j")), r=["Wraw"], w=[W2n])
                S.op("pool", lambda: nc.gpsimd.tensor_copy(W2[cb][64:128, :, :, 0], Wraw[64:128, :, ::-1].rearrange("p j t -> p t j")), r=["Wraw"], w=[W2n])
                for s_ in range(2):
                    S.op("sp", lambda: nc.sync.dma_start(out=BKraw[s_ * 8:(s_ + 1) * 8, :, 0:64],
                                                         in_=BT[s_, 0, fw_, :].rearrange("t (j k) -> j t k", k=64)), r=["BT"], w=["BKraw"])
                    S.op("sp", lambda: nc.sync.dma_start(out=BKraw[32 + s_ * 8:32 + (s_ + 1) * 8, :, 64:128],
                                                         in_=BT[s_, 1, bw_, :].rearrange("t (j k) -> j t k", k=64)), r=["BT"], w=["BKraw"])
                S.op("pool", lambda: nc.gpsimd.tensor_copy(BK2[cb][0:16], BKraw[0:16]), r=["BKraw"], w=[B2n])
                S.op("pool", lambda: nc.gpsimd.tensor_copy(BK2[cb][32:48], BKraw[32:48, ::-1, :]), r=["BKraw"], w=[B2n])
                for s in range(TC):
                    st = s0 + s
                    i_f, tl = st // 128, st % 128
                    i_b = NT - 1 - i_f
                    ob = (st // 8) % 2
                    HH = (0, 1)
                    csl = [slice(h_ * 256, (h_ + 1) * 256) for h_ in HH]
                    for h_ in HH:
                        S.op("pe", lambda: nc.tensor.matmul(ps[h_][0:48, 0:256], selF[:, tl, :], Vtm[:, i_f, csl[h_]], start=True, stop=False),
                             r=["selF", "Vtm"], w=[PSN[h_]])
                        S.op("pe", lambda: nc.tensor.matmul(ps[h_][0:48, 0:256], selB[:, tl, :], Vtm[:, i_b, csl[h_]], start=False, stop=False),
                             r=["selB", "Vtm"], w=[PSN[h_]])
                        S.op("pe", lambda: nc.tensor.matmul(ps[h_][0:48, 0:256], AR2[cb][:, s, 0:48], Sbf[:, csl[h_]], start=False, stop=True),
                             r=[A2n, "Sbf%d" % h_], w=[PSN[h_]])
                    for h_ in HH:
                        S.op("dve", lambda: nc.vector.tensor_tensor(rhs2[:, csl[h_]], ps[h_][0:48, 0:256], m48[:, csl[h_]], ALU.mult),
                             r=[PSN[h_], "m48"], w=["rhs2_%d" % h_])
                    for h_ in HH:
                        S.op("dve", lambda: nc.vector.tensor_tensor(St[:, csl[h_]].rearrange("p (j v) -> p j v", j=4), St[:, csl[h_]].rearrange("p (j v) -> p j v", j=4),
                                                                    W2[cb][:, s, h_ * 4:(h_ + 1) * 4, :].to_broadcast([128, 4, 64]), ALU.mult),
                             r=["St%d" % h_, W2n], w=["St%d" % h_])
                    for h_ in HH:
                        S.op("pe", lambda: nc.tensor.matmul(ps[2 + h_][:, 0:256], BK2[cb][0:48, s, :], rhs2[:, csl[h_]], start=True, stop=True),
                             r=[B2n, "rhs2_%d" % h_], w=[PSN[2 + h_]])
                    for h_ in HH:
                        S.op("dve", lambda: nc.vector.tensor_tensor(St[:, csl[h_]], St[:, csl[h_]], ps[2 + h_][:, 0:256], ALU.add),
                             r=["St%d" % h_, PSN[2 + h_]], w=["St%d" % h_])
                    for h_ in HH:
                        S.op("act", lambda: nc.scalar.copy(Sbf[:, csl[h_]], St[:, csl[h_]]), r=["St%d" % h_], w=["Sbf%d" % h_])
                    for h_ in HH:
                        S.op("pe", lambda: nc.tensor.matmul(ps[4 + h_][0:40, 0:256], AR2[cb][:, s, 64:104], Sbf[:, csl[h_]], start=True, stop=True),
                             r=[A2n, "Sbf%d" % h_], w=[PSN[4 + h_]])
                    for h_ in HH:
                        S.op("act", lambda: nc.scalar.copy(Oraw[ob][:, st % 8, csl[h_]], ps[4 + h_][0:40, 0:256]), r=[PSN[4 + h_]], w=["Oraw%d" % ob])
                    if st % 8 == 7:
                        st0 = st - 7
                        S.op("pool", lambda: nc.gpsimd.tensor_tensor(Oraw[ob][:], Oraw[ob][:], m40[:].to_broadcast([40, 8, 512]), ALU.mult),
                             r=["Oraw%d" % ob, "m40"], w=["Oraw%d" % ob])
                        S.op("dve", lambda: nc.vector.tensor_reduce(out=Ored[ob][:], in_=Oraw[ob][:].rearrange("p s (j v) -> p s v j", j=8),
                                                                     axis=AX.X, op=ALU.add),
                             r=["Oraw%d" % ob], w=["Ored%d" % ob])
                        S.op("poolq", lambda: nc.gpsimd.dma_start(out=OD[0, st0:st0 + 8, :].rearrange("s (j v) -> j s v", v=64), in_=Ored[ob][0:8]),
                             r=["Ored%d" % ob], w=["OD"])
                        S.op("poolq", lambda: nc.gpsimd.dma_start(out=OD[1, st0:st0 + 8, :].rearrange("s (j v) -> j s v", v=64), in_=Ored[ob][32:40]),
                             r=["Ored%d" % ob], w=["OD"])
            if DBG:
                S.op("poolq", lambda: nc.gpsimd.dma_start(out=dbg_od, in_=OD), r=["OD"])
            S.barrier()

        with ExitStack() as esP:
            sbP = lambda name, shape, dt=F32: esP.enter_context(nc.sbuf_tensor(name, shape, dt))
            pvec = lambda ap_row, p=128: ap_row.rearrange("o (g p) -> p (o g)", p=p)
            yT = sbP("yT", [128, 8, T], BF16)
            ldb = [sbP("ldb%d" % i, [128, 1024]) for i in range(2)]
            n = 0
            for g in range(4):
                for c4 in range(4):
                    b = n % 2
                    n += 1
                    S.op("sp", lambda: nc.sync.dma_start(out=ldb[b][:], in_=YD[g, :, c4 * 1024:(c4 + 1) * 1024]), r=["YD"], w=["ldb%d" % b])
                    S.op("pool", lambda: nc.gpsimd.tensor_copy(yT[:, g, c4 * 1024:(c4 + 1) * 1024], ldb[b][:]), r=["ldb%d" % b], w=["yT"])
            blk2 = sbP("blk2", [128, 128])
            lnw_t = sbP("lnw_t", [128, 4])
            lnb_t = sbP("lnb_t", [128, 4])
            S.op("sp", lambda: nc.sync.dma_start(out=blk2[:], in_=blk_in[:, :]), w=["blk2"])
            S.op("sp", lambda: nc.sync.dma_start(out=lnw_t[:], in_=pvec(rw_ln_w[0:1, :])), w=["lnw_t"])
            S.op("sp", lambda: nc.sync.dma_start(out=lnb_t[:], in_=pvec(rw_ln_b[0:1, :])), w=["lnb_t"])
            oft = [sbP("oft%d" % i, [128, 4, 128]) for i in range(2)]
            obt = [sbP("obt%d" % i, [128, 4, 128]) for i in range(2)]
            bonc = [sbP("bonc%d" % i, [128, 512]) for i in range(2)]
            ggc = [sbP("ggc%d" % i, [128, 512]) for i in range(2)]
            obr = sbP("obr", [128, 4, 128])
            sfm = sbP("sfm", [128, 512])
            cen = sbP("cen", [128, 512])
            sqp = sbP("sqp", [128, 512])
            rstd = sbP("rstd", [128, 512])
            n = 0
            for g4 in range(4):
                rows = slice(g4 * 128, (g4 + 1) * 128)
                for i4 in range(8):
                    b = n % 2
                    n += 1
                    cs_ = slice(i4 * 512, (i4 + 1) * 512)
                    S.op("sp", lambda: nc.sync.dma_start(out=oft[b][:], in_=OD[0, cs_, rows].rearrange("(q p) c -> p q c", p=128)), r=["OD"], w=["oft%d" % b])
                    S.op("sp", lambda: nc.sync.dma_start(out=obt[b][:], in_=OD[1, T - (i4 + 1) * 512:T - i4 * 512, rows].rearrange("(m p) c -> p m c", p=128)),
                         r=["OD"], w=["obt%d" % b])
                    S.op("sp", lambda: nc.sync.dma_start(out=bonc[b][:], in_=BON[rows, cs_]), r=["BON"], w=["bonc%d" % b])
                    S.op("sp", lambda: nc.sync.dma_start(out=ggc[b][:], in_=GG[rows, cs_]), r=["GG"], w=["ggc%d" % b])
                    for q in range(4):
                        S.op("pe", lambda: nc.tensor.transpose(ps[0][:, q * 128:(q + 1) * 128], oft[b][:, q, :], ident[:]), r=["oft%d" % b, "ident"], w=[PSN[0]])
                    for q in range(4):
                        S.op("pe", lambda: nc.tensor.transpose(ps[1][:, q * 128:(q + 1) * 128], obt[b][:, 3 - q, :], ident[:]), r=["obt%d" % b, "ident"], w=[PSN[1]])
                    S.op("act", lambda: nc.scalar.copy(obr[:], ps[1][:].rearrange("p (q t) -> p q t", q=4)[:, :, ::-1]), r=[PSN[1]], w=["obr"])
                    S.op("dve", lambda: nc.vector.tensor_tensor(sfm[:], ps[0][:], obr[:].rearrange("p q t -> p (q t)"), ALU.add), r=[PSN[0], "obr"], w=["sfm"])
                    S.op("pe", lambda: nc.tensor.matmul(ps[2][:], blk2[:], sfm[:], start=True, stop=True), r=["blk2", "sfm"], w=[PSN[2]])
                    S.op("dve", lambda: nc.vector.scalar_tensor_tensor(cen[:], ps[2][:], -1.0 / 64, sfm[:], ALU.mult, ALU.add), r=[PSN[2], "sfm"], w=["cen"])
                    S.op("act", lambda: nc.scalar.activation(out=sqp[:], in_=cen[:], func=AF.Square), r=["cen"], w=["sqp"])
                    S.op("pe", lambda: nc.tensor.matmul(ps[3][:], blk2[:], sqp[:], start=True, stop=True), r=["blk2", "sqp"], w=[PSN[3]])
                    S.op("dve", lambda: nc.vector.tensor_scalar(rstd[:], ps[3][:], 1.0 / 64, 64e-5, ALU.mult, ALU.add), r=[PSN[3]], w=["rstd"])
                    S.op("act", lambda: nc.scalar.activation(out=rstd[:], in_=rstd[:], func=AF.Sqrt), r=["rstd"], w=["rstd"])
                    S.op("dve", lambda: nc.vector.reciprocal(rstd[:], rstd[:]), r=["rstd"], w=["rstd"])
                    S.op("dve", lambda: nc.vector.tensor_tensor(cen[:], cen[:], rstd[:], ALU.mult), r=["cen", "rstd"], w=["cen"])
                    S.op("dve", lambda: nc.vector.tensor_scalar(cen[:], cen[:], lnw_t[:, g4:g4 + 1], lnb_t[:, g4:g4 + 1], ALU.mult, ALU.add),
                         r=["cen", "lnw_t", "lnb_t"], w=["cen"])
                    S.op("pool", lambda: nc.gpsimd.tensor_tensor(cen[:], cen[:], bonc[b][:], ALU.add), r=["cen", "bonc%d" % b], w=["cen"])
                    S.op("pool", lambda: nc.gpsimd.tensor_tensor(yT[:, 4 + g4, cs_], cen[:], ggc[b][:], ALU.mult), r=["cen", "ggc%d" % b], w=["yT"])
            wo = sbP("wo", [128, 8, D], BF16)
            g1t = sbP("g1t", [128, D])
            xin = [sbP("xin%d" % i, [128, D]) for i in range(2)]
            x1t = [sbP("x1t%d" % i, [128, D]) for i in range(2)]
            for kc in range(8):
                b = kc % 2
                S.op("sp", lambda: nc.sync.dma_start(out=ldb[b][:], in_=w_out[kc * 128:(kc + 1) * 128, :]), w=["ldb%d" % b])
                S.op("pool", lambda: nc.gpsimd.tensor_copy(wo[:, kc, :], ldb[b][:]), r=["ldb%d" % b], w=["wo"])
            S.op("sp", lambda: nc.sync.dma_start(out=g1t[:], in_=MODD[:, 2 * D:3 * D]), r=["MODD"], w=["g1t"])
            for i in range(NT):
                b = i % 2
                S.op("sp", lambda: nc.sync.dma_start(out=xin[b][:], in_=x[i * 128:(i + 1) * 128, :]), w=["xin%d" % b])
                for half in range(2):
                    hs = slice(half * 512, (half + 1) * 512)
                    for kc in range(8):
                        S.op("pe", lambda: nc.tensor.matmul(ps[4 + half][:], yT[:, kc, i * 128:(i + 1) * 128], wo[:, kc, hs], start=(kc == 0), stop=(kc == 7)),
                             r=["yT", "wo"], w=[PSN[4 + half]])
                    S.op("dve", lambda: nc.vector.tensor_tensor(x1t[b][:, hs], ps[4 + half][:], g1t[:, hs], ALU.mult), r=[PSN[4 + half], "g1t"], w=["x1t%d" % b])
                    S.op("pool", lambda: nc.gpsimd.tensor_tensor(x1t[b][:, hs], x1t[b][:, hs], xin[b][:, hs], ALU.add), r=["x1t%d" % b, "xin%d" % b], w=["x1t%d" % b])
                S.op("poolq", lambda: nc.gpsimd.dma_start(out=X1D[i * 128:(i + 1) * 128, :], in_=x1t[b][:]), r=["x1t%d" % b], w=["X1D"])
            if DBG:
                S.op("poolq", lambda: nc.gpsimd.dma_start(out=dbg_x1, in_=X1D), r=["X1D"])
            S.barrier()

        NE = 256
        CAP = 1024
        NST = CAP // 128
        with ExitStack() as esF:
            sbF = lambda name, shape, dt=F32: esF.enter_context(nc.sbuf_tensor(name, shape, dt))
            g2t = sbF("g2t", [128, D])
            S.op("sp", lambda: nc.sync.dma_start(out=g2t[:], in_=MODD[:, 5 * D:6 * D]), r=["MODD"], w=["g2t"])
            keys = sbF("keys", [128, 2, CAP])
            idxI = sbF("idxI", [128, NST, NE], I32)
            with ExitStack() as esG:
                sbG = lambda name, shape, dt=F32: esG.enter_context(nc.sbuf_tensor(name, shape, dt))
                gm2 = sbG("gm2", [128, D]); sc2 = sbG("sc2", [128, D]); sh2 = sbG("sh2", [128, D])
                S.op("sp", lambda: nc.sync.dma_start(out=gm2[:], in_=bcast_rows(norm2_g[0:1, :])), w=["gm2"])
                S.op("sp", lambda: nc.sync.dma_start(out=sh2[:], in_=MODD[:, 3 * D:4 * D]), r=["MODD"], w=["sh2"])
                S.op("sp", lambda: nc.sync.dma_start(out=sc2[:], in_=MODD[:, 4 * D:5 * D]), r=["MODD"], w=["sc2"])
                S.op("dve", lambda: nc.vector.scalar_tensor_tensor(gm2[:], sc2[:], 1.0, gm2[:], ALU.add, ALU.mult), r=["sc2", "gm2"], w=["gm2"])
                rwt = sbG("rwt", [128, 8, NE])
                S.op("sp", lambda: nc.sync.dma_start(out=rwt[:], in_=router_w.rearrange("(kc p) n -> p kc n", p=128)), w=["rwt"])
                rbias = sbG("rbias", [128, NE])
                S.op("sp", lambda: nc.sync.dma_start(out=rbias[:], in_=bcast_rows(router_bias[0:1, :])), w=["rbias"])
                tokid = sbG("tokid_sb", [128, NT])
                S.op("sp", lambda: nc.sync.dma_start(out=tokid[:], in_=tokid_in[:, :]), w=["tokid"])
                zrow = sbG("zrow", [1, D + NE])
                S.op("pool", lambda: nc.gpsimd.memset(zrow[:], 0.0), w=["zrow"])
                S.op("poolq", lambda: nc.gpsimd.dma_start(out=H2E[T:T + 1, :], in_=zrow[:]), r=["zrow"], w=["H2E"])
                S.op("poolq", lambda: nc.gpsimd.dma_start(out=OUTACC[T:T + 1, :], in_=zrow[:, 0:D]), r=["zrow"], w=["OUTACC"])
                shgu = sbG("shgu", [128, 8, 512], BF16)
                shd = sbG("shd", [128, 2, D], BF16)
                stg = sbG("stg", [128, 8, 256])
                S.op("sp", lambda: nc.sync.dma_start(out=stg[:], in_=sh_w_gate.rearrange("(kc p) n -> p kc n", p=128)), w=["stg"])
                S.op("pool", lambda: nc.gpsimd.tensor_copy(shgu[:, :, 0:256], stg[:]), r=["stg"], w=["shgu"])
                S.op("sp", lambda: nc.sync.dma_start(out=stg[:], in_=sh_w_up.rearrange("(kc p) n -> p kc n", p=128)), w=["stg"])
                S.op("pool", lambda: nc.gpsimd.tensor_copy(shgu[:, :, 256:512], stg[:]), r=["stg"], w=["shgu"])
                S.op("sp", lambda: nc.sync.dma_start(out=stg[:].rearrange("p a b -> p (a b)").rearrange("p (j n) -> p j n", j=2),
                                                     in_=sh_w_down.rearrange("(jc p) n -> p jc n", p=128)), w=["stg"])
                S.op("pool", lambda: nc.gpsimd.tensor_copy(shd[:], stg[:].rearrange("p a b -> p (a b)").rearrange("p (j n) -> p j n", j=2)), r=["stg"], w=["shd"])
                KT = sbG("KT", [128, 2, T])
                xt2 = [sbG("xt2_%d" % i, [128, D]) for i in range(2)]
                h2 = [sbG("h2_%d" % i, [128, D]) for i in range(2)]
                sq2 = sbG("sq2", [128, D])
                ss2 = [sbG("ss2_%d" % i, [128, 1]) for i in range(2)]
                rs2 = [sbG("rs2_%d" % i, [128, 1]) for i in range(2)]
                h2T = sbG("h2T", [128, 8, 128])
                h2Tb = sbG("h2Tb", [128, 8, 128], BF16)
                sc = sbG("sc", [128, NE]); bia = sbG("bia", [128, NE]); msk = sbG("msk", [128, NE]); sel = sbG("sel", [128, NE])
                gat = [sbG("gat%d" % i, [128, NE]) for i in range(2)]
                key = sbG("key", [128, NE])
                m8 = sbG("m8", [128, 8, 8]); gs = sbG("gs", [128, 8]); gm8 = sbG("gm8", [128, 8]); gmk = sbG("gmk", [128, 8, 1]); pen = sbG("pen", [128, 8, 1])
                t8 = sbG("t8", [128, 8]); den = sbG("den", [128, 1])
                sgs = sbG("sgs", [128, 256]); acs = sbG("acs", [128, 256]); acT = sbG("acT", [128, 2, 128], BF16)
                acc = [sbG("acc%d" % i, [128, D]) for i in range(2)]
                for i in range(NT):
                    b = i % 2
                    tsl = slice(i * 128, (i + 1) * 128)
                    S.op("sp", lambda: nc.sync.dma_start(out=xt2[b][:], in_=X1D[tsl, :]), r=["X1D"], w=["xt2_%d" % b])
                    rstd_of(sq2, ss2[b], rs2[b], "n2_%d" % b, xt2[b][:], "xt2_%d" % b)
                    S.op("dve", lambda: nc.vector.scalar_tensor_tensor(h2[b][:], xt2[b][:], rs2[b][:, 0:1], gm2[:], ALU.mult, ALU.mult),
                         r=["xt2_%d" % b, "rsn2_%d" % b, "gm2"], w=["h2_%d" % b])
                    S.op("pool", lambda: nc.gpsimd.tensor_tensor(h2[b][:], h2[b][:], sh2[:], ALU.add), r=["h2_%d" % b, "sh2"], w=["h2_%d" % b])
                    S.op("poolq", lambda: nc.gpsimd.dma_start(out=H2E[tsl, 0:D], in_=h2[b][:]), r=["h2_%d" % b], w=["H2E"])
                    for half in range(2):
                        pb = half
                        for q in range(4):
                            kc = half * 4 + q
                            S.op("pe", lambda: nc.tensor.transpose(ps[pb][:, q * 128:(q + 1) * 128], h2[b][:, kc * 128:(kc + 1) * 128], ident[:]),
                                 r=["h2_%d" % b, "ident"], w=[PSN[pb]])
                        S.op("act", lambda: nc.scalar.copy(h2T[:, half * 4:(half + 1) * 4, :], ps[pb][:].rearrange("p (q t) -> p q t", q=4)), r=[PSN[pb]], w=["h2T"])
                    S.op("pool", lambda: nc.gpsimd.tensor_copy(h2Tb[:], h2T[:]), r=["h2T"], w=["h2Tb"])
                    for kc in range(8):
                        S.op("pe", lambda: nc.tensor.matmul(ps[2][:, 0:NE], h2T[:, kc, :], rwt[:, kc, :], start=(kc == 0), stop=(kc == 7)), r=["h2T", "rwt"], w=[PSN[2]])
                    S.op("act", lambda: nc.scalar.activation(out=sc[:], in_=ps[2][:, 0:NE], func=AF.Sigmoid), r=[PSN[2]], w=["sc"])
                    S.op("dve", lambda: nc.vector.tensor_tensor(bia[:], sc[:], rbias[:], ALU.add), r=["sc", "rbias"], w=["bia"])
                    for g in range(8):
                        S.op("dve", lambda: nc.vector.max(out=m8[:, g, :], in_=bia[:, g * 32:(g + 1) * 32]), r=["bia"], w=["m8"])
                    S.op("dve", lambda: nc.vector.tensor_tensor(gs[:], m8[:, :, 0], m8[:, :, 1], ALU.add), r=["m8"], w=["gs"])
                    S.op("dve", lambda: nc.vector.max(out=gm8[:], in_=gs[:]), r=["gs"], w=["gm8"])
                    S.op("dve", lambda: nc.vector.tensor_scalar(gmk[:, :, 0], gs[:], gm8[:, 3:4], None, ALU.is_ge), r=["gs", "gm8"], w=["gmk"])
                    S.op("dve", lambda: nc.vector.tensor_scalar(pen[:, :, 0], gmk[:, :, 0], 1e9, -1e9, ALU.mult, ALU.add), r=["gmk"], w=["pen"])
                    S.op("dve", lambda: nc.vector.tensor_tensor(msk[:].rearrange("p (g e) -> p g e", g=8), bia[:].rearrange("p (g e) -> p g e", g=8),
                                                                gmk[:].to_broadcast([128, 8, 32]), ALU.mult), r=["bia", "gmk"], w=["msk"])
                    S.op("dve", lambda: nc.vector.tensor_tensor(msk[:].rearrange("p (g e) -> p g e", g=8), msk[:].rearrange("p (g e) -> p g e", g=8),
                                                                pen[:].to_broadcast([128, 8, 32]), ALU.add), r=["msk", "pen"], w=["msk"])
                    S.op("dve", lambda: nc.vector.max(out=t8[:], in_=msk[:]), r=["msk"], w=["t8"])
                    S.op("dve", lambda: nc.vector.tensor_scalar(sel[:], msk[:], t8[:, 7:8], None, ALU.is_ge), r=["msk", "t8"], w=["sel"])
                    S.op("dve", lambda: nc.vector.tensor_tensor(gat[b][:], sc[:], sel[:], ALU.mult), r=["sc", "sel"], w=["gat%d" % b])
                    S.op("dve", lambda: nc.vector.tensor_reduce(out=den[:], in_=gat[b][:], axis=AX.X, op=ALU.add), r=["gat%d" % b], w=["den"])
                    S.op("dve", lambda: nc.vector.reciprocal(den[:], den[:]), r=["den"], w=["den"])
                    S.op("dve", lambda: nc.vector.tensor_scalar(gat[b][:], gat[b][:], den[:, 0:1], 2.5, ALU.mult, ALU.mult), r=["gat%d" % b, "den"], w=["gat%d" % b])
                    S.op("poolq", lambda: nc.gpsimd.dma_start(out=H2E[tsl, D:D + NE], in_=gat[b][:]), r=["gat%d" % b], w=["H2E"])
                    S.op("dve", lambda: nc.vector.tensor_scalar(key[:], sel[:], tokid[:, i:i + 1], None, ALU.mult), r=["sel", "tokid"], w=["key"])
                    for eh in range(2):
                        S.op("pe", lambda: nc.tensor.transpose(ps[3][:, eh * 128:(eh + 1) * 128], key[:, eh * 128:(eh + 1) * 128], ident[:]), r=["key", "ident"], w=[PSN[3]])
                    S.op("act", lambda: nc.scalar.copy(KT[:, :, tsl], ps[3][:, 0:256].rearrange("p (e t) -> p e t", e=2)), r=[PSN[3]], w=["KT"])
                    for kc in range(8):
                        S.op("pe", lambda: nc.tensor.matmul(ps[4][:], h2Tb[:, kc, :], shgu[:, kc, :], start=(kc == 0), stop=(kc == 7)), r=["h2Tb", "shgu"], w=[PSN[4]])
                    S.op("act", lambda: nc.scalar.activation(out=sgs[:], in_=ps[4][:, 0:256], func=AF.Silu), r=[PSN[4]], w=["sgs"])
                    S.op("dve", lambda: nc.vector.tensor_tensor(acs[:], sgs[:], ps[4][:, 256:512], ALU.mult), r=["sgs", PSN[4]], w=["acs"])
                    for jc in range(2):
                        S.op("pe", lambda: nc.tensor.transpose(ps[5][:, jc * 128:(jc + 1) * 128], acs[:, jc * 128:(jc + 1) * 128], ident[:]), r=["acs", "ident"], w=[PSN[5]])
                    S.op("act", lambda: nc.scalar.copy(acT[:], ps[5][:, 0:256].rearrange("p (j t) -> p j t", j=2)), r=[PSN[5]], w=["acT"])
                    for half in range(2):
                        hs = slice(half * 512, (half + 1) * 512)
                        for jc in range(2):
                            S.op("pe", lambda: nc.tensor.matmul(ps[6 + half][:], acT[:, jc, :], shd[:, jc, hs], start=(jc == 0), stop=(jc == 1)), r=["acT", "shd"], w=[PSN[6 + half]])
                        S.op("dve", lambda: nc.vector.tensor_tensor(acc[b][:, hs], ps[6 + half][:], g2t[:, hs], ALU.mult), r=[PSN[6 + half], "g2t"], w=["acc%d" % b])
                        S.op("pool", lambda: nc.gpsimd.tensor_tensor(acc[b][:, hs], acc[b][:, hs], xt2[b][:, hs], ALU.add), r=["acc%d" % b, "xt2_%d" % b], w=["acc%d" % b])
                    S.op("poolq", lambda: nc.gpsimd.dma_start(out=OUTACC[tsl, :], in_=acc[b][:]), r=["acc%d" % b], w=["OUTACC"])
                for eh in range(2):
                    for rnd in range(CAP // 8):
                        S.op("dve", lambda: nc.vector.max(out=keys[:, eh, rnd * 8:(rnd + 1) * 8], in_=KT[:, eh, :]), r=["KT"], w=["keys"])
                        S.op("dve", lambda: nc.vector.match_replace(out=KT[:, eh, :], in_to_replace=keys[:, eh, rnd * 8:(rnd + 1) * 8],
                                                                    in_values=KT[:, eh, :], imm_value=0.0), r=["KT", "keys"], w=["KT"])
                zk = sbG("zk", [128, 2, CAP])
                S.op("dve", lambda: nc.vector.tensor_scalar(zk[:], keys[:], 0.0, float(T + 1), ALU.is_equal, ALU.mult), r=["keys"], w=["zk"])
                S.op("dve", lambda: nc.vector.scalar_tensor_tensor(keys[:], keys[:], -1.0, zk[:], ALU.add, ALU.add), r=["keys", "zk"], w=["keys"])
                idxT = sbG("idxT", [128, NST, NE])
                for shf in range(NST):
                    for eh in range(2):
                        S.op("pe", lambda: nc.tensor.transpose(ps[0][:, eh * 128:(eh + 1) * 128], keys[:, eh, shf * 128:(shf + 1) * 128], ident[:]), r=["keys", "ident"], w=[PSN[0]])
                    S.op("act", lambda: nc.scalar.copy(idxT[:, shf, :], ps[0][:, 0:NE]), r=[PSN[0]], w=["idxT"])
                S.op("dve", lambda: nc.vector.tensor_copy(idxI[:], idxT[:]), r=["idxT"], w=["idxI"])
                if DBG:
                    S.op("poolq", lambda: nc.gpsimd.dma_start(out=dbg_idx, in_=idxI[:]), r=["idxI"])
                S.barrier()
            with ExitStack() as esE:
                sbE = lambda name, shape, dt=F32: esE.enter_context(nc.sbuf_tensor(name, shape, dt))
                wgs = [sbE("wgs%d" % i, [128, 8, 512]) for i in range(2)]
                wgb = [sbE("wgb%d" % i, [128, 8, 512], BF16) for i in range(2)]
                wds = [sbE("wds%d" % i, [128, 2, D]) for i in range(2)]
                wdb = [sbE("wdb%d" % i, [128, 2, D], BF16) for i in range(2)]
                Xg = [sbE("Xg%d" % i, [128, D + NE]) for i in range(4)]
                XgT = [sbE("XgT%d" % i, [128, 8, 128], BF16) for i in range(2)]
                sge = [sbE("sge%d" % i, [128, 256]) for i in range(2)]
                ace = [sbE("ace%d" % i, [128, 256]) for i in range(2)]
                aeT = [sbE("aeT%d" % i, [128, 2, 128], BF16) for i in range(2)]
                yo = [sbE("yo%d" % i, [128, D]) for i in range(3)]
                tiles = [(e, shf) for e in range(N_EXP_RUN) for shf in range(NST)]
                NTL = len(tiles)

                def load_w(e):
                    wb = e % 2
                    S.op("sp", lambda: nc.sync.dma_start(out=wgs[wb][:, :, 0:256], in_=exp_w_gate[e].rearrange("(kc p) n -> p kc n", p=128)), w=["wgs%d" % wb])
                    S.op("sp", lambda: nc.sync.dma_start(out=wgs[wb][:, :, 256:512], in_=exp_w_up[e].rearrange("(kc p) n -> p kc n", p=128)), w=["wgs%d" % wb])
                    S.op("sp", lambda: nc.sync.dma_start(out=wds[wb][:], in_=exp_w_down[e].rearrange("(jc p) n -> p jc n", p=128)), w=["wds%d" % wb])

                def cast_w(e):
                    wb = e % 2
                    S.op("dve", lambda: nc.vector.tensor_copy(wgb[wb][:], wgs[wb][:]), r=["wgs%d" % wb], w=["wgb%d" % wb])
                    S.op("act", lambda: nc.scalar.copy(wdb[wb][:], wds[wb][:]), r=["wds%d" % wb], w=["wdb%d" % wb])

                def gather(k):
                    e, shf = tiles[k]
                    xb = k % 4
                    S.op("poolq", lambda: nc.gpsimd.indirect_dma_start(out=Xg[xb][:, :], out_offset=None, in_=H2E[:, :],
                                                                       in_offset=bass.IndirectOffsetOnAxis(ap=idxI[:, shf, e:e + 1], axis=0)),
                         r=["H2E", "idxI"], w=["Xg%d" % xb])

                def stageA(k):
                    xb, tb = k % 4, k % 2
                    for half in range(2):
                        for q in range(4):
                            kc = half * 4 + q
                            S.op("pe", lambda: nc.tensor.transpose(ps[half][:, q * 128:(q + 1) * 128], Xg[xb][:, kc * 128:(kc + 1) * 128], ident[:]),
                                 r=["Xg%d" % xb, "ident"], w=[PSN[half]])
                        S.op("act", lambda: nc.scalar.copy(XgT[tb][:, half * 4:(half + 1) * 4, :], ps[half][:].rearrange("p (q t) -> p q t", q=4)),
                             r=[PSN[half]], w=["XgT%d" % tb])

                def stageB(k):
                    e, shf = tiles[k]
                    wb, tb, gb = e % 2, k % 2, 2 + k % 2
                    for kc in range(8):
                        S.op("pe", lambda: nc.tensor.matmul(ps[gb][:], XgT[tb][:, kc, :], wgb[wb][:, kc, :], start=(kc == 0), stop=(kc == 7)),
                             r=["XgT%d" % tb, "wgb%d" % wb], w=[PSN[gb]])
                    S.op("act", lambda: nc.scalar.activation(out=sge[tb][:], in_=ps[gb][:, 0:256], func=AF.Silu), r=[PSN[gb]], w=["sge%d" % tb])
                    S.op("dve", lambda: nc.vector.tensor_tensor(ace[tb][:], sge[tb][:], ps[gb][:, 256:512], ALU.mult), r=["sge%d" % tb, PSN[gb]], w=["ace%d" % tb])

                def stageC(k):
                    tb = k % 2
                    for jc in range(2):
                        S.op("pe", lambda: nc.tensor.transpose(ps[4][:, jc * 128:(jc + 1) * 128], ace[tb][:, jc * 128:(jc + 1) * 128], ident[:]),
                             r=["ace%d" % tb, "ident"], w=[PSN[4]])
                    S.op("act", lambda: nc.scalar.copy(aeT[tb][:], ps[4][:, 0:256].rearrange("p (j t) -> p j t", j=2)), r=[PSN[4]], w=["aeT%d" % tb])

                def stageD(k):
                    e, shf = tiles[k]
                    wb, tb, xb, yb = e % 2, k % 2, k % 4, k % 3
                    for half in range(2):
                        hs = slice(half * 512, (half + 1) * 512)
                        for jc in range(2):
                            S.op("pe", lambda: nc.tensor.matmul(ps[6 + half][:], aeT[tb][:, jc, :], wdb[wb][:, jc, hs], start=(jc == 0), stop=(jc == 1)),
                                 r=["aeT%d" % tb, "wdb%d" % wb], w=[PSN[6 + half]])
                        S.op("dve", lambda: nc.vector.scalar_tensor_tensor(yo[yb][:, hs], ps[6 + half][:], Xg[xb][:, D + e:D + e + 1], g2t[:, hs], ALU.mult, ALU.mult),
                             r=[PSN[6 + half], "Xg%d" % xb, "g2t"], w=["yo%d" % yb])
                    S.op("poolq", lambda: nc.gpsimd.indirect_dma_start(out=OUTACC[:, :], out_offset=bass.IndirectOffsetOnAxis(ap=idxI[:, shf, e:e + 1], axis=0),
                                                                       in_=yo[yb][:, :], in_offset=None, compute_op=ALU.add),
                         r=["yo%d" % yb, "idxI"], w=["OUTACC"])

                if NTL > 0:
                    load_w(0)
                    cast_w(0)
                    if N_EXP_RUN > 1:
                        load_w(1)
                    gather(0)
                    if NTL > 1:
                        gather(1)
                    stageA(0)
                    for k in range(NTL):
                        e, shf = tiles[k]
                        if k + 2 < NTL:
                            gather(k + 2)
                        if k + 1 < NTL:
                            stageA(k + 1)
                        stageB(k)
                        if k >= 1:
                            stageD(k - 1)
                        stageC(k)
                        if shf == NST - 1 and e + 1 < N_EXP_RUN:
                            cast_w(e + 1)
                            if e + 2 < N_EXP_RUN:
                                load_w(e + 2)
                    stageD(NTL - 1)
                S.barrier()

        S.barrier()
        gf = sb("gf", [128, D])
        ot = [sb("ot%d" % i, [128, D]) for i in range(2)]
        xt = [sb("xf%d" % i, [128, D]) for i in range(2)]
        sq = sb("sqf2", [128, D])
        ss = [sb("ssf%d" % i, [128, 1]) for i in range(2)]
        rs = [sb("rsf%d" % i, [128, 1]) for i in range(2)]

        def rstd_fin(b, src, srcname):
            S.op("act", lambda: nc.scalar.activation(out=sq[:], in_=src, func=AF.Square, accum_out=ss[b][:]),
                 r=[srcname], w=["sqf2", "ssf%d" % b])
            S.op("dve", lambda: nc.vector.tensor_scalar(rs[b][:], ss[b][:], 1.0 / D, NORM_EPS, ALU.mult, ALU.add),
                 r=["ssf%d" % b], w=["rsf%d" % b])
            S.op("act", lambda: nc.scalar.activation(out=rs[b][:], in_=rs[b][:], func=AF.Sqrt), r=["rsf%d" % b], w=["rsf%d" % b])
            S.op("dve", lambda: nc.vector.reciprocal(rs[b][:], rs[b][:]), r=["rsf%d" % b], w=["rsf%d" % b])

        S.op("sp", lambda: nc.sync.dma_start(out=gf[:], in_=bcast_rows(normf_g[0:1, :])), w=["gf"])
        for i in range(NT):
            b = i % 2
            S.op("sp", lambda: nc.sync.dma_start(out=xt[b][:], in_=OUTACC[i * 128:(i + 1) * 128, :]), r=["OUTACC"], w=["xf%d" % b])
            rstd_fin(b, xt[b][:], "xf%d" % b)
            S.op("dve", lambda: nc.vector.scalar_tensor_tensor(ot[b][:], xt[b][:], rs[b][:, 0:1], gf[:], ALU.mult, ALU.mult),
                 r=["xf%d" % b, "rsf%d" % b, "gf"], w=["ot%d" % b])
            S.op("poolq", lambda: nc.gpsimd.dma_start(out=out[i * 128:(i + 1) * 128, :], in_=ot[b][:]), r=["ot%d" % b])
        S.finish("pool")
    return nc


_NC_CACHE = {}


_CONST = {}


def _constants():
    if _CONST:
        return _CONST
    import ml_dtypes
    L = T
    N = 2 * T
    f32 = np.float32
    pos = np.arange(L, dtype=f32)
    t = pos / f32(L - 1)
    bands = np.linspace(1e-4, 15.0, 16, dtype=f32)
    ang = (f32(2.0 * np.pi / L) * pos[:, None]) * bands[None]
    feats = np.concatenate([t[:, None], np.cos(ang), -np.sin(ang)], axis=-1).astype(f32)
    deltas = np.abs(np.linspace(np.log(1e-2) / 1.5, np.log(1e-2) / 0.3, 512, dtype=f32))
    env = np.exp(-t[:, None] * deltas[None]).astype(f32)
    tt = np.arange(L, dtype=np.int64)
    ff = np.arange(L, dtype=np.int64)
    ph = (tt[:, None] * ff[None, :]) % N
    angm = ph.astype(np.float64) * (2.0 * np.pi / N)
    cosm = np.cos(angm)
    sinm = np.sin(angm)
    fw = np.empty((L, N), np.float32)
    fw[:, :L] = cosm
    fw[:, L:] = -sinm
    fw[:, L] = np.where(tt % 2 == 0, 1.0, -1.0)
    dfw = fw.reshape(NT, 128, 64, 128).transpose(2, 1, 0, 3)
    iv = np.empty((N, L), np.float32)
    iv[:L] = (2.0 / N) * cosm.T
    iv[0] = 1.0 / N
    iv[L:] = -(2.0 / N) * sinm.T
    iv[L] = np.where(tt % 2 == 0, 1.0, -1.0) / N
    dinv = iv.reshape(64, 128, 16, 256).transpose(2, 1, 0, 3)
    m48 = np.zeros((48, 512), np.float32)
    m40 = np.zeros((40, 512), np.float32)
    for base in (0, 8, 32, 40):
        for j in range(8):
            m48[base + j, j * 64:(j + 1) * 64] = 1.0
    for base in (0, 32):
        for j in range(8):
            m40[base + j, j * 64:(j + 1) * 64] = 1.0
    selF = np.zeros((128, 128, 48), np.float32)
    selB = np.zeros((128, 128, 48), np.float32)
    for tl in range(128):
        selF[tl, tl, 8:16] = 1.0
        selB[127 - tl, tl, 40:48] = 1.0
    _CONST.update({
        "m48": m48, "m40": m40,
        "selF": selF.astype(ml_dtypes.bfloat16), "selB": selB.astype(ml_dtypes.bfloat16),
        "featsT": np.ascontiguousarray(feats.T),
        "env": env,
        "dfw": np.ascontiguousarray(dfw).astype(ml_dtypes.bfloat16),
        "dinv": np.ascontiguousarray(dinv).astype(ml_dtypes.bfloat16),
    })
    return _CONST


def _f32(a):
    return np.ascontiguousarray(a, dtype=np.float32)


def make_in_maps(inputs, ncores=NCORES):
    x = _f32(inputs["x"])
    c = _f32(inputs["c"])
    shared = {
        "normf_g": _f32(inputs["normf_g"]).reshape(1, D),
        "norm1_g": _f32(inputs["norm1_g"]).reshape(1, D),
        "w_ada": _f32(inputs["w_ada"]).reshape(D, 6 * D),
        "b_ada": _f32(inputs["b_ada"]).reshape(1, 6 * D),
        "w_in": _f32(inputs["w_in"]).reshape(D, N_IN),
        "hy_conv_w": _f32(inputs["hy_conv_w"]).reshape(3, HY_COLS),
        "hy_conv_b": _f32(inputs["hy_conv_b"]).reshape(1, HY_COLS),
        "ident": np.eye(128, dtype=np.float32),
        "hy_pos_w1": _f32(inputs["hy_pos_w1"]).reshape(33, 64),
        "hy_pos_b1": _f32(inputs["hy_pos_b1"]).reshape(64, 1),
        "hy_pos_w2": _f32(inputs["hy_pos_w2"]).reshape(2, 64, 64),
        "hy_pos_b2": _f32(inputs["hy_pos_b2"]).reshape(2, 64, 1),
        "hy_pos_w3": _f32(inputs["hy_pos_w3"]).reshape(64, 2048),
        "hy_sin_freq": _f32(inputs["hy_sin_freq"]).reshape(64, 1),
        "hy_skip": _f32(inputs["hy_skip"]).reshape(1, 1024),
        "w_out": _f32(inputs["w_out"]).reshape(D, D),
        "norm2_g": _f32(inputs["norm2_g"]).reshape(1, D),
        "router_w": _f32(inputs["router_w"]).reshape(D, 256),
        "router_bias": _f32(inputs["router_bias"]).reshape(1, 256),
        "sh_w_gate": _f32(inputs["sh_w_gate"]).reshape(D, 256),
        "sh_w_up": _f32(inputs["sh_w_up"]).reshape(D, 256),
        "sh_w_down": _f32(inputs["sh_w_down"]).reshape(256, D),
        "exp_w_gate": _f32(inputs["exp_w_gate"]).reshape(256, D, 256),
        "exp_w_up": _f32(inputs["exp_w_up"]).reshape(256, D, 256),
        "exp_w_down": _f32(inputs["exp_w_down"]).reshape(256, 256, D),
        "tokid": (np.arange(T, dtype=np.float32).reshape(NT, 128).T + 1.0).copy(),
        "rw_mu": _f32(inputs["rw_mu"]).reshape(1, 1760),
        "rw_w0": _f32(inputs["rw_w0"]).reshape(2, 512),
        "rw_w_up": _f32(inputs["rw_w_up"]).reshape(2, 32, 512),
        "rw_a0": _f32(inputs["rw_a0"]).reshape(2, 512),
        "rw_a_up": _f32(inputs["rw_a_up"]).reshape(2, 32, 512),
        "rw_g_up": _f32(inputs["rw_g_up"]).reshape(96, 512),
        "rw_k_k": _f32(inputs["rw_k_k"]).reshape(1, 512),
        "rw_k_a": _f32(inputs["rw_k_a"]).reshape(1, 512),
        "rw_r_k": _f32(inputs["rw_r_k"]).reshape(1, 512),
        "rw_ln_w": _f32(inputs["rw_ln_w"]).reshape(1, 512),
        "rw_ln_b": _f32(inputs["rw_ln_b"]).reshape(1, 512),
        "blk": np.kron(np.eye(2, dtype=np.float32), np.ones((64, 64), np.float32)),
    }
    shared.update(_constants())
    in_maps = []
    for cidx in range(ncores):
        m = dict(shared)
        m["x"] = x[cidx]
        m["c"] = c[cidx:cidx + 1]
        in_maps.append(m)
    return in_maps


def kernel(**inputs):
    if "nc" not in _NC_CACHE:
        _NC_CACHE["nc"] = build_nc()
    nc = _NC_CACHE["nc"]
    in_maps = make_in_maps(inputs)
    res = run_bass_kernel_spmd(nc, in_maps, core_ids=list(range(NCORES)))
    return np.stack([res.results[c]["out"] for c in range(NCORES)], axis=0)
```
